# Optimizing a Trainium2 kernel written in Bass

```python
import math
import jax
import jax.numpy as jnp
from jax import lax
import numpy as np

D_MODEL = 1024
BATCH = 16
SEQ = 2048
DEPTH = 2

GRID_W = 64
CTX_LEN = 256
NORM_EPS = 1e-6
F32 = jnp.float32

DN_HEADS = 4
DN_DK = 128
DN_DV = 128
DN_CONV = 5
DN_CHUNK = 64
DN_QK_W = DN_HEADS * DN_DK
DN_V_W = DN_HEADS * DN_DV

S5_WIDTH = 512
S5_GROUP = 16
S5_GROUPS = S5_WIDTH // S5_GROUP
S5_STATE = 64

AB_SIZES = (DN_QK_W, DN_QK_W, DN_V_W, DN_V_W, 2 * DN_HEADS, 2 * DN_HEADS, S5_WIDTH)
AB_IN_W = sum(AB_SIZES)
MIX_W = DN_V_W + S5_WIDTH

MLA_HEADS = 8
MLA_Q_RANK = 384
MLA_KV_RANK = 256
MLA_NOPE = 128
MLA_ROPE = 64
MLA_V = 128
MLA_IN_W = MLA_Q_RANK + MLA_KV_RANK + MLA_ROPE
MLA_SCALE = (MLA_NOPE + MLA_ROPE) ** -0.5
ROPE_THETA = 10000.0
Q_BLOCK = 128

N_EXPERTS = 16
N_EXPERT_GROUPS = 4
EXPERTS_PER_GROUP = N_EXPERTS // N_EXPERT_GROUPS
TOP_K = 2
D_EXPERT = 512

kernel_name = 'hybrid_deltanet_s5_mla_moe_dit'


def rmsnorm(x, g):
    xf = x.astype(F32)
    y = xf * lax.rsqrt(jnp.mean(xf * xf, axis=-1, keepdims=True) + NORM_EPS)
    return (y * g.astype(F32)).astype(x.dtype)


def l2norm(x):
    xf = x.astype(F32)
    return xf * lax.rsqrt(jnp.sum(xf * xf, axis=-1, keepdims=True) + NORM_EPS)


def adaln(cond, w, b):
    m = jax.nn.silu(cond) @ w + b
    return [t[:, None, :] for t in jnp.split(m, 6, axis=-1)]


def modulate(x, g, shift, scale):
    return rmsnorm(x, g) * (1.0 + scale) + shift


def split_cols(x, sizes):
    offsets, acc = [], 0
    for s in sizes[:-1]:
        acc += s
        offsets.append(acc)
    return jnp.split(x, offsets, axis=-1)


def flip_t(t):
    return jnp.flip(t, axis=1)


def identity_t(t):
    return t


def short_conv(x, w):
    out = lax.conv_general_dilated(
        x, w[:, None, :].astype(x.dtype), window_strides=(1,),
        padding=[(DN_CONV // 2, DN_CONV // 2)],
        dimension_numbers=('NWC', 'WIO', 'NWC'), feature_group_count=x.shape[-1])
    return jax.nn.silu(out)


def dn_prepare(pq, pk, pv, pa, pb, conv_w, A_log, dt_bias):
    Bn, T = pq.shape[:2]
    qkv = short_conv(jnp.concatenate([pq, pk, pv], axis=-1), conv_w)
    q, k, v = jnp.split(qkv, [DN_QK_W, 2 * DN_QK_W], axis=-1)
    q = l2norm(q.reshape(Bn, T, DN_HEADS, DN_DK)) * (DN_DK ** -0.5)
    k = l2norm(k.reshape(Bn, T, DN_HEADS, DN_DK))
    v = v.reshape(Bn, T, DN_HEADS, DN_DV).astype(F32)
    a = pa.reshape(Bn, T, 2, DN_HEADS).astype(F32)
    beta = jax.nn.sigmoid(pb.reshape(Bn, T, 2, DN_HEADS).astype(F32))
    g = -jnp.exp(A_log.astype(F32)) * jax.nn.softplus(a + dt_bias.astype(F32))
    return q, k, v, g, beta


def chunk_gated_delta(q, k, v, g, beta, s0):
    Bn, T, H, K = q.shape
    Vd = v.shape[-1]
    n = T // DN_CHUNK

    def chunks(t):
        t = t.reshape((Bn, n, DN_CHUNK, H) + t.shape[3:])
        return jnp.moveaxis(jnp.moveaxis(t, 1, 0), 3, 2)

    q, k, v, g, beta = (chunks(t) for t in (q, k, v, g, beta))
    gc = jnp.cumsum(g, axis=-1)
    pos = jnp.arange(DN_CHUNK)
    incl = pos[:, None] >= pos[None, :]
    strict = pos[:, None] > pos[None, :]
    diff = gc[..., :, None] - gc[..., None, :]
    decay = jnp.where(incl, jnp.exp(jnp.where(incl, diff, 0.0)), 0.0)
    k_beta = k * beta[..., None]
    a_low = jnp.where(strict, jnp.einsum('nbhik,nbhjk->nbhij', k_beta, k) * decay, 0.0)
    lower = a_low + jnp.eye(DN_CHUNK, dtype=F32)
    rhs = jnp.concatenate([v * beta[..., None], k_beta * jnp.exp(gc)[..., None]], axis=-1)
    sol = lax.linalg.triangular_solve(lower, rhs, left_side=True, lower=True, unit_diagonal=True)
    u, w = sol[..., :Vd], sol[..., Vd:]
    qk = jnp.where(incl, jnp.einsum('nbhik,nbhjk->nbhij', q, k) * decay, 0.0)
    q_dec = q * jnp.exp(gc)[..., None]
    k_dec = k * jnp.exp(gc[..., -1:] - gc)[..., None]
    g_last = jnp.exp(gc[..., -1])

    def step(S, xs):
        u_i, w_i, qk_i, qd_i, kd_i, gl_i = xs
        v_new = u_i - jnp.einsum('bhck,bhkv->bhcv', w_i, S)
        o_i = jnp.einsum('bhck,bhkv->bhcv', qd_i, S) + jnp.einsum('bhij,bhjv->bhiv', qk_i, v_new)
        S = S * gl_i[..., None, None] + jnp.einsum('bhck,bhcv->bhkv', kd_i, v_new)
        return S, o_i

    S, o = lax.scan(step, s0, (u, w, qk, q_dec, k_dec, g_last))
    o = jnp.moveaxis(jnp.moveaxis(o, 2, 3), 0, 1).reshape(Bn, T, H, Vd)
    return o, S


def bidir_delta(ctx_in, lat_in):
    qc, kc, vc, gc, bc = ctx_in
    ql, kl, vl, gl, bl = lat_in
    s0 = jnp.zeros((qc.shape[0], DN_HEADS, DN_DK, DN_DV), F32)
    outs = []
    for d in range(2):
        orient = identity_t if d == 0 else flip_t
        o_c, s_c = chunk_gated_delta(orient(qc), orient(kc), orient(vc),
                                     orient(gc[:, :, d]), orient(bc[:, :, d]), s0)
        o_l, _ = chunk_gated_delta(orient(ql), orient(kl), orient(vl),
                                   orient(gl[:, :, d]), orient(bl[:, :, d]), s_c)
        outs.append((orient(o_c), orient(o_l)))
    return outs[0][0] + outs[1][0], outs[0][1] + outs[1][1]


def dn_output(o, z, norm_g):
    Bn, T = o.shape[:2]
    gated = rmsnorm(o, norm_g) * jax.nn.silu(z.reshape(Bn, T, DN_HEADS, DN_DV).astype(F32))
    return gated.reshape(Bn, T, DN_V_W)


def cmul(ar, ai, br, bi):
    return ar * br - ai * bi, ar * bi + ai * br


def s5_discretise(A_re, A_im, log_dt):
    A_re, A_im = A_re.astype(F32), A_im.astype(F32)
    dt = jnp.exp(log_dt.astype(F32))[..., None]
    mag = jnp.exp(A_re * dt)
    lam_re, lam_im = mag * jnp.cos(A_im * dt), mag * jnp.sin(A_im * dt)
    den = A_re * A_re + A_im * A_im
    nr, ni = lam_re - 1.0, lam_im
    coef_re = (nr * A_re + ni * A_im) / den
    coef_im = (ni * A_re - nr * A_im) / den
    return lam_re, lam_im, coef_re, coef_im


def s5_scan(b_re, b_im, lam_re, lam_im, h_re, h_im):
    hr, hi = cmul(lam_re, lam_im, h_re, h_im)
    b_re = b_re.at[:, 0].add(hr)
    b_im = b_im.at[:, 0].add(hi)
    T = b_re.shape[1]
    a_re = jnp.broadcast_to(lam_re, (1, T) + lam_re.shape)
    a_im = jnp.broadcast_to(lam_im, (1, T) + lam_im.shape)

    def combine(e1, e2):
        a1r, a1i, b1r, b1i = e1
        a2r, a2i, b2r, b2i = e2
        ar, ai = cmul(a2r, a2i, a1r, a1i)
        br, bi = cmul(a2r, a2i, b1r, b1i)
        return ar, ai, br + b2r, bi + b2i

    _, _, x_re, x_im = lax.associative_scan(combine, (a_re, a_im, b_re, b_im), axis=1)
    return x_re, x_im


def s5_states(u_c, u_l, A_re, A_im, log_dt, B_re, B_im):
    lam_re, lam_im, coef_re, coef_im = s5_discretise(A_re, A_im, log_dt)
    B_re, B_im = B_re.astype(F32), B_im.astype(F32)

    def drive(u):
        ug = u.reshape(u.shape[:2] + (S5_GROUPS, S5_GROUP)).astype(F32)
        return jnp.einsum('btgh,gph->btgp', ug, B_re), jnp.einsum('btgh,gph->btgp', ug, B_im)

    bc_re, bc_im = drive(u_c)
    bl_re, bl_im = drive(u_l)
    h0 = jnp.zeros((u_c.shape[0], S5_GROUPS, S5_STATE), F32)
    per_dir = []
    for d in range(2):
        orient = identity_t if d == 0 else flip_t
        dc = cmul(coef_re[d], coef_im[d], orient(bc_re), orient(bc_im))
        xcr, xci = s5_scan(dc[0], dc[1], lam_re[d], lam_im[d], h0, h0)
        dl = cmul(coef_re[d], coef_im[d], orient(bl_re), orient(bl_im))
        xlr, xli = s5_scan(dl[0], dl[1], lam_re[d], lam_im[d], xcr[:, -1], xci[:, -1])
        per_dir.append((orient(xcr), orient(xci), orient(xlr), orient(xli)))
    xc_re, xc_im, xl_re, xl_im = (a + b for a, b in zip(per_dir[0], per_dir[1]))
    return xc_re, xc_im, xl_re, xl_im


def s5_readout(u, x_re, x_im, C_re, C_im, D_skip, glu_w, glu_b):
    Bn, T = u.shape[:2]
    ug = u.reshape(Bn, T, S5_GROUPS, S5_GROUP).astype(F32)
    y = (jnp.einsum('btgp,ghp->btgh', x_re, C_re.astype(F32))
         - jnp.einsum('btgp,ghp->btgh', x_im, C_im.astype(F32))
         + D_skip.astype(F32) * ug)
    y = jax.nn.gelu(y.reshape(Bn, T, S5_WIDTH))
    return y * jax.nn.sigmoid(y @ glu_w.astype(F32) + glu_b.astype(F32))


def mixer_ab(h_ctx, h_lat, w_in, conv_w, A_log, dt_bias, dn_norm_g, A_re, A_im, log_dt,
             B_re, B_im, C_re, C_im, D_skip, glu_w, glu_b, w_out, need_ctx):
    pc = split_cols(h_ctx @ w_in, AB_SIZES)
    pl = split_cols(h_lat @ w_in, AB_SIZES)
    dc = dn_prepare(pc[0], pc[1], pc[2], pc[4], pc[5], conv_w, A_log, dt_bias)
    dl = dn_prepare(pl[0], pl[1], pl[2], pl[4], pl[5], conv_w, A_log, dt_bias)
    o_dn_c, o_dn_l = bidir_delta(dc, dl)
    xc_re, xc_im, xl_re, xl_im = s5_states(pc[6], pl[6], A_re, A_im, log_dt, B_re, B_im)

    def merge(o_dn, z, u, x_re, x_im, h):
        a_out = dn_output(o_dn, z, dn_norm_g)
        b_out = s5_readout(u, x_re, x_im, C_re, C_im, D_skip, glu_w, glu_b)
        return jnp.concatenate([a_out, b_out], axis=-1).astype(h.dtype) @ w_out

    o_lat = merge(o_dn_l, pl[3], pl[6], xl_re, xl_im, h_lat)
    o_ctx = merge(o_dn_c, pc[3], pc[6], xc_re, xc_im, h_ctx) if need_ctx else None
    return o_ctx, o_lat


def axial_rope(T):
    rows = T // GRID_W
    row = jnp.repeat(jnp.arange(rows, dtype=F32), GRID_W)
    col = jnp.tile(jnp.arange(GRID_W, dtype=F32), rows)
    n_freq = MLA_ROPE // 4
    inv = ROPE_THETA ** (-jnp.arange(n_freq, dtype=F32) / n_freq)
    ang = jnp.concatenate([row[:, None] * inv, col[:, None] * inv], axis=-1)
    return jnp.cos(ang), jnp.sin(ang)


def apply_rope(x, cos, sin):
    shape = (1, x.shape[1]) + (1,) * (x.ndim - 3) + (cos.shape[-1],)
    cos, sin = cos.reshape(shape), sin.reshape(shape)
    x1, x2 = jnp.split(x.astype(F32), 2, axis=-1)
    return jnp.concatenate([x1 * cos - x2 * sin, x1 * sin + x2 * cos], axis=-1).astype(x.dtype)


def softmax_attend(q, k, v):
    s = jnp.einsum('bqhd,bkhd->bhqk', q, k).astype(F32) * MLA_SCALE
    p = jax.nn.softmax(s, axis=-1).astype(v.dtype)
    return jnp.einsum('bhqk,bkhd->bqhd', p, v)


def mixer_mla(h_ctx, h_lat, w_in, q_norm_g, w_q_up, kv_norm_g, w_kv_up, w_out, cos, sin, need_ctx):
    def queries(cq, rope):
        Bn, T = cq.shape[:2]
        q = (rmsnorm(cq, q_norm_g) @ w_q_up).reshape(Bn, T, MLA_HEADS, MLA_NOPE + MLA_ROPE)
        q_rope = apply_rope(q[..., MLA_NOPE:], cos, sin) if rope else q[..., MLA_NOPE:]
        return jnp.concatenate([q[..., :MLA_NOPE], q_rope], axis=-1)

    def keys_values(ckv, k_rope, rope):
        Bn, T = ckv.shape[:2]
        kv = (rmsnorm(ckv, kv_norm_g) @ w_kv_up).reshape(Bn, T, MLA_HEADS, MLA_NOPE + MLA_V)
        if rope:
            k_rope = apply_rope(k_rope, cos, sin)
        k_rope = jnp.broadcast_to(k_rope[:, :, None, :], (Bn, T, MLA_HEADS, MLA_ROPE))
        return jnp.concatenate([kv[..., :MLA_NOPE], k_rope], axis=-1), kv[..., MLA_NOPE:]

    cq_c, ckv_c, kr_c = split_cols(h_ctx @ w_in, (MLA_Q_RANK, MLA_KV_RANK, MLA_ROPE))
    cq_l, ckv_l, kr_l = split_cols(h_lat @ w_in, (MLA_Q_RANK, MLA_KV_RANK, MLA_ROPE))
    k_c, v_c = keys_values(ckv_c, kr_c, False)
    k_l, v_l = keys_values(ckv_l, kr_l, True)
    q_l = queries(cq_l, True)
    k_all = jnp.concatenate([k_c, k_l], axis=1)
    v_all = jnp.concatenate([v_c, v_l], axis=1)
    Bn, T = q_l.shape[:2]
    nb = T // Q_BLOCK
    q_blocks = jnp.moveaxis(q_l.reshape(Bn, nb, Q_BLOCK, MLA_HEADS, MLA_NOPE + MLA_ROPE), 1, 0)
    o_blocks = lax.map(lambda qb: softmax_attend(qb, k_all, v_all), q_blocks)
    o_l = jnp.moveaxis(o_blocks, 0, 1).reshape(Bn, T, MLA_HEADS * MLA_V) @ w_out
    o_c = None
    if need_ctx:
        q_c = queries(cq_c, False)
        o_c = softmax_attend(q_c, k_c, v_c).reshape(Bn, -1, MLA_HEADS * MLA_V) @ w_out
    return o_c, o_l


def grouped_moe(h, router_w, router_bias, w_gate, w_up, w_down):
    tok = h.reshape(-1, h.shape[-1])
    scores = jax.nn.sigmoid((tok @ router_w).astype(F32))
    choice = scores + router_bias.astype(F32)
    grouped = choice.reshape(-1, N_EXPERT_GROUPS, EXPERTS_PER_GROUP)
    group_score = jnp.sum(lax.top_k(grouped, 2)[0], axis=-1)
    best_group = jnp.argmax(group_score, axis=-1)
    in_group = (jnp.arange(N_EXPERTS) // EXPERTS_PER_GROUP)[None, :] == best_group[:, None]
    _, idx = lax.top_k(jnp.where(in_group, choice, -jnp.inf), TOP_K)
    wts = jnp.take_along_axis(scores, idx, axis=-1)
    wts = wts / jnp.sum(wts, axis=-1, keepdims=True)
    gates = jnp.sum(jax.nn.one_hot(idx, N_EXPERTS, dtype=F32) * wts[..., None], axis=1)
    out = jnp.zeros(tok.shape, F32)
    for e in range(N_EXPERTS):
        act = jax.nn.silu(tok @ w_gate[e]) * (tok @ w_up[e])
        out = out + gates[:, e:e + 1] * (act @ w_down[e])
    return out.reshape(h.shape).astype(h.dtype)


def setup_inputs(seed: int = 0) -> dict:
    key = jax.random.key(seed)
    keys = iter(jax.random.split(key, 40))

    def normal(shape, scale):
        return scale * jax.random.normal(next(keys), shape, F32)

    def gain(shape):
        return 1.0 + normal(shape, 0.05)

    def log_uniform(shape, lo, hi):
        return jax.random.uniform(next(keys), shape, F32, math.log(lo), math.log(hi))

    NE, NO = (DEPTH + 1) // 2, DEPTH // 2
    D = D_MODEL
    dn_dt = jnp.exp(log_uniform((NE, 2, DN_HEADS), 1e-3, 1e-1))
    state_idx = jnp.arange(S5_STATE, dtype=F32)
    return {
        'x': normal((BATCH, SEQ, D), 1.0),
        'c': normal((BATCH, D), 1.0),
        'ctx': normal((BATCH, CTX_LEN, D), 1.0),
        'c_ctx': normal((D,), 1.0),
        'ada_w': normal((DEPTH, D, 6 * D), 0.5 * D ** -0.5),
        'ada_b': normal((DEPTH, 6 * D), 0.02),
        'norm1_g': gain((DEPTH, D)),
        'norm2_g': gain((DEPTH, D)),
        'ab_w_in': normal((NE, D, AB_IN_W), D ** -0.5),
        'dn_conv_w': normal((NE, DN_CONV, 2 * DN_QK_W + DN_V_W), DN_CONV ** -0.5),
        'dn_A_log': jnp.log(jax.random.uniform(next(keys), (NE, 2, DN_HEADS), F32, 1.0, 16.0)),
        'dn_dt_bias': dn_dt + jnp.log(-jnp.expm1(-dn_dt)),
        'dn_norm_g': gain((NE, DN_DV)),
        's5_A_re': -0.5 + normal((NE, 2, S5_GROUPS, S5_STATE), 0.01),
        's5_A_im': math.pi * state_idx + normal((NE, 2, S5_GROUPS, S5_STATE), 0.01),
        's5_log_dt': log_uniform((NE, 2, S5_GROUPS), 1e-3, 1e-1),
        's5_B_re': normal((NE, S5_GROUPS, S5_STATE, S5_GROUP), (2 * S5_GROUP) ** -0.5),
        's5_B_im': normal((NE, S5_GROUPS, S5_STATE, S5_GROUP), (2 * S5_GROUP) ** -0.5),
        's5_C_re': normal((NE, S5_GROUPS, S5_GROUP, S5_STATE), (2 * S5_STATE) ** -0.5),
        's5_C_im': normal((NE, S5_GROUPS, S5_GROUP, S5_STATE), (2 * S5_STATE) ** -0.5),
        's5_D': normal((NE, S5_GROUPS, S5_GROUP), 0.5),
        's5_glu_w': normal((NE, S5_WIDTH, S5_WIDTH), S5_WIDTH ** -0.5),
        's5_glu_b': normal((NE, S5_WIDTH), 0.01),
        'ab_w_out': normal((NE, MIX_W, D), MIX_W ** -0.5),
        'mla_w_in': normal((NO, D, MLA_IN_W), D ** -0.5),
        'mla_q_norm_g': gain((NO, MLA_Q_RANK)),
        'mla_w_q_up': normal((NO, MLA_Q_RANK, MLA_HEADS * (MLA_NOPE + MLA_ROPE)), MLA_Q_RANK ** -0.5),
        'mla_kv_norm_g': gain((NO, MLA_KV_RANK)),
        'mla_w_kv_up': normal((NO, MLA_KV_RANK, MLA_HEADS * (MLA_NOPE + MLA_V)), MLA_KV_RANK ** -0.5),
        'mla_w_out': normal((NO, MLA_HEADS * MLA_V, D), (MLA_HEADS * MLA_V) ** -0.5),
        'router_w': normal((D, N_EXPERTS), D ** -0.5),
        'router_bias': normal((N_EXPERTS,), 0.01),
        'moe_w_gate': normal((DEPTH, N_EXPERTS, D, D_EXPERT), D ** -0.5),
        'moe_w_up': normal((DEPTH, N_EXPERTS, D, D_EXPERT), D ** -0.5),
        'moe_w_down': normal((DEPTH, N_EXPERTS, D_EXPERT, D), D_EXPERT ** -0.5),
        'final_norm_g': gain((D,)),
    }


def reference(x, c, ctx, c_ctx, ada_w, ada_b, norm1_g, norm2_g, ab_w_in, dn_conv_w, dn_A_log,
              dn_dt_bias, dn_norm_g, s5_A_re, s5_A_im, s5_log_dt, s5_B_re, s5_B_im, s5_C_re,
              s5_C_im, s5_D, s5_glu_w, s5_glu_b, ab_w_out, mla_w_in, mla_q_norm_g, mla_w_q_up,
              mla_kv_norm_g, mla_w_kv_up, mla_w_out, router_w, router_bias, moe_w_gate, moe_w_up,
              moe_w_down, final_norm_g):
    T = x.shape[1]
    cos, sin = axial_rope(T)
    cond_ctx = c_ctx[None, :]
    for i in range(DEPTH):
        last = i == DEPTH - 1
        j = i // 2
        sh1, sc1, g1, sh2, sc2, g2 = adaln(c, ada_w[i], ada_b[i])
        sh1c, sc1c, g1c, sh2c, sc2c, g2c = adaln(cond_ctx, ada_w[i], ada_b[i])
        h_l = modulate(x, norm1_g[i], sh1, sc1)
        h_c = modulate(ctx, norm1_g[i], sh1c, sc1c)
        if i % 2 == 0:
            o_c, o_l = mixer_ab(h_c, h_l, ab_w_in[j], dn_conv_w[j], dn_A_log[j], dn_dt_bias[j],
                                dn_norm_g[j], s5_A_re[j], s5_A_im[j], s5_log_dt[j], s5_B_re[j],
                                s5_B_im[j], s5_C_re[j], s5_C_im[j], s5_D[j], s5_glu_w[j],
                                s5_glu_b[j], ab_w_out[j], not last)
        else:
            o_c, o_l = mixer_mla(h_c, h_l, mla_w_in[j], mla_q_norm_g[j], mla_w_q_up[j],
                                 mla_kv_norm_g[j], mla_w_kv_up[j], mla_w_out[j], cos, sin, not last)
        x = x + g1 * o_l
        x = x + g2 * grouped_moe(modulate(x, norm2_g[i], sh2, sc2), router_w, router_bias,
                                 moe_w_gate[i], moe_w_up[i], moe_w_down[i])
        if not last:
            ctx = ctx + g1c * o_c
            ctx = ctx + g2c * grouped_moe(modulate(ctx, norm2_g[i], sh2c, sc2c), router_w, router_bias,
                                          moe_w_gate[i], moe_w_up[i], moe_w_down[i])
    return rmsnorm(x, final_norm_g)
```

```python
import math
from contextlib import ExitStack
import numpy as np
import concourse.bass as bass
import concourse.mybir as mybir
from concourse.bass_utils import run_bass_kernel_spmd

F32 = mybir.dt.float32
BF16 = mybir.dt.bfloat16
ALU = mybir.AluOpType
AF = mybir.ActivationFunctionType
AX = mybir.AxisListType

NCORES = 8
NB = 2
D = 1024
SEQ = 2048
CTX = 256
T = SEQ + CTX
NT = T // 128
EPS = 1e-6
KD = D // 128
MAGIC = 12582912.0
TWO_PI = 2.0 * math.pi


import os as _os
STORE_Q = _os.environ.get("STORE_Q", "pool")
OP_TRACE = bool(_os.environ.get("OP_TRACE"))
OP_LIMIT = int(_os.environ.get("OP_LIMIT", "1000000000"))


class Buf:
    __slots__ = ("name", "w", "r", "excl")

    def __init__(self, name="", excl=False):
        self.name = name
        self.w = None
        self.r = {}
        self.excl = excl


class Prog:
    SEM_ROT = 30000
    N_DMA_SEMS = 32

    def __init__(self, nc):
        self.nc = nc
        self.eng = {"pe": nc.tensor, "dve": nc.vector, "act": nc.scalar,
                    "pool": nc.gpsimd, "sp": nc.sync}
        self.sems = {}
        self.cur = {}
        self.cnt = {}
        self.waited = {e: {} for e in self.eng}
        self.gen = {e: 0 for e in self.eng}
        for e in self.eng:
            self._new_sem(e)
        self.dma_keys = []
        for i in range(self.N_DMA_SEMS):
            k = "dma%d" % i
            self.sems[k] = nc.alloc_semaphore(name=k)
            self.cnt[k] = 0
            self.dma_keys.append(k)
        self.dma_rr = 0
        self.ninst = {e: 0 for e in self.eng}

    def _new_sem(self, e):
        k = "%s_%d" % (e, self.gen[e])
        self.gen[e] += 1
        self.sems[k] = self.nc.alloc_semaphore(name=k)
        self.cnt[k] = 0
        self.cur[e] = k

    def _wait(self, e, key, val):
        if val <= 0 or self.waited[e].get(key, 0) >= val:
            return
        self.eng[e].wait_ge(self.sems[key], val)
        self.waited[e][key] = val
        self.ninst[e] += 1

    def _deps(self, e, r, w):
        need = {}
        own0 = self.cur[e]
        for b in r:
            if b.w is not None:
                k, v = b.w
                need[k] = max(need.get(k, 0), v)
            if b.excl:
                for k, v in b.r.items():
                    if k != own0:
                        need[k] = max(need.get(k, 0), v)
        for b in w:
            if b.w is not None:
                k, v = b.w
                need[k] = max(need.get(k, 0), v)
            for k, v in b.r.items():
                need[k] = max(need.get(k, 0), v)
        own = self.cur[e]
        for k, v in need.items():
            if k == own and e == "pe":
                continue
            self._wait(e, k, v)

    def op(self, e, fn, r=(), w=()):
        self.total = getattr(self, "total", 0) + 1
        if self.total > OP_LIMIT:
            return None
        if OP_TRACE:
            import sys as _sys
            print("OP", self.total, e, _sys._getframe(1).f_lineno)
        self._deps(e, r, w)
        inst = fn(self.eng[e])
        k = self.cur[e]
        inst.then_inc(self.sems[k], 1)
        self.cnt[k] += 1
        v = self.cnt[k]
        for b in r:
            b.r[k] = v
        for b in w:
            b.w = (k, v)
            b.r = {}
        self.ninst[e] += 1
        if v >= self.SEM_ROT:
            self._new_sem(e)
        return inst

    def dma(self, e, out, in_, r=(), w=(), **kw):
        self.total = getattr(self, "total", 0) + 1
        if self.total > OP_LIMIT:
            return None
        if STORE_Q and "DRAM" in str(out.space):
            e = STORE_Q
        k = self.dma_keys[self.dma_rr]
        self.dma_rr = (self.dma_rr + 1) % len(self.dma_keys)
        self._wait(e, k, self.cnt[k])
        self._deps(e, r, w)
        inst = self.eng[e].dma_start(out=out, in_=in_, **kw)
        inst.then_inc(self.sems[k], 16)
        self.cnt[k] += 16
        v = self.cnt[k]
        for b in r:
            b.r[k] = v
        for b in w:
            b.w = (k, v)
            b.r = {}
        self.ninst[e] += 1
        return inst

    def barrier(self, engines=None):
        for e in (engines or self.eng):
            for k in list(self.cnt):
                self._wait(e, k, self.cnt[k])


class KB:
    def __init__(self, scr_kinds=None):
        self.nc = bass.Bass("TRN2", target_bir_lowering=False)
        self.P = Prog(self.nc)
        self.scr_kinds = scr_kinds or {}
        self.dr = {}
        self.stack = None
        self.uid = 0
        self.dq = 0

    def dram(self, name, shape, dtype=F32, kind="Internal"):
        kind = self.scr_kinds.get(name, kind)
        ap = self.nc.dram_tensor(name, list(shape), dtype, kind=kind).ap()
        self.dr[name] = (ap, Buf(name))
        return self.dr[name]

    def sb(self, shape, dtype=F32, name=None):
        self.uid += 1
        nm = "%s_%d" % (name or "sb", self.uid)
        t = self.stack.enter_context(self.nc.sbuf_tensor(nm, list(shape), dtype))
        return t.ap(), Buf(nm)

    def ps(self, shape, dtype=F32, name=None):
        self.uid += 1
        nm = "%s_%d" % (name or "ps", self.uid)
        t = self.stack.enter_context(self.nc.psum_tensor(nm, [128, 512], F32))
        ap = t.ap()
        shape = list(shape)
        n = 1
        for v in shape[1:]:
            n *= v
        assert dtype == F32 and n <= 512, shape
        v = ap[0:shape[0], 0:n]
        if len(shape) == 3:
            v = v.rearrange("p (a b) -> p a b", b=shape[2])
        return v, Buf(nm, excl=True)

    def phase(self):
        kb = self

        class _Ph:
            def __enter__(s):
                s.saved = kb.stack
                kb.stack = ExitStack()
                kb.stack.__enter__()
                return kb.stack

            def __exit__(s, *a):
                kb.P.barrier()
                kb.stack.__exit__(*a)
                kb.stack = s.saved
        return _Ph()

    def dmaq(self):
        self.dq ^= 1
        return "sp"

    def consts(self):
        P, nc = self.P, self.nc
        self.ident, self.b_ident = self.sb([128, 128], F32, "ident")
        P.op("pool", lambda e: e.memset(self.ident, 0.0), w=[self.b_ident])
        P.op("pool", lambda e: e.affine_select(out=self.ident, in_=self.ident, pattern=[[-1, 128]],
                                               compare_op=ALU.not_equal, fill=1.0, base=0, channel_multiplier=1),
             r=[self.b_ident], w=[self.b_ident])
        self.identb, self.b_identb = self.sb([128, 128], BF16, "identb")
        P.op("dve", lambda e: e.tensor_copy(out=self.identb, in_=self.ident), r=[self.b_ident], w=[self.b_identb])
        self.ones, self.b_ones = self.sb([128, 128], F32, "ones")
        P.op("pool", lambda e: e.memset(self.ones, 1.0), w=[self.b_ones])
        self.shiftc = {}
        for sh in (0.0, math.pi / 2, MAGIC, -MAGIC):
            t, bt = self.sb([128, 1], F32, "shc")
            P.op("pool", lambda e: e.memset(t, sh), w=[bt])
            self.shiftc[sh] = (t, bt)

    def load_vec_fm(self, src1d, n, dst, b_dst, tmp_ps):
        P = self.P
        st, b_st = self.sb([n, 128], F32, "lv")
        P.dma("sp", st, src1d.rearrange("(k p) -> k p", p=128), w=[b_st])
        ps, b_ps = tmp_ps
        P.op("pe", lambda e: e.transpose(out=ps[:, 0:n], in_=st, identity=self.ident[0:n, 0:n]),
             r=[b_st, self.b_ident], w=[b_ps])
        P.op("dve", lambda e: e.tensor_copy(out=dst, in_=ps[:, 0:n]), r=[b_ps], w=[b_dst])

    def load_w_bf16(self, dst, b_dst, src, rows, cols, stage, colblk=2048):
        P = self.P
        i = 0
        for c0 in range(0, cols, colblk):
            cw = min(colblk, cols - c0)
            st, b_st = stage[self.uid % len(stage)]
            self.uid += 1
            P.dma("sp", st[0:rows, 0:cw], src[:, c0:c0 + cw], w=[b_st])
            self.castrr = getattr(self, "castrr", 0) + 1
            eng = ("pool", "act", "dve")[self.castrr % 3]
            i += 1
            if eng == "act":
                P.op("act", lambda e: e.copy(out=dst[:, c0:c0 + cw], in_=st[0:rows, 0:cw]), r=[b_st], w=[b_dst])
            else:
                P.op(eng, lambda e: e.tensor_copy(out=dst[:, c0:c0 + cw], in_=st[0:rows, 0:cw]), r=[b_st], w=[b_dst])


def phase_adaln(kb, li, IN):
    P, nc = kb.P, kb.nc
    modd, b_modd = kb.dr["modd%d" % li]
    with kb.phase():
        condT, b_condT = kb.sb([128, 8, 3], F32, "condT")
        crow, b_crow = kb.sb([3, 1024], F32, "crow")
        P.dma("sp", crow[0:2, :], IN["c"], w=[b_crow])
        P.dma("sp", crow[2:3, :], IN["c_ctx"].rearrange("(o n) -> o n", o=1), w=[b_crow])
        pst, b_pst = kb.ps([128, 8, 3], F32, "pst")
        for k in range(8):
            P.op("pe", lambda e: e.transpose(out=pst[:, k, :], in_=crow[0:3, k * 128:(k + 1) * 128],
                                             identity=kb.ident[0:3, 0:3]), r=[b_crow, kb.b_ident], w=[b_pst])
        P.op("act", lambda e: e.activation(out=condT, in_=pst, func=AF.Silu), r=[b_pst], w=[b_condT])
        wb = [kb.sb([128, 8, 512], F32, "adaw") for _ in range(2)]
        bb = [kb.sb([3, 512], F32, "adab") for _ in range(2)]
        rs = [kb.sb([3, 512], F32, "adar") for _ in range(2)]
        pm = [kb.ps([3, 512], F32, "adaps") for _ in range(2)]
        for blk in range(12):
            w, b_w = wb[blk % 2]
            bt, b_bt = bb[blk % 2]
            r_, b_r = rs[blk % 2]
            ps, b_ps = pm[blk % 2]
            cs = slice(blk * 512, (blk + 1) * 512)
            P.dma("sp", w, IN["ada_w"][li, :, cs].rearrange("(k p) n -> p k n", p=128), w=[b_w])
            P.dma("sp", bt, IN["ada_b"][li, cs].partition_broadcast(3), w=[b_bt])
            for k in range(8):
                P.op("pe", lambda e: e.matmul(ps, lhsT=condT[:, k, :], rhs=w[:, k, :], start=(k == 0), stop=(k == 7)),
                     r=[b_condT, b_w], w=[b_ps])
            P.op("dve", lambda e: e.tensor_tensor(out=r_, in0=ps, in1=bt, op=ALU.add), r=[b_ps, b_bt], w=[b_r])
            P.dma("sp", modd[:, cs], r_, r=[b_r], w=[b_modd])


def adaln_gen(kb, li, IN):
    P = kb.P
    modd, b_modd = kb.dr["modd%d" % li]
    condT, b_condT = kb.sb([128, 8, 3], F32, "condT")
    crow, b_crow = kb.sb([3, 1024], F32, "crow")
    P.dma("sp", crow[0:2, :], IN["c"], w=[b_crow])
    P.dma("sp", crow[2:3, :], IN["c_ctx"].rearrange("(o n) -> o n", o=1), w=[b_crow])
    pst, b_pst = kb.ps([128, 8, 3], F32, "pst")
    for k in range(8):
        P.op("pe", lambda e: e.transpose(out=pst[:, k, :], in_=crow[0:3, k * 128:(k + 1) * 128],
                                         identity=kb.ident[0:3, 0:3]), r=[b_crow, kb.b_ident], w=[b_pst])
    P.op("act", lambda e: e.activation(out=condT, in_=pst, func=AF.Silu), r=[b_pst], w=[b_condT])
    wb = [kb.sb([128, 8, 512], F32, "adaw") for _ in range(2)]
    bb = [kb.sb([3, 512], F32, "adab") for _ in range(2)]
    rs = [kb.sb([3, 512], F32, "adar") for _ in range(2)]
    pm = [kb.ps([3, 512], F32, "adaps") for _ in range(2)]
    yield
    for blk in range(12):
        w, b_w = wb[blk % 2]
        bt, b_bt = bb[blk % 2]
        r_, b_r = rs[blk % 2]
        ps, b_ps = pm[blk % 2]
        cs = slice(blk * 512, (blk + 1) * 512)
        P.dma("sp", w, IN["ada_w"][li, :, cs].rearrange("(k p) n -> p k n", p=128), w=[b_w])
        P.dma("sp", bt, IN["ada_b"][li, cs].partition_broadcast(3), w=[b_bt])
        for k in range(8):
            P.op("pe", lambda e: e.matmul(ps, lhsT=condT[:, k, :], rhs=w[:, k, :], start=(k == 0), stop=(k == 7)),
                 r=[b_condT, b_w], w=[b_ps])
        P.op("dve", lambda e: e.tensor_tensor(out=r_, in0=ps, in1=bt, op=ALU.add), r=[b_ps, b_bt], w=[b_r])
        P.dma("sp", modd[:, cs], r_, r=[b_r], w=[b_modd])
        yield


def load_mod_fm(kb, li, IN, gname):
    P = kb.P
    modd, b_modd = kb.dr["modd%d" % li]
    modF, b_modF = kb.sb([128, 48, 3], F32, "modF")
    As = [kb.sb([128, 8, 3], F32, "modA") for _ in range(2)]
    gs = [kb.sb([128, 8], F32, "ng") for _ in range(2)]
    with kb.phase():
        mrow, b_mrow = kb.sb([3, 6144], F32, "mrow")
        P.dma("sp", mrow, modd, r=[b_modd], w=[b_mrow])
        pst, b_pst = kb.ps([128, 48, 3], F32, "modps")
        for j in range(48):
            P.op("pe", lambda e: e.transpose(out=pst[:, j, :], in_=mrow[0:3, j * 128:(j + 1) * 128],
                                             identity=kb.ident[0:3, 0:3]), r=[b_mrow, kb.b_ident], w=[b_pst])
        P.op("dve", lambda e: e.tensor_copy(out=modF, in_=pst), r=[b_pst], w=[b_modF])
        for i, (nm, so) in enumerate(((gname[0], 8), (gname[1], 32))):
            g, b_g = gs[i]
            A, b_A = As[i]
            kb.load_vec_fm(IN[nm][li], 8, g, b_g, (pst.rearrange("p a c -> p (a c)"), b_pst))
            P.op("dve", lambda e: e.tensor_scalar(out=A, in0=modF[:, so:so + 8, :], scalar1=1.0, scalar2=None, op0=ALU.add),
                 r=[b_modF], w=[b_A])
            P.op("dve", lambda e: e.tensor_tensor(out=A, in0=A, in1=g.unsqueeze(2).to_broadcast([128, 8, 3]), op=ALU.mult),
                 r=[b_A, b_g], w=[b_A])
    return modF, b_modF, As[0], As[1]


def norm_mod_tile(kb, xs_ap, b_xs, rows, cond, A, b_A, modF, b_modF, shift_off, work, hT_list, col0, cols=None):
    P = kb.P
    (xt, b_xt), (sq, b_sq), (ss, b_ss), (pt, b_pt) = work
    P.dma("sp", xt, xs_ap[rows, :], r=[b_xs], w=[b_xt])
    P.op("act", lambda e: e.activation(out=sq, in_=xt, func=AF.Square, accum_out=ss[:, 0:1]), r=[b_xt], w=[b_sq, b_ss])
    P.op("dve", lambda e: e.tensor_scalar(out=ss[:, 1:2], in0=ss[:, 0:1], scalar1=1.0 / D, scalar2=EPS,
                                          op0=ALU.mult, op1=ALU.add), r=[b_ss], w=[b_ss])
    P.op("act", lambda e: e.activation(out=ss[:, 2:3], in_=ss[:, 1:2], func=AF.Sqrt), r=[b_ss], w=[b_ss])
    P.op("dve", lambda e: e.reciprocal(out=ss[:, 3:4], in_=ss[:, 2:3]), r=[b_ss], w=[b_ss])
    P.op("act", lambda e: e.activation(out=sq, in_=xt, func=AF.Copy, scale=ss[:, 3:4]), r=[b_xt, b_ss], w=[b_sq])
    for half in range(2):
        for k4 in range(4):
            k = half * 4 + k4
            P.op("pe", lambda e: e.transpose(out=pt[:, k4, :], in_=sq[:, k * 128:(k + 1) * 128], identity=kb.ident),
                 r=[b_sq, kb.b_ident], w=[b_pt])
        for k4 in range(4):
            k = half * 4 + k4
            for hi, (hT, b_hT) in enumerate(hT_list):
                eng = "dve"
                c0_ = cols[hi] if cols is not None else col0
                P.op(eng, lambda e: e.tensor_scalar(out=hT[:, k, c0_:c0_ + 128], in0=pt[:, k4, :],
                                                    scalar1=A[:, k, cond:cond + 1],
                                                    scalar2=modF[:, shift_off + k, cond:cond + 1],
                                                    op0=ALU.mult, op1=ALU.add),
                     r=[b_pt, b_A, b_modF], w=[b_hT])


def norm_mod_batch(kb, xs_ap, b_xs, toks, conds, A, b_A, modF, b_modF, shift_off, bufs, outs):
    P = kb.P
    G = len(toks)
    ss, b_ss = bufs["ss"]
    for i, tok0 in enumerate(toks):
        (xt, b_xt), (sq, b_sq) = bufs["xt"][i], bufs["sq"][i]
        P.dma("sp", xt, xs_ap[tok0:tok0 + 128, :], r=[b_xs], w=[b_xt])
        P.op("act", lambda e: e.activation(out=sq, in_=xt, func=AF.Square, accum_out=ss[:, 0, i:i + 1]), r=[b_xt], w=[b_sq, b_ss])
    P.op("dve", lambda e: e.tensor_scalar(out=ss[:, 1, 0:G], in0=ss[:, 0, 0:G], scalar1=1.0 / D, scalar2=EPS,
                                          op0=ALU.mult, op1=ALU.add), r=[b_ss], w=[b_ss])
    P.op("act", lambda e: e.activation(out=ss[:, 2, 0:G], in_=ss[:, 1, 0:G], func=AF.Sqrt), r=[b_ss], w=[b_ss])
    P.op("dve", lambda e: e.reciprocal(out=ss[:, 3, 0:G], in_=ss[:, 2, 0:G]), r=[b_ss], w=[b_ss])
    rot = bufs.setdefault("rot", [0])
    for i, tok0 in enumerate(toks):
        (xt, b_xt), (sq, b_sq) = bufs["xt"][i], bufs["sq"][i]
        cond = conds[i]
        P.op("act", lambda e: e.activation(out=sq, in_=xt, func=AF.Copy, scale=ss[:, 3, i:i + 1]), r=[b_xt, b_ss], w=[b_sq])
        for half in range(2):
            pt, b_pt = bufs["pts"][rot[0] % len(bufs["pts"])]
            rot[0] += 1
            for k4 in range(4):
                k = half * 4 + k4
                P.op("pe", lambda e: e.transpose(out=pt[:, k4, :], in_=sq[:, k * 128:(k + 1) * 128], identity=kb.ident),
                     r=[b_sq, kb.b_ident], w=[b_pt])
            for k4 in range(4):
                k = half * 4 + k4
                for hi, (hT, b_hT, colfn) in enumerate(outs):
                    c0_ = colfn(i)
                    if (k4 + hi) % 2 == 0:
                        P.op("dve", lambda e: e.tensor_scalar(out=hT[:, k, c0_:c0_ + 128], in0=pt[:, k4, :],
                                                              scalar1=A[:, k, cond:cond + 1],
                                                              scalar2=modF[:, shift_off + k, cond:cond + 1],
                                                              op0=ALU.mult, op1=ALU.add),
                             r=[b_pt, b_A, b_modF], w=[b_hT])
                    else:
                        P.op("act", lambda e: e.activation(out=hT[:, k, c0_:c0_ + 128], in_=pt[:, k4, :], func=AF.Identity,
                                                           scale=A[:, k, cond:cond + 1],
                                                           bias=modF[:, shift_off + k, cond:cond + 1]),
                             r=[b_pt, b_A, b_modF], w=[b_hT])


def make_nm_bufs(kb, G, npt):
    return dict(xt=[kb.sb([128, 1024], F32, "xt") for _ in range(G)], sq=[kb.sb([128, 1024], F32, "sq") for _ in range(G)],
                ss=kb.sb([128, 4, G], F32, "ss"), pts=[kb.ps([128, 4, 128], F32, "pt") for _ in range(npt)])


AB_W = 2576


def phase_l0_inproj(kb, IN):
    P = kb.P
    xs, b_xs = kb.dr["xs"]
    qkvu, b_qkvu = kb.dr["qkvu"]
    zab, b_zab = kb.dr["zab"]
    with kb.phase():
        modF, b_modF, (A1, b_A1), _ = load_mod_fm(kb, 0, IN, ("norm1_g", "norm2_g"))
        W, b_W = kb.sb([128, 8, AB_W], BF16, "win")
        stage = [kb.sb([128, 2576], F32, "wst") for _ in range(2)]
        for k in range(8):
            kb.load_w_bf16(W[:, k, :], b_W, IN["ab_w_in"][0, k * 128:(k + 1) * 128, :], 128, AB_W, stage, colblk=2576)
        pts_shared = [kb.ps([128, 4, 128], F32, "pt") for _ in range(4)]
        nmb = []
        for _i in range(2):
            bb_ = make_nm_bufs(kb, 2, 0)
            bb_["pts"] = pts_shared
            nmb.append(bb_)
        nmb[1]["rot"] = nmb[0].setdefault("rot", [0])
        hTs = [kb.sb([128, 8, 256], BF16, "hT") for _ in range(2)]
        outs = [kb.sb([128, 16, 256], F32, "ost") for _ in range(2)]
        zst = [kb.sb([128, 528], F32, "zst") for _ in range(2)]
        pf = [kb.ps([128, 512], F32, "pf") for _ in range(2)]
        pz = [kb.ps([128, 512], F32, "pz") for _ in range(1)]
        pab = kb.ps([128, 512], F32, "pab")
        fm_cols = [c * 128 for c in range(12)] + [2064 + c * 128 for c in range(4)]
        ui = 0
        for b in range(NB):
            for un in range(T // 256):
                hT, b_hT = hTs[ui % 2]
                ost, b_ost = outs[ui % 2]
                toks_ = [un * 256, un * 256 + 128]
                norm_mod_batch(kb, xs[b], b_xs, toks_, [2 if t_ < CTX else b for t_ in toks_], A1, b_A1, modF, b_modF, 0,
                               nmb[ui % 2], [(hT, b_hT, lambda i: i * 128)])
                for ci, c0 in enumerate(fm_cols):
                    ps, b_ps = pf[ci % 2]
                    for k in range(8):
                        P.op("pe", lambda e: e.matmul(ps[:, 0:256], lhsT=W[:, k, c0:c0 + 128], rhs=hT[:, k, :],
                                                      start=(k == 0), stop=(k == 7)), r=[b_W, b_hT], w=[b_ps])
                    if ci % 2 == 0:
                        P.op("act", lambda e: e.copy(out=ost[:, ci, :], in_=ps[:, 0:256]), r=[b_ps], w=[b_ost])
                    else:
                        P.op("dve", lambda e: e.tensor_copy(out=ost[:, ci, :], in_=ps[:, 0:256]), r=[b_ps], w=[b_ost])
                P.dma("sp", qkvu[b, :, un * 256:(un + 1) * 256].rearrange("(c p) t -> p c t", p=128), ost,
                      r=[b_ost], w=[b_qkvu])
                for tt in range(2):
                    z, b_z = zst[tt]
                    ps, b_ps = pz[0]
                    for k in range(8):
                        P.op("pe", lambda e: e.matmul(ps[:, 0:512], lhsT=hT[:, k, tt * 128:(tt + 1) * 128],
                                                      rhs=W[:, k, 1536:2048], start=(k == 0), stop=(k == 7)),
                             r=[b_W, b_hT], w=[b_ps])
                    for k in range(8):
                        P.op("pe", lambda e: e.matmul(pab[0][:, 0:16], lhsT=hT[:, k, tt * 128:(tt + 1) * 128],
                                                      rhs=W[:, k, 2048:2064], start=(k == 0), stop=(k == 7)),
                             r=[b_W, b_hT], w=[pab[1]])
                    P.op("act", lambda e: e.copy(out=z[:, 0:512], in_=ps), r=[b_ps], w=[b_z])
                    P.op("dve", lambda e: e.tensor_copy(out=z[:, 512:528], in_=pab[0][:, 0:16]), r=[pab[1]], w=[b_z])
                    tok0 = un * 256 + tt * 128
                    P.dma("sp", zab[b, tok0:tok0 + 128, :], z, r=[b_z], w=[b_zab])
                ui += 1


SEGS = ((0, CTX), (CTX, T))
CPIECES = [(0, CTX)] + [(CTX + 512 * q_, CTX + 512 * (q_ + 1)) for q_ in range(4)]
ADA1_IN_CONV = bool(int(_os.environ.get("ADA1_IN_CONV", "1")))
PE_CONV = bool(int(_os.environ.get("PE_CONV", "0")))


def phase_l0_conv(kb, IN):
    P = kb.P
    qkvu, b_qkvu = kb.dr["qkvu"]
    qkvn, b_qkvn = kb.dr["qkvn"]
    with kb.phase():
        cwr, b_cwr = kb.sb([5, 1536], F32, "cwr")
        P.dma("sp", cwr, IN["dn_conv_w"][0], w=[b_cwr])
        pcw, b_pcw = kb.ps([128, 12, 5], F32, "pcw")
        for c in range(12):
            P.op("pe", lambda e: e.transpose(out=pcw[:, c, :], in_=cwr[0:5, c * 128:(c + 1) * 128],
                                             identity=kb.ident[0:5, 0:5]), r=[b_cwr, kb.b_ident], w=[b_pcw])
        cw, b_cw = kb.sb([128, 12, 5], F32, "cw")
        P.op("dve", lambda e: e.tensor_copy(out=cw, in_=pcw), r=[b_pcw], w=[b_cw])
        epsc, b_epsc = kb.sb([128, 1], F32, "epsc")
        P.op("pool", lambda e: e.memset(epsc, EPS), w=[b_epsc])
        bufs = [dict(x=kb.sb([128, T], F32, "cx"), a=kb.sb([128, T], F32, "ca"), y=kb.sb([128, T], F32, "cy"),
                     q=kb.sb([128, T], F32, "cq")) for _ in range(2)]
        pss = [kb.ps([128, 512], F32, "cps") for _ in range(3)]
        pcv = [kb.ps([128, 512], F32, "pcv") for _ in range(1)] * 2
        ada_it = adaln_gen(kb, 1, IN) if ADA1_IN_CONV else iter(())
        next(ada_it, None)
        diags = [kb.sb([128, 5, 128], F32, "cdiag") for _ in range(2)]
        it = 0
        for b in range(NB):
            for c in range(12):
                B_ = bufs[it % 2]
                (x, b_x), (a, b_a), (y, b_y), (q, b_q) = B_["x"], B_["a"], B_["y"], B_["q"]
                P.dma("sp", x, qkvu[b, c * 128:(c + 1) * 128, :], r=[b_qkvu], w=[b_x])
                if PE_CONV and (it % 2 == 1):
                    (dg_, b_dg) = diags[(it // 2) % 2]
                    for j in range(5):
                        P.op("pool", lambda e: e.tensor_scalar(out=dg_[:, j, :], in0=kb.ident, scalar1=cw[:, c, j:j + 1], scalar2=None, op0=ALU.mult),
                             r=[kb.b_ident, b_cw], w=[b_dg])
                    for pi, (p0, p1) in enumerate(CPIECES):
                        ps, b_ps = pcv[pi % 2]
                        s0, s1 = (0, CTX) if p0 < CTX else (CTX, T)
                        for ji, j in enumerate((2, 0, 1, 3, 4)):
                            o = j - 2
                            t0, t1 = max(p0, s0 - o), min(p1, s1 - o)
                            P.op("pe", lambda e: e.matmul(ps[:, t0 - p0:t1 - p0], lhsT=dg_[:, j, :], rhs=x[:, t0 + o:t1 + o],
                                                          start=(ji == 0), stop=(ji == 4)), r=[b_dg, b_x], w=[b_ps])
                        P.op("act", lambda e: e.activation(out=y[:, p0:p1], in_=ps[:, 0:p1 - p0], func=AF.Silu), r=[b_ps], w=[b_y])
                else:
                    P.op("act", lambda e: e.activation(out=a, in_=x, func=AF.Copy, scale=cw[:, c, 2:3]), r=[b_x, b_cw], w=[b_a])
                    for j in (0, 1, 3, 4):
                        o = j - 2
                        for (s0, s1) in SEGS:
                            t0, t1 = max(s0, s0 - o), min(s1, s1 - o)
                            P.op("dve", lambda e: e.scalar_tensor_tensor(out=a[:, t0:t1], in0=x[:, t0 + o:t1 + o],
                                                                         scalar=cw[:, c, j:j + 1], in1=a[:, t0:t1],
                                                                         op0=ALU.mult, op1=ALU.add),
                                 r=[b_x, b_cw, b_a], w=[b_a])
                    P.op("act", lambda e: e.activation(out=y, in_=a, func=AF.Silu), r=[b_a], w=[b_y])
                if c < 8:
                    P.op("act", lambda e: e.activation(out=q, in_=y, func=AF.Square), r=[b_y], w=[b_q])
                    for pi, t0 in enumerate(range(0, T, 512)):
                        t1 = min(T, t0 + 512)
                        ps, b_ps = pss[pi % 3]
                        P.op("pe", lambda e: e.matmul(ps[:, 0:t1 - t0], lhsT=kb.ones, rhs=q[:, t0:t1], start=True, stop=True),
                             r=[kb.b_ones, b_q], w=[b_ps])
                        P.op("act", lambda e: e.activation(out=a[:, t0:t1], in_=ps[:, 0:t1 - t0], func=AF.Sqrt, bias=epsc[:, 0:1]),
                             r=[b_ps, b_epsc], w=[b_a])
                    P.op("dve", lambda e: e.reciprocal(out=q, in_=a), r=[b_a], w=[b_q])
                    sc = (128.0 ** -0.5) if c < 4 else 1.0
                    P.op("dve", lambda e: e.scalar_tensor_tensor(out=y, in0=y, scalar=sc, in1=q, op0=ALU.mult, op1=ALU.mult),
                         r=[b_y, b_q], w=[b_y])
                P.dma("sp", qkvn[b, c * 128:(c + 1) * 128, :], y, r=[b_y], w=[b_qkvn])
                it += 1
                if it % 2 == 0:
                    next(ada_it, None)
        for _ in ada_it:
            pass


BIG = 30000.0


def phase_l0_delta(kb, IN):
    P = kb.P
    qkvn, b_qkvn = kb.dr["qkvn"]
    zab, b_zab = kb.dr["zab"]
    mixT, b_mixT = kb.dr["mixT"]
    ident, b_ident, ones, b_ones = kb.ident, kb.b_ident, kb.ones, kb.b_ones
    with kb.phase():
        def mk(nm):
            return kb.sb([128, 128], F32, nm)
        (Lo, b_Lo), (Up, b_Up) = mk("Lo"), mk("Up")
        for (M, b_M, op_) in ((Lo, b_Lo, ALU.is_ge), (Up, b_Up, ALU.is_ge)):
            P.op("pool", lambda e: e.memset(M, 0.0), w=[b_M])
            for r0 in (0, 64):
                blk = M[r0:r0 + 64, r0:r0 + 64]
                P.op("pool", lambda e: e.memset(blk, 1.0), r=[b_M], w=[b_M])
                if M is Lo:
                    P.op("pool", lambda e: e.affine_select(out=blk, in_=blk, pattern=[[-1, 64]], compare_op=ALU.is_ge,
                                                           fill=0.0, base=0, channel_multiplier=1), r=[b_M], w=[b_M])
                else:
                    P.op("pool", lambda e: e.affine_select(out=blk, in_=blk, pattern=[[1, 64]], compare_op=ALU.is_ge,
                                                           fill=0.0, base=0, channel_multiplier=-1), r=[b_M], w=[b_M])
        masks = {}
        for nm, (M, b_M) in (("Lo", (Lo, b_Lo)), ("Up", (Up, b_Up))):
            for sgn in (1.0, -1.0):
                N_, b_N = mk("N" + nm)
                P.op("dve", lambda e: e.tensor_scalar(out=N_, in0=M, scalar1=-sgn * BIG, scalar2=sgn * BIG,
                                                      op0=ALU.mult, op1=ALU.add), r=[b_M], w=[b_N])
                masks[(nm, sgn)] = (N_, b_N)
        dirs = [dict(LT=(Up, b_Up), M1=masks[("Lo", 1.0)], M2=masks[("Up", -1.0)], last=(63, 127), order=(0, 64)),
                dict(LT=(Lo, b_Lo), M1=masks[("Up", 1.0)], M2=masks[("Lo", -1.0)], last=(0, 64), order=(64, 0))]
        dtb, b_dtb = kb.sb([128, 8], F32, "dtb")
        P.dma("sp", dtb, IN["dn_dt_bias"][0].rearrange("a b -> (a b)").partition_broadcast(128), w=[b_dtb])
        nA, b_nA = kb.sb([128, 8], F32, "nA")
        P.dma("sp", nA, IN["dn_A_log"][0].rearrange("a b -> (a b)").partition_broadcast(128), w=[b_nA])
        P.op("act", lambda e: e.activation(out=nA, in_=nA, func=AF.Exp), r=[b_nA], w=[b_nA])
        P.op("dve", lambda e: e.tensor_scalar(out=nA, in0=nA, scalar1=-1.0, scalar2=None, op0=ALU.mult), r=[b_nA], w=[b_nA])
        gB, b_gB = kb.sb([128, 128], F32, "gB")
        P.dma("sp", gB, IN["dn_norm_g"][0].partition_broadcast(128), w=[b_gB])

        gat, b_gat = kb.sb([128, NT, 16], F32, "gat")
        gg, b_gg = kb.sb([128, NT, 8], F32, "gg")
        bet, b_bet = kb.sb([128, NT, 8], F32, "bet")
        qT, b_qT = kb.sb([128, T], F32, "qT")
        kT, b_kT = kb.sb([128, T], F32, "kT")
        vT, b_vT = kb.sb([128, T], F32, "vT")
        zt, b_zt = kb.sb([128, NT, 128], F32, "zt")
        dst_, b_dst = kb.sb([128, 4, NT], F32, "dnst")
        qTb, b_qTb = kb.sb([128, T], BF16, "qTb")
        kTb, b_kTb = kb.sb([128, T], BF16, "kTb")
        vTb, b_vTb = kb.sb([128, T], BF16, "vTb")
        identb, b_identb = kb.identb, kb.b_identb
        osums = [kb.sb([128, NT, 128], F32, "osum") for _ in range(2)]
        aT, b_aT = kb.sb([128, T], F32, "aT")
        banks = [kb.ps([128, 4, 128], F32, "dps") for _ in range(8)]

        def slot(bk, i):
            ap, bf = banks[bk]
            return ap[:, i, :], bf
        chains = []
        NTMP = 21
        for c in range(2):
            o = 4 * c
            chains.append(dict(
                ps=[slot(o, 0), slot(o, 1), slot(o, 2), slot(o, 3), slot(o + 1, 0), slot(o + 1, 1), slot(o + 1, 2), slot(o + 1, 3),
                    slot(o + 2, 0), slot(o + 2, 1), slot(o + 2, 2), slot(o + 2, 3)],
                prec=[slot(o + 3, 0), slot(o + 3, 1), slot(o + 3, 2)],
                tset=[[kb.sb([128, 128], F32, "dt%d" % i) for i in range(NTMP)] for _ in range(2)],
                tsetb=[[kb.sb([128, 128], BF16, "db%d" % i) for i in range(14)] for _ in range(2)],
                Sb=kb.sb([128, 128], BF16, "Sb"),
                cset=[kb.sb([128, 8], F32, "dc") for _ in range(2)],
                vnew=[kb.sb([128, 128], BF16, "vnew") for _ in range(2)],
                S=kb.sb([128, 128], F32, "S"),
                osum=osums[c]))
        import os
        lim = [int(v) for v in os.environ.get("DELTA_LIM", "2,4,2").split(",")]

        def chain_gen(ch, d, h):
            dd = dirs[d]
            (LT, b_LT), (M1, b_M1), (M2, b_M2) = dd["LT"], dd["M1"], dd["M2"]
            col = d * 4 + h
            S, b_S = ch["S"]
            osum, b_osum = ch["osum"]
            P.op("pool", lambda e: e.memset(S, 0.0), w=[b_S])
            Sb_, b_Sb_ = ch["Sb"]
            P.op("pool", lambda e: e.memset(Sb_, 0.0), w=[b_Sb_])
            order = list(range(NT)) if d == 0 else [1, 0] + list(range(NT - 1, 1, -1))
            for it, j in enumerate(order):
                tm = ch["tset"][it % 2]
                (cs, b_cs) = ch["cset"][it % 2]
                tk = slice(j * 128, (j + 1) * 128)
                tb_ = ch["tsetb"][it % 2]
                (Lg, b_Lg), (Erow, b_Erow), (dec, b_dec), (decT, b_decT), (u, b_u) = tm[0:5]
                (A, b_A), (AT, b_AT), (TT0, b_TT0), (TT1, b_TT1) = tb_[0:4]
                Pm = [tb_[4], tb_[5]]
                PTm = [tb_[6], tb_[7]]
                (Xu, b_Xu), (Xw, b_Xw), (wT, b_wT), (qdT, b_qdT), (kdec, b_kdec), (qkT, b_qkT) = tb_[8:14]
                Sb, b_Sb = ch["Sb"]
                (pGr, b_pGr), (pgc, b_pgc), (pKK, b_pKK), (pQK, b_pQK) = ch["ps"][0:4]
                (pAT, b_pAT), (pP, b_pP), (pPT, b_pPT), (pTT, b_pTT) = ch["ps"][4:8]
                (pu, b_pu), (pwT, b_pwT), (pkt, b_pkt), (pvt, b_pvt) = ch["ps"][8:12]
                pATb = pAT.bitcast(BF16)[:, 0:128]
                pktb = pkt.bitcast(BF16)[:, 0:128]
                pvtb = pvt.bitcast(BF16)[:, 0:128]
                gcol = gg[:, j, col:col + 1]
                bcol = bet[:, j, col:col + 1]
                P.op("dve", lambda e: e.tensor_scalar(out=Lg, in0=LT, scalar1=gcol, scalar2=None, op0=ALU.mult),
                     r=[b_LT, b_gg], w=[b_Lg])
                P.op("pe", lambda e: e.matmul(pGr, lhsT=ones, rhs=Lg, start=True, stop=True), r=[b_ones, b_Lg], w=[b_pGr])
                P.op("pe", lambda e: e.matmul(pgc[:, 0:1], lhsT=Lg, rhs=ones[:, 0:1], start=True, stop=True),
                     r=[b_ones, b_Lg], w=[b_pgc])
                P.op("pe", lambda e: e.matmul(pKK, lhsT=kTb[:, tk], rhs=kTb[:, tk], start=True, stop=True), r=[b_kTb], w=[b_pKK])
                P.op("pe", lambda e: e.matmul(pQK, lhsT=kTb[:, tk], rhs=qTb[:, tk], start=True, stop=True), r=[b_kTb, b_qTb], w=[b_pQK])
                yield
                P.op("dve", lambda e: e.tensor_copy(out=cs[:, 0:1], in_=pgc[:, 0:1]), r=[b_pgc], w=[b_cs])
                P.op("act", lambda e: e.activation(out=cs[:, 1:2], in_=pgc[:, 0:1], func=AF.Copy, scale=-1.0), r=[b_pgc], w=[b_cs])
                P.op("dve", lambda e: e.tensor_tensor(out=dec, in0=pGr, in1=M1, op=ALU.add), r=[b_pGr, b_M1], w=[b_dec])
                P.op("dve", lambda e: e.tensor_tensor(out=decT, in0=pGr, in1=M2, op=ALU.add), r=[b_pGr, b_M2], w=[b_decT])
                P.op("act", lambda e: e.activation(out=Erow, in_=pGr, func=AF.Exp), r=[b_pGr], w=[b_Erow])
                for ci, r0 in enumerate((0, 64)):
                    lc = dd["last"][ci]
                    P.op("act", lambda e: e.activation(out=cs[r0:r0 + 64, 3:4], in_=pGr[r0:r0 + 64, lc:lc + 1], func=AF.Exp,
                                                       bias=cs[r0:r0 + 64, 1:2], scale=1.0), r=[b_pGr, b_cs], w=[b_cs])
                P.op("act", lambda e: e.activation(out=dec, in_=dec, func=AF.Exp, bias=cs[:, 0:1], scale=-1.0), r=[b_dec, b_cs], w=[b_dec])
                P.op("act", lambda e: e.activation(out=decT, in_=decT, func=AF.Exp, bias=cs[:, 1:2], scale=1.0), r=[b_decT, b_cs], w=[b_decT])
                P.op("act", lambda e: e.activation(out=cs[:, 2:3], in_=cs[:, 0:1], func=AF.Exp), r=[b_cs], w=[b_cs])
                P.op("dve", lambda e: e.tensor_tensor(out=cs[:, 4:5], in0=cs[:, 2:3], in1=bcol, op=ALU.mult), r=[b_cs, b_bet], w=[b_cs])
                yield
                P.op("pool", lambda e: e.tensor_tensor(out=dec, in0=dec, in1=ident, op=ALU.subtract), r=[b_dec, b_ident], w=[b_dec])
                P.op("dve", lambda e: e.scalar_tensor_tensor(out=A, in0=pKK, scalar=bcol, in1=dec, op0=ALU.mult, op1=ALU.mult),
                     r=[b_pKK, b_bet, b_dec], w=[b_A])
                P.op("dve", lambda e: e.tensor_tensor(out=qkT, in0=pQK, in1=decT, op=ALU.mult), r=[b_pQK, b_decT], w=[b_qkT])
                P.op("pe", lambda e: e.transpose(out=pATb, in_=A, identity=identb), r=[b_A, b_identb], w=[b_pAT])
                P.op("pe", lambda e: e.transpose(out=pktb, in_=kTb[:, tk], identity=identb), r=[b_kTb, b_identb], w=[b_pkt])
                P.op("pe", lambda e: e.transpose(out=pvtb, in_=vTb[:, tk], identity=identb), r=[b_vTb, b_identb], w=[b_pvt])
                yield
                P.op("act", lambda e: e.copy(out=AT, in_=pATb), r=[b_pAT], w=[b_AT])
                P.op("pool", lambda e: e.tensor_tensor(out=TT0, in0=ident, in1=AT, op=ALU.subtract), r=[b_ident, b_AT], w=[b_TT0])
                P.op("act", lambda e: e.activation(out=Xu, in_=pvtb, func=AF.Copy, scale=bcol), r=[b_pvt, b_bet], w=[b_Xu])
                P.op("act", lambda e: e.activation(out=Xw, in_=pktb, func=AF.Copy, scale=cs[:, 4:5]), r=[b_pkt, b_cs], w=[b_Xw])
                P.op("dve", lambda e: e.tensor_scalar(out=kdec, in0=pktb, scalar1=cs[:, 3:4], scalar2=0.0, op0=ALU.mult, op1=ALU.add),
                     r=[b_pkt, b_cs], w=[b_kdec])
                P.op("pool", lambda e: e.tensor_tensor(out=qdT, in0=qT[:, tk], in1=Erow, op=ALU.mult), r=[b_qT, b_Erow], w=[b_qdT])
                curP, curPT = (A, b_A), (AT, b_AT)
                TTc, TTn = (TT0, b_TT0), (TT1, b_TT1)
                for m in range(1, 6):
                    (nP, b_nP), (nPT, b_nPT) = Pm[m % 2], PTm[m % 2]
                    P.op("pe", lambda e: e.matmul(pP, lhsT=curPT[0], rhs=curP[0], start=True, stop=True),
                         r=[curPT[1], curP[1]], w=[b_pP])
                    if m < 5:
                        P.op("pe", lambda e: e.matmul(pPT, lhsT=curP[0], rhs=curPT[0], start=True, stop=True),
                             r=[curPT[1], curP[1]], w=[b_pPT])
                    yield
                    P.op("act", lambda e: e.copy(out=nP, in_=pP), r=[b_pP], w=[b_nP])
                    if m < 5:
                        P.op("dve", lambda e: e.tensor_copy(out=nPT, in_=pPT), r=[b_pPT], w=[b_nPT])
                    P.op("pe", lambda e: e.matmul(pTT, lhsT=nP, rhs=TTc[0], start=True, stop=True), r=[b_nP, TTc[1]], w=[b_pTT])
                    yield
                    P.op("dve", lambda e: e.tensor_tensor(out=TTn[0], in0=pTT, in1=TTc[0], op=ALU.add),
                         r=[b_pTT, TTc[1]], w=[TTn[1]])
                    curP, curPT = (nP, b_nP), (nPT, b_nPT)
                    TTc, TTn = TTn, TTc
                TT, b_TT = TTc
                P.op("pe", lambda e: e.matmul(pu, lhsT=TT, rhs=Xu, start=True, stop=True), r=[b_TT, b_Xu], w=[b_pu])
                P.op("pe", lambda e: e.matmul(pwT, lhsT=Xw, rhs=TT, start=True, stop=True), r=[b_TT, b_Xw], w=[b_pwT])
                yield
                P.op("act", lambda e: e.copy(out=u, in_=pu), r=[b_pu], w=[b_u])
                P.op("act", lambda e: e.copy(out=wT, in_=pwT), r=[b_pwT], w=[b_wT])
                for r0 in dd["order"]:
                    rs = slice(r0, r0 + 64)
                    lc = dd["last"][r0 // 64]
                    (vn, b_vn) = ch["vnew"][(r0 // 64)]
                    (p1, b_p1), (p2, b_p2), (p3, b_p3) = ch["prec"]
                    P.op("pe", lambda e: e.matmul(p1[rs, :], lhsT=wT[:, rs], rhs=Sb, start=True, stop=True), r=[b_wT, b_Sb], w=[b_p1])
                    yield
                    P.op("dve", lambda e: e.tensor_tensor(out=vn[rs, :], in0=u[rs, :], in1=p1[rs, :], op=ALU.subtract),
                         r=[b_u, b_p1], w=[b_vn])
                    P.op("pe", lambda e: e.matmul(p2[rs, :], lhsT=qdT[:, rs], rhs=Sb, start=True, stop=False), r=[b_qdT, b_Sb], w=[b_p2])
                    P.op("pe", lambda e: e.matmul(p2[rs, :], lhsT=qkT[rs, rs], rhs=vn[rs, :], start=False, stop=True),
                         r=[b_qkT, b_vn], w=[b_p2])
                    P.op("pe", lambda e: e.matmul(p3, lhsT=kdec[rs, :], rhs=vn[rs, :], start=True, stop=True), r=[b_kdec, b_vn], w=[b_p3])
                    yield
                    P.op("act", lambda e: e.copy(out=osum[rs, j, :], in_=p2[rs, :]), r=[b_p2], w=[b_osum])
                    P.op("dve", lambda e: e.scalar_tensor_tensor(out=S, in0=S, scalar=Erow[:, lc:lc + 1], in1=p3,
                                                                 op0=ALU.mult, op1=ALU.add), r=[b_S, b_Erow, b_p3], w=[b_S])
                    P.op("act", lambda e: e.copy(out=Sb, in_=S), r=[b_S], w=[b_Sb])

        for b in range(lim[0]):
            P.dma("sp", gat, zab[b, :, 512:528].rearrange("(j p) c -> p j c", p=128), r=[b_zab], w=[b_gat])
            P.op("dve", lambda e: e.tensor_tensor(out=gg, in0=gat[:, :, 0:8], in1=dtb.unsqueeze(1).to_broadcast([128, NT, 8]),
                                                  op=ALU.add), r=[b_gat, b_dtb], w=[b_gg])
            P.op("act", lambda e: e.activation(out=gg, in_=gg, func=AF.Exp), r=[b_gg], w=[b_gg])
            P.op("act", lambda e: e.activation(out=gg, in_=gg, func=AF.Ln, bias=1.0), r=[b_gg], w=[b_gg])
            P.op("dve", lambda e: e.tensor_tensor(out=gg, in0=gg, in1=nA.unsqueeze(1).to_broadcast([128, NT, 8]),
                                                  op=ALU.mult), r=[b_gg, b_nA], w=[b_gg])
            P.op("act", lambda e: e.activation(out=bet, in_=gat[:, :, 8:16], func=AF.Sigmoid), r=[b_gat], w=[b_bet])
            for h in range(lim[1]):
                P.dma("sp", qT, qkvn[b, h * 128:(h + 1) * 128, :], r=[b_qkvn], w=[b_qT])
                P.dma("sp", kT, qkvn[b, 512 + h * 128:512 + (h + 1) * 128, :], r=[b_qkvn], w=[b_kT])
                P.dma("sp", vT, qkvn[b, 1024 + h * 128:1024 + (h + 1) * 128, :], r=[b_qkvn], w=[b_vT])
                P.dma("sp", zt, zab[b, :, h * 128:(h + 1) * 128].rearrange("(j p) c -> p j c", p=128), r=[b_zab], w=[b_zt])
                P.op("act", lambda e: e.copy(out=qTb, in_=qT), r=[b_qT], w=[b_qTb])
                P.op("pool", lambda e: e.tensor_copy(out=kTb, in_=kT), r=[b_kT], w=[b_kTb])
                P.op("act", lambda e: e.copy(out=vTb, in_=vT), r=[b_vT], w=[b_vTb])
                gens = [chain_gen(chains[d], d, h) for d in range(2)]
                alive = [True, True]
                while any(alive):
                    for gi, g_ in enumerate(gens):
                        if alive[gi]:
                            try:
                                next(g_)
                            except StopIteration:
                                alive[gi] = False
                osum, b_osum = osums[0]
                P.op("pool", lambda e: e.tensor_tensor(out=osum, in0=osum, in1=osums[1][0], op=ALU.add), r=[b_osum, osums[1][1]], w=[b_osum])
                tset = chains[0]["tset"]
                cset = [[chains[0]["cset"][0]], [chains[0]["cset"][1]]]
                pset = [chains[0]["ps"], chains[1]["ps"]]
                itn = 0
                (sqj, b_sqj) = tset[0][0]
                for j in range(NT):
                    P.op("act", lambda e: e.activation(out=sqj, in_=osum[:, j, :], func=AF.Square, accum_out=dst_[:, 0, j:j + 1]),
                         r=[b_osum], w=[b_sqj, b_dst])
                P.op("dve", lambda e: e.tensor_scalar(out=dst_[:, 1, :], in0=dst_[:, 0, :], scalar1=1.0 / 128, scalar2=EPS,
                                                      op0=ALU.mult, op1=ALU.add), r=[b_dst], w=[b_dst])
                P.op("act", lambda e: e.activation(out=dst_[:, 2, :], in_=dst_[:, 1, :], func=AF.Sqrt), r=[b_dst], w=[b_dst])
                P.op("dve", lambda e: e.reciprocal(out=dst_[:, 3, :], in_=dst_[:, 2, :]), r=[b_dst], w=[b_dst])
                P.op("act", lambda e: e.activation(out=zt, in_=zt, func=AF.Silu), r=[b_zt], w=[b_zt])
                P.op("dve", lambda e: e.tensor_tensor(out=osum, in0=osum, in1=dst_[:, 3, :].unsqueeze(2).to_broadcast([128, NT, 128]), op=ALU.mult),
                     r=[b_osum, b_dst], w=[b_osum])
                P.op("pool", lambda e: e.tensor_tensor(out=zt, in0=zt, in1=gB.unsqueeze(1).to_broadcast([128, NT, 128]), op=ALU.mult),
                     r=[b_zt, b_gB], w=[b_zt])
                P.op("dve", lambda e: e.tensor_tensor(out=osum, in0=osum, in1=zt, op=ALU.mult), r=[b_osum, b_zt], w=[b_osum])
                for j0 in range(0, NT, 4):
                    nj = min(4, NT - j0)
                    bk_ap, bk_b = banks[(j0 // 4) % 8]
                    for jj in range(nj):
                        P.op("pe", lambda e: e.transpose(out=bk_ap[:, jj, :], in_=osum[:, j0 + jj, :], identity=ident), r=[b_osum, b_ident], w=[bk_b])
                    src_ = bk_ap[:, 0:nj, :]
                    dstv = aT[:, j0 * 128:(j0 + nj) * 128].rearrange("p (a b) -> p a b", b=128)
                    if (j0 // 4) % 2 == 0:
                        P.op("act", lambda e: e.copy(out=dstv, in_=src_), r=[bk_b], w=[b_aT])
                    else:
                        P.op("dve", lambda e: e.tensor_copy(out=dstv, in_=src_), r=[bk_b], w=[b_aT])
                P.dma("sp", mixT[b, h * 128:(h + 1) * 128, :], aT, r=[b_aT], w=[b_mixT])


def phase_l0_s5(kb, IN):
    P = kb.P
    qkvu, b_qkvu = kb.dr["qkvu"]
    ygT, b_ygT = kb.dr["ygT"]
    ident, b_ident = kb.ident, kb.b_ident
    with kb.phase():
        pbk = [kb.ps([128, 512], F32, "s5ps") for _ in range(7)]
        def stacked_T(src_a, src_b, nm):
            st, b_st = kb.sb([64, 128], F32, nm + "s")
            P.dma("sp", st[:, 0:64], src_a, w=[b_st])
            P.dma("sp", st[:, 64:128], src_b, w=[b_st])
            ps, b_ps = pbk[0]
            P.op("pe", lambda e: e.transpose(out=ps[:, 0:64], in_=st, identity=ident[0:64, 0:64]), r=[b_st, b_ident], w=[b_ps])
            o, b_o = kb.sb([128, 64], F32, nm)
            P.op("dve", lambda e: e.tensor_copy(out=o, in_=ps[:, 0:64]), r=[b_ps], w=[b_o])
            return o, b_o
        Are_src = IN["s5_A_re"][0].rearrange("d g p -> (d g) p")
        Aim_src = IN["s5_A_im"][0].rearrange("d g p -> (d g) p")
        Are, b_Are = stacked_T(Are_src, Are_src, "Are")
        Aim, b_Aim = stacked_T(Aim_src, Aim_src, "Aim")
        dt, b_dt = kb.sb([128, 64], F32, "dt")
        P.dma("sp", dt, IN["s5_log_dt"][0].rearrange("d g -> (d g)").partition_broadcast(128), w=[b_dt])
        P.op("act", lambda e: e.activation(out=dt, in_=dt, func=AF.Exp), r=[b_dt], w=[b_dt])
        NS = 14
        sm = [kb.sb([128, 64], F32, "s5sm%d" % i) for i in range(NS)]
        (rr, b_rr), (th, b_th), (t0_, b_t0), (t1_, b_t1), (cs_, b_cs), (sn_, b_sn), (lre, b_lre), (lim, b_lim) = sm[0:8]
        (den, b_den), (cre, b_cre), (cim, b_cim), (cimS, b_cimS), (creS, b_creS), (t2_, b_t2) = sm[8:14]

        def tt(eng, out, b_out, a, b_a, b, b_b, op):
            P.op(eng, lambda e: e.tensor_tensor(out=out, in0=a, in1=b, op=op), r=[b_a, b_b], w=[b_out])

        def ts(eng, out, b_out, a, b_a, s1, s2, op0, op1):
            P.op(eng, lambda e: e.tensor_scalar(out=out, in0=a, scalar1=s1, scalar2=s2, op0=op0, op1=op1), r=[b_a], w=[b_out])
        tt("dve", rr, b_rr, Are, b_Are, dt, b_dt, ALU.mult)
        P.op("act", lambda e: e.activation(out=rr, in_=rr, func=AF.Exp), r=[b_rr], w=[b_rr])
        tt("dve", th, b_th, Aim, b_Aim, dt, b_dt, ALU.mult)

        def sincos(dst, b_dst, shift):
            ts("dve", t0_, b_t0, th, b_th, shift, 1.0 / TWO_PI, ALU.add, ALU.mult)
            ts("dve", t1_, b_t1, t0_, b_t0, MAGIC, None, ALU.add, ALU.bypass)
            ts("dve", t1_, b_t1, t1_, b_t1, -MAGIC, -TWO_PI, ALU.add, ALU.mult)
            P.op("dve", lambda e: e.scalar_tensor_tensor(out=t0_, in0=th, scalar=shift, in1=t1_, op0=ALU.add, op1=ALU.add),
                 r=[b_th, b_t1], w=[b_t0])
            P.op("act", lambda e: e.activation(out=dst, in_=t0_, func=AF.Sin), r=[b_t0], w=[b_dst])
        sincos(cs_, b_cs, math.pi / 2)
        sincos(sn_, b_sn, 0.0)
        tt("dve", lre, b_lre, rr, b_rr, cs_, b_cs, ALU.mult)
        tt("dve", lim, b_lim, rr, b_rr, sn_, b_sn, ALU.mult)
        ts("dve", lre, b_lre, lre, b_lre, -1.0, None, ALU.add, ALU.bypass)
        tt("dve", den, b_den, Are, b_Are, Are, b_Are, ALU.mult)
        tt("dve", t2_, b_t2, Aim, b_Aim, Aim, b_Aim, ALU.mult)
        tt("dve", den, b_den, den, b_den, t2_, b_t2, ALU.add)
        P.op("dve", lambda e: e.reciprocal(out=den, in_=den), r=[b_den], w=[b_den])
        tt("dve", cre, b_cre, lre, b_lre, Are, b_Are, ALU.mult)
        tt("dve", t2_, b_t2, lim, b_lim, Aim, b_Aim, ALU.mult)
        tt("dve", cre, b_cre, cre, b_cre, t2_, b_t2, ALU.add)
        tt("dve", cre, b_cre, cre, b_cre, den, b_den, ALU.mult)
        tt("dve", cim, b_cim, lim, b_lim, Are, b_Are, ALU.mult)
        tt("dve", t2_, b_t2, lre, b_lre, Aim, b_Aim, ALU.mult)
        tt("dve", cim, b_cim, cim, b_cim, t2_, b_t2, ALU.subtract)
        tt("dve", cim, b_cim, cim, b_cim, den, b_den, ALU.mult)
        ts("dve", cimS[0:64, :], b_cimS, cim[0:64, :], b_cim, -1.0, None, ALU.mult, ALU.bypass)
        P.op("dve", lambda e: e.tensor_copy(out=cimS[64:128, :], in_=cim[64:128, :]), r=[b_cim], w=[b_cimS])
        P.op("dve", lambda e: e.tensor_copy(out=creS[0:64, :], in_=cre[0:64, :]), r=[b_cre], w=[b_creS])
        ts("dve", creS[64:128, :], b_creS, cre[64:128, :], b_cre, -1.0, None, ALU.mult, ALU.bypass)
        Bst1, b_Bst1 = kb.sb([128, 32, 16], F32, "Bst1")
        BstS, b_BstS = kb.sb([128, 32, 16], F32, "BstS")
        Bre_src = IN["s5_B_re"][0].rearrange("g p h -> p g h")
        Bim_src = IN["s5_B_im"][0].rearrange("g p h -> p g h")
        P.dma("sp", Bst1[0:64], Bre_src, w=[b_Bst1]); P.dma("sp", Bst1[64:128], Bim_src, w=[b_Bst1])
        P.dma("sp", BstS[0:64], Bim_src, w=[b_BstS]); P.dma("sp", BstS[64:128], Bre_src, w=[b_BstS])
        Wst, b_Wst = kb.sb([128, 4, 2, 2, 2, 128], BF16, "Wst")
        Wst3, b_Wst3 = kb.sb([128, 4, 2, 2, 2, 128], BF16, "Wst3")
        src, b_src = kb.sb([128, 128], F32, "wsrc")
        wtmp, b_wtmp = kb.sb([128, 16], F32, "wtmp")
        for c in range(4):
            for m in range(2):
                for d in range(2):
                    for var in range(2):
                        P.op("pool", lambda e: e.memset(src, 0.0), w=[b_src])
                        for pr in range(4):
                            g = 8 * c + 2 * pr + m
                            dg = d * 32 + g
                            dst = src[:, 32 * pr + 16 * m:32 * pr + 16 * m + 16]
                            if var == 0:
                                P.op("dve", lambda e: e.tensor_scalar(out=wtmp, in0=Bst1[:, g, :], scalar1=cre[:, dg:dg + 1], scalar2=None,
                                                                      op0=ALU.mult), r=[b_Bst1, b_cre], w=[b_wtmp])
                                P.op("dve", lambda e: e.scalar_tensor_tensor(out=dst, in0=BstS[:, g, :], scalar=cimS[:, dg:dg + 1], in1=wtmp,
                                                                             op0=ALU.mult, op1=ALU.add), r=[b_BstS, b_cimS, b_wtmp], w=[b_src])
                            else:
                                P.op("dve", lambda e: e.tensor_scalar(out=wtmp, in0=BstS[:, g, :], scalar1=creS[:, dg:dg + 1], scalar2=None,
                                                                      op0=ALU.mult), r=[b_BstS, b_creS], w=[b_wtmp])
                                P.op("dve", lambda e: e.scalar_tensor_tensor(out=dst, in0=Bst1[:, g, :], scalar=cim[:, dg:dg + 1], in1=wtmp,
                                                                             op0=ALU.mult, op1=ALU.add), r=[b_Bst1, b_cim, b_wtmp], w=[b_src])
                        ps, b_ps = pbk[1]
                        P.op("pe", lambda e: e.transpose(out=ps[:, 0:128], in_=src, identity=ident), r=[b_src, b_ident], w=[b_ps])
                        P.op("act", lambda e: e.copy(out=Wst[:, c, m, d, var, :], in_=ps[:, 0:128]), r=[b_ps], w=[b_Wst])
                        P.op("act", lambda e: e.copy(out=Wst3[64:128, c, m, d, var, :], in_=ps[64:128, 0:128]), r=[b_ps], w=[b_Wst3])
                        P.op("pool", lambda e: e.memset(Wst3[64:96, c, m, d, var, :], 0.0), r=[b_Wst3], w=[b_Wst3])
        CrPad, b_CrPad = kb.sb([128, 32, 2, 128], BF16, "CrPad")
        P.op("pool", lambda e: e.memset(CrPad, 0.0), w=[b_CrPad])
        Cre_src = IN["s5_C_re"][0].rearrange("g h p -> (g h) p")
        Cim_src = IN["s5_C_im"][0].rearrange("g h p -> (g h) p")
        cst, b_cst = kb.sb([128, 128], F32, "cst")
        for var in range(2):
            for q in range(4):
                rows = slice(q * 128, (q + 1) * 128)
                if var == 0:
                    P.dma("sp", cst[:, 0:64], Cre_src[rows, :], w=[b_cst]); P.dma("sp", cst[:, 64:128], Cim_src[rows, :], w=[b_cst])
                else:
                    P.dma("sp", cst[:, 0:64], Cim_src[rows, :], w=[b_cst]); P.dma("sp", cst[:, 64:128], Cre_src[rows, :], w=[b_cst])
                ps, b_ps = pbk[2]
                P.op("pe", lambda e: e.transpose(out=ps[:, 0:128], in_=cst, identity=ident), r=[b_cst, b_ident], w=[b_ps])
                for gl in range(8):
                    g = q * 8 + gl
                    cols = slice(gl * 16, gl * 16 + 16)
                    sgn_top = 1.0 if var == 0 else -1.0
                    P.op("act", lambda e: e.activation(out=CrPad[0:64, g, var, cols], in_=ps[0:64, cols], func=AF.Copy, scale=sgn_top),
                         r=[b_ps], w=[b_CrPad])
                    P.op("act", lambda e: e.activation(out=CrPad[64:128, g, var, cols], in_=ps[64:128, cols], func=AF.Copy, scale=-1.0),
                         r=[b_ps], w=[b_CrPad])
        Dc, b_Dc = kb.sb([128, 4], F32, "Dc")
        kb.load_vec_fm(IN["s5_D"][0].rearrange("g h -> (g h)"), 4, Dc, b_Dc, pbk[0])
        from concourse.mybir import dt as _dt
        iof, b_iof = kb.sb([128, T], F32, "iof")
        with kb.phase():
            ioi, b_ioi = kb.sb([128, T], _dt.int32, "ioi")
            P.op("pool", lambda e: e.iota(ioi, pattern=[[1, T]], base=0, channel_multiplier=0), w=[b_ioi])
            P.op("dve", lambda e: e.tensor_copy(out=iof, in_=ioi), r=[b_ioi], w=[b_iof])
        ub = [kb.sb([128, T], BF16, "ub") for _ in range(NB)]
        ysb = [kb.sb([128, T], F32, "ysb") for _ in range(NB)]
        tabs = [dict(C=kb.sb([128, T], F32, "tC"), S=kb.sb([128, T], F32, "tS")) for _ in range(2)]
        ph1, b_ph1 = kb.sb([128, T], F32, "ph1")
        ph2, b_ph2 = ph1, b_ph1
        tmps5 = [dict(bt=kb.sb([128, T], F32, "bt"), tmp=kb.sb([128, T], F32, "tmp"), ww=kb.sb([128, T], F32, "ww"),
                      Q1=kb.sb([128, T], BF16, "Q1"), Q2=kb.sb([128, T], BF16, "Q2")) for _ in range(2)]
        s5it = 0
        pieces = [(0, 256)] + [(256 + 512 * q, 256 + 512 * (q + 1)) for q in range(4)]

        def possl(d, t0, t1):
            if d == 0:
                return slice(t0, t1)
            if t1 <= CTX:
                hi, lo = CTX - 1 - t0, CTX - t1
            else:
                hi, lo = (T + CTX - 1) - t0, (T + CTX) - t1
            return slice(hi, lo - 1 if lo > 0 else None, -1)
        import os
        glim = int(os.environ.get("S5_GLIM", "32"))
        tix = 0
        for c in range(4):
            for b in range(NB):
                P.dma("sp", ysb[b][0], qkvu[b, 1536 + c * 128:1536 + (c + 1) * 128, :], r=[b_qkvu], w=[ysb[b][1]])
                P.op("act", lambda e: e.copy(out=ub[b][0], in_=ysb[b][0]), r=[ysb[b][1]], w=[ub[b][1]])
                P.op("dve", lambda e: e.tensor_scalar(out=ysb[b][0], in0=ysb[b][0], scalar1=Dc[:, c:c + 1], scalar2=None, op0=ALU.mult),
                     r=[ysb[b][1], b_Dc], w=[ysb[b][1]])
            its = [(gl, d, b) for gl in range(8) if 8 * c + gl < glim for d in range(2) for b in range(NB)]
            nit = len(its)

            def info(t):
                gl, d, b = its[t]
                g = 8 * c + gl
                pr, m = gl // 2, gl % 2
                tb = tabs[(t // 2) % 2]
                tq = tmps5[t % 2]
                return g, d, b, pr, m, d * 32 + g, tb["C"], tb["S"], tq

            def S_tables(t):
                g, d, b, pr, m, dg, (tC, b_tC), (tS, b_tS), tq = info(t)
                for (tab, b_tab, shift, ph, b_ph) in ((tC, b_tC, math.pi / 2, ph1, b_ph1), (tS, b_tS, 0.0, ph2, b_ph2)):
                    P.op("act", lambda e: e.activation(out=ph, in_=iof, func=AF.Identity, scale=th[:, dg:dg + 1],
                                                       bias=kb.shiftc[shift][0][:, 0:1]), r=[b_iof, b_th, kb.shiftc[shift][1]], w=[b_ph])
                    P.op("act", lambda e: e.activation(out=tab, in_=ph, func=AF.Identity, scale=1.0 / TWO_PI,
                                                       bias=kb.shiftc[MAGIC][0][:, 0:1]), r=[b_ph, kb.shiftc[MAGIC][1]], w=[b_tab])
                    P.op("act", lambda e: e.activation(out=tab, in_=tab, func=AF.Identity, scale=1.0,
                                                       bias=kb.shiftc[-MAGIC][0][:, 0:1]), r=[b_tab, kb.shiftc[-MAGIC][1]], w=[b_tab])
                    P.op("dve", lambda e: e.scalar_tensor_tensor(out=ph, in0=tab, scalar=-TWO_PI, in1=ph, op0=ALU.mult, op1=ALU.add),
                         r=[b_tab, b_ph], w=[b_ph])
                    P.op("act", lambda e: e.activation(out=tab, in_=ph, func=AF.Sin), r=[b_ph], w=[b_tab])

            def S_A(t):
                g, d, b, pr, m, dg, (tC, b_tC), (tS, b_tS), tq = info(t)
                (bt, b_bt), (tmp, b_tmp) = tq["bt"], tq["tmp"]
                prs = slice(32 * pr, 32 * pr + 32)
                for pi, (t0, t1) in enumerate(pieces):
                    n = t1 - t0
                    psl = possl(d, t0, t1)
                    (pA1, b_pA1), (pA2, b_pA2) = pbk[(pi % 2) * 2], pbk[(pi % 2) * 2 + 1]
                    Wuse, b_Wuse, prs_ = (Wst, b_Wst, prs) if pr < 3 else (Wst3, b_Wst3, slice(64, 128))
                    P.op("pe", lambda e: e.matmul(pA1[:, 0:n], lhsT=Wuse[prs_, c, m, d, 0, :], rhs=ub[b][0][prs_, psl], start=True, stop=True),
                         r=[b_Wuse, ub[b][1]], w=[b_pA1])
                    P.op("pe", lambda e: e.matmul(pA2[:, 0:n], lhsT=Wuse[prs_, c, m, d, 1, :], rhs=ub[b][0][prs_, psl], start=True, stop=True),
                         r=[b_Wuse, ub[b][1]], w=[b_pA2])
                    P.op("dve", lambda e: e.tensor_tensor(out=bt[:, t0:t1], in0=pA1[:, 0:n], in1=tC[:, t0:t1], op=ALU.mult),
                         r=[b_pA1, b_tC], w=[b_bt])
                    P.op("dve", lambda e: e.tensor_tensor(out=tmp[:, t0:t1], in0=pA2[:, 0:n], in1=tS[:, t0:t1], op=ALU.mult),
                         r=[b_pA2, b_tS], w=[b_tmp])

            def S_BC(t):
                g, d, b, pr, m, dg, (tC, b_tC), (tS, b_tS), tq = info(t)
                (bt, b_bt), (tmp, b_tmp), (ww, b_ww) = tq["bt"], tq["tmp"], tq["ww"]
                P.op("pool", lambda e: e.tensor_tensor(out=bt, in0=bt, in1=tmp, op=ALU.add), r=[b_bt, b_tmp], w=[b_bt])
                P.op("dve", lambda e: e.tensor_tensor_scan(out=ww, data0=rr[:, dg:dg + 1].to_broadcast([128, T]), data1=bt,
                                                           initial=0.0, op0=ALU.mult, op1=ALU.add), r=[b_bt, b_rr], w=[b_ww])

            def S_D(t):
                g, d, b, pr, m, dg, (tC, b_tC), (tS, b_tS), tq = info(t)
                (ww, b_ww), (Q1, b_Q1), (Q2, b_Q2) = tq["ww"], tq["Q1"], tq["Q2"]
                P.op("pool", lambda e: e.tensor_tensor(out=Q1, in0=ww, in1=tC, op=ALU.mult), r=[b_ww, b_tC], w=[b_Q1])
                P.op("pool", lambda e: e.tensor_tensor(out=Q2, in0=ww, in1=tS, op=ALU.mult), r=[b_ww, b_tS], w=[b_Q2])

            def S_E(t):
                g, d, b, pr, m, dg, (tC, b_tC), (tS, b_tS), tq = info(t)
                (Q1, b_Q1), (Q2, b_Q2) = tq["Q1"], tq["Q2"]
                for pi, (t0, t1) in enumerate(pieces):
                    n = t1 - t0
                    psl = possl(d, t0, t1)
                    (pY, b_pY) = pbk[4 + pi % 3]
                    P.op("pe", lambda e: e.matmul(pY[:, 0:n], lhsT=CrPad[:, g, 0, :], rhs=Q1[:, t0:t1], start=True, stop=False),
                         r=[b_CrPad, b_Q1], w=[b_pY])
                    P.op("pe", lambda e: e.matmul(pY[:, 0:n], lhsT=CrPad[:, g, 1, :], rhs=Q2[:, t0:t1], start=False, stop=True),
                         r=[b_CrPad, b_Q2], w=[b_pY])
                    yv = ysb[b][0][:, psl]
                    P.op("dve", lambda e: e.tensor_tensor(out=yv, in0=pY[:, 0:n], in1=yv, op=ALU.add),
                         r=[b_pY, ysb[b][1]], w=[ysb[b][1]])

            for step in range(nit + 3):
                if step < nit and step % 2 == 0:
                    S_tables(step)
                if step < nit:
                    S_A(step)
                if 0 <= step - 1 < nit:
                    S_BC(step - 1)
                if 0 <= step - 2 < nit:
                    S_D(step - 2)
                if 0 <= step - 3 < nit:
                    S_E(step - 3)
            for b in range(NB):
                P.op("act", lambda e: e.activation(out=ysb[b][0], in_=ysb[b][0], func=AF.Gelu), r=[ysb[b][1]], w=[ysb[b][1]])
                P.dma("sp", ygT[b, c * 128:(c + 1) * 128, :], ysb[b][0], r=[ysb[b][1]], w=[b_ygT])


def load_gate_rows(kb, li, off):
    P = kb.P
    modd, b_modd = kb.dr["modd%d" % li]
    out = []
    for c in range(3):
        g, b_g = kb.sb([128, 1024], F32, "gB")
        P.dma("sp", g, modd[c, off:off + 1024].partition_broadcast(128), r=[b_modd], w=[b_g])
        out.append((g, b_g))
    return out


def load_wchunks_bf16(kb, dst, b_dst, src2d, nk, cols, stage, engines=("pool",)):
    P = kb.P
    for k in range(nk):
        st, b_st = stage[kb.uid % len(stage)]
        kb.uid += 1
        P.dma("sp", st[:, 0:cols], src2d[k * 128:(k + 1) * 128, :], w=[b_st])
        eng = engines[k % len(engines)]
        if eng == "act":
            P.op("act", lambda e: e.copy(out=dst[:, k, :], in_=st[:, 0:cols]), r=[b_st], w=[b_dst])
        else:
            P.op(eng, lambda e: e.tensor_copy(out=dst[:, k, :], in_=st[:, 0:cols]), r=[b_st], w=[b_dst])


def phase_l0_out(kb, IN):
    P = kb.P
    xs, b_xs = kb.dr["xs"]
    mixT, b_mixT = kb.dr["mixT"]
    ygT, b_ygT = kb.dr["ygT"]
    with kb.phase():
        gB = load_gate_rows(kb, 0, 2048)
        stage = [kb.sb([128, 1024], F32, "wst") for _ in range(2)]
        Wglu, b_Wglu = kb.sb([128, 4, 512], BF16, "wglu")
        load_wchunks_bf16(kb, Wglu, b_Wglu, IN["s5_glu_w"][0], 4, 512, stage, ("pool", "act", "dve"))
        Wout, b_Wout = kb.sb([128, 8, 1024], BF16, "wout")
        load_wchunks_bf16(kb, Wout, b_Wout, IN["ab_w_out"][0], 8, 1024, stage, ("pool", "act", "dve"))
        pgl = [kb.ps([128, 512], F32, "pgl") for _ in range(2)]
        pdd = [kb.ps([128, 512], F32, "pdd") for _ in range(2)]
        glub, b_glub = kb.sb([128, 4], F32, "glub")
        kb.load_vec_fm(IN["s5_glu_b"][0], 4, glub, b_glub, pgl[0])
        ygs = [kb.sb([128, 4, 256], F32, "yg32") for _ in range(2)]
        ygbs = [kb.sb([128, 4, 256], BF16, "ygb") for _ in range(2)]
        a32s = [kb.sb([128, 4, 256], F32, "a32") for _ in range(2)]
        mixbs = [kb.sb([128, 8, 256], BF16, "mixb") for _ in range(2)]
        sigs = [kb.sb([128, 256], F32, "sig") for _ in range(2)]
        xts = [kb.sb([128, 1024], F32, "xt") for _ in range(2)]
        tms = [kb.sb([128, 512], F32, "tm") for _ in range(2)]
        ui = 0
        for b in range(NB):
            for un in range(T // 256):
                tsl = slice(un * 256, (un + 1) * 256)
                (yg, b_yg), (ygb, b_ygb), (a32, b_a32), (mixb, b_mixb) = ygs[ui % 2], ygbs[ui % 2], a32s[ui % 2], mixbs[ui % 2]
                ui += 1
                P.dma("sp", yg, ygT[b, :, tsl].rearrange("(c p) t -> p c t", p=128), r=[b_ygT], w=[b_yg])
                P.dma("sp", a32, mixT[b, 0:512, tsl].rearrange("(c p) t -> p c t", p=128), r=[b_mixT], w=[b_a32])
                P.op("act", lambda e: e.copy(out=ygb, in_=yg), r=[b_yg], w=[b_ygb])
                P.op("pool", lambda e: e.tensor_copy(out=mixb[:, 0:4, :], in_=a32), r=[b_a32], w=[b_mixb])
                for f in range(4):
                    ps, b_ps = pgl[f % 2]
                    sg, b_sg = sigs[f % 2]
                    for k in range(4):
                        P.op("pe", lambda e: e.matmul(ps[:, 0:256], lhsT=Wglu[:, k, f * 128:(f + 1) * 128], rhs=ygb[:, k, :],
                                                      start=(k == 0), stop=(k == 3)), r=[b_Wglu, b_ygb], w=[b_ps])
                    P.op("act", lambda e: e.activation(out=sg, in_=ps[:, 0:256], func=AF.Sigmoid, bias=glub[:, f:f + 1]),
                         r=[b_ps, b_glub], w=[b_sg])
                    P.op("dve", lambda e: e.tensor_tensor(out=mixb[:, 4 + f, :], in0=yg[:, f, :], in1=sg, op=ALU.mult),
                         r=[b_yg, b_sg], w=[b_mixb])
                for tt in range(2):
                    tok0 = un * 256 + tt * 128
                    cond = 2 if tok0 < CTX else b
                    xt, b_xt = xts[tt]
                    P.dma("sp", xt, xs[b, tok0:tok0 + 128, :], r=[b_xs], w=[b_xt])
                    for half in range(2):
                        pd, b_pd = pdd[half]
                        tm, b_tm = tms[half]
                        hs = slice(half * 512, (half + 1) * 512)
                        for k in range(8):
                            P.op("pe", lambda e: e.matmul(pd, lhsT=mixb[:, k, tt * 128:(tt + 1) * 128], rhs=Wout[:, k, hs],
                                                          start=(k == 0), stop=(k == 7)), r=[b_mixb, b_Wout], w=[b_pd])
                        P.op("dve", lambda e: e.tensor_tensor(out=tm, in0=pd, in1=gB[cond][0][:, hs], op=ALU.mult),
                             r=[b_pd, gB[cond][1]], w=[b_tm])
                        P.op("pool", lambda e: e.tensor_tensor(out=xt[:, hs], in0=xt[:, hs], in1=tm, op=ALU.add),
                             r=[b_xt, b_tm], w=[b_xt])
                    P.dma("sp", xs[b, tok0:tok0 + 128, :], xt, r=[b_xt], w=[b_xs])


def phase_moe(kb, IN, li, tok_lo):
    P = kb.P
    xs, b_xs = kb.dr["xs"]
    ident, b_ident = kb.ident, kb.b_ident
    ntile_all = (T - tok_lo) // 128
    nblk = int(_os.environ.get("MOE_NBLK", "1"))
    tiles_per_blk = ntile_all // nblk
    TB = tiles_per_blk * 128
    with kb.phase():
        modF, b_modF, _, (A2, b_A2) = load_mod_fm(kb, li, IN, ("norm1_g", "norm2_g"))
        rw, b_rw = kb.sb([128, 8, 16], F32, "rw")
        P.dma("sp", rw, IN["router_w"].rearrange("(k p) e -> p k e", p=128), w=[b_rw])
        rb, b_rb = kb.sb([128, 16], F32, "rb")
        P.dma("sp", rb, IN["router_bias"].partition_broadcast(128), w=[b_rb])
        Esel, b_Esel = kb.sb([16, 16, 128], F32, "Esel")
        P.op("dve", lambda e: e.tensor_copy(out=Esel, in_=ident[0:16, 0:16].unsqueeze(2).to_broadcast([16, 16, 128])),
             r=[b_ident], w=[b_Esel])
        hT, b_hT = kb.sb([128, 8, TB], BF16, "hTm")
        gT, b_gT = kb.sb([16, TB], F32, "gT")
        import os
        elim = int(os.environ.get("MOE_ELIM", "16"))
        for b in range(NB):
            for blk in range(nblk):
                base = tok_lo + blk * TB
                with kb.phase():
                    GB = 3 if tiles_per_blk % 3 == 0 else 4
                    nmb = make_nm_bufs(kb, GB, 4)
                    h32s = [kb.sb([128, 8, 128], F32, "h32") for _ in range(GB)]
                    plog, b_plog = kb.ps([128, 512], F32, "plog")
                    pgT, b_pgT = kb.ps([128, 512], F32, "pgT")
                    nt_ = tiles_per_blk
                    h32all, b_h32all = kb.sb([128, 8, GB * 128], F32, "h32all")
                    for j0 in range(0, tiles_per_blk, GB):
                        toks_ = [base + (j0 + i) * 128 for i in range(GB)]
                        norm_mod_batch(kb, xs[b], b_xs, toks_, [2 if t_ < CTX else b for t_ in toks_], A2, b_A2, modF, b_modF, 24,
                                       nmb, [(hT, b_hT, lambda i, j0=j0: (j0 + i) * 128), (h32all, b_h32all, lambda i: i * 128)])
                        for i in range(GB):
                            j = j0 + i
                            for k in range(8):
                                P.op("pe", lambda e: e.matmul(plog[:, j * 16:(j + 1) * 16], lhsT=h32all[:, k, i * 128:(i + 1) * 128], rhs=rw[:, k, :],
                                                              start=(k == 0), stop=(k == 7)), r=[b_h32all, b_rw], w=[b_plog])
                    def T3(nm, inner=16):
                        return kb.sb([128, nt_, inner], F32, nm)
                    (sc, b_sc), (ch, b_ch), (em, b_em), (cm, b_cm), (sel, b_sel) = T3("sc"), T3("ch"), T3("em"), T3("cm"), T3("sel")
                    (q4, b_q4) = kb.sb([128, 4, nt_, 4], F32, "q4")
                    (r4, b_r4) = kb.sb([128, 4, nt_, 4], F32, "r4")
                    (gs, b_gs), (gm, b_gm) = T3("gs", 4), T3("gm", 4)
                    (col, b_col) = kb.sb([128, 6, nt_], F32, "col")

                    def tt(out, i0, i1, op, rr, ww):
                        P.op("dve", lambda e: e.tensor_tensor(out=out, in0=i0, in1=i1, op=op), r=rr, w=ww)
                    P.op("act", lambda e: e.activation(out=sc, in_=plog[:, 0:nt_ * 16].rearrange("p (j e) -> p j e", e=16), func=AF.Sigmoid),
                         r=[b_plog], w=[b_sc])
                    tt(ch, sc, rb.unsqueeze(1).to_broadcast([128, nt_, 16]), ALU.add, [b_sc, b_rb], [b_ch])
                    ch4 = ch.rearrange("p j (g e) -> p j g e", e=4)
                    a_, b_, c_, d_ = ch4[:, :, :, 0], ch4[:, :, :, 1], ch4[:, :, :, 2], ch4[:, :, :, 3]
                    tt(q4[:, 0], a_, b_, ALU.max, [b_ch], [b_q4])
                    tt(q4[:, 1], a_, b_, ALU.min, [b_ch], [b_q4])
                    tt(q4[:, 2], c_, d_, ALU.max, [b_ch], [b_q4])
                    tt(q4[:, 3], c_, d_, ALU.min, [b_ch], [b_q4])
                    tt(r4[:, 0], q4[:, 0], q4[:, 2], ALU.max, [b_q4], [b_r4])
                    tt(r4[:, 1], q4[:, 0], q4[:, 2], ALU.min, [b_q4], [b_r4])
                    tt(r4[:, 2], q4[:, 1], q4[:, 3], ALU.max, [b_q4], [b_r4])
                    tt(r4[:, 3], r4[:, 1], r4[:, 2], ALU.max, [b_r4], [b_r4])
                    tt(gs, r4[:, 0], r4[:, 3], ALU.add, [b_r4], [b_gs])
                    P.op("dve", lambda e: e.tensor_reduce(out=col[:, 0, :], in_=gs, axis=AX.X, op=ALU.max), r=[b_gs], w=[b_col])
                    tt(gm, gs, col[:, 0, :].unsqueeze(2).to_broadcast([128, nt_, 4]), ALU.is_ge, [b_gs, b_col], [b_gm])
                    em4 = em.rearrange("p j (g e) -> p j g e", e=4)
                    P.op("dve", lambda e: e.tensor_copy(out=em4, in_=gm.unsqueeze(3).to_broadcast([128, nt_, 4, 4])), r=[b_gm], w=[b_em])
                    tt(cm, ch, em, ALU.mult, [b_ch, b_em], [b_cm])
                    P.op("dve", lambda e: e.tensor_scalar(out=em, in0=em, scalar1=BIG, scalar2=-BIG, op0=ALU.mult, op1=ALU.add), r=[b_em], w=[b_em])
                    tt(cm, cm, em, ALU.add, [b_cm, b_em], [b_cm])
                    P.op("dve", lambda e: e.tensor_reduce(out=col[:, 1, :], in_=cm, axis=AX.X, op=ALU.max), r=[b_cm], w=[b_col])
                    tt(sel, cm, col[:, 1, :].unsqueeze(2).to_broadcast([128, nt_, 16]), ALU.is_ge, [b_cm, b_col], [b_sel])
                    P.op("dve", lambda e: e.scalar_tensor_tensor(out=cm, in0=sel, scalar=-4.0 * BIG, in1=cm, op0=ALU.mult, op1=ALU.add),
                         r=[b_sel, b_cm], w=[b_cm])
                    P.op("dve", lambda e: e.tensor_reduce(out=col[:, 2, :], in_=cm, axis=AX.X, op=ALU.max), r=[b_cm], w=[b_col])
                    tt(em, cm, col[:, 2, :].unsqueeze(2).to_broadcast([128, nt_, 16]), ALU.is_ge, [b_cm, b_col], [b_em])
                    tt(sel, sel, em, ALU.add, [b_sel, b_em], [b_sel])
                    tt(sel, sel, sc, ALU.mult, [b_sel, b_sc], [b_sel])
                    P.op("dve", lambda e: e.tensor_reduce(out=col[:, 3, :], in_=sel, axis=AX.X, op=ALU.add), r=[b_sel], w=[b_col])
                    P.op("dve", lambda e: e.reciprocal(out=col[:, 4, :], in_=col[:, 3, :]), r=[b_col], w=[b_col])
                    tt(sel, sel, col[:, 4, :].unsqueeze(2).to_broadcast([128, nt_, 16]), ALU.mult, [b_sel, b_col], [b_sel])
                    for j0 in range(0, nt_, 4):
                        nj = min(4, nt_ - j0)
                        for jj in range(nj):
                            P.op("pe", lambda e: e.transpose(out=pgT[0:16, jj * 128:(jj + 1) * 128], in_=sel[:, j0 + jj, :], identity=ident),
                                 r=[b_sel, b_ident], w=[b_pgT])
                        P.op("act", lambda e: e.copy(out=gT[:, j0 * 128:(j0 + nj) * 128], in_=pgT[0:16, 0:nj * 128]), r=[b_pgT], w=[b_gT])
                with kb.phase():
                  acc, b_acc = kb.sb([128, tiles_per_blk, 1024], F32, "acc")
                  with kb.phase():
                    stage = [kb.sb([128, 1024], F32, "mst") for _ in range(3)]
                    Wg = [kb.sb([128, 8, 512], BF16, "Wg") for _ in range(2)]
                    Wu = [kb.sb([128, 8, 512], BF16, "Wu") for _ in range(2)]
                    Wd = [kb.sb([128, 4, 1024], BF16, "Wd") for _ in range(2)]
                    pg = [kb.ps([128, 512], F32, "pg") for _ in range(2)]
                    pu = [kb.ps([128, 512], F32, "pu") for _ in range(2)]
                    pgb = kb.ps([128, 512], F32, "pgb")
                    pd = [kb.ps([128, 512], F32, "pd") for _ in range(2)]
                    actT = [kb.sb([128, 4, 512], BF16, "actT") for _ in range(2)]
                    sgs = [kb.sb([128, 512], F32, "sg") for _ in range(2)]
                    tms = [kb.sb([128, 512], F32, "tmm") for _ in range(2)]
                    pieces = [(t0, min(TB, t0 + 512)) for t0 in range(0, TB, 512)]
                    def gateup(ex, pi, aTb):
                        (wg, b_wg), (wu, b_wu) = Wg[ex % 2], Wu[ex % 2]
                        t0, t1 = pieces[pi]
                        n = t1 - t0
                        aT, b_aT = aTb
                        P.op("pe", lambda e: e.matmul(pgb[0][:, 0:n], lhsT=Esel[:, ex, :], rhs=gT[:, t0:t1], start=True, stop=True),
                             r=[b_Esel, b_gT], w=[pgb[1]])
                        for f in range(4):
                            (g_, b_g), (u_, b_u) = pg[f % 2], pu[f % 2]
                            sg, b_sg = sgs[f % 2]
                            tm, b_tm = tms[f % 2]
                            fs = slice(f * 128, (f + 1) * 128)
                            for k in range(8):
                                P.op("pe", lambda e: e.matmul(g_[:, 0:n], lhsT=wg[:, k, fs], rhs=hT[:, k, t0:t1], start=(k == 0), stop=(k == 7)),
                                     r=[b_wg, b_hT], w=[b_g])
                            for k in range(8):
                                P.op("pe", lambda e: e.matmul(u_[:, 0:n], lhsT=wu[:, k, fs], rhs=hT[:, k, t0:t1], start=(k == 0), stop=(k == 7)),
                                     r=[b_wu, b_hT], w=[b_u])
                            P.op("act", lambda e: e.activation(out=sg[:, 0:n], in_=g_[:, 0:n], func=AF.Silu), r=[b_g], w=[b_sg])
                            P.op("dve", lambda e: e.tensor_tensor(out=tm[:, 0:n], in0=u_[:, 0:n], in1=sg[:, 0:n], op=ALU.mult), r=[b_u, b_sg], w=[b_tm])
                            P.op("dve", lambda e: e.tensor_tensor(out=aT[:, f, 0:n], in0=pgb[0][:, 0:n], in1=tm[:, 0:n], op=ALU.mult),
                                 r=[pgb[1], b_tm], w=[b_aT])

                    def down(ex, pi, aTb):
                        (wd, b_wd) = Wd[ex % 2]
                        t0, t1 = pieces[pi]
                        n = t1 - t0
                        aT, b_aT = aTb
                        for tt_ in range(n // 128):
                            j = t0 // 128 + tt_
                            for half in range(2):
                                d_, b_d = pd[half]
                                hs = slice(half * 512, (half + 1) * 512)
                                for f in range(4):
                                    P.op("pe", lambda e: e.matmul(d_, lhsT=aT[:, f, tt_ * 128:(tt_ + 1) * 128], rhs=wd[:, f, hs],
                                                                  start=(f == 0), stop=(f == 3)), r=[b_aT, b_wd], w=[b_d])
                                if ex == 0:
                                    P.op("act", lambda e: e.copy(out=acc[:, j, hs], in_=d_), r=[b_d], w=[b_acc])
                                else:
                                    P.op("dve", lambda e: e.tensor_tensor(out=acc[:, j, hs], in0=d_, in1=acc[:, j, hs], op=ALU.add),
                                         r=[b_d, b_acc], w=[b_acc])

                    items = [(ex, pi) for ex in range(elim) for pi in range(len(pieces))]
                    prev = None
                    for idx, (ex, pi) in enumerate(items):
                        if pi == 0:
                            load_wchunks_bf16(kb, Wg[ex % 2][0], Wg[ex % 2][1], IN["moe_w_gate"][li, ex], 8, 512, stage)
                            load_wchunks_bf16(kb, Wu[ex % 2][0], Wu[ex % 2][1], IN["moe_w_up"][li, ex], 8, 512, stage)
                            load_wchunks_bf16(kb, Wd[ex % 2][0], Wd[ex % 2][1], IN["moe_w_down"][li, ex], 4, 1024, stage)
                        gateup(ex, pi, actT[idx % 2])
                        if prev is not None:
                            down(prev[0], prev[1], actT[(idx - 1) % 2])
                        prev = (ex, pi)
                    down(prev[0], prev[1], actT[(len(items) - 1) % 2])
                  with kb.phase():
                    gB = load_gate_rows(kb, li, 5 * 1024)
                    xts = [kb.sb([128, 1024], F32, "xtm") for _ in range(2)]
                    for j in range(tiles_per_blk):
                        tok0 = base + j * 128
                        cond = 2 if tok0 < CTX else b
                        xt, b_xt = xts[j % 2]
                        P.dma("sp", xt, xs[b, tok0:tok0 + 128, :], r=[b_xs], w=[b_xt])
                        P.op("dve", lambda e: e.tensor_tensor(out=acc[:, j, :], in0=acc[:, j, :], in1=gB[cond][0], op=ALU.mult),
                             r=[b_acc, gB[cond][1]], w=[b_acc])
                        P.op("dve" if j % 2 == 0 else "pool", lambda e: e.tensor_tensor(out=xt, in0=xt, in1=acc[:, j, :], op=ALU.add),
                             r=[b_xt, b_acc], w=[b_xt])
                        P.dma("sp", xs[b, tok0:tok0 + 128, :], xt, r=[b_xt], w=[b_xs])


MLA_SCALE = 192.0 ** -0.5
I32 = mybir.dt.int32


def range_reduce_sin(kb, eng, out, b_out, ang, b_ang, shift, tmp, b_tmp):
    P = kb.P
    P.op(eng, lambda e: e.tensor_scalar(out=tmp, in0=ang, scalar1=shift, scalar2=1.0 / TWO_PI, op0=ALU.add, op1=ALU.mult), r=[b_ang], w=[b_tmp])
    P.op(eng, lambda e: e.tensor_scalar(out=tmp, in0=tmp, scalar1=MAGIC, scalar2=None, op0=ALU.add), r=[b_tmp], w=[b_tmp])
    P.op(eng, lambda e: e.tensor_scalar(out=tmp, in0=tmp, scalar1=-MAGIC, scalar2=-TWO_PI, op0=ALU.add, op1=ALU.mult), r=[b_tmp], w=[b_tmp])
    P.op(eng, lambda e: e.tensor_tensor(out=tmp, in0=tmp, in1=ang, op=ALU.add), r=[b_tmp, b_ang], w=[b_tmp])
    P.op("act", lambda e: e.activation(out=out, in_=tmp, func=AF.Sin, bias=kb.shiftc[shift][0][0:out.shape[0], 0:1]), r=[b_tmp, kb.shiftc[shift][1]], w=[b_out])


def phase_l1_mla(kb, IN):
    P = kb.P
    xs, b_xs = kb.dr["xs"]
    attnT, b_attnT = kb.dr["attnT"]
    ident, b_ident, identb, b_identb, ones, b_ones = kb.ident, kb.b_ident, kb.identb, kb.b_identb, kb.ones, kb.b_ones
    with kb.phase():
        modF, b_modF, (A1, b_A1), _ = load_mod_fm(kb, 1, IN, ("norm1_g", "norm2_g"))
        pmisc, b_pmisc = kb.ps([128, 512], F32, "pmisc")
        Win, b_Win = kb.sb([128, 8, 768], BF16, "Win")
        Wq, b_Wq = kb.sb([128, 3, 2048], BF16, "Wq")
        Wkv, b_Wkv = kb.sb([128, 2, 2048], BF16, "Wkv")
        cosT, b_cosT = kb.sb([64, SEQ], F32, "cosT")
        sinT, b_sinT = kb.sb([64, SEQ], F32, "sinT")
        rc, b_rc = kb.sb([64, 4], F32, "rc")
        fr_i, b_fr_i = kb.sb([1, 128], I32, "fri")
        fr_f, b_fr_f = kb.sb([1, 128], F32, "frf")
        gq, b_gq = kb.sb([128, 3], F32, "gq")
        gkv, b_gkv = kb.sb([128, 2], F32, "gkv")
        epsc, b_epsc = kb.sb([128, 1], F32, "epsc")
        _scope = kb.phase()
        _scope.__enter__()
        stage = [kb.sb([128, 2048], F32, "mst") for _ in range(2)]
        for k in range(8):
            st, b_st = stage[k % 2]
            P.dma("sp", st[:, 0:704], IN["mla_w_in"][0, k * 128:(k + 1) * 128, :], w=[b_st])
            P.op("pool", lambda e: e.tensor_copy(out=Win[:, k, 0:704], in_=st[:, 0:704]), r=[b_st], w=[b_Win])
            P.op("pool", lambda e: e.tensor_scalar(out=Win[:, k, 704:736], in0=st[:, 672:704], scalar1=-1.0, scalar2=None, op0=ALU.mult), r=[b_st], w=[b_Win])
            P.op("pool", lambda e: e.tensor_copy(out=Win[:, k, 736:768], in_=st[:, 640:672]), r=[b_st], w=[b_Win])
        for k in range(3):
            st, b_st = stage[k % 2]
            P.dma("sp", st[:, 0:1536], IN["mla_w_q_up"][0, k * 128:(k + 1) * 128, :], w=[b_st])
            P.op("pool", lambda e: e.tensor_copy(out=Wq[:, k, 0:1536], in_=st[:, 0:1536]), r=[b_st], w=[b_Wq])
            for h in range(8):
                P.op("pool", lambda e: e.tensor_scalar(out=Wq[:, k, 1536 + h * 64:1536 + h * 64 + 32], in0=st[:, h * 192 + 160:h * 192 + 192],
                                                       scalar1=-1.0, scalar2=None, op0=ALU.mult), r=[b_st], w=[b_Wq])
                P.op("pool", lambda e: e.tensor_copy(out=Wq[:, k, 1536 + h * 64 + 32:1536 + h * 64 + 64], in_=st[:, h * 192 + 128:h * 192 + 160]),
                     r=[b_st], w=[b_Wq])
        load_wchunks_bf16(kb, Wkv, b_Wkv, IN["mla_w_kv_up"][0], 2, 2048, stage)
        kb.load_vec_fm(IN["mla_q_norm_g"][0], 3, gq, b_gq, (pmisc, b_pmisc))
        kb.load_vec_fm(IN["mla_kv_norm_g"][0], 2, gkv, b_gkv, (pmisc, b_pmisc))
        P.op("pool", lambda e: e.memset(epsc, EPS), w=[b_epsc])
        P.op("pool", lambda e: e.iota(fr_i[:, 0:64], pattern=[[0, 2], [0, 2], [1, 16]], base=0, channel_multiplier=0), w=[b_fr_i])
        P.op("pool", lambda e: e.iota(fr_i[:, 64:128], pattern=[[0, 2], [1, 2], [0, 16]], base=0, channel_multiplier=0), r=[b_fr_i], w=[b_fr_i])
        P.op("dve", lambda e: e.tensor_copy(out=fr_f, in_=fr_i), r=[b_fr_i], w=[b_fr_f])
        P.op("pe", lambda e: e.transpose(out=pmisc[0:64, 0:1], in_=fr_f[0:1, 0:64], identity=ident[0:1, 0:1]), r=[b_fr_f, b_ident], w=[b_pmisc])
        P.op("pe", lambda e: e.transpose(out=pmisc[0:64, 1:2], in_=fr_f[0:1, 64:128], identity=ident[0:1, 0:1]), r=[b_fr_f, b_ident], w=[b_pmisc])
        P.op("act", lambda e: e.activation(out=rc[:, 0:1], in_=pmisc[0:64, 0:1], func=AF.Exp, scale=-math.log(10000.0) / 16.0), r=[b_pmisc], w=[b_rc])
        P.op("dve", lambda e: e.tensor_scalar(out=rc[:, 1:2], in0=pmisc[0:64, 1:2], scalar1=-1.0, scalar2=1.0, op0=ALU.mult, op1=ALU.add), r=[b_pmisc], w=[b_rc])
        pos_i, b_pos_i = kb.sb([64, SEQ], I32, "posi")
        rowf, b_rowf = kb.sb([64, SEQ], F32, "rowf")
        colf, b_colf = kb.sb([64, SEQ], F32, "colf")
        P.op("pool", lambda e: e.iota(pos_i, pattern=[[1, 32], [0, 64]], base=0, channel_multiplier=0), w=[b_pos_i])
        P.op("dve", lambda e: e.tensor_copy(out=rowf, in_=pos_i), r=[b_pos_i], w=[b_rowf])
        P.op("pool", lambda e: e.iota(pos_i, pattern=[[0, 32], [1, 64]], base=0, channel_multiplier=0), r=[b_pos_i], w=[b_pos_i])
        P.op("dve", lambda e: e.tensor_copy(out=colf, in_=pos_i), r=[b_pos_i], w=[b_colf])
        P.op("dve", lambda e: e.tensor_tensor(out=rowf, in0=rowf, in1=colf, op=ALU.subtract), r=[b_rowf, b_colf], w=[b_rowf])
        P.op("dve", lambda e: e.scalar_tensor_tensor(out=rowf, in0=rowf, scalar=rc[:, 1:2], in1=colf, op0=ALU.mult, op1=ALU.add), r=[b_rowf, b_rc, b_colf], w=[b_rowf])
        P.op("dve", lambda e: e.tensor_scalar(out=rowf, in0=rowf, scalar1=rc[:, 0:1], scalar2=None, op0=ALU.mult), r=[b_rowf, b_rc], w=[b_rowf])
        range_reduce_sin(kb, "dve", cosT, b_cosT, rowf, b_rowf, math.pi / 2, colf, b_colf)
        range_reduce_sin(kb, "dve", sinT, b_sinT, rowf, b_rowf, 0.0, colf, b_colf)
        _scope.__exit__(None, None, None)
        cqn, b_cqn = kb.sb([128, 3, SEQ], BF16, "cqn")
        ckvn, b_ckvn = kb.sb([128, 2, T], BF16, "ckvn")
        krT, b_krT = kb.sb([64, T], BF16, "krT")
        nmb = make_nm_bufs(kb, 2, 1)
        hT, b_hT = kb.sb([128, 8, 256], BF16, "hT1")
        c32, b_c32 = kb.sb([128, 3, 256], F32, "c32")
        csq, b_csq = kb.sb([128, 3, 256], F32, "csq")
        rin, b_rin = kb.sb([128, 256], F32, "rin")
        kr1, b_kr1 = kb.sb([64, 256], F32, "kr1")
        kr2, b_kr2 = kb.sb([64, 256], F32, "kr2")
        pA, b_pA = kb.ps([128, 512], F32, "pA")
        pB, b_pB = kb.ps([128, 512], F32, "pB")
        knT, b_knT = kb.sb([128, T], BF16, "knT")
        Vh, b_Vh = kb.sb([128, NT, 128], BF16, "Vh")
        qnT, b_qnT = kb.sb([128, SEQ], BF16, "qnT")
        qrT, b_qrT = kb.sb([64, SEQ], BF16, "qrT")
        q32a, b_q32a = kb.sb([64, 512], F32, "q32a")
        q32b, b_q32b = kb.sb([64, 512], F32, "q32b")
        Pb = [kb.sb([128, T], BF16, "Pb") for _ in range(2)]
        PTs = [kb.sb([128, NT, 128], BF16, "PTs") for _ in range(2)]
        smx = [kb.sb([128, 16], F32, "smx") for _ in range(4)]
        o32 = [kb.sb([128, 128], F32, "o32") for _ in range(2)]
        aTs, b_aTs = kb.sb([128, SEQ], F32, "aTs")
        s1b = [(pA, b_pA), (pB, b_pB)] + [kb.ps([128, 512], F32, "s2b") for _ in range(1)]
        Ssb = [kb.sb([128, T], F32, "Ssb") for _ in range(2)]
        ptbs = []
        for _i in range(3):
            kb.uid += 1
            ptb_t = kb.stack.enter_context(kb.nc.psum_tensor("ptb_%d" % kb.uid, [128, 1024], BF16))
            ptbs.append((ptb_t.ap().rearrange("p (a b) -> p a b", b=128), Buf("ptb%d" % _i, excl=True)))
        po, b_po = pmisc, b_pmisc
        kpieces = [(t0, min(T, t0 + 512)) for t0 in range(0, T, 512)]
        import os
        hlim = int(os.environ.get("MLA_HLIM", "8"))
        qlim = int(os.environ.get("MLA_QLIM", "16"))

        def rms_scale(src_ps_list, nchunk, width, gcol, dst, b_dst, dcol0, dfeat):
            for c, (ps, b_ps) in enumerate(src_ps_list):
                P.op("act", lambda e: e.copy(out=c32[:, c, 0:width], in_=ps), r=[b_ps], w=[b_c32])
                P.op("act", lambda e: e.activation(out=csq[:, c, 0:width], in_=c32[:, c, 0:width], func=AF.Square), r=[b_c32], w=[b_csq])
            return

        for b in range(NB):
            for un in range(T // 256):
                toks_ = [un * 256, un * 256 + 128]
                norm_mod_batch(kb, xs[b], b_xs, toks_, [2 if t_ < CTX else b for t_ in toks_], A1, b_A1, modF, b_modF, 0,
                               nmb, [(hT, b_hT, lambda i: i * 128)])
                tsl = slice(un * 256, (un + 1) * 256)
                lat = un >= 1
                lsl = slice(un * 256 - CTX, (un + 1) * 256 - CTX)
                groups = ([("q", 0, 3, 384.0, gq, b_gq)] if lat else []) + [("kv", 3, 2, 256.0, gkv, b_gkv)]
                for (nm, c0, nch, dfeat, gcol, b_gcol) in groups:
                    for c in range(nch):
                        ps, b_ps = (pA, b_pA) if c % 2 == 0 else (pB, b_pB)
                        for k in range(8):
                            P.op("pe", lambda e: e.matmul(ps[:, 0:256], lhsT=Win[:, k, (c0 + c) * 128:(c0 + c + 1) * 128], rhs=hT[:, k, :],
                                                          start=(k == 0), stop=(k == 7)), r=[b_Win, b_hT], w=[b_ps])
                        P.op("act", lambda e: e.copy(out=c32[:, c, :], in_=ps[:, 0:256]), r=[b_ps], w=[b_c32])
                        P.op("act", lambda e: e.activation(out=csq[:, c, :], in_=c32[:, c, :], func=AF.Square), r=[b_c32], w=[b_csq])
                    for c in range(nch):
                        P.op("pe", lambda e: e.matmul(pA[:, 256:512], lhsT=ones, rhs=csq[:, c, :], start=(c == 0), stop=(c == nch - 1)),
                             r=[b_ones, b_csq], w=[b_pA])
                    P.op("dve", lambda e: e.tensor_scalar(out=rin, in0=pA[:, 256:512], scalar1=1.0 / dfeat, scalar2=EPS, op0=ALU.mult, op1=ALU.add),
                         r=[b_pA], w=[b_rin])
                    P.op("act", lambda e: e.activation(out=rin, in_=rin, func=AF.Sqrt), r=[b_rin], w=[b_rin])
                    P.op("dve", lambda e: e.reciprocal(out=rin, in_=rin), r=[b_rin], w=[b_rin])
                    for c in range(nch):
                        if nm == "q":
                            dst, b_dst, dsl = cqn, b_cqn, lsl
                        else:
                            dst, b_dst, dsl = ckvn, b_ckvn, tsl
                        P.op("dve", lambda e: e.scalar_tensor_tensor(out=dst[:, c, dsl], in0=c32[:, c, :], scalar=gcol[:, c:c + 1], in1=rin,
                                                                     op0=ALU.mult, op1=ALU.mult), r=[b_c32, b_gcol, b_rin], w=[b_dst])
                for k in range(8):
                    P.op("pe", lambda e: e.matmul(pB[0:64, 0:256], lhsT=Win[:, k, 640:704], rhs=hT[:, k, :], start=(k == 0), stop=(k == 7)),
                         r=[b_Win, b_hT], w=[b_pB])
                if not lat:
                    P.op("act", lambda e: e.copy(out=krT[:, tsl], in_=pB[0:64, 0:256]), r=[b_pB], w=[b_krT])
                else:
                    for k in range(8):
                        P.op("pe", lambda e: e.matmul(pB[0:64, 256:512], lhsT=Win[:, k, 704:768], rhs=hT[:, k, :], start=(k == 0), stop=(k == 7)),
                             r=[b_Win, b_hT], w=[b_pB])
                    P.op("dve", lambda e: e.tensor_tensor(out=kr1, in0=pB[0:64, 0:256], in1=cosT[:, lsl], op=ALU.mult), r=[b_pB, b_cosT], w=[b_kr1])
                    P.op("dve", lambda e: e.tensor_tensor(out=kr2, in0=pB[0:64, 256:512], in1=sinT[:, lsl], op=ALU.mult), r=[b_pB, b_sinT], w=[b_kr2])
                    P.op("pool", lambda e: e.tensor_tensor(out=krT[:, tsl], in0=kr1, in1=kr2, op=ALU.add), r=[b_kr1, b_kr2], w=[b_krT])
            for h in range(hlim):
                for pi, (t0, t1) in enumerate(kpieces):
                    n = t1 - t0
                    for k in range(2):
                        P.op("pe", lambda e: e.matmul(pA[:, 0:n], lhsT=Wkv[:, k, h * 256:h * 256 + 128], rhs=ckvn[:, k, t0:t1], start=(k == 0), stop=(k == 1)),
                             r=[b_Wkv, b_ckvn], w=[b_pA])
                    P.op("act", lambda e: e.copy(out=knT[:, t0:t1], in_=pA[:, 0:n]), r=[b_pA], w=[b_knT])
                for j0 in range(0, NT, 4):
                    nj = min(4, NT - j0)
                    for jj in range(nj):
                        j = j0 + jj
                        for k in range(2):
                            P.op("pe", lambda e: e.matmul(pB[:, jj * 128:(jj + 1) * 128], lhsT=ckvn[:, k, j * 128:(j + 1) * 128],
                                                          rhs=Wkv[:, k, h * 256 + 128:h * 256 + 256], start=(k == 0), stop=(k == 1)),
                                 r=[b_Wkv, b_ckvn], w=[b_pB])
                    P.op("dve", lambda e: e.tensor_copy(out=Vh[:, j0:j0 + nj, :], in_=pB[:, 0:nj * 128].rearrange("p (a b) -> p a b", b=128)),
                         r=[b_pB], w=[b_Vh])
                for qp in range(4):
                    qsl = slice(qp * 512, (qp + 1) * 512)
                    for k in range(3):
                        P.op("pe", lambda e: e.matmul(pA, lhsT=Wq[:, k, h * 192:h * 192 + 128], rhs=cqn[:, k, qsl], start=(k == 0), stop=(k == 2)),
                             r=[b_Wq, b_cqn], w=[b_pA])
                    P.op("act", lambda e: e.copy(out=qnT[:, qsl], in_=pA), r=[b_pA], w=[b_qnT])
                    for k in range(3):
                        P.op("pe", lambda e: e.matmul(pB[0:64, :], lhsT=Wq[:, k, h * 192 + 128:h * 192 + 192], rhs=cqn[:, k, qsl], start=(k == 0), stop=(k == 2)),
                             r=[b_Wq, b_cqn], w=[b_pB])
                    P.op("dve", lambda e: e.tensor_tensor(out=q32a, in0=pB[0:64, :], in1=cosT[:, qsl], op=ALU.mult), r=[b_pB, b_cosT], w=[b_q32a])
                    for k in range(3):
                        P.op("pe", lambda e: e.matmul(pB[0:64, :], lhsT=Wq[:, k, 1536 + h * 64:1536 + h * 64 + 64], rhs=cqn[:, k, qsl], start=(k == 0), stop=(k == 2)),
                             r=[b_Wq, b_cqn], w=[b_pB])
                    P.op("dve", lambda e: e.tensor_tensor(out=q32b, in0=pB[0:64, :], in1=sinT[:, qsl], op=ALU.mult), r=[b_pB, b_sinT], w=[b_q32b])
                    P.op("pool", lambda e: e.tensor_tensor(out=qrT[:, qsl], in0=q32a, in1=q32b, op=ALU.add), r=[b_q32a, b_q32b], w=[b_qrT])
                def S1(qt):
                    qs = slice(qt * 128, (qt + 1) * 128)
                    (sx, b_sx) = smx[qt % 4]
                    (ssb, b_ssb) = Ssb[qt % 2]
                    for pi, (t0, t1) in enumerate(kpieces):
                        n = t1 - t0
                        sc_, b_sc = s1b[(qt * 5 + pi) % 3]
                        P.op("pe", lambda e: e.matmul(sc_[:, 0:n], lhsT=qnT[:, qs], rhs=knT[:, t0:t1], start=True, stop=False),
                             r=[b_qnT, b_knT], w=[b_sc])
                        P.op("pe", lambda e: e.matmul(sc_[:, 0:n], lhsT=qrT[:, qs], rhs=krT[:, t0:t1], start=False, stop=True),
                             r=[b_qrT, b_krT], w=[b_sc])
                        if pi % 2 == 0:
                            P.op("act", lambda e: e.copy(out=ssb[:, t0:t1], in_=sc_[:, 0:n]), r=[b_sc], w=[b_ssb])
                        else:
                            P.op("dve", lambda e: e.tensor_copy(out=ssb[:, t0:t1], in_=sc_[:, 0:n]), r=[b_sc], w=[b_ssb])
                    P.op("dve", lambda e: e.tensor_reduce(out=sx[:, 5:6], in_=ssb, axis=AX.X, op=ALU.max), r=[b_ssb], w=[b_sx])
                    P.op("dve", lambda e: e.tensor_scalar(out=sx[:, 6:7], in0=sx[:, 5:6], scalar1=-MLA_SCALE, scalar2=None, op0=ALU.mult), r=[b_sx], w=[b_sx])

                def S2(qt):
                    (sx, b_sx) = smx[qt % 4]
                    (pb_, b_pb) = Pb[qt % 2]
                    (ssb, b_ssb) = Ssb[qt % 2]
                    P.op("act", lambda e: e.activation(out=pb_, in_=ssb, func=AF.Exp, scale=MLA_SCALE, bias=sx[:, 6:7],
                                                       accum_out=sx[:, 7:8]), r=[b_ssb, b_sx], w=[b_pb, b_sx])
                    P.op("dve", lambda e: e.reciprocal(out=sx[:, 13:14], in_=sx[:, 7:8]), r=[b_sx], w=[b_sx])

                def S3(qt):
                    (pb_, b_pb), (pts, b_pts) = Pb[qt % 2], PTs[qt % 2]
                    for gi, j0 in enumerate(range(0, NT, 8)):
                        nj = min(8, NT - j0)
                        ptb, b_ptb = ptbs[gi % 3]
                        for jj in range(nj):
                            j = j0 + jj
                            P.op("pe", lambda e: e.transpose(out=ptb[:, jj, :], in_=pb_[:, j * 128:(j + 1) * 128], identity=identb),
                                 r=[b_pb, b_identb], w=[b_ptb])
                        if gi % 2 == 0:
                            P.op("act", lambda e: e.copy(out=pts[:, j0:j0 + nj, :], in_=ptb[:, 0:nj, :]), r=[b_ptb], w=[b_pts])
                        else:
                            P.op("dve", lambda e: e.tensor_copy(out=pts[:, j0:j0 + nj, :], in_=ptb[:, 0:nj, :]), r=[b_ptb], w=[b_pts])

                def S4(qt):
                    qs = slice(qt * 128, (qt + 1) * 128)
                    (pts, b_pts), (sx, b_sx), (o3, b_o3) = PTs[qt % 2], smx[qt % 4], o32[qt % 2]
                    for j in range(NT):
                        P.op("pe", lambda e: e.matmul(po[:, 0:128], lhsT=pts[:, j, :], rhs=Vh[:, j, :], start=(j == 0), stop=(j == NT - 1)),
                             r=[b_pts, b_Vh], w=[b_po])
                    P.op("act", lambda e: e.activation(out=o3, in_=po[:, 0:128], func=AF.Copy, scale=sx[:, 13:14]), r=[b_po, b_sx], w=[b_o3])
                    P.op("pe", lambda e: e.transpose(out=po[:, 128:256], in_=o3, identity=ident), r=[b_o3, b_ident], w=[b_po])
                    P.op("dve", lambda e: e.tensor_copy(out=aTs[:, qs], in_=po[:, 128:256]), r=[b_po], w=[b_aTs])

                for t in range(qlim + 3):
                    if t < qlim:
                        S1(t)
                    if 0 <= t - 2 < qlim:
                        S3(t - 2)
                    if 0 <= t - 3 < qlim:
                        S4(t - 3)
                    if 0 <= t - 1 < qlim:
                        S2(t - 1)
                P.dma("sp", attnT[b, h * 128:(h + 1) * 128, :], aTs, r=[b_aTs], w=[b_attnT])


def phase_proj_res(kb, IN, li, src_name, wsrc, gate_off, tok_lo):
    P = kb.P
    xs, b_xs = kb.dr["xs"]
    srcT, b_srcT = kb.dr[src_name]
    ntok = T - tok_lo
    with kb.phase():
        gB = load_gate_rows(kb, li, gate_off)
        stage = [kb.sb([128, 1024], F32, "wst") for _ in range(2)]
        W, b_W = kb.sb([128, 8, 1024], BF16, "wout")
        load_wchunks_bf16(kb, W, b_W, wsrc, 8, 1024, stage, ("pool", "act", "dve"))
        a32s = [kb.sb([128, 8, 256], F32, "a32") for _ in range(2)]
        mixbs = [kb.sb([128, 8, 256], BF16, "mixb") for _ in range(2)]
        pdd = [kb.ps([128, 512], F32, "pdd") for _ in range(2)]
        xts = [kb.sb([128, 1024], F32, "xt") for _ in range(2)]
        tms = [kb.sb([128, 512], F32, "tm") for _ in range(2)]
        ui = 0
        for b in range(NB):
            for un in range(ntok // 256):
                tsl = slice(un * 256, (un + 1) * 256)
                (a32, b_a32), (mixb, b_mixb) = a32s[ui % 2], mixbs[ui % 2]
                ui += 1
                P.dma("sp", a32, srcT[b, :, tsl].rearrange("(c p) t -> p c t", p=128), r=[b_srcT], w=[b_a32])
                P.op("act", lambda e: e.copy(out=mixb, in_=a32), r=[b_a32], w=[b_mixb])
                for tt in range(2):
                    tok0 = tok_lo + un * 256 + tt * 128
                    cond = 2 if tok0 < CTX else b
                    xt, b_xt = xts[tt]
                    P.dma("sp", xt, xs[b, tok0:tok0 + 128, :], r=[b_xs], w=[b_xt])
                    for half in range(2):
                        pd, b_pd = pdd[half]
                        tm, b_tm = tms[half]
                        hs = slice(half * 512, (half + 1) * 512)
                        for k in range(8):
                            P.op("pe", lambda e: e.matmul(pd, lhsT=mixb[:, k, tt * 128:(tt + 1) * 128], rhs=W[:, k, hs],
                                                          start=(k == 0), stop=(k == 7)), r=[b_mixb, b_W], w=[b_pd])
                        P.op("dve", lambda e: e.tensor_tensor(out=tm, in0=pd, in1=gB[cond][0][:, hs], op=ALU.mult),
                             r=[b_pd, gB[cond][1]], w=[b_tm])
                        P.op("pool", lambda e: e.tensor_tensor(out=xt[:, hs], in0=xt[:, hs], in1=tm, op=ALU.add),
                             r=[b_xt, b_tm], w=[b_xt])
                    P.dma("sp", xs[b, tok0:tok0 + 128, :], xt, r=[b_xt], w=[b_xs])


def phase_final(kb, IN, out_ap, b_out):
    P = kb.P
    xs, b_xs = kb.dr["xs"]
    with kb.phase():
        gF, b_gF = kb.sb([128, 1024], F32, "gF")
        P.dma("sp", gF, IN["final_norm_g"].partition_broadcast(128), w=[b_gF])
        xts = [kb.sb([128, 1024], F32, "xt") for _ in range(2)]
        sqs = [kb.sb([128, 1024], F32, "sq") for _ in range(2)]
        sss = [kb.sb([128, 4], F32, "ss") for _ in range(2)]
        it = 0
        for b in range(NB):
            for j in range(SEQ // 128):
                (xt, b_xt), (sq, b_sq), (ss, b_ss) = xts[it % 2], sqs[it % 2], sss[it % 2]
                it += 1
                P.dma("sp", xt, xs[b, CTX + j * 128:CTX + (j + 1) * 128, :], r=[b_xs], w=[b_xt])
                P.op("act", lambda e: e.activation(out=sq, in_=xt, func=AF.Square, accum_out=ss[:, 0:1]), r=[b_xt], w=[b_sq, b_ss])
                P.op("dve", lambda e: e.tensor_scalar(out=ss[:, 1:2], in0=ss[:, 0:1], scalar1=1.0 / D, scalar2=EPS, op0=ALU.mult, op1=ALU.add), r=[b_ss], w=[b_ss])
                P.op("act", lambda e: e.activation(out=ss[:, 2:3], in_=ss[:, 1:2], func=AF.Sqrt), r=[b_ss], w=[b_ss])
                P.op("dve", lambda e: e.reciprocal(out=ss[:, 3:4], in_=ss[:, 2:3]), r=[b_ss], w=[b_ss])
                P.op("dve", lambda e: e.scalar_tensor_tensor(out=sq, in0=xt, scalar=ss[:, 3:4], in1=gF, op0=ALU.mult, op1=ALU.mult),
                     r=[b_xt, b_ss, b_gF], w=[b_sq])
                P.dma("sp", out_ap[b, j * 128:(j + 1) * 128, :], sq, r=[b_sq], w=[b_out])


def build(phases, scr_kinds=None):
    kb = KB(scr_kinds)
    nc = kb.nc
    shapes = {}

    def inp(name, shape):
        shapes[name] = list(shape)

    class LazyIn(dict):
        def __missing__(self, name):
            ap = nc.dram_tensor(name, shapes[name], F32, kind="ExternalInput").ap()
            self[name] = ap
            return ap

    IN = LazyIn()
    kb.IN = IN
    inp("x", [NB, SEQ, D]); inp("c", [NB, D]); inp("ctx", [NB, CTX, D]); inp("c_ctx", [D])
    inp("ada_w", [2, D, 6 * D]); inp("ada_b", [2, 6 * D]); inp("norm1_g", [2, D]); inp("norm2_g", [2, D])
    inp("ab_w_in", [1, D, AB_W]); inp("dn_conv_w", [1, 5, 1536]); inp("dn_A_log", [1, 2, 4]); inp("dn_dt_bias", [1, 2, 4])
    inp("dn_norm_g", [1, 128])
    inp("s5_A_re", [1, 2, 32, 64]); inp("s5_A_im", [1, 2, 32, 64]); inp("s5_log_dt", [1, 2, 32])
    inp("s5_B_re", [1, 32, 64, 16]); inp("s5_B_im", [1, 32, 64, 16]); inp("s5_C_re", [1, 32, 16, 64]); inp("s5_C_im", [1, 32, 16, 64])
    inp("s5_D", [1, 32, 16]); inp("s5_glu_w", [1, 512, 512]); inp("s5_glu_b", [1, 512]); inp("ab_w_out", [1, 1024, D])
    inp("mla_w_in", [1, D, 704]); inp("mla_q_norm_g", [1, 384]); inp("mla_w_q_up", [1, 384, 1536]); inp("mla_kv_norm_g", [1, 256])
    inp("mla_w_kv_up", [1, 256, 2048]); inp("mla_w_out", [1, 1024, D])
    inp("router_w", [D, 16]); inp("router_bias", [16])
    inp("moe_w_gate", [2, 16, D, 512]); inp("moe_w_up", [2, 16, D, 512]); inp("moe_w_down", [2, 16, 512, D])
    inp("final_norm_g", [D])
    out_ap = nc.dram_tensor("out", [NB, SEQ, D], F32, kind="ExternalOutput").ap()
    b_out = Buf("out")

    kb.dram("modd0", [3, 6144]); kb.dram("modd1", [3, 6144])
    kb.dram("xs", [NB, T, D])
    kb.dram("qkvu", [NB, 2048, T])
    kb.dram("zab", [NB, T, 528])
    kb.dram("qkvn", [NB, 1536, T])
    kb.dram("mixT", [NB, 1024, T])
    kb.dram("ygT", [NB, 512, T])
    kb.dram("attnT", [NB, 1024, SEQ])

    with ExitStack() as top:
        kb.stack = top
        kb.consts()
        P = kb.P
        if "init" in phases:
            xs, b_xs = kb.dr["xs"]
            for b in range(NB):
                P.dma("sp", xs[b, 0:CTX, :], IN["ctx"][b], w=[b_xs])
                for q in range(4):
                    P.dma("sp", xs[b, CTX + q * 512:CTX + (q + 1) * 512, :], IN["x"][b, q * 512:(q + 1) * 512, :], w=[b_xs])
        if "ada0" in phases:
            phase_adaln(kb, 0, IN)
        if "l0in" in phases:
            phase_l0_inproj(kb, IN)
        if "l0conv" in phases:
            phase_l0_conv(kb, IN)
        if "l0delta" in phases:
            phase_l0_delta(kb, IN)
        if "l0s5" in phases:
            phase_l0_s5(kb, IN)
        if "l0out" in phases:
            phase_l0_out(kb, IN)
        if "moe0" in phases:
            phase_moe(kb, IN, 0, 0)
        if "ada1" in phases and not (ADA1_IN_CONV and "l0conv" in phases):
            phase_adaln(kb, 1, IN)
        if "l1mla" in phases:
            phase_l1_mla(kb, IN)
        if "l1out" in phases:
            phase_proj_res(kb, IN, 1, "attnT", IN["mla_w_out"][0], 2048, CTX)
        if "moe1" in phases:
            phase_moe(kb, IN, 1, CTX)
        if "final" in phases:
            phase_final(kb, IN, out_ap, b_out)
        if "dumpxs" in phases:
            xs, b_xs = kb.dr["xs"]
            xd = nc.dram_tensor("xs_dump", [NB, T, D], F32, kind="ExternalOutput").ap()
            b_xd = Buf("xd")
            for b in range(NB):
                P.dma("sp", xd[b], xs[b], r=[b_xs], w=[b_xd])
        P.barrier()
    return kb


def _in_maps(inputs, names=None):
    maps = []
    for core in range(NCORES):
        m = {}
        for k, v in inputs.items():
            if names is not None and k not in names:
                continue
            a = np.asarray(v)
            if k in ("x", "c", "ctx"):
                a = a[core * NB:(core + 1) * NB]
            m[k] = np.ascontiguousarray(a, dtype=np.float32)
        maps.append(m)
    return maps


def kernel(**inputs):
    kb = build(ALL_PHASES)
    res = run_bass_kernel_spmd(kb.nc, _in_maps(inputs, set(kb.IN.keys())), core_ids=list(range(NCORES)))
    return np.concatenate([np.asarray(r["out"]) for r in res.results], axis=0).astype(np.float32)


ALL_PHASES = ("init", "ada0", "l0in", "l0conv", "l0delta", "l0s5", "l0out", "moe0",
              "ada1", "l1mla", "l1out", "moe1", "final")
```

```python
import math
from contextlib import ExitStack
import numpy as np
import concourse.bass as bass
import concourse.mybir as mybir
from concourse.bass_utils import run_bass_kernel_spmd

F32 = mybir.dt.float32
BF16 = mybir.dt.bfloat16
ALU = mybir.AluOpType
AF = mybir.ActivationFunctionType
AX = mybir.AxisListType

NCORES = 8
NB = 2
D = 1024
SEQ = 2048
CTX = 256
T = SEQ + CTX
NT = T // 128
EPS = 1e-6
KD = D // 128
MAGIC = 12582912.0
TWO_PI = 2.0 * math.pi


import os as _os
STORE_Q = _os.environ.get("STORE_Q", "pool")
OP_TRACE = bool(_os.environ.get("OP_TRACE"))
OP_LIMIT = int(_os.environ.get("OP_LIMIT", "1000000000"))


class Buf:
    __slots__ = ("name", "w", "r", "excl")

    def __init__(self, name="", excl=False):
        self.name = name
        self.w = None
        self.r = {}
        self.excl = excl


class Prog:
    SEM_ROT = 30000
    N_DMA_SEMS = 32

    def __init__(self, nc):
        self.nc = nc
        self.eng = {"pe": nc.tensor, "dve": nc.vector, "act": nc.scalar,
                    "pool": nc.gpsimd, "sp": nc.sync}
        self.sems = {}
        self.cur = {}
        self.cnt = {}
        self.waited = {e: {} for e in self.eng}
        self.gen = {e: 0 for e in self.eng}
        for e in self.eng:
            self._new_sem(e)
        self.dma_keys = []
        for i in range(self.N_DMA_SEMS):
            k = "dma%d" % i
            self.sems[k] = nc.alloc_semaphore(name=k)
            self.cnt[k] = 0
            self.dma_keys.append(k)
        self.dma_rr = 0
        self.ninst = {e: 0 for e in self.eng}

    def _new_sem(self, e):
        k = "%s_%d" % (e, self.gen[e])
        self.gen[e] += 1
        self.sems[k] = self.nc.alloc_semaphore(name=k)
        self.cnt[k] = 0
        self.cur[e] = k

    def _wait(self, e, key, val):
        if val <= 0 or self.waited[e].get(key, 0) >= val:
            return
        self.eng[e].wait_ge(self.sems[key], val)
        self.waited[e][key] = val
        self.ninst[e] += 1

    def _deps(self, e, r, w):
        need = {}
        own0 = self.cur[e]
        for b in r:
            if b.w is not None:
                k, v = b.w
                need[k] = max(need.get(k, 0), v)
            if b.excl:
                for k, v in b.r.items():
                    if k != own0:
                        need[k] = max(need.get(k, 0), v)
        for b in w:
            if b.w is not None:
                k, v = b.w
                need[k] = max(need.get(k, 0), v)
            for k, v in b.r.items():
                need[k] = max(need.get(k, 0), v)
        own = self.cur[e]
        for k, v in need.items():
            if k == own and e == "pe":
                continue
            self._wait(e, k, v)

    def op(self, e, fn, r=(), w=()):
        self.total = getattr(self, "total", 0) + 1
        if self.total > OP_LIMIT:
            return None
        if OP_TRACE:
            import sys as _sys
            print("OP", self.total, e, _sys._getframe(1).f_lineno)
        self._deps(e, r, w)
        inst = fn(self.eng[e])
        k = self.cur[e]
        inst.then_inc(self.sems[k], 1)
        self.cnt[k] += 1
        v = self.cnt[k]
        for b in r:
            b.r[k] = v
        for b in w:
            b.w = (k, v)
            b.r = {}
        self.ninst[e] += 1
        if v >= self.SEM_ROT:
            self._new_sem(e)
        return inst

    def dma(self, e, out, in_, r=(), w=(), **kw):
        self.total = getattr(self, "total", 0) + 1
        if self.total > OP_LIMIT:
            return None
        if STORE_Q and "DRAM" in str(out.space):
            e = STORE_Q
        k = self.dma_keys[self.dma_rr]
        self.dma_rr = (self.dma_rr + 1) % len(self.dma_keys)
        self._wait(e, k, self.cnt[k])
        self._deps(e, r, w)
        inst = self.eng[e].dma_start(out=out, in_=in_, **kw)
        inst.then_inc(self.sems[k], 16)
        self.cnt[k] += 16
        v = self.cnt[k]
        for b in r:
            b.r[k] = v
        for b in w:
            b.w = (k, v)
            b.r = {}
        self.ninst[e] += 1
        return inst

    def barrier(self, engines=None):
        for e in (engines or self.eng):
            for k in list(self.cnt):
                self._wait(e, k, self.cnt[k])


class KB:
    def __init__(self, scr_kinds=None):
        self.nc = bass.Bass("TRN2", target_bir_lowering=False)
        self.P = Prog(self.nc)
        self.scr_kinds = scr_kinds or {}
        self.dr = {}
        self.stack = None
        self.uid = 0
        self.dq = 0

    def dram(self, name, shape, dtype=F32, kind="Internal"):
        kind = self.scr_kinds.get(name, kind)
        ap = self.nc.dram_tensor(name, list(shape), dtype, kind=kind).ap()
        self.dr[name] = (ap, Buf(name))
        return self.dr[name]

    def sb(self, shape, dtype=F32, name=None):
        self.uid += 1
        nm = "%s_%d" % (name or "sb", self.uid)
        t = self.stack.enter_context(self.nc.sbuf_tensor(nm, list(shape), dtype))
        return t.ap(), Buf(nm)

    def ps(self, shape, dtype=F32, name=None):
        self.uid += 1
        nm = "%s_%d" % (name or "ps", self.uid)
        t = self.stack.enter_context(self.nc.psum_tensor(nm, [128, 512], F32))
        ap = t.ap()
        shape = list(shape)
        n = 1
        for v in shape[1:]:
            n *= v
        assert dtype == F32 and n <= 512, shape
        v = ap[0:shape[0], 0:n]
        if len(shape) == 3:
            v = v.rearrange("p (a b) -> p a b", b=shape[2])
        return v, Buf(nm, excl=True)

    def phase(self):
        kb = self

        class _Ph:
            def __enter__(s):
                s.saved = kb.stack
                kb.stack = ExitStack()
                kb.stack.__enter__()
                return kb.stack

            def __exit__(s, *a):
                kb.P.barrier()
                kb.stack.__exit__(*a)
                kb.stack = s.saved
        return _Ph()

    def dmaq(self):
        self.dq ^= 1
        return "sp"

    def consts(self):
        P, nc = self.P, self.nc
        self.ident, self.b_ident = self.sb([128, 128], F32, "ident")
        P.op("pool", lambda e: e.memset(self.ident, 0.0), w=[self.b_ident])
        P.op("pool", lambda e: e.affine_select(out=self.ident, in_=self.ident, pattern=[[-1, 128]],
                                               compare_op=ALU.not_equal, fill=1.0, base=0, channel_multiplier=1),
             r=[self.b_ident], w=[self.b_ident])
        self.identb, self.b_identb = self.sb([128, 128], BF16, "identb")
        P.op("dve", lambda e: e.tensor_copy(out=self.identb, in_=self.ident), r=[self.b_ident], w=[self.b_identb])
        self.ones, self.b_ones = self.sb([128, 128], F32, "ones")
        P.op("pool", lambda e: e.memset(self.ones, 1.0), w=[self.b_ones])
        self.shiftc = {}
        for sh in (0.0, math.pi / 2, MAGIC, -MAGIC):
            t, bt = self.sb([128, 1], F32, "shc")
            P.op("pool", lambda e: e.memset(t, sh), w=[bt])
            self.shiftc[sh] = (t, bt)

    def load_vec_fm(self, src1d, n, dst, b_dst, tmp_ps):
        P = self.P
        st, b_st = self.sb([n, 128], F32, "lv")
        P.dma("sp", st, src1d.rearrange("(k p) -> k p", p=128), w=[b_st])
        ps, b_ps = tmp_ps
        P.op("pe", lambda e: e.transpose(out=ps[:, 0:n], in_=st, identity=self.ident[0:n, 0:n]),
             r=[b_st, self.b_ident], w=[b_ps])
        P.op("dve", lambda e: e.tensor_copy(out=dst, in_=ps[:, 0:n]), r=[b_ps], w=[b_dst])

    def load_w_bf16(self, dst, b_dst, src, rows, cols, stage, colblk=2048):
        P = self.P
        i = 0
        for c0 in range(0, cols, colblk):
            cw = min(colblk, cols - c0)
            st, b_st = stage[self.uid % len(stage)]
            self.uid += 1
            P.dma("sp", st[0:rows, 0:cw], src[:, c0:c0 + cw], w=[b_st])
            self.castrr = getattr(self, "castrr", 0) + 1
            eng = ("pool", "act", "dve")[self.castrr % 3]
            i += 1
            if eng == "act":
                P.op("act", lambda e: e.copy(out=dst[:, c0:c0 + cw], in_=st[0:rows, 0:cw]), r=[b_st], w=[b_dst])
            else:
                P.op(eng, lambda e: e.tensor_copy(out=dst[:, c0:c0 + cw], in_=st[0:rows, 0:cw]), r=[b_st], w=[b_dst])


def phase_adaln(kb, li, IN):
    P, nc = kb.P, kb.nc
    modd, b_modd = kb.dr["modd%d" % li]
    with kb.phase():
        condT, b_condT = kb.sb([128, 8, 3], F32, "condT")
        crow, b_crow = kb.sb([3, 1024], F32, "crow")
        P.dma("sp", crow[0:2, :], IN["c"], w=[b_crow])
        P.dma("sp", crow[2:3, :], IN["c_ctx"].rearrange("(o n) -> o n", o=1), w=[b_crow])
        pst, b_pst = kb.ps([128, 8, 3], F32, "pst")
        for k in range(8):
            P.op("pe", lambda e: e.transpose(out=pst[:, k, :], in_=crow[0:3, k * 128:(k + 1) * 128],
                                             identity=kb.ident[0:3, 0:3]), r=[b_crow, kb.b_ident], w=[b_pst])
        P.op("act", lambda e: e.activation(out=condT, in_=pst, func=AF.Silu), r=[b_pst], w=[b_condT])
        wb = [kb.sb([128, 8, 512], F32, "adaw") for _ in range(2)]
        bb = [kb.sb([3, 512], F32, "adab") for _ in range(2)]
        rs = [kb.sb([3, 512], F32, "adar") for _ in range(2)]
        pm = [kb.ps([3, 512], F32, "adaps") for _ in range(2)]
        for blk in range(12):
            w, b_w = wb[blk % 2]
            bt, b_bt = bb[blk % 2]
            r_, b_r = rs[blk % 2]
            ps, b_ps = pm[blk % 2]
            cs = slice(blk * 512, (blk + 1) * 512)
            P.dma("sp", w, IN["ada_w"][li, :, cs].rearrange("(k p) n -> p k n", p=128), w=[b_w])
            P.dma("sp", bt, IN["ada_b"][li, cs].partition_broadcast(3), w=[b_bt])
            for k in range(8):
                P.op("pe", lambda e: e.matmul(ps, lhsT=condT[:, k, :], rhs=w[:, k, :], start=(k == 0), stop=(k == 7)),
                     r=[b_condT, b_w], w=[b_ps])
            P.op("dve", lambda e: e.tensor_tensor(out=r_, in0=ps, in1=bt, op=ALU.add), r=[b_ps, b_bt], w=[b_r])
            P.dma("sp", modd[:, cs], r_, r=[b_r], w=[b_modd])


def adaln_gen(kb, li, IN):
    P = kb.P
    modd, b_modd = kb.dr["modd%d" % li]
    condT, b_condT = kb.sb([128, 8, 3], F32, "condT")
    crow, b_crow = kb.sb([3, 1024], F32, "crow")
    P.dma("sp", crow[0:2, :], IN["c"], w=[b_crow])
    P.dma("sp", crow[2:3, :], IN["c_ctx"].rearrange("(o n) -> o n", o=1), w=[b_crow])
    pst, b_pst = kb.ps([128, 8, 3], F32, "pst")
    for k in range(8):
        P.op("pe", lambda e: e.transpose(out=pst[:, k, :], in_=crow[0:3, k * 128:(k + 1) * 128],
                                         identity=kb.ident[0:3, 0:3]), r=[b_crow, kb.b_ident], w=[b_pst])
    P.op("act", lambda e: e.activation(out=condT, in_=pst, func=AF.Silu), r=[b_pst], w=[b_condT])
    wb = [kb.sb([128, 8, 512], F32, "adaw") for _ in range(2)]
    bb = [kb.sb([3, 512], F32, "adab") for _ in range(2)]
    rs = [kb.sb([3, 512], F32, "adar") for _ in range(2)]
    pm = [kb.ps([3, 512], F32, "adaps") for _ in range(2)]
    yield
    for blk in range(12):
        w, b_w = wb[blk % 2]
        bt, b_bt = bb[blk % 2]
        r_, b_r = rs[blk % 2]
        ps, b_ps = pm[blk % 2]
        cs = slice(blk * 512, (blk + 1) * 512)
        P.dma("sp", w, IN["ada_w"][li, :, cs].rearrange("(k p) n -> p k n", p=128), w=[b_w])
        P.dma("sp", bt, IN["ada_b"][li, cs].partition_broadcast(3), w=[b_bt])
        for k in range(8):
            P.op("pe", lambda e: e.matmul(ps, lhsT=condT[:, k, :], rhs=w[:, k, :], start=(k == 0), stop=(k == 7)),
                 r=[b_condT, b_w], w=[b_ps])
        P.op("dve", lambda e: e.tensor_tensor(out=r_, in0=ps, in1=bt, op=ALU.add), r=[b_ps, b_bt], w=[b_r])
        P.dma("sp", modd[:, cs], r_, r=[b_r], w=[b_modd])
        yield


def load_mod_fm(kb, li, IN, gname):
    P = kb.P
    modd, b_modd = kb.dr["modd%d" % li]
    modF, b_modF = kb.sb([128, 48, 3], F32, "modF")
    As = [kb.sb([128, 8, 3], F32, "modA") for _ in range(2)]
    gs = [kb.sb([128, 8], F32, "ng") for _ in range(2)]
    with kb.phase():
        mrow, b_mrow = kb.sb([3, 6144], F32, "mrow")
        P.dma("sp", mrow, modd, r=[b_modd], w=[b_mrow])
        pst, b_pst = kb.ps([128, 48, 3], F32, "modps")
        for j in range(48):
            P.op("pe", lambda e: e.transpose(out=pst[:, j, :], in_=mrow[0:3, j * 128:(j + 1) * 128],
                                             identity=kb.ident[0:3, 0:3]), r=[b_mrow, kb.b_ident], w=[b_pst])
        P.op("dve", lambda e: e.tensor_copy(out=modF, in_=pst), r=[b_pst], w=[b_modF])
        for i, (nm, so) in enumerate(((gname[0], 8), (gname[1], 32))):
            g, b_g = gs[i]
            A, b_A = As[i]
            kb.load_vec_fm(IN[nm][li], 8, g, b_g, (pst.rearrange("p a c -> p (a c)"), b_pst))
            P.op("dve", lambda e: e.tensor_scalar(out=A, in0=modF[:, so:so + 8, :], scalar1=1.0, scalar2=None, op0=ALU.add),
                 r=[b_modF], w=[b_A])
            P.op("dve", lambda e: e.tensor_tensor(out=A, in0=A, in1=g.unsqueeze(2).to_broadcast([128, 8, 3]), op=ALU.mult),
                 r=[b_A, b_g], w=[b_A])
    return modF, b_modF, As[0], As[1]


def norm_mod_tile(kb, xs_ap, b_xs, rows, cond, A, b_A, modF, b_modF, shift_off, work, hT_list, col0, cols=None):
    P = kb.P
    (xt, b_xt), (sq, b_sq), (ss, b_ss), (pt, b_pt) = work
    P.dma("sp", xt, xs_ap[rows, :], r=[b_xs], w=[b_xt])
    P.op("act", lambda e: e.activation(out=sq, in_=xt, func=AF.Square, accum_out=ss[:, 0:1]), r=[b_xt], w=[b_sq, b_ss])
    P.op("dve", lambda e: e.tensor_scalar(out=ss[:, 1:2], in0=ss[:, 0:1], scalar1=1.0 / D, scalar2=EPS,
                                          op0=ALU.mult, op1=ALU.add), r=[b_ss], w=[b_ss])
    P.op("act", lambda e: e.activation(out=ss[:, 2:3], in_=ss[:, 1:2], func=AF.Sqrt), r=[b_ss], w=[b_ss])
    P.op("dve", lambda e: e.reciprocal(out=ss[:, 3:4], in_=ss[:, 2:3]), r=[b_ss], w=[b_ss])
    P.op("act", lambda e: e.activation(out=sq, in_=xt, func=AF.Copy, scale=ss[:, 3:4]), r=[b_xt, b_ss], w=[b_sq])
    for half in range(2):
        for k4 in range(4):
            k = half * 4 + k4
            P.op("pe", lambda e: e.transpose(out=pt[:, k4, :], in_=sq[:, k * 128:(k + 1) * 128], identity=kb.ident),
                 r=[b_sq, kb.b_ident], w=[b_pt])
        for k4 in range(4):
            k = half * 4 + k4
            for hi, (hT, b_hT) in enumerate(hT_list):
                eng = "dve"
                c0_ = cols[hi] if cols is not None else col0
                P.op(eng, lambda e: e.tensor_scalar(out=hT[:, k, c0_:c0_ + 128], in0=pt[:, k4, :],
                                                    scalar1=A[:, k, cond:cond + 1],
                                                    scalar2=modF[:, shift_off + k, cond:cond + 1],
                                                    op0=ALU.mult, op1=ALU.add),
                     r=[b_pt, b_A, b_modF], w=[b_hT])


def norm_mod_batch(kb, xs_ap, b_xs, toks, conds, A, b_A, modF, b_modF, shift_off, bufs, outs):
    P = kb.P
    G = len(toks)
    ss, b_ss = bufs["ss"]
    for i, tok0 in enumerate(toks):
        (xt, b_xt), (sq, b_sq) = bufs["xt"][i], bufs["sq"][i]
        P.dma("sp", xt, xs_ap[tok0:tok0 + 128, :], r=[b_xs], w=[b_xt])
        P.op("act", lambda e: e.activation(out=sq, in_=xt, func=AF.Square, accum_out=ss[:, 0, i:i + 1]), r=[b_xt], w=[b_sq, b_ss])
    P.op("dve", lambda e: e.tensor_scalar(out=ss[:, 1, 0:G], in0=ss[:, 0, 0:G], scalar1=1.0 / D, scalar2=EPS,
                                          op0=ALU.mult, op1=ALU.add), r=[b_ss], w=[b_ss])
    P.op("act", lambda e: e.activation(out=ss[:, 2, 0:G], in_=ss[:, 1, 0:G], func=AF.Sqrt), r=[b_ss], w=[b_ss])
    P.op("dve", lambda e: e.reciprocal(out=ss[:, 3, 0:G], in_=ss[:, 2, 0:G]), r=[b_ss], w=[b_ss])
    rot = bufs.setdefault("rot", [0])
    for i, tok0 in enumerate(toks):
        (xt, b_xt), (sq, b_sq) = bufs["xt"][i], bufs["sq"][i]
        cond = conds[i]
        P.op("act", lambda e: e.activation(out=sq, in_=xt, func=AF.Copy, scale=ss[:, 3, i:i + 1]), r=[b_xt, b_ss], w=[b_sq])
        for half in range(2):
            pt, b_pt = bufs["pts"][rot[0] % len(bufs["pts"])]
            rot[0] += 1
            for k4 in range(4):
                k = half * 4 + k4
                P.op("pe", lambda e: e.transpose(out=pt[:, k4, :], in_=sq[:, k * 128:(k + 1) * 128], identity=kb.ident),
                     r=[b_sq, kb.b_ident], w=[b_pt])
            for k4 in range(4):
                k = half * 4 + k4
                for hi, (hT, b_hT, colfn) in enumerate(outs):
                    c0_ = colfn(i)
                    if (k4 + hi) % 2 == 0:
                        P.op("dve", lambda e: e.tensor_scalar(out=hT[:, k, c0_:c0_ + 128], in0=pt[:, k4, :],
                                                              scalar1=A[:, k, cond:cond + 1],
                                                              scalar2=modF[:, shift_off + k, cond:cond + 1],
                                                              op0=ALU.mult, op1=ALU.add),
                             r=[b_pt, b_A, b_modF], w=[b_hT])
                    else:
                        P.op("act", lambda e: e.activation(out=hT[:, k, c0_:c0_ + 128], in_=pt[:, k4, :], func=AF.Identity,
                                                           scale=A[:, k, cond:cond + 1],
                                                           bias=modF[:, shift_off + k, cond:cond + 1]),
                             r=[b_pt, b_A, b_modF], w=[b_hT])


def make_nm_bufs(kb, G, npt):
    return dict(xt=[kb.sb([128, 1024], F32, "xt") for _ in range(G)], sq=[kb.sb([128, 1024], F32, "sq") for _ in range(G)],
                ss=kb.sb([128, 4, G], F32, "ss"), pts=[kb.ps([128, 4, 128], F32, "pt") for _ in range(npt)])


AB_W = 2576


def phase_l0_inproj(kb, IN):
    P = kb.P
    xs, b_xs = kb.dr["xs"]
    qkvu, b_qkvu = kb.dr["qkvu"]
    zab, b_zab = kb.dr["zab"]
    with kb.phase():
        modF, b_modF, (A1, b_A1), _ = load_mod_fm(kb, 0, IN, ("norm1_g", "norm2_g"))
        W, b_W = kb.sb([128, 8, AB_W], BF16, "win")
        stage = [kb.sb([128, 2576], F32, "wst") for _ in range(2)]
        for k in range(8):
            kb.load_w_bf16(W[:, k, :], b_W, IN["ab_w_in"][0, k * 128:(k + 1) * 128, :], 128, AB_W, stage, colblk=2576)
        pts_shared = [kb.ps([128, 4, 128], F32, "pt") for _ in range(4)]
        nmb = []
        for _i in range(2):
            bb_ = make_nm_bufs(kb, 2, 0)
            bb_["pts"] = pts_shared
            nmb.append(bb_)
        nmb[1]["rot"] = nmb[0].setdefault("rot", [0])
        hTs = [kb.sb([128, 8, 256], BF16, "hT") for _ in range(2)]
        outs = [kb.sb([128, 16, 256], F32, "ost") for _ in range(2)]
        zst = [kb.sb([128, 528], F32, "zst") for _ in range(2)]
        pf = [kb.ps([128, 512], F32, "pf") for _ in range(2)]
        pz = [kb.ps([128, 512], F32, "pz") for _ in range(1)]
        pab = kb.ps([128, 512], F32, "pab")
        fm_cols = [c * 128 for c in range(12)] + [2064 + c * 128 for c in range(4)]
        ui = 0
        for b in range(NB):
            for un in range(T // 256):
                hT, b_hT = hTs[ui % 2]
                ost, b_ost = outs[ui % 2]
                toks_ = [un * 256, un * 256 + 128]
                norm_mod_batch(kb, xs[b], b_xs, toks_, [2 if t_ < CTX else b for t_ in toks_], A1, b_A1, modF, b_modF, 0,
                               nmb[ui % 2], [(hT, b_hT, lambda i: i * 128)])
                for ci, c0 in enumerate(fm_cols):
                    ps, b_ps = pf[ci % 2]
                    for k in range(8):
                        P.op("pe", lambda e: e.matmul(ps[:, 0:256], lhsT=W[:, k, c0:c0 + 128], rhs=hT[:, k, :],
                                                      start=(k == 0), stop=(k == 7)), r=[b_W, b_hT], w=[b_ps])
                    if ci % 2 == 0:
                        P.op("act", lambda e: e.copy(out=ost[:, ci, :], in_=ps[:, 0:256]), r=[b_ps], w=[b_ost])
                    else:
                        P.op("dve", lambda e: e.tensor_copy(out=ost[:, ci, :], in_=ps[:, 0:256]), r=[b_ps], w=[b_ost])
                P.dma("sp", qkvu[b, :, un * 256:(un + 1) * 256].rearrange("(c p) t -> p c t", p=128), ost,
                      r=[b_ost], w=[b_qkvu])
                for tt in range(2):
                    z, b_z = zst[tt]
                    ps, b_ps = pz[0]
                    for k in range(8):
                        P.op("pe", lambda e: e.matmul(ps[:, 0:512], lhsT=hT[:, k, tt * 128:(tt + 1) * 128],
                                                      rhs=W[:, k, 1536:2048], start=(k == 0), stop=(k == 7)),
                             r=[b_W, b_hT], w=[b_ps])
                    for k in range(8):
                        P.op("pe", lambda e: e.matmul(pab[0][:, 0:16], lhsT=hT[:, k, tt * 128:(tt + 1) * 128],
                                                      rhs=W[:, k, 2048:2064], start=(k == 0), stop=(k == 7)),
                             r=[b_W, b_hT], w=[pab[1]])
                    P.op("act", lambda e: e.copy(out=z[:, 0:512], in_=ps), r=[b_ps], w=[b_z])
                    P.op("dve", lambda e: e.tensor_copy(out=z[:, 512:528], in_=pab[0][:, 0:16]), r=[pab[1]], w=[b_z])
                    tok0 = un * 256 + tt * 128
                    P.dma("sp", zab[b, tok0:tok0 + 128, :], z, r=[b_z], w=[b_zab])
                ui += 1


SEGS = ((0, CTX), (CTX, T))
CPIECES = [(0, CTX)] + [(CTX + 512 * q_, CTX + 512 * (q_ + 1)) for q_ in range(4)]
ADA1_IN_CONV = bool(int(_os.environ.get("ADA1_IN_CONV", "1")))
PE_CONV = bool(int(_os.environ.get("PE_CONV", "0")))


def phase_l0_conv(kb, IN):
    P = kb.P
    qkvu, b_qkvu = kb.dr["qkvu"]
    qkvn, b_qkvn = kb.dr["qkvn"]
    with kb.phase():
        cwr, b_cwr = kb.sb([5, 1536], F32, "cwr")
        P.dma("sp", cwr, IN["dn_conv_w"][0], w=[b_cwr])
        pcw, b_pcw = kb.ps([128, 12, 5], F32, "pcw")
        for c in range(12):
            P.op("pe", lambda e: e.transpose(out=pcw[:, c, :], in_=cwr[0:5, c * 128:(c + 1) * 128],
                                             identity=kb.ident[0:5, 0:5]), r=[b_cwr, kb.b_ident], w=[b_pcw])
        cw, b_cw = kb.sb([128, 12, 5], F32, "cw")
        P.op("dve", lambda e: e.tensor_copy(out=cw, in_=pcw), r=[b_pcw], w=[b_cw])
        epsc, b_epsc = kb.sb([128, 1], F32, "epsc")
        P.op("pool", lambda e: e.memset(epsc, EPS), w=[b_epsc])
        bufs = [dict(x=kb.sb([128, T], F32, "cx"), a=kb.sb([128, T], F32, "ca"), y=kb.sb([128, T], F32, "cy"),
                     q=kb.sb([128, T], F32, "cq")) for _ in range(2)]
        pss = [kb.ps([128, 512], F32, "cps") for _ in range(3)]
        pcv = [kb.ps([128, 512], F32, "pcv") for _ in range(1)] * 2
        ada_it = adaln_gen(kb, 1, IN) if ADA1_IN_CONV else iter(())
        next(ada_it, None)
        diags = [kb.sb([128, 5, 128], F32, "cdiag") for _ in range(2)]
        it = 0
        for b in range(NB):
            for c in range(12):
                B_ = bufs[it % 2]
                (x, b_x), (a, b_a), (y, b_y), (q, b_q) = B_["x"], B_["a"], B_["y"], B_["q"]
                P.dma("sp", x, qkvu[b, c * 128:(c + 1) * 128, :], r=[b_qkvu], w=[b_x])
                if PE_CONV and (it % 2 == 1):
                    (dg_, b_dg) = diags[(it // 2) % 2]
                    for j in range(5):
                        P.op("pool", lambda e: e.tensor_scalar(out=dg_[:, j, :], in0=kb.ident, scalar1=cw[:, c, j:j + 1], scalar2=None, op0=ALU.mult),
                             r=[kb.b_ident, b_cw], w=[b_dg])
                    for pi, (p0, p1) in enumerate(CPIECES):
                        ps, b_ps = pcv[pi % 2]
                        s0, s1 = (0, CTX) if p0 < CTX else (CTX, T)
                        for ji, j in enumerate((2, 0, 1, 3, 4)):
                            o = j - 2
                            t0, t1 = max(p0, s0 - o), min(p1, s1 - o)
                            P.op("pe", lambda e: e.matmul(ps[:, t0 - p0:t1 - p0], lhsT=dg_[:, j, :], rhs=x[:, t0 + o:t1 + o],
                                                          start=(ji == 0), stop=(ji == 4)), r=[b_dg, b_x], w=[b_ps])
                        P.op("act", lambda e: e.activation(out=y[:, p0:p1], in_=ps[:, 0:p1 - p0], func=AF.Silu), r=[b_ps], w=[b_y])
                else:
                    P.op("act", lambda e: e.activation(out=a, in_=x, func=AF.Copy, scale=cw[:, c, 2:3]), r=[b_x, b_cw], w=[b_a])
                    for j in (0, 1, 3, 4):
                        o = j - 2
                        for (s0, s1) in SEGS:
                            t0, t1 = max(s0, s0 - o), min(s1, s1 - o)
                            P.op("dve", lambda e: e.scalar_tensor_tensor(out=a[:, t0:t1], in0=x[:, t0 + o:t1 + o],
                                                                         scalar=cw[:, c, j:j + 1], in1=a[:, t0:t1],
                                                                         op0=ALU.mult, op1=ALU.add),
                                 r=[b_x, b_cw, b_a], w=[b_a])
                    P.op("act", lambda e: e.activation(out=y, in_=a, func=AF.Silu), r=[b_a], w=[b_y])
                if c < 8:
                    P.op("act", lambda e: e.activation(out=q, in_=y, func=AF.Square), r=[b_y], w=[b_q])
                    for pi, t0 in enumerate(range(0, T, 512)):
                        t1 = min(T, t0 + 512)
                        ps, b_ps = pss[pi % 3]
                        P.op("pe", lambda e: e.matmul(ps[:, 0:t1 - t0], lhsT=kb.ones, rhs=q[:, t0:t1], start=True, stop=True),
                             r=[kb.b_ones, b_q], w=[b_ps])
                        P.op("act", lambda e: e.activation(out=a[:, t0:t1], in_=ps[:, 0:t1 - t0], func=AF.Sqrt, bias=epsc[:, 0:1]),
                             r=[b_ps, b_epsc], w=[b_a])
                    P.op("dve", lambda e: e.reciprocal(out=q, in_=a), r=[b_a], w=[b_q])
                    sc = (128.0 ** -0.5) if c < 4 else 1.0
                    P.op("dve", lambda e: e.scalar_tensor_tensor(out=y, in0=y, scalar=sc, in1=q, op0=ALU.mult, op1=ALU.mult),
                         r=[b_y, b_q], w=[b_y])
                P.dma("sp", qkvn[b, c * 128:(c + 1) * 128, :], y, r=[b_y], w=[b_qkvn])
                it += 1
                if it % 2 == 0:
                    next(ada_it, None)
        for _ in ada_it:
            pass


BIG = 30000.0


def phase_l0_delta(kb, IN):
    P = kb.P
    qkvn, b_qkvn = kb.dr["qkvn"]
    zab, b_zab = kb.dr["zab"]
    mixT, b_mixT = kb.dr["mixT"]
    ident, b_ident, ones, b_ones = kb.ident, kb.b_ident, kb.ones, kb.b_ones
    with kb.phase():
        def mk(nm):
            return kb.sb([128, 128], F32, nm)
        (Lo, b_Lo), (Up, b_Up) = mk("Lo"), mk("Up")
        for (M, b_M, op_) in ((Lo, b_Lo, ALU.is_ge), (Up, b_Up, ALU.is_ge)):
            P.op("pool", lambda e: e.memset(M, 0.0), w=[b_M])
            for r0 in (0, 64):
                blk = M[r0:r0 + 64, r0:r0 + 64]
                P.op("pool", lambda e: e.memset(blk, 1.0), r=[b_M], w=[b_M])
                if M is Lo:
                    P.op("pool", lambda e: e.affine_select(out=blk, in_=blk, pattern=[[-1, 64]], compare_op=ALU.is_ge,
                                                           fill=0.0, base=0, channel_multiplier=1), r=[b_M], w=[b_M])
                else:
                    P.op("pool", lambda e: e.affine_select(out=blk, in_=blk, pattern=[[1, 64]], compare_op=ALU.is_ge,
                                                           fill=0.0, base=0, channel_multiplier=-1), r=[b_M], w=[b_M])
        masks = {}
        for nm, (M, b_M) in (("Lo", (Lo, b_Lo)), ("Up", (Up, b_Up))):
            for sgn in (1.0, -1.0):
                N_, b_N = mk("N" + nm)
                P.op("dve", lambda e: e.tensor_scalar(out=N_, in0=M, scalar1=-sgn * BIG, scalar2=sgn * BIG,
                                                      op0=ALU.mult, op1=ALU.add), r=[b_M], w=[b_N])
                masks[(nm, sgn)] = (N_, b_N)
        dirs = [dict(LT=(Up, b_Up), M1=masks[("Lo", 1.0)], M2=masks[("Up", -1.0)], last=(63, 127), order=(0, 64)),
                dict(LT=(Lo, b_Lo), M1=masks[("Up", 1.0)], M2=masks[("Lo", -1.0)], last=(0, 64), order=(64, 0))]
        dtb, b_dtb = kb.sb([128, 8], F32, "dtb")
        P.dma("sp", dtb, IN["dn_dt_bias"][0].rearrange("a b -> (a b)").partition_broadcast(128), w=[b_dtb])
        nA, b_nA = kb.sb([128, 8], F32, "nA")
        P.dma("sp", nA, IN["dn_A_log"][0].rearrange("a b -> (a b)").partition_broadcast(128), w=[b_nA])
        P.op("act", lambda e: e.activation(out=nA, in_=nA, func=AF.Exp), r=[b_nA], w=[b_nA])
        P.op("dve", lambda e: e.tensor_scalar(out=nA, in0=nA, scalar1=-1.0, scalar2=None, op0=ALU.mult), r=[b_nA], w=[b_nA])
        gB, b_gB = kb.sb([128, 128], F32, "gB")
        P.dma("sp", gB, IN["dn_norm_g"][0].partition_broadcast(128), w=[b_gB])

        gat, b_gat = kb.sb([128, NT, 16], F32, "gat")
        gg, b_gg = kb.sb([128, NT, 8], F32, "gg")
        bet, b_bet = kb.sb([128, NT, 8], F32, "bet")
        qT, b_qT = kb.sb([128, T], F32, "qT")
        kT, b_kT = kb.sb([128, T], F32, "kT")
        vT, b_vT = kb.sb([128, T], F32, "vT")
        zt, b_zt = kb.sb([128, NT, 128], F32, "zt")
        dst_, b_dst = kb.sb([128, 4, NT], F32, "dnst")
        qTb, b_qTb = kb.sb([128, T], BF16, "qTb")
        kTb, b_kTb = kb.sb([128, T], BF16, "kTb")
        vTb, b_vTb = kb.sb([128, T], BF16, "vTb")
        identb, b_identb = kb.identb, kb.b_identb
        osums = [kb.sb([128, NT, 128], F32, "osum") for _ in range(2)]
        aT, b_aT = kb.sb([128, T], F32, "aT")
        banks = [kb.ps([128, 4, 128], F32, "dps") for _ in range(8)]

        def slot(bk, i):
            ap, bf = banks[bk]
            return ap[:, i, :], bf
        chains = []
        NTMP = 21
        for c in range(2):
            o = 4 * c
            chains.append(dict(
                ps=[slot(o, 0), slot(o, 1), slot(o, 2), slot(o, 3), slot(o + 1, 0), slot(o + 1, 1), slot(o + 1, 2), slot(o + 1, 3),
                    slot(o + 2, 0), slot(o + 2, 1), slot(o + 2, 2), slot(o + 2, 3)],
                prec=[slot(o + 3, 0), slot(o + 3, 1), slot(o + 3, 2)],
                tset=[[kb.sb([128, 128], F32, "dt%d" % i) for i in range(NTMP)] for _ in range(2)],
                tsetb=[[kb.sb([128, 128], BF16, "db%d" % i) for i in range(14)] for _ in range(2)],
                Sb=kb.sb([128, 128], BF16, "Sb"),
                cset=[kb.sb([128, 8], F32, "dc") for _ in range(2)],
                vnew=[kb.sb([128, 128], BF16, "vnew") for _ in range(2)],
                S=kb.sb([128, 128], F32, "S"),
                osum=osums[c]))
        import os
        lim = [int(v) for v in os.environ.get("DELTA_LIM", "2,4,2").split(",")]

        def chain_gen(ch, d, h):
            dd = dirs[d]
            (LT, b_LT), (M1, b_M1), (M2, b_M2) = dd["LT"], dd["M1"], dd["M2"]
            col = d * 4 + h
            S, b_S = ch["S"]
            osum, b_osum = ch["osum"]
            P.op("pool", lambda e: e.memset(S, 0.0), w=[b_S])
            Sb_, b_Sb_ = ch["Sb"]
            P.op("pool", lambda e: e.memset(Sb_, 0.0), w=[b_Sb_])
            order = list(range(NT)) if d == 0 else [1, 0] + list(range(NT - 1, 1, -1))
            for it, j in enumerate(order):
                tm = ch["tset"][it % 2]
                (cs, b_cs) = ch["cset"][it % 2]
                tk = slice(j * 128, (j + 1) * 128)
                tb_ = ch["tsetb"][it % 2]
                (Lg, b_Lg), (Erow, b_Erow), (dec, b_dec), (decT, b_decT), (u, b_u) = tm[0:5]
                (A, b_A), (AT, b_AT), (TT0, b_TT0), (TT1, b_TT1) = tb_[0:4]
                Pm = [tb_[4], tb_[5]]
                PTm = [tb_[6], tb_[7]]
                (Xu, b_Xu), (Xw, b_Xw), (wT, b_wT), (qdT, b_qdT), (kdec, b_kdec), (qkT, b_qkT) = tb_[8:14]
                Sb, b_Sb = ch["Sb"]
                (pGr, b_pGr), (pgc, b_pgc), (pKK, b_pKK), (pQK, b_pQK) = ch["ps"][0:4]
                (pAT, b_pAT), (pP, b_pP), (pPT, b_pPT), (pTT, b_pTT) = ch["ps"][4:8]
                (pu, b_pu), (pwT, b_pwT), (pkt, b_pkt), (pvt, b_pvt) = ch["ps"][8:12]
                pATb = pAT.bitcast(BF16)[:, 0:128]
                pktb = pkt.bitcast(BF16)[:, 0:128]
                pvtb = pvt.bitcast(BF16)[:, 0:128]
                gcol = gg[:, j, col:col + 1]
                bcol = bet[:, j, col:col + 1]
                P.op("dve", lambda e: e.tensor_scalar(out=Lg, in0=LT, scalar1=gcol, scalar2=None, op0=ALU.mult),
                     r=[b_LT, b_gg], w=[b_Lg])
                P.op("pe", lambda e: e.matmul(pGr, lhsT=ones, rhs=Lg, start=True, stop=True), r=[b_ones, b_Lg], w=[b_pGr])
                P.op("pe", lambda e: e.matmul(pgc[:, 0:1], lhsT=Lg, rhs=ones[:, 0:1], start=True, stop=True),
                     r=[b_ones, b_Lg], w=[b_pgc])
                P.op("pe", lambda e: e.matmul(pKK, lhsT=kTb[:, tk], rhs=kTb[:, tk], start=True, stop=True), r=[b_kTb], w=[b_pKK])
                P.op("pe", lambda e: e.matmul(pQK, lhsT=kTb[:, tk], rhs=qTb[:, tk], start=True, stop=True), r=[b_kTb, b_qTb], w=[b_pQK])
                yield
                P.op("dve", lambda e: e.tensor_copy(out=cs[:, 0:1], in_=pgc[:, 0:1]), r=[b_pgc], w=[b_cs])
                P.op("act", lambda e: e.activation(out=cs[:, 1:2], in_=pgc[:, 0:1], func=AF.Copy, scale=-1.0), r=[b_pgc], w=[b_cs])
                P.op("dve", lambda e: e.tensor_tensor(out=dec, in0=pGr, in1=M1, op=ALU.add), r=[b_pGr, b_M1], w=[b_dec])
                P.op("dve", lambda e: e.tensor_tensor(out=decT, in0=pGr, in1=M2, op=ALU.add), r=[b_pGr, b_M2], w=[b_decT])
                P.op("act", lambda e: e.activation(out=Erow, in_=pGr, func=AF.Exp), r=[b_pGr], w=[b_Erow])
                for ci, r0 in enumerate((0, 64)):
                    lc = dd["last"][ci]
                    P.op("act", lambda e: e.activation(out=cs[r0:r0 + 64, 3:4], in_=pGr[r0:r0 + 64, lc:lc + 1], func=AF.Exp,
                                                       bias=cs[r0:r0 + 64, 1:2], scale=1.0), r=[b_pGr, b_cs], w=[b_cs])
                P.op("act", lambda e: e.activation(out=dec, in_=dec, func=AF.Exp, bias=cs[:, 0:1], scale=-1.0), r=[b_dec, b_cs], w=[b_dec])
                P.op("act", lambda e: e.activation(out=decT, in_=decT, func=AF.Exp, bias=cs[:, 1:2], scale=1.0), r=[b_decT, b_cs], w=[b_decT])
                P.op("act", lambda e: e.activation(out=cs[:, 2:3], in_=cs[:, 0:1], func=AF.Exp), r=[b_cs], w=[b_cs])
                P.op("dve", lambda e: e.tensor_tensor(out=cs[:, 4:5], in0=cs[:, 2:3], in1=bcol, op=ALU.mult), r=[b_cs, b_bet], w=[b_cs])
                yield
                P.op("pool", lambda e: e.tensor_tensor(out=dec, in0=dec, in1=ident, op=ALU.subtract), r=[b_dec, b_ident], w=[b_dec])
                P.op("dve", lambda e: e.scalar_tensor_tensor(out=A, in0=pKK, scalar=bcol, in1=dec, op0=ALU.mult, op1=ALU.mult),
                     r=[b_pKK, b_bet, b_dec], w=[b_A])
                P.op("dve", lambda e: e.tensor_tensor(out=qkT, in0=pQK, in1=decT, op=ALU.mult), r=[b_pQK, b_decT], w=[b_qkT])
                P.op("pe", lambda e: e.transpose(out=pATb, in_=A, identity=identb), r=[b_A, b_identb], w=[b_pAT])
                P.op("pe", lambda e: e.transpose(out=pktb, in_=kTb[:, tk], identity=identb), r=[b_kTb, b_identb], w=[b_pkt])
                P.op("pe", lambda e: e.transpose(out=pvtb, in_=vTb[:, tk], identity=identb), r=[b_vTb, b_identb], w=[b_pvt])
                yield
                P.op("act", lambda e: e.copy(out=AT, in_=pATb), r=[b_pAT], w=[b_AT])
                P.op("pool", lambda e: e.tensor_tensor(out=TT0, in0=ident, in1=AT, op=ALU.subtract), r=[b_ident, b_AT], w=[b_TT0])
                P.op("act", lambda e: e.activation(out=Xu, in_=pvtb, func=AF.Copy, scale=bcol), r=[b_pvt, b_bet], w=[b_Xu])
                P.op("act", lambda e: e.activation(out=Xw, in_=pktb, func=AF.Copy, scale=cs[:, 4:5]), r=[b_pkt, b_cs], w=[b_Xw])
                P.op("dve", lambda e: e.tensor_scalar(out=kdec, in0=pktb, scalar1=cs[:, 3:4], scalar2=0.0, op0=ALU.mult, op1=ALU.add),
                     r=[b_pkt, b_cs], w=[b_kdec])
                P.op("pool", lambda e: e.tensor_tensor(out=qdT, in0=qT[:, tk], in1=Erow, op=ALU.mult), r=[b_qT, b_Erow], w=[b_qdT])
                curP, curPT = (A, b_A), (AT, b_AT)
                TTc, TTn = (TT0, b_TT0), (TT1, b_TT1)
                for m in range(1, 6):
                    (nP, b_nP), (nPT, b_nPT) = Pm[m % 2], PTm[m % 2]
                    P.op("pe", lambda e: e.matmul(pP, lhsT=curPT[0], rhs=curP[0], start=True, stop=True),
                         r=[curPT[1], curP[1]], w=[b_pP])
                    if m < 5:
                        P.op("pe", lambda e: e.matmul(pPT, lhsT=curP[0], rhs=curPT[0], start=True, stop=True),
                             r=[curPT[1], curP[1]], w=[b_pPT])
                    yield
                    P.op("act", lambda e: e.copy(out=nP, in_=pP), r=[b_pP], w=[b_nP])
                    if m < 5:
                        P.op("dve", lambda e: e.tensor_copy(out=nPT, in_=pPT), r=[b_pPT], w=[b_nPT])
                    P.op("pe", lambda e: e.matmul(pTT, lhsT=nP, rhs=TTc[0], start=True, stop=True), r=[b_nP, TTc[1]], w=[b_pTT])
                    yield
                    P.op("dve", lambda e: e.tensor_tensor(out=TTn[0], in0=pTT, in1=TTc[0], op=ALU.add),
                         r=[b_pTT, TTc[1]], w=[TTn[1]])
                    curP, curPT = (nP, b_nP), (nPT, b_nPT)
                    TTc, TTn = TTn, TTc
                TT, b_TT = TTc
                P.op("pe", lambda e: e.matmul(pu, lhsT=TT, rhs=Xu, start=True, stop=True), r=[b_TT, b_Xu], w=[b_pu])
                P.op("pe", lambda e: e.matmul(pwT, lhsT=Xw, rhs=TT, start=True, stop=True), r=[b_TT, b_Xw], w=[b_pwT])
                yield
                P.op("act", lambda e: e.copy(out=u, in_=pu), r=[b_pu], w=[b_u])
                P.op("act", lambda e: e.copy(out=wT, in_=pwT), r=[b_pwT], w=[b_wT])
                for r0 in dd["order"]:
                    rs = slice(r0, r0 + 64)
                    lc = dd["last"][r0 // 64]
                    (vn, b_vn) = ch["vnew"][(r0 // 64)]
                    (p1, b_p1), (p2, b_p2), (p3, b_p3) = ch["prec"]
                    P.op("pe", lambda e: e.matmul(p1[rs, :], lhsT=wT[:, rs], rhs=Sb, start=True, stop=True), r=[b_wT, b_Sb], w=[b_p1])
                    yield
                    P.op("dve", lambda e: e.tensor_tensor(out=vn[rs, :], in0=u[rs, :], in1=p1[rs, :], op=ALU.subtract),
                         r=[b_u, b_p1], w=[b_vn])
                    P.op("pe", lambda e: e.matmul(p2[rs, :], lhsT=qdT[:, rs], rhs=Sb, start=True, stop=False), r=[b_qdT, b_Sb], w=[b_p2])
                    P.op("pe", lambda e: e.matmul(p2[rs, :], lhsT=qkT[rs, rs], rhs=vn[rs, :], start=False, stop=True),
                         r=[b_qkT, b_vn], w=[b_p2])
                    P.op("pe", lambda e: e.matmul(p3, lhsT=kdec[rs, :], rhs=vn[rs, :], start=True, stop=True), r=[b_kdec, b_vn], w=[b_p3])
                    yield
                    P.op("act", lambda e: e.copy(out=osum[rs, j, :], in_=p2[rs, :]), r=[b_p2], w=[b_osum])
                    P.op("dve", lambda e: e.scalar_tensor_tensor(out=S, in0=S, scalar=Erow[:, lc:lc + 1], in1=p3,
                                                                 op0=ALU.mult, op1=ALU.add), r=[b_S, b_Erow, b_p3], w=[b_S])
                    P.op("act", lambda e: e.copy(out=Sb, in_=S), r=[b_S], w=[b_Sb])

        for b in range(lim[0]):
            P.dma("sp", gat, zab[b, :, 512:528].rearrange("(j p) c -> p j c", p=128), r=[b_zab], w=[b_gat])
            P.op("dve", lambda e: e.tensor_tensor(out=gg, in0=gat[:, :, 0:8], in1=dtb.unsqueeze(1).to_broadcast([128, NT, 8]),
                                                  op=ALU.add), r=[b_gat, b_dtb], w=[b_gg])
            P.op("act", lambda e: e.activation(out=gg, in_=gg, func=AF.Exp), r=[b_gg], w=[b_gg])
            P.op("act", lambda e: e.activation(out=gg, in_=gg, func=AF.Ln, bias=1.0), r=[b_gg], w=[b_gg])
            P.op("dve", lambda e: e.tensor_tensor(out=gg, in0=gg, in1=nA.unsqueeze(1).to_broadcast([128, NT, 8]),
                                                  op=ALU.mult), r=[b_gg, b_nA], w=[b_gg])
            P.op("act", lambda e: e.activation(out=bet, in_=gat[:, :, 8:16], func=AF.Sigmoid), r=[b_gat], w=[b_bet])
            for h in range(lim[1]):
                P.dma("sp", qT, qkvn[b, h * 128:(h + 1) * 128, :], r=[b_qkvn], w=[b_qT])
                P.dma("sp", kT, qkvn[b, 512 + h * 128:512 + (h + 1) * 128, :], r=[b_qkvn], w=[b_kT])
                P.dma("sp", vT, qkvn[b, 1024 + h * 128:1024 + (h + 1) * 128, :], r=[b_qkvn], w=[b_vT])
                P.dma("sp", zt, zab[b, :, h * 128:(h + 1) * 128].rearrange("(j p) c -> p j c", p=128), r=[b_zab], w=[b_zt])
                P.op("act", lambda e: e.copy(out=qTb, in_=qT), r=[b_qT], w=[b_qTb])
                P.op("pool", lambda e: e.tensor_copy(out=kTb, in_=kT), r=[b_kT], w=[b_kTb])
                P.op("act", lambda e: e.copy(out=vTb, in_=vT), r=[b_vT], w=[b_vTb])
                gens = [chain_gen(chains[d], d, h) for d in range(2)]
                alive = [True, True]
                while any(alive):
                    for gi, g_ in enumerate(gens):
                        if alive[gi]:
                            try:
                                next(g_)
                            except StopIteration:
                                alive[gi] = False
                osum, b_osum = osums[0]
                P.op("pool", lambda e: e.tensor_tensor(out=osum, in0=osum, in1=osums[1][0], op=ALU.add), r=[b_osum, osums[1][1]], w=[b_osum])
                tset = chains[0]["tset"]
                cset = [[chains[0]["cset"][0]], [chains[0]["cset"][1]]]
                pset = [chains[0]["ps"], chains[1]["ps"]]
                itn = 0
                (sqj, b_sqj) = tset[0][0]
                for j in range(NT):
                    P.op("act", lambda e: e.activation(out=sqj, in_=osum[:, j, :], func=AF.Square, accum_out=dst_[:, 0, j:j + 1]),
                         r=[b_osum], w=[b_sqj, b_dst])
                P.op("dve", lambda e: e.tensor_scalar(out=dst_[:, 1, :], in0=dst_[:, 0, :], scalar1=1.0 / 128, scalar2=EPS,
                                                      op0=ALU.mult, op1=ALU.add), r=[b_dst], w=[b_dst])
                P.op("act", lambda e: e.activation(out=dst_[:, 2, :], in_=dst_[:, 1, :], func=AF.Sqrt), r=[b_dst], w=[b_dst])
                P.op("dve", lambda e: e.reciprocal(out=dst_[:, 3, :], in_=dst_[:, 2, :]), r=[b_dst], w=[b_dst])
                P.op("act", lambda e: e.activation(out=zt, in_=zt, func=AF.Silu), r=[b_zt], w=[b_zt])
                P.op("dve", lambda e: e.tensor_tensor(out=osum, in0=osum, in1=dst_[:, 3, :].unsqueeze(2).to_broadcast([128, NT, 128]), op=ALU.mult),
                     r=[b_osum, b_dst], w=[b_osum])
                P.op("pool", lambda e: e.tensor_tensor(out=zt, in0=zt, in1=gB.unsqueeze(1).to_broadcast([128, NT, 128]), op=ALU.mult),
                     r=[b_zt, b_gB], w=[b_zt])
                P.op("dve", lambda e: e.tensor_tensor(out=osum, in0=osum, in1=zt, op=ALU.mult), r=[b_osum, b_zt], w=[b_osum])
                for j0 in range(0, NT, 4):
                    nj = min(4, NT - j0)
                    bk_ap, bk_b = banks[(j0 // 4) % 8]
                    for jj in range(nj):
                        P.op("pe", lambda e: e.transpose(out=bk_ap[:, jj, :], in_=osum[:, j0 + jj, :], identity=ident), r=[b_osum, b_ident], w=[bk_b])
                    src_ = bk_ap[:, 0:nj, :]
                    dstv = aT[:, j0 * 128:(j0 + nj) * 128].rearrange("p (a b) -> p a b", b=128)
                    if (j0 // 4) % 2 == 0:
                        P.op("act", lambda e: e.copy(out=dstv, in_=src_), r=[bk_b], w=[b_aT])
                    else:
                        P.op("dve", lambda e: e.tensor_copy(out=dstv, in_=src_), r=[bk_b], w=[b_aT])
                P.dma("sp", mixT[b, h * 128:(h + 1) * 128, :], aT, r=[b_aT], w=[b_mixT])


def phase_l0_s5(kb, IN):
    P = kb.P
    qkvu, b_qkvu = kb.dr["qkvu"]
    ygT, b_ygT = kb.dr["ygT"]
    ident, b_ident = kb.ident, kb.b_ident
    with kb.phase():
        pbk = [kb.ps([128, 512], F32, "s5ps") for _ in range(7)]
        def stacked_T(src_a, src_b, nm):
            st, b_st = kb.sb([64, 128], F32, nm + "s")
            P.dma("sp", st[:, 0:64], src_a, w=[b_st])
            P.dma("sp", st[:, 64:128], src_b, w=[b_st])
            ps, b_ps = pbk[0]
            P.op("pe", lambda e: e.transpose(out=ps[:, 0:64], in_=st, identity=ident[0:64, 0:64]), r=[b_st, b_ident], w=[b_ps])
            o, b_o = kb.sb([128, 64], F32, nm)
            P.op("dve", lambda e: e.tensor_copy(out=o, in_=ps[:, 0:64]), r=[b_ps], w=[b_o])
            return o, b_o
        Are_src = IN["s5_A_re"][0].rearrange("d g p -> (d g) p")
        Aim_src = IN["s5_A_im"][0].rearrange("d g p -> (d g) p")
        Are, b_Are = stacked_T(Are_src, Are_src, "Are")
        Aim, b_Aim = stacked_T(Aim_src, Aim_src, "Aim")
        dt, b_dt = kb.sb([128, 64], F32, "dt")
        P.dma("sp", dt, IN["s5_log_dt"][0].rearrange("d g -> (d g)").partition_broadcast(128), w=[b_dt])
        P.op("act", lambda e: e.activation(out=dt, in_=dt, func=AF.Exp), r=[b_dt], w=[b_dt])
        NS = 14
        sm = [kb.sb([128, 64], F32, "s5sm%d" % i) for i in range(NS)]
        (rr, b_rr), (th, b_th), (t0_, b_t0), (t1_, b_t1), (cs_, b_cs), (sn_, b_sn), (lre, b_lre), (lim, b_lim) = sm[0:8]
        (den, b_den), (cre, b_cre), (cim, b_cim), (cimS, b_cimS), (creS, b_creS), (t2_, b_t2) = sm[8:14]

        def tt(eng, out, b_out, a, b_a, b, b_b, op):
            P.op(eng, lambda e: e.tensor_tensor(out=out, in0=a, in1=b, op=op), r=[b_a, b_b], w=[b_out])

        def ts(eng, out, b_out, a, b_a, s1, s2, op0, op1):
            P.op(eng, lambda e: e.tensor_scalar(out=out, in0=a, scalar1=s1, scalar2=s2, op0=op0, op1=op1), r=[b_a], w=[b_out])
        tt("dve", rr, b_rr, Are, b_Are, dt, b_dt, ALU.mult)
        P.op("act", lambda e: e.activation(out=rr, in_=rr, func=AF.Exp), r=[b_rr], w=[b_rr])
        tt("dve", th, b_th, Aim, b_Aim, dt, b_dt, ALU.mult)

        def sincos(dst, b_dst, shift):
            ts("dve", t0_, b_t0, th, b_th, shift, 1.0 / TWO_PI, ALU.add, ALU.mult)
            ts("dve", t1_, b_t1, t0_, b_t0, MAGIC, None, ALU.add, ALU.bypass)
            ts("dve", t1_, b_t1, t1_, b_t1, -MAGIC, -TWO_PI, ALU.add, ALU.mult)
            P.op("dve", lambda e: e.scalar_tensor_tensor(out=t0_, in0=th, scalar=shift, in1=t1_, op0=ALU.add, op1=ALU.add),
                 r=[b_th, b_t1], w=[b_t0])
            P.op("act", lambda e: e.activation(out=dst, in_=t0_, func=AF.Sin), r=[b_t0], w=[b_dst])
        sincos(cs_, b_cs, math.pi / 2)
        sincos(sn_, b_sn, 0.0)
        tt("dve", lre, b_lre, rr, b_rr, cs_, b_cs, ALU.mult)
        tt("dve", lim, b_lim, rr, b_rr, sn_, b_sn, ALU.mult)
        ts("dve", lre, b_lre, lre, b_lre, -1.0, None, ALU.add, ALU.bypass)
        tt("dve", den, b_den, Are, b_Are, Are, b_Are, ALU.mult)
        tt("dve", t2_, b_t2, Aim, b_Aim, Aim, b_Aim, ALU.mult)
        tt("dve", den, b_den, den, b_den, t2_, b_t2, ALU.add)
        P.op("dve", lambda e: e.reciprocal(out=den, in_=den), r=[b_den], w=[b_den])
        tt("dve", cre, b_cre, lre, b_lre, Are, b_Are, ALU.mult)
        tt("dve", t2_, b_t2, lim, b_lim, Aim, b_Aim, ALU.mult)
        tt("dve", cre, b_cre, cre, b_cre, t2_, b_t2, ALU.add)
        tt("dve", cre, b_cre, cre, b_cre, den, b_den, ALU.mult)
        tt("dve", cim, b_cim, lim, b_lim, Are, b_Are, ALU.mult)
        tt("dve", t2_, b_t2, lre, b_lre, Aim, b_Aim, ALU.mult)
        tt("dve", cim, b_cim, cim, b_cim, t2_, b_t2, ALU.subtract)
        tt("dve", cim, b_cim, cim, b_cim, den, b_den, ALU.mult)
        ts("dve", cimS[0:64, :], b_cimS, cim[0:64, :], b_cim, -1.0, None, ALU.mult, ALU.bypass)
        P.op("dve", lambda e: e.tensor_copy(out=cimS[64:128, :], in_=cim[64:128, :]), r=[b_cim], w=[b_cimS])
        P.op("dve", lambda e: e.tensor_copy(out=creS[0:64, :], in_=cre[0:64, :]), r=[b_cre], w=[b_creS])
        ts("dve", creS[64:128, :], b_creS, cre[64:128, :], b_cre, -1.0, None, ALU.mult, ALU.bypass)
        Bst1, b_Bst1 = kb.sb([128, 32, 16], F32, "Bst1")
        BstS, b_BstS = kb.sb([128, 32, 16], F32, "BstS")
        Bre_src = IN["s5_B_re"][0].rearrange("g p h -> p g h")
        Bim_src = IN["s5_B_im"][0].rearrange("g p h -> p g h")
        P.dma("sp", Bst1[0:64], Bre_src, w=[b_Bst1]); P.dma("sp", Bst1[64:128], Bim_src, w=[b_Bst1])
        P.dma("sp", BstS[0:64], Bim_src, w=[b_BstS]); P.dma("sp", BstS[64:128], Bre_src, w=[b_BstS])
        Wst, b_Wst = kb.sb([128, 4, 2, 2, 2, 128], BF16, "Wst")
        Wst3, b_Wst3 = kb.sb([128, 4, 2, 2, 2, 128], BF16, "Wst3")
        src, b_src = kb.sb([128, 128], F32, "wsrc")
        wtmp, b_wtmp = kb.sb([128, 16], F32, "wtmp")
        for c in range(4):
            for m in range(2):
                for d in range(2):
                    for var in range(2):
                        P.op("pool", lambda e: e.memset(src, 0.0), w=[b_src])
                        for pr in range(4):
                            g = 8 * c + 2 * pr + m
                            dg = d * 32 + g
                            dst = src[:, 32 * pr + 16 * m:32 * pr + 16 * m + 16]
                            if var == 0:
                                P.op("dve", lambda e: e.tensor_scalar(out=wtmp, in0=Bst1[:, g, :], scalar1=cre[:, dg:dg + 1], scalar2=None,
                                                                      op0=ALU.mult), r=[b_Bst1, b_cre], w=[b_wtmp])
                                P.op("dve", lambda e: e.scalar_tensor_tensor(out=dst, in0=BstS[:, g, :], scalar=cimS[:, dg:dg + 1], in1=wtmp,
                                                                             op0=ALU.mult, op1=ALU.add), r=[b_BstS, b_cimS, b_wtmp], w=[b_src])
                            else:
                                P.op("dve", lambda e: e.tensor_scalar(out=wtmp, in0=BstS[:, g, :], scalar1=creS[:, dg:dg + 1], scalar2=None,
                                                                      op0=ALU.mult), r=[b_BstS, b_creS], w=[b_wtmp])
                                P.op("dve", lambda e: e.scalar_tensor_tensor(out=dst, in0=Bst1[:, g, :], scalar=cim[:, dg:dg + 1], in1=wtmp,
                                                                             op0=ALU.mult, op1=ALU.add), r=[b_Bst1, b_cim, b_wtmp], w=[b_src])
                        ps, b_ps = pbk[1]
                        P.op("pe", lambda e: e.transpose(out=ps[:, 0:128], in_=src, identity=ident), r=[b_src, b_ident], w=[b_ps])
                        P.op("act", lambda e: e.copy(out=Wst[:, c, m, d, var, :], in_=ps[:, 0:128]), r=[b_ps], w=[b_Wst])
                        P.op("act", lambda e: e.copy(out=Wst3[64:128, c, m, d, var, :], in_=ps[64:128, 0:128]), r=[b_ps], w=[b_Wst3])
                        P.op("pool", lambda e: e.memset(Wst3[64:96, c, m, d, var, :], 0.0), r=[b_Wst3], w=[b_Wst3])
        CrPad, b_CrPad = kb.sb([128, 32, 2, 128], BF16, "CrPad")
        P.op("pool", lambda e: e.memset(CrPad, 0.0), w=[b_CrPad])
        Cre_src = IN["s5_C_re"][0].rearrange("g h p -> (g h) p")
        Cim_src = IN["s5_C_im"][0].rearrange("g h p -> (g h) p")
        cst, b_cst = kb.sb([128, 128], F32, "cst")
        for var in range(2):
            for q in range(4):
                rows = slice(q * 128, (q + 1) * 128)
                if var == 0:
                    P.dma("sp", cst[:, 0:64], Cre_src[rows, :], w=[b_cst]); P.dma("sp", cst[:, 64:128], Cim_src[rows, :], w=[b_cst])
                else:
                    P.dma("sp", cst[:, 0:64], Cim_src[rows, :], w=[b_cst]); P.dma("sp", cst[:, 64:128], Cre_src[rows, :], w=[b_cst])
                ps, b_ps = pbk[2]
                P.op("pe", lambda e: e.transpose(out=ps[:, 0:128], in_=cst, identity=ident), r=[b_cst, b_ident], w=[b_ps])
                for gl in range(8):
                    g = q * 8 + gl
                    cols = slice(gl * 16, gl * 16 + 16)
                    sgn_top = 1.0 if var == 0 else -1.0
                    P.op("act", lambda e: e.activation(out=CrPad[0:64, g, var, cols], in_=ps[0:64, cols], func=AF.Copy, scale=sgn_top),
                         r=[b_ps], w=[b_CrPad])
                    P.op("act", lambda e: e.activation(out=CrPad[64:128, g, var, cols], in_=ps[64:128, cols], func=AF.Copy, scale=-1.0),
                         r=[b_ps], w=[b_CrPad])
        Dc, b_Dc = kb.sb([128, 4], F32, "Dc")
        kb.load_vec_fm(IN["s5_D"][0].rearrange("g h -> (g h)"), 4, Dc, b_Dc, pbk[0])
        from concourse.mybir import dt as _dt
        iof, b_iof = kb.sb([128, T], F32, "iof")
        with kb.phase():
            ioi, b_ioi = kb.sb([128, T], _dt.int32, "ioi")
            P.op("pool", lambda e: e.iota(ioi, pattern=[[1, T]], base=0, channel_multiplier=0), w=[b_ioi])
            P.op("dve", lambda e: e.tensor_copy(out=iof, in_=ioi), r=[b_ioi], w=[b_iof])
        ub = [kb.sb([128, T], BF16, "ub") for _ in range(NB)]
        ysb = [kb.sb([128, T], F32, "ysb") for _ in range(NB)]
        tabs = [dict(C=kb.sb([128, T], F32, "tC"), S=kb.sb([128, T], F32, "tS")) for _ in range(2)]
        ph1, b_ph1 = kb.sb([128, T], F32, "ph1")
        ph2, b_ph2 = ph1, b_ph1
        tmps5 = [dict(bt=kb.sb([128, T], F32, "bt"), tmp=kb.sb([128, T], F32, "tmp"), ww=kb.sb([128, T], F32, "ww"),
                      Q1=kb.sb([128, T], BF16, "Q1"), Q2=kb.sb([128, T], BF16, "Q2")) for _ in range(2)]
        s5it = 0
        pieces = [(0, 256)] + [(256 + 512 * q, 256 + 512 * (q + 1)) for q in range(4)]

        def possl(d, t0, t1):
            if d == 0:
                return slice(t0, t1)
            if t1 <= CTX:
                hi, lo = CTX - 1 - t0, CTX - t1
            else:
                hi, lo = (T + CTX - 1) - t0, (T + CTX) - t1
            return slice(hi, lo - 1 if lo > 0 else None, -1)
        import os
        glim = int(os.environ.get("S5_GLIM", "32"))
        tix = 0
        for c in range(4):
            for b in range(NB):
                P.dma("sp", ysb[b][0], qkvu[b, 1536 + c * 128:1536 + (c + 1) * 128, :], r=[b_qkvu], w=[ysb[b][1]])
                P.op("act", lambda e: e.copy(out=ub[b][0], in_=ysb[b][0]), r=[ysb[b][1]], w=[ub[b][1]])
                P.op("dve", lambda e: e.tensor_scalar(out=ysb[b][0], in0=ysb[b][0], scalar1=Dc[:, c:c + 1], scalar2=None, op0=ALU.mult),
                     r=[ysb[b][1], b_Dc], w=[ysb[b][1]])
            its = [(gl, d, b) for gl in range(8) if 8 * c + gl < glim for d in range(2) for b in range(NB)]
            nit = len(its)

            def info(t):
                gl, d, b = its[t]
                g = 8 * c + gl
                pr, m = gl // 2, gl % 2
                tb = tabs[(t // 2) % 2]
                tq = tmps5[t % 2]
                return g, d, b, pr, m, d * 32 + g, tb["C"], tb["S"], tq

            def S_tables(t):
                g, d, b, pr, m, dg, (tC, b_tC), (tS, b_tS), tq = info(t)
                for (tab, b_tab, shift, ph, b_ph) in ((tC, b_tC, math.pi / 2, ph1, b_ph1), (tS, b_tS, 0.0, ph2, b_ph2)):
                    P.op("act", lambda e: e.activation(out=ph, in_=iof, func=AF.Identity, scale=th[:, dg:dg + 1],
                                                       bias=kb.shiftc[shift][0][:, 0:1]), r=[b_iof, b_th, kb.shiftc[shift][1]], w=[b_ph])
                    P.op("act", lambda e: e.activation(out=tab, in_=ph, func=AF.Identity, scale=1.0 / TWO_PI,
                                                       bias=kb.shiftc[MAGIC][0][:, 0:1]), r=[b_ph, kb.shiftc[MAGIC][1]], w=[b_tab])
                    P.op("act", lambda e: e.activation(out=tab, in_=tab, func=AF.Identity, scale=1.0,
                                                       bias=kb.shiftc[-MAGIC][0][:, 0:1]), r=[b_tab, kb.shiftc[-MAGIC][1]], w=[b_tab])
                    P.op("dve", lambda e: e.scalar_tensor_tensor(out=ph, in0=tab, scalar=-TWO_PI, in1=ph, op0=ALU.mult, op1=ALU.add),
                         r=[b_tab, b_ph], w=[b_ph])
                    P.op("act", lambda e: e.activation(out=tab, in_=ph, func=AF.Sin), r=[b_ph], w=[b_tab])

            def S_A(t):
                g, d, b, pr, m, dg, (tC, b_tC), (tS, b_tS), tq = info(t)
                (bt, b_bt), (tmp, b_tmp) = tq["bt"], tq["tmp"]
                prs = slice(32 * pr, 32 * pr + 32)
                for pi, (t0, t1) in enumerate(pieces):
                    n = t1 - t0
                    psl = possl(d, t0, t1)
                    (pA1, b_pA1), (pA2, b_pA2) = pbk[(pi % 2) * 2], pbk[(pi % 2) * 2 + 1]
                    Wuse, b_Wuse, prs_ = (Wst, b_Wst, prs) if pr < 3 else (Wst3, b_Wst3, slice(64, 128))
                    P.op("pe", lambda e: e.matmul(pA1[:, 0:n], lhsT=Wuse[prs_, c, m, d, 0, :], rhs=ub[b][0][prs_, psl], start=True, stop=True),
                         r=[b_Wuse, ub[b][1]], w=[b_pA1])
                    P.op("pe", lambda e: e.matmul(pA2[:, 0:n], lhsT=Wuse[prs_, c, m, d, 1, :], rhs=ub[b][0][prs_, psl], start=True, stop=True),
                         r=[b_Wuse, ub[b][1]], w=[b_pA2])
                    P.op("dve", lambda e: e.tensor_tensor(out=bt[:, t0:t1], in0=pA1[:, 0:n], in1=tC[:, t0:t1], op=ALU.mult),
                         r=[b_pA1, b_tC], w=[b_bt])
                    P.op("dve", lambda e: e.tensor_tensor(out=tmp[:, t0:t1], in0=pA2[:, 0:n], in1=tS[:, t0:t1], op=ALU.mult),
                         r=[b_pA2, b_tS], w=[b_tmp])

            def S_BC(t):
                g, d, b, pr, m, dg, (tC, b_tC), (tS, b_tS), tq = info(t)
                (bt, b_bt), (tmp, b_tmp), (ww, b_ww) = tq["bt"], tq["tmp"], tq["ww"]
                P.op("pool", lambda e: e.tensor_tensor(out=bt, in0=bt, in1=tmp, op=ALU.add), r=[b_bt, b_tmp], w=[b_bt])
                P.op("dve", lambda e: e.tensor_tensor_scan(out=ww, data0=rr[:, dg:dg + 1].to_broadcast([128, T]), data1=bt,
                                                           initial=0.0, op0=ALU.mult, op1=ALU.add), r=[b_bt, b_rr], w=[b_ww])

            def S_D(t):
                g, d, b, pr, m, dg, (tC, b_tC), (tS, b_tS), tq = info(t)
                (ww, b_ww), (Q1, b_Q1), (Q2, b_Q2) = tq["ww"], tq["Q1"], tq["Q2"]
                P.op("pool", lambda e: e.tensor_tensor(out=Q1, in0=ww, in1=tC, op=ALU.mult), r=[b_ww, b_tC], w=[b_Q1])
                P.op("pool", lambda e: e.tensor_tensor(out=Q2, in0=ww, in1=tS, op=ALU.mult), r=[b_ww, b_tS], w=[b_Q2])

            def S_E(t):
                g, d, b, pr, m, dg, (tC, b_tC), (tS, b_tS), tq = info(t)
                (Q1, b_Q1), (Q2, b_Q2) = tq["Q1"], tq["Q2"]
                for pi, (t0, t1) in enumerate(pieces):
                    n = t1 - t0
                    psl = possl(d, t0, t1)
                    (pY, b_pY) = pbk[4 + pi % 3]
                    P.op("pe", lambda e: e.matmul(pY[:, 0:n], lhsT=CrPad[:, g, 0, :], rhs=Q1[:, t0:t1], start=True, stop=False),
                         r=[b_CrPad, b_Q1], w=[b_pY])
                    P.op("pe", lambda e: e.matmul(pY[:, 0:n], lhsT=CrPad[:, g, 1, :], rhs=Q2[:, t0:t1], start=False, stop=True),
                         r=[b_CrPad, b_Q2], w=[b_pY])
                    yv = ysb[b][0][:, psl]
                    P.op("dve", lambda e: e.tensor_tensor(out=yv, in0=pY[:, 0:n], in1=yv, op=ALU.add),
                         r=[b_pY, ysb[b][1]], w=[ysb[b][1]])

            for step in range(nit + 3):
                if step < nit and step % 2 == 0:
                    S_tables(step)
                if step < nit:
                    S_A(step)
                if 0 <= step - 1 < nit:
                    S_BC(step - 1)
                if 0 <= step - 2 < nit:
                    S_D(step - 2)
                if 0 <= step - 3 < nit:
                    S_E(step - 3)
            for b in range(NB):
                P.op("act", lambda e: e.activation(out=ysb[b][0], in_=ysb[b][0], func=AF.Gelu), r=[ysb[b][1]], w=[ysb[b][1]])
                P.dma("sp", ygT[b, c * 128:(c + 1) * 128, :], ysb[b][0], r=[ysb[b][1]], w=[b_ygT])


def load_gate_rows(kb, li, off):
    P = kb.P
    modd, b_modd = kb.dr["modd%d" % li]
    out = []
    for c in range(3):
        g, b_g = kb.sb([128, 1024], F32, "gB")
        P.dma("sp", g, modd[c, off:off + 1024].partition_broadcast(128), r=[b_modd], w=[b_g])
        out.append((g, b_g))
    return out


def load_wchunks_bf16(kb, dst, b_dst, src2d, nk, cols, stage, engines=("pool",)):
    P = kb.P
    for k in range(nk):
        st, b_st = stage[kb.uid % len(stage)]
        kb.uid += 1
        P.dma("sp", st[:, 0:cols], src2d[k * 128:(k + 1) * 128, :], w=[b_st])
        eng = engines[k % len(engines)]
        if eng == "act":
            P.op("act", lambda e: e.copy(out=dst[:, k, :], in_=st[:, 0:cols]), r=[b_st], w=[b_dst])
        else:
            P.op(eng, lambda e: e.tensor_copy(out=dst[:, k, :], in_=st[:, 0:cols]), r=[b_st], w=[b_dst])


def phase_l0_out(kb, IN):
    P = kb.P
    xs, b_xs = kb.dr["xs"]
    mixT, b_mixT = kb.dr["mixT"]
    ygT, b_ygT = kb.dr["ygT"]
    with kb.phase():
        gB = load_gate_rows(kb, 0, 2048)
        stage = [kb.sb([128, 1024], F32, "wst") for _ in range(2)]
        Wglu, b_Wglu = kb.sb([128, 4, 512], BF16, "wglu")
        load_wchunks_bf16(kb, Wglu, b_Wglu, IN["s5_glu_w"][0], 4, 512, stage, ("pool", "act", "dve"))
        Wout, b_Wout = kb.sb([128, 8, 1024], BF16, "wout")
        load_wchunks_bf16(kb, Wout, b_Wout, IN["ab_w_out"][0], 8, 1024, stage, ("pool", "act", "dve"))
        pgl = [kb.ps([128, 512], F32, "pgl") for _ in range(2)]
        pdd = [kb.ps([128, 512], F32, "pdd") for _ in range(2)]
        glub, b_glub = kb.sb([128, 4], F32, "glub")
        kb.load_vec_fm(IN["s5_glu_b"][0], 4, glub, b_glub, pgl[0])
        ygs = [kb.sb([128, 4, 256], F32, "yg32") for _ in range(2)]
        ygbs = [kb.sb([128, 4, 256], BF16, "ygb") for _ in range(2)]
        a32s = [kb.sb([128, 4, 256], F32, "a32") for _ in range(2)]
        mixbs = [kb.sb([128, 8, 256], BF16, "mixb") for _ in range(2)]
        sigs = [kb.sb([128, 256], F32, "sig") for _ in range(2)]
        xts = [kb.sb([128, 1024], F32, "xt") for _ in range(2)]
        tms = [kb.sb([128, 512], F32, "tm") for _ in range(2)]
        ui = 0
        for b in range(NB):
            for un in range(T // 256):
                tsl = slice(un * 256, (un + 1) * 256)
                (yg, b_yg), (ygb, b_ygb), (a32, b_a32), (mixb, b_mixb) = ygs[ui % 2], ygbs[ui % 2], a32s[ui % 2], mixbs[ui % 2]
                ui += 1
                P.dma("sp", yg, ygT[b, :, tsl].rearrange("(c p) t -> p c t", p=128), r=[b_ygT], w=[b_yg])
                P.dma("sp", a32, mixT[b, 0:512, tsl].rearrange("(c p) t -> p c t", p=128), r=[b_mixT], w=[b_a32])
                P.op("act", lambda e: e.copy(out=ygb, in_=yg), r=[b_yg], w=[b_ygb])
                P.op("pool", lambda e: e.tensor_copy(out=mixb[:, 0:4, :], in_=a32), r=[b_a32], w=[b_mixb])
                for f in range(4):
                    ps, b_ps = pgl[f % 2]
                    sg, b_sg = sigs[f % 2]
                    for k in range(4):
                        P.op("pe", lambda e: e.matmul(ps[:, 0:256], lhsT=Wglu[:, k, f * 128:(f + 1) * 128], rhs=ygb[:, k, :],
                                                      start=(k == 0), stop=(k == 3)), r=[b_Wglu, b_ygb], w=[b_ps])
                    P.op("act", lambda e: e.activation(out=sg, in_=ps[:, 0:256], func=AF.Sigmoid, bias=glub[:, f:f + 1]),
                         r=[b_ps, b_glub], w=[b_sg])
                    P.op("dve", lambda e: e.tensor_tensor(out=mixb[:, 4 + f, :], in0=yg[:, f, :], in1=sg, op=ALU.mult),
                         r=[b_yg, b_sg], w=[b_mixb])
                for tt in range(2):
                    tok0 = un * 256 + tt * 128
                    cond = 2 if tok0 < CTX else b
                    xt, b_xt = xts[tt]
                    P.dma("sp", xt, xs[b, tok0:tok0 + 128, :], r=[b_xs], w=[b_xt])
                    for half in range(2):
                        pd, b_pd = pdd[half]
                        tm, b_tm = tms[half]
                        hs = slice(half * 512, (half + 1) * 512)
                        for k in range(8):
                            P.op("pe", lambda e: e.matmul(pd, lhsT=mixb[:, k, tt * 128:(tt + 1) * 128], rhs=Wout[:, k, hs],
                                                          start=(k == 0), stop=(k == 7)), r=[b_mixb, b_Wout], w=[b_pd])
                        P.op("dve", lambda e: e.tensor_tensor(out=tm, in0=pd, in1=gB[cond][0][:, hs], op=ALU.mult),
                             r=[b_pd, gB[cond][1]], w=[b_tm])
                        P.op("pool", lambda e: e.tensor_tensor(out=xt[:, hs], in0=xt[:, hs], in1=tm, op=ALU.add),
                             r=[b_xt, b_tm], w=[b_xt])
                    P.dma("sp", xs[b, tok0:tok0 + 128, :], xt, r=[b_xt], w=[b_xs])


def phase_moe(kb, IN, li, tok_lo):
    P = kb.P
    xs, b_xs = kb.dr["xs"]
    ident, b_ident = kb.ident, kb.b_ident
    ntile_all = (T - tok_lo) // 128
    nblk = int(_os.environ.get("MOE_NBLK", "1"))
    tiles_per_blk = ntile_all // nblk
    TB = tiles_per_blk * 128
    with kb.phase():
        modF, b_modF, _, (A2, b_A2) = load_mod_fm(kb, li, IN, ("norm1_g", "norm2_g"))
        rw, b_rw = kb.sb([128, 8, 16], F32, "rw")
        P.dma("sp", rw, IN["router_w"].rearrange("(k p) e -> p k e", p=128), w=[b_rw])
        rb, b_rb = kb.sb([128, 16], F32, "rb")
        P.dma("sp", rb, IN["router_bias"].partition_broadcast(128), w=[b_rb])
        Esel, b_Esel = kb.sb([16, 16, 128], F32, "Esel")
        P.op("dve", lambda e: e.tensor_copy(out=Esel, in_=ident[0:16, 0:16].unsqueeze(2).to_broadcast([16, 16, 128])),
             r=[b_ident], w=[b_Esel])
        hT, b_hT = kb.sb([128, 8, TB], BF16, "hTm")
        gT, b_gT = kb.sb([16, TB], F32, "gT")
        import os
        elim = int(os.environ.get("MOE_ELIM", "16"))
        for b in range(NB):
            for blk in range(nblk):
                base = tok_lo + blk * TB
                with kb.phase():
                    GB = 3 if tiles_per_blk % 3 == 0 else 4
                    nmb = make_nm_bufs(kb, GB, 4)
                    h32s = [kb.sb([128, 8, 128], F32, "h32") for _ in range(GB)]
                    plog, b_plog = kb.ps([128, 512], F32, "plog")
                    pgT, b_pgT = kb.ps([128, 512], F32, "pgT")
                    nt_ = tiles_per_blk
                    h32all, b_h32all = kb.sb([128, 8, GB * 128], F32, "h32all")
                    for j0 in range(0, tiles_per_blk, GB):
                        toks_ = [base + (j0 + i) * 128 for i in range(GB)]
                        norm_mod_batch(kb, xs[b], b_xs, toks_, [2 if t_ < CTX else b for t_ in toks_], A2, b_A2, modF, b_modF, 24,
                                       nmb, [(hT, b_hT, lambda i, j0=j0: (j0 + i) * 128), (h32all, b_h32all, lambda i: i * 128)])
                        for i in range(GB):
                            j = j0 + i
                            for k in range(8):
                                P.op("pe", lambda e: e.matmul(plog[:, j * 16:(j + 1) * 16], lhsT=h32all[:, k, i * 128:(i + 1) * 128], rhs=rw[:, k, :],
                                                              start=(k == 0), stop=(k == 7)), r=[b_h32all, b_rw], w=[b_plog])
                    def T3(nm, inner=16):
                        return kb.sb([128, nt_, inner], F32, nm)
                    (sc, b_sc), (ch, b_ch), (em, b_em), (cm, b_cm), (sel, b_sel) = T3("sc"), T3("ch"), T3("em"), T3("cm"), T3("sel")
                    (q4, b_q4) = kb.sb([128, 4, nt_, 4], F32, "q4")
                    (r4, b_r4) = kb.sb([128, 4, nt_, 4], F32, "r4")
                    (gs, b_gs), (gm, b_gm) = T3("gs", 4), T3("gm", 4)
                    (col, b_col) = kb.sb([128, 6, nt_], F32, "col")

                    def tt(out, i0, i1, op, rr, ww):
                        P.op("dve", lambda e: e.tensor_tensor(out=out, in0=i0, in1=i1, op=op), r=rr, w=ww)
                    P.op("act", lambda e: e.activation(out=sc, in_=plog[:, 0:nt_ * 16].rearrange("p (j e) -> p j e", e=16), func=AF.Sigmoid),
                         r=[b_plog], w=[b_sc])
                    tt(ch, sc, rb.unsqueeze(1).to_broadcast([128, nt_, 16]), ALU.add, [b_sc, b_rb], [b_ch])
                    ch4 = ch.rearrange("p j (g e) -> p j g e", e=4)
                    a_, b_, c_, d_ = ch4[:, :, :, 0], ch4[:, :, :, 1], ch4[:, :, :, 2], ch4[:, :, :, 3]
                    tt(q4[:, 0], a_, b_, ALU.max, [b_ch], [b_q4])
                    tt(q4[:, 1], a_, b_, ALU.min, [b_ch], [b_q4])
                    tt(q4[:, 2], c_, d_, ALU.max, [b_ch], [b_q4])
                    tt(q4[:, 3], c_, d_, ALU.min, [b_ch], [b_q4])
                    tt(r4[:, 0], q4[:, 0], q4[:, 2], ALU.max, [b_q4], [b_r4])
                    tt(r4[:, 1], q4[:, 0], q4[:, 2], ALU.min, [b_q4], [b_r4])
                    tt(r4[:, 2], q4[:, 1], q4[:, 3], ALU.max, [b_q4], [b_r4])
                    tt(r4[:, 3], r4[:, 1], r4[:, 2], ALU.max, [b_r4], [b_r4])
                    tt(gs, r4[:, 0], r4[:, 3], ALU.add, [b_r4], [b_gs])
                    P.op("dve", lambda e: e.tensor_reduce(out=col[:, 0, :], in_=gs, axis=AX.X, op=ALU.max), r=[b_gs], w=[b_col])
                    tt(gm, gs, col[:, 0, :].unsqueeze(2).to_broadcast([128, nt_, 4]), ALU.is_ge, [b_gs, b_col], [b_gm])
                    em4 = em.rearrange("p j (g e) -> p j g e", e=4)
                    P.op("dve", lambda e: e.tensor_copy(out=em4, in_=gm.unsqueeze(3).to_broadcast([128, nt_, 4, 4])), r=[b_gm], w=[b_em])
                    tt(cm, ch, em, ALU.mult, [b_ch, b_em], [b_cm])
                    P.op("dve", lambda e: e.tensor_scalar(out=em, in0=em, scalar1=BIG, scalar2=-BIG, op0=ALU.mult, op1=ALU.add), r=[b_em], w=[b_em])
                    tt(cm, cm, em, ALU.add, [b_cm, b_em], [b_cm])
                    P.op("dve", lambda e: e.tensor_reduce(out=col[:, 1, :], in_=cm, axis=AX.X, op=ALU.max), r=[b_cm], w=[b_col])
                    tt(sel, cm, col[:, 1, :].unsqueeze(2).to_broadcast([128, nt_, 16]), ALU.is_ge, [b_cm, b_col], [b_sel])
                    P.op("dve", lambda e: e.scalar_tensor_tensor(out=cm, in0=sel, scalar=-4.0 * BIG, in1=cm, op0=ALU.mult, op1=ALU.add),
                         r=[b_sel, b_cm], w=[b_cm])
                    P.op("dve", lambda e: e.tensor_reduce(out=col[:, 2, :], in_=cm, axis=AX.X, op=ALU.max), r=[b_cm], w=[b_col])
                    tt(em, cm, col[:, 2, :].unsqueeze(2).to_broadcast([128, nt_, 16]), ALU.is_ge, [b_cm, b_col], [b_em])
                    tt(sel, sel, em, ALU.add, [b_sel, b_em], [b_sel])
                    tt(sel, sel, sc, ALU.mult, [b_sel, b_sc], [b_sel])
                    P.op("dve", lambda e: e.tensor_reduce(out=col[:, 3, :], in_=sel, axis=AX.X, op=ALU.add), r=[b_sel], w=[b_col])
                    P.op("dve", lambda e: e.reciprocal(out=col[:, 4, :], in_=col[:, 3, :]), r=[b_col], w=[b_col])
                    tt(sel, sel, col[:, 4, :].unsqueeze(2).to_broadcast([128, nt_, 16]), ALU.mult, [b_sel, b_col], [b_sel])
                    for j0 in range(0, nt_, 4):
                        nj = min(4, nt_ - j0)
                        for jj in range(nj):
                            P.op("pe", lambda e: e.transpose(out=pgT[0:16, jj * 128:(jj + 1) * 128], in_=sel[:, j0 + jj, :], identity=ident),
                                 r=[b_sel, b_ident], w=[b_pgT])
                        P.op("act", lambda e: e.copy(out=gT[:, j0 * 128:(j0 + nj) * 128], in_=pgT[0:16, 0:nj * 128]), r=[b_pgT], w=[b_gT])
                with kb.phase():
                  acc, b_acc = kb.sb([128, tiles_per_blk, 1024], F32, "acc")
                  with kb.phase():
                    stage = [kb.sb([128, 1024], F32, "mst") for _ in range(3)]
                    Wg = [kb.sb([128, 8, 512], BF16, "Wg") for _ in range(2)]
                    Wu = [kb.sb([128, 8, 512], BF16, "Wu") for _ in range(2)]
                    Wd = [kb.sb([128, 4, 1024], BF16, "Wd") for _ in range(2)]
                    pg = [kb.ps([128, 512], F32, "pg") for _ in range(2)]
                    pu = [kb.ps([128, 512], F32, "pu") for _ in range(2)]
                    pgb = kb.ps([128, 512], F32, "pgb")
                    pd = [kb.ps([128, 512], F32, "pd") for _ in range(2)]
                    actT = [kb.sb([128, 4, 512], BF16, "actT") for _ in range(2)]
                    sgs = [kb.sb([128, 512], F32, "sg") for _ in range(2)]
                    tms = [kb.sb([128, 512], F32, "tmm") for _ in range(2)]
                    pieces = [(t0, min(TB, t0 + 512)) for t0 in range(0, TB, 512)]
                    def gateup(ex, pi, aTb):
                        (wg, b_wg), (wu, b_wu) = Wg[ex % 2], Wu[ex % 2]
                        t0, t1 = pieces[pi]
                        n = t1 - t0
                        aT, b_aT = aTb
                        P.op("pe", lambda e: e.matmul(pgb[0][:, 0:n], lhsT=Esel[:, ex, :], rhs=gT[:, t0:t1], start=True, stop=True),
                             r=[b_Esel, b_gT], w=[pgb[1]])
                        for f in range(4):
                            (g_, b_g), (u_, b_u) = pg[f % 2], pu[f % 2]
                            sg, b_sg = sgs[f % 2]
                            tm, b_tm = tms[f % 2]
                            fs = slice(f * 128, (f + 1) * 128)
                            for k in range(8):
                                P.op("pe", lambda e: e.matmul(g_[:, 0:n], lhsT=wg[:, k, fs], rhs=hT[:, k, t0:t1], start=(k == 0), stop=(k == 7)),
                                     r=[b_wg, b_hT], w=[b_g])
                            for k in range(8):
                                P.op("pe", lambda e: e.matmul(u_[:, 0:n], lhsT=wu[:, k, fs], rhs=hT[:, k, t0:t1], start=(k == 0), stop=(k == 7)),
                                     r=[b_wu, b_hT], w=[b_u])
                            P.op("act", lambda e: e.activation(out=sg[:, 0:n], in_=g_[:, 0:n], func=AF.Silu), r=[b_g], w=[b_sg])
                            P.op("dve", lambda e: e.tensor_tensor(out=tm[:, 0:n], in0=u_[:, 0:n], in1=sg[:, 0:n], op=ALU.mult), r=[b_u, b_sg], w=[b_tm])
                            P.op("dve", lambda e: e.tensor_tensor(out=aT[:, f, 0:n], in0=pgb[0][:, 0:n], in1=tm[:, 0:n], op=ALU.mult),
                                 r=[pgb[1], b_tm], w=[b_aT])

                    def down(ex, pi, aTb):
                        (wd, b_wd) = Wd[ex % 2]
                        t0, t1 = pieces[pi]
                        n = t1 - t0
                        aT, b_aT = aTb
                        for tt_ in range(n // 128):
                            j = t0 // 128 + tt_
                            for half in range(2):
                                d_, b_d = pd[half]
                                hs = slice(half * 512, (half + 1) * 512)
                                for f in range(4):
                                    P.op("pe", lambda e: e.matmul(d_, lhsT=aT[:, f, tt_ * 128:(tt_ + 1) * 128], rhs=wd[:, f, hs],
                                                                  start=(f == 0), stop=(f == 3)), r=[b_aT, b_wd], w=[b_d])
                                if ex == 0:
                                    P.op("act", lambda e: e.copy(out=acc[:, j, hs], in_=d_), r=[b_d], w=[b_acc])
                                else:
                                    P.op("dve", lambda e: e.tensor_tensor(out=acc[:, j, hs], in0=d_, in1=acc[:, j, hs], op=ALU.add),
                                         r=[b_d, b_acc], w=[b_acc])

                    items = [(ex, pi) for ex in range(elim) for pi in range(len(pieces))]
                    prev = None
                    for idx, (ex, pi) in enumerate(items):
                        if pi == 0:
                            load_wchunks_bf16(kb, Wg[ex % 2][0], Wg[ex % 2][1], IN["moe_w_gate"][li, ex], 8, 512, stage)
                            load_wchunks_bf16(kb, Wu[ex % 2][0], Wu[ex % 2][1], IN["moe_w_up"][li, ex], 8, 512, stage)
                            load_wchunks_bf16(kb, Wd[ex % 2][0], Wd[ex % 2][1], IN["moe_w_down"][li, ex], 4, 1024, stage)
                        gateup(ex, pi, actT[idx % 2])
                        if prev is not None:
                            down(prev[0], prev[1], actT[(idx - 1) % 2])
                        prev = (ex, pi)
                    down(prev[0], prev[1], actT[(len(items) - 1) % 2])
                  with kb.phase():
                    gB = load_gate_rows(kb, li, 5 * 1024)
                    xts = [kb.sb([128, 1024], F32, "xtm") for _ in range(2)]
                    for j in range(tiles_per_blk):
                        tok0 = base + j * 128
                        cond = 2 if tok0 < CTX else b
                        xt, b_xt = xts[j % 2]
                        P.dma("sp", xt, xs[b, tok0:tok0 + 128, :], r=[b_xs], w=[b_xt])
                        P.op("dve", lambda e: e.tensor_tensor(out=acc[:, j, :], in0=acc[:, j, :], in1=gB[cond][0], op=ALU.mult),
                             r=[b_acc, gB[cond][1]], w=[b_acc])
                        P.op("dve" if j % 2 == 0 else "pool", lambda e: e.tensor_tensor(out=xt, in0=xt, in1=acc[:, j, :], op=ALU.add),
                             r=[b_xt, b_acc], w=[b_xt])
                        P.dma("sp", xs[b, tok0:tok0 + 128, :], xt, r=[b_xt], w=[b_xs])


MLA_SCALE = 192.0 ** -0.5
I32 = mybir.dt.int32


def range_reduce_sin(kb, eng, out, b_out, ang, b_ang, shift, tmp, b_tmp):
    P = kb.P
    P.op(eng, lambda e: e.tensor_scalar(out=tmp, in0=ang, scalar1=shift, scalar2=1.0 / TWO_PI, op0=ALU.add, op1=ALU.mult), r=[b_ang], w=[b_tmp])
    P.op(eng, lambda e: e.tensor_scalar(out=tmp, in0=tmp, scalar1=MAGIC, scalar2=None, op0=ALU.add), r=[b_tmp], w=[b_tmp])
    P.op(eng, lambda e: e.tensor_scalar(out=tmp, in0=tmp, scalar1=-MAGIC, scalar2=-TWO_PI, op0=ALU.add, op1=ALU.mult), r=[b_tmp], w=[b_tmp])
    P.op(eng, lambda e: e.tensor_tensor(out=tmp, in0=tmp, in1=ang, op=ALU.add), r=[b_tmp, b_ang], w=[b_tmp])
    P.op("act", lambda e: e.activation(out=out, in_=tmp, func=AF.Sin, bias=kb.shiftc[shift][0][0:out.shape[0], 0:1]), r=[b_tmp, kb.shiftc[shift][1]], w=[b_out])


def phase_l1_mla(kb, IN):
    P = kb.P
    xs, b_xs = kb.dr["xs"]
    attnT, b_attnT = kb.dr["attnT"]
    ident, b_ident, identb, b_identb, ones, b_ones = kb.ident, kb.b_ident, kb.identb, kb.b_identb, kb.ones, kb.b_ones
    with kb.phase():
        modF, b_modF, (A1, b_A1), _ = load_mod_fm(kb, 1, IN, ("norm1_g", "norm2_g"))
        pmisc, b_pmisc = kb.ps([128, 512], F32, "pmisc")
        Win, b_Win = kb.sb([128, 8, 768], BF16, "Win")
        Wq, b_Wq = kb.sb([128, 3, 2048], BF16, "Wq")
        Wkv, b_Wkv = kb.sb([128, 2, 2048], BF16, "Wkv")
        cosT, b_cosT = kb.sb([64, SEQ], F32, "cosT")
        sinT, b_sinT = kb.sb([64, SEQ], F32, "sinT")
        rc, b_rc = kb.sb([64, 4], F32, "rc")
        fr_i, b_fr_i = kb.sb([1, 128], I32, "fri")
        fr_f, b_fr_f = kb.sb([1, 128], F32, "frf")
        gq, b_gq = kb.sb([128, 3], F32, "gq")
        gkv, b_gkv = kb.sb([128, 2], F32, "gkv")
        epsc, b_epsc = kb.sb([128, 1], F32, "epsc")
        _scope = kb.phase()
        _scope.__enter__()
        stage = [kb.sb([128, 2048], F32, "mst") for _ in range(2)]
        for k in range(8):
            st, b_st = stage[k % 2]
            P.dma("sp", st[:, 0:704], IN["mla_w_in"][0, k * 128:(k + 1) * 128, :], w=[b_st])
            P.op("pool", lambda e: e.tensor_copy(out=Win[:, k, 0:704], in_=st[:, 0:704]), r=[b_st], w=[b_Win])
            P.op("pool", lambda e: e.tensor_scalar(out=Win[:, k, 704:736], in0=st[:, 672:704], scalar1=-1.0, scalar2=None, op0=ALU.mult), r=[b_st], w=[b_Win])
            P.op("pool", lambda e: e.tensor_copy(out=Win[:, k, 736:768], in_=st[:, 640:672]), r=[b_st], w=[b_Win])
        for k in range(3):
            st, b_st = stage[k % 2]
            P.dma("sp", st[:, 0:1536], IN["mla_w_q_up"][0, k * 128:(k + 1) * 128, :], w=[b_st])
            P.op("pool", lambda e: e.tensor_copy(out=Wq[:, k, 0:1536], in_=st[:, 0:1536]), r=[b_st], w=[b_Wq])
            for h in range(8):
                P.op("pool", lambda e: e.tensor_scalar(out=Wq[:, k, 1536 + h * 64:1536 + h * 64 + 32], in0=st[:, h * 192 + 160:h * 192 + 192],
                                                       scalar1=-1.0, scalar2=None, op0=ALU.mult), r=[b_st], w=[b_Wq])
                P.op("pool", lambda e: e.tensor_copy(out=Wq[:, k, 1536 + h * 64 + 32:1536 + h * 64 + 64], in_=st[:, h * 192 + 128:h * 192 + 160]),
                     r=[b_st], w=[b_Wq])
        load_wchunks_bf16(kb, Wkv, b_Wkv, IN["mla_w_kv_up"][0], 2, 2048, stage)
        kb.load_vec_fm(IN["mla_q_norm_g"][0], 3, gq, b_gq, (pmisc, b_pmisc))
        kb.load_vec_fm(IN["mla_kv_norm_g"][0], 2, gkv, b_gkv, (pmisc, b_pmisc))
        P.op("pool", lambda e: e.memset(epsc, EPS), w=[b_epsc])
        P.op("pool", lambda e: e.iota(fr_i[:, 0:64], pattern=[[0, 2], [0, 2], [1, 16]], base=0, channel_multiplier=0), w=[b_fr_i])
        P.op("pool", lambda e: e.iota(fr_i[:, 64:128], pattern=[[0, 2], [1, 2], [0, 16]], base=0, channel_multiplier=0), r=[b_fr_i], w=[b_fr_i])
        P.op("dve", lambda e: e.tensor_copy(out=fr_f, in_=fr_i), r=[b_fr_i], w=[b_fr_f])
        P.op("pe", lambda e: e.transpose(out=pmisc[0:64, 0:1], in_=fr_f[0:1, 0:64], identity=ident[0:1, 0:1]), r=[b_fr_f, b_ident], w=[b_pmisc])
        P.op("pe", lambda e: e.transpose(out=pmisc[0:64, 1:2], in_=fr_f[0:1, 64:128], identity=ident[0:1, 0:1]), r=[b_fr_f, b_ident], w=[b_pmisc])
        P.op("act", lambda e: e.activation(out=rc[:, 0:1], in_=pmisc[0:64, 0:1], func=AF.Exp, scale=-math.log(10000.0) / 16.0), r=[b_pmisc], w=[b_rc])
        P.op("dve", lambda e: e.tensor_scalar(out=rc[:, 1:2], in0=pmisc[0:64, 1:2], scalar1=-1.0, scalar2=1.0, op0=ALU.mult, op1=ALU.add), r=[b_pmisc], w=[b_rc])
        pos_i, b_pos_i = kb.sb([64, SEQ], I32, "posi")
        rowf, b_rowf = kb.sb([64, SEQ], F32, "rowf")
        colf, b_colf = kb.sb([64, SEQ], F32, "colf")
        P.op("pool", lambda e: e.iota(pos_i, pattern=[[1, 32], [0, 64]], base=0, channel_multiplier=0), w=[b_pos_i])
        P.op("dve", lambda e: e.tensor_copy(out=rowf, in_=pos_i), r=[b_pos_i], w=[b_rowf])
        P.op("pool", lambda e: e.iota(pos_i, pattern=[[0, 32], [1, 64]], base=0, channel_multiplier=0), r=[b_pos_i], w=[b_pos_i])
        P.op("dve", lambda e: e.tensor_copy(out=colf, in_=pos_i), r=[b_pos_i], w=[b_colf])
        P.op("dve", lambda e: e.tensor_tensor(out=rowf, in0=rowf, in1=colf, op=ALU.subtract), r=[b_rowf, b_colf], w=[b_rowf])
        P.op("dve", lambda e: e.scalar_tensor_tensor(out=rowf, in0=rowf, scalar=rc[:, 1:2], in1=colf, op0=ALU.mult, op1=ALU.add), r=[b_rowf, b_rc, b_colf], w=[b_rowf])
        P.op("dve", lambda e: e.tensor_scalar(out=rowf, in0=rowf, scalar1=rc[:, 0:1], scalar2=None, op0=ALU.mult), r=[b_rowf, b_rc], w=[b_rowf])
        range_reduce_sin(kb, "dve", cosT, b_cosT, rowf, b_rowf, math.pi / 2, colf, b_colf)
        range_reduce_sin(kb, "dve", sinT, b_sinT, rowf, b_rowf, 0.0, colf, b_colf)
        _scope.__exit__(None, None, None)
        cqn, b_cqn = kb.sb([128, 3, SEQ], BF16, "cqn")
        ckvn, b_ckvn = kb.sb([128, 2, T], BF16, "ckvn")
        krT, b_krT = kb.sb([64, T], BF16, "krT")
        nmb = make_nm_bufs(kb, 2, 1)
        hT, b_hT = kb.sb([128, 8, 512], BF16, "hT1")
        c32, b_c32 = kb.sb([128, 3, 512], F32, "c32")
        csq, b_csq = kb.sb([128, 3, 512], F32, "csq")
        rin, b_rin = kb.sb([128, 512], F32, "rin")
        kr1, b_kr1 = kb.sb([64, 512], F32, "kr1")
        kr2, b_kr2 = kb.sb([64, 512], F32, "kr2")
        pA, b_pA = kb.ps([128, 512], F32, "pA")
        pB, b_pB = kb.ps([128, 512], F32, "pB")
        knT, b_knT = kb.sb([128, T], BF16, "knT")
        Vh, b_Vh = kb.sb([128, NT, 128], BF16, "Vh")
        qnT, b_qnT = kb.sb([128, SEQ], BF16, "qnT")
        qrT, b_qrT = kb.sb([64, SEQ], BF16, "qrT")
        q32a, b_q32a = kb.sb([64, 512], F32, "q32a")
        q32b, b_q32b = kb.sb([64, 512], F32, "q32b")
        Pb = [kb.sb([128, T], BF16, "Pb") for _ in range(2)]
        PTs = [kb.sb([128, NT, 128], BF16, "PTs") for _ in range(2)]
        smx = [kb.sb([128, 16], F32, "smx") for _ in range(4)]
        o32 = [kb.sb([128, 128], F32, "o32") for _ in range(2)]
        aTs, b_aTs = kb.sb([128, SEQ], F32, "aTs")
        s1b = [(pA, b_pA), (pB, b_pB)] + [kb.ps([128, 512], F32, "s2b") for _ in range(1)]
        Ssb = [kb.sb([128, T], F32, "Ssb") for _ in range(2)]
        ptbs = []
        for _i in range(3):
            kb.uid += 1
            ptb_t = kb.stack.enter_context(kb.nc.psum_tensor("ptb_%d" % kb.uid, [128, 1024], BF16))
            ptbs.append((ptb_t.ap().rearrange("p (a b) -> p a b", b=128), Buf("ptb%d" % _i, excl=True)))
        po, b_po = pmisc, b_pmisc
        kpieces = [(t0, min(T, t0 + 512)) for t0 in range(0, T, 512)]
        import os
        hlim = int(os.environ.get("MLA_HLIM", "8"))
        qlim = int(os.environ.get("MLA_QLIM", "16"))

        def rms_scale(src_ps_list, nchunk, width, gcol, dst, b_dst, dcol0, dfeat):
            for c, (ps, b_ps) in enumerate(src_ps_list):
                P.op("act", lambda e: e.copy(out=c32[:, c, 0:width], in_=ps), r=[b_ps], w=[b_c32])
                P.op("act", lambda e: e.activation(out=csq[:, c, 0:width], in_=c32[:, c, 0:width], func=AF.Square), r=[b_c32], w=[b_csq])
            return

        for b in range(NB):
            units = [(0, CTX)] + [(CTX + 512 * q_, CTX + 512 * (q_ + 1)) for q_ in range(4)]
            pC_, b_pC = s1b[2]
            for (u0, u1) in units:
                W_ = u1 - u0
                for half in range(W_ // 256):
                    toks_ = [u0 + half * 256, u0 + half * 256 + 128]
                    norm_mod_batch(kb, xs[b], b_xs, toks_, [2 if t_ < CTX else b for t_ in toks_], A1, b_A1, modF, b_modF, 0,
                                   nmb, [(hT, b_hT, lambda i, half=half: half * 256 + i * 128)])
                tsl = slice(u0, u1)
                lat = u0 >= CTX
                lsl = slice(u0 - CTX, u1 - CTX)
                groups = ([("q", 0, 3, 384.0, gq, b_gq)] if lat else []) + [("kv", 3, 2, 256.0, gkv, b_gkv)]
                for (nm, c0, nch, dfeat, gcol, b_gcol) in groups:
                    for c in range(nch):
                        ps, b_ps = (pA, b_pA) if c % 2 == 0 else (pB, b_pB)
                        for k in range(8):
                            P.op("pe", lambda e: e.matmul(ps[:, 0:W_], lhsT=Win[:, k, (c0 + c) * 128:(c0 + c + 1) * 128], rhs=hT[:, k, 0:W_],
                                                          start=(k == 0), stop=(k == 7)), r=[b_Win, b_hT], w=[b_ps])
                        P.op("act", lambda e: e.copy(out=c32[:, c, 0:W_], in_=ps[:, 0:W_]), r=[b_ps], w=[b_c32])
                        P.op("act", lambda e: e.activation(out=csq[:, c, 0:W_], in_=c32[:, c, 0:W_], func=AF.Square), r=[b_c32], w=[b_csq])
                    for c in range(nch):
                        P.op("pe", lambda e: e.matmul(pC_[:, 0:W_], lhsT=ones, rhs=csq[:, c, 0:W_], start=(c == 0), stop=(c == nch - 1)),
                             r=[b_ones, b_csq], w=[b_pC])
                    P.op("dve", lambda e: e.tensor_scalar(out=rin[:, 0:W_], in0=pC_[:, 0:W_], scalar1=1.0 / dfeat, scalar2=EPS, op0=ALU.mult, op1=ALU.add),
                         r=[b_pC], w=[b_rin])
                    P.op("act", lambda e: e.activation(out=rin[:, 0:W_], in_=rin[:, 0:W_], func=AF.Sqrt), r=[b_rin], w=[b_rin])
                    P.op("dve", lambda e: e.reciprocal(out=rin[:, 0:W_], in_=rin[:, 0:W_]), r=[b_rin], w=[b_rin])
                    for c in range(nch):
                        if nm == "q":
                            dst, b_dst, dsl = cqn, b_cqn, lsl
                        else:
                            dst, b_dst, dsl = ckvn, b_ckvn, tsl
                        P.op("dve", lambda e: e.scalar_tensor_tensor(out=dst[:, c, dsl], in0=c32[:, c, 0:W_], scalar=gcol[:, c:c + 1], in1=rin[:, 0:W_],
                                                                     op0=ALU.mult, op1=ALU.mult), r=[b_c32, b_gcol, b_rin], w=[b_dst])
                for k in range(8):
                    P.op("pe", lambda e: e.matmul(pB[0:64, 0:W_], lhsT=Win[:, k, 640:704], rhs=hT[:, k, 0:W_], start=(k == 0), stop=(k == 7)),
                         r=[b_Win, b_hT], w=[b_pB])
                if not lat:
                    P.op("act", lambda e: e.copy(out=krT[:, tsl], in_=pB[0:64, 0:W_]), r=[b_pB], w=[b_krT])
                else:
                    for k in range(8):
                        P.op("pe", lambda e: e.matmul(pA[0:64, 0:W_], lhsT=Win[:, k, 704:768], rhs=hT[:, k, 0:W_], start=(k == 0), stop=(k == 7)),
                             r=[b_Win, b_hT], w=[b_pA])
                    P.op("dve", lambda e: e.tensor_tensor(out=kr1[:, 0:W_], in0=pB[0:64, 0:W_], in1=cosT[:, lsl], op=ALU.mult), r=[b_pB, b_cosT], w=[b_kr1])
                    P.op("dve", lambda e: e.tensor_tensor(out=kr2[:, 0:W_], in0=pA[0:64, 0:W_], in1=sinT[:, lsl], op=ALU.mult), r=[b_pA, b_sinT], w=[b_kr2])
                    P.op("pool", lambda e: e.tensor_tensor(out=krT[:, tsl], in0=kr1[:, 0:W_], in1=kr2[:, 0:W_], op=ALU.add), r=[b_kr1, b_kr2], w=[b_krT])
            for h in range(hlim):
                for pi, (t0, t1) in enumerate(kpieces):
                    n = t1 - t0
                    for k in range(2):
                        P.op("pe", lambda e: e.matmul(pA[:, 0:n], lhsT=Wkv[:, k, h * 256:h * 256 + 128], rhs=ckvn[:, k, t0:t1], start=(k == 0), stop=(k == 1)),
                             r=[b_Wkv, b_ckvn], w=[b_pA])
                    P.op("act", lambda e: e.copy(out=knT[:, t0:t1], in_=pA[:, 0:n]), r=[b_pA], w=[b_knT])
                for j0 in range(0, NT, 4):
                    nj = min(4, NT - j0)
                    for jj in range(nj):
                        j = j0 + jj
                        for k in range(2):
                            P.op("pe", lambda e: e.matmul(pB[:, jj * 128:(jj + 1) * 128], lhsT=ckvn[:, k, j * 128:(j + 1) * 128],
                                                          rhs=Wkv[:, k, h * 256 + 128:h * 256 + 256], start=(k == 0), stop=(k == 1)),
                                 r=[b_Wkv, b_ckvn], w=[b_pB])
                    P.op("dve", lambda e: e.tensor_copy(out=Vh[:, j0:j0 + nj, :], in_=pB[:, 0:nj * 128].rearrange("p (a b) -> p a b", b=128)),
                         r=[b_pB], w=[b_Vh])
                for qp in range(4):
                    qsl = slice(qp * 512, (qp + 1) * 512)
                    for k in range(3):
                        P.op("pe", lambda e: e.matmul(pA, lhsT=Wq[:, k, h * 192:h * 192 + 128], rhs=cqn[:, k, qsl], start=(k == 0), stop=(k == 2)),
                             r=[b_Wq, b_cqn], w=[b_pA])
                    P.op("act", lambda e: e.copy(out=qnT[:, qsl], in_=pA), r=[b_pA], w=[b_qnT])
                    for k in range(3):
                        P.op("pe", lambda e: e.matmul(pB[0:64, :], lhsT=Wq[:, k, h * 192 + 128:h * 192 + 192], rhs=cqn[:, k, qsl], start=(k == 0), stop=(k == 2)),
                             r=[b_Wq, b_cqn], w=[b_pB])
                    P.op("dve", lambda e: e.tensor_tensor(out=q32a, in0=pB[0:64, :], in1=cosT[:, qsl], op=ALU.mult), r=[b_pB, b_cosT], w=[b_q32a])
                    for k in range(3):
                        P.op("pe", lambda e: e.matmul(pB[0:64, :], lhsT=Wq[:, k, 1536 + h * 64:1536 + h * 64 + 64], rhs=cqn[:, k, qsl], start=(k == 0), stop=(k == 2)),
                             r=[b_Wq, b_cqn], w=[b_pB])
                    P.op("dve", lambda e: e.tensor_tensor(out=q32b, in0=pB[0:64, :], in1=sinT[:, qsl], op=ALU.mult), r=[b_pB, b_sinT], w=[b_q32b])
                    P.op("pool", lambda e: e.tensor_tensor(out=qrT[:, qsl], in0=q32a, in1=q32b, op=ALU.add), r=[b_q32a, b_q32b], w=[b_qrT])
                def S1(qt):
                    qs = slice(qt * 128, (qt + 1) * 128)
                    (sx, b_sx) = smx[qt % 4]
                    (ssb, b_ssb) = Ssb[qt % 2]
                    for pi, (t0, t1) in enumerate(kpieces):
                        n = t1 - t0
                        sc_, b_sc = s1b[(qt * 5 + pi) % 3]
                        P.op("pe", lambda e: e.matmul(sc_[:, 0:n], lhsT=qnT[:, qs], rhs=knT[:, t0:t1], start=True, stop=False),
                             r=[b_qnT, b_knT], w=[b_sc])
                        P.op("pe", lambda e: e.matmul(sc_[:, 0:n], lhsT=qrT[:, qs], rhs=krT[:, t0:t1], start=False, stop=True),
                             r=[b_qrT, b_krT], w=[b_sc])
                        if pi % 2 == 0:
                            P.op("act", lambda e: e.copy(out=ssb[:, t0:t1], in_=sc_[:, 0:n]), r=[b_sc], w=[b_ssb])
                        else:
                            P.op("dve", lambda e: e.tensor_copy(out=ssb[:, t0:t1], in_=sc_[:, 0:n]), r=[b_sc], w=[b_ssb])
                    P.op("dve", lambda e: e.tensor_reduce(out=sx[:, 5:6], in_=ssb, axis=AX.X, op=ALU.max), r=[b_ssb], w=[b_sx])
                    P.op("dve", lambda e: e.tensor_scalar(out=sx[:, 6:7], in0=sx[:, 5:6], scalar1=-MLA_SCALE, scalar2=None, op0=ALU.mult), r=[b_sx], w=[b_sx])

                def S2(qt):
                    (sx, b_sx) = smx[qt % 4]
                    (pb_, b_pb) = Pb[qt % 2]
                    (ssb, b_ssb) = Ssb[qt % 2]
                    P.op("act", lambda e: e.activation(out=pb_, in_=ssb, func=AF.Exp, scale=MLA_SCALE, bias=sx[:, 6:7],
                                                       accum_out=sx[:, 7:8]), r=[b_ssb, b_sx], w=[b_pb, b_sx])
                    P.op("dve", lambda e: e.reciprocal(out=sx[:, 13:14], in_=sx[:, 7:8]), r=[b_sx], w=[b_sx])

                def S3(qt):
                    (pb_, b_pb), (pts, b_pts) = Pb[qt % 2], PTs[qt % 2]
                    for gi, j0 in enumerate(range(0, NT, 8)):
                        nj = min(8, NT - j0)
                        ptb, b_ptb = ptbs[gi % 3]
                        for jj in range(nj):
                            j = j0 + jj
                            P.op("pe", lambda e: e.transpose(out=ptb[:, jj, :], in_=pb_[:, j * 128:(j + 1) * 128], identity=identb),
                                 r=[b_pb, b_identb], w=[b_ptb])
                        if gi % 2 == 0:
                            P.op("act", lambda e: e.copy(out=pts[:, j0:j0 + nj, :], in_=ptb[:, 0:nj, :]), r=[b_ptb], w=[b_pts])
                        else:
                            P.op("dve", lambda e: e.tensor_copy(out=pts[:, j0:j0 + nj, :], in_=ptb[:, 0:nj, :]), r=[b_ptb], w=[b_pts])

                def S4(qt):
                    qs = slice(qt * 128, (qt + 1) * 128)
                    (pts, b_pts), (sx, b_sx), (o3, b_o3) = PTs[qt % 2], smx[qt % 4], o32[qt % 2]
                    for j in range(NT):
                        P.op("pe", lambda e: e.matmul(po[:, 0:128], lhsT=pts[:, j, :], rhs=Vh[:, j, :], start=(j == 0), stop=(j == NT - 1)),
                             r=[b_pts, b_Vh], w=[b_po])
                    P.op("act", lambda e: e.activation(out=o3, in_=po[:, 0:128], func=AF.Copy, scale=sx[:, 13:14]), r=[b_po, b_sx], w=[b_o3])
                    P.op("pe", lambda e: e.transpose(out=po[:, 128:256], in_=o3, identity=ident), r=[b_o3, b_ident], w=[b_po])
                    P.op("dve", lambda e: e.tensor_copy(out=aTs[:, qs], in_=po[:, 128:256]), r=[b_po], w=[b_aTs])

                for t in range(qlim + 3):
                    if t < qlim:
                        S1(t)
                    if 0 <= t - 2 < qlim:
                        S3(t - 2)
                    if 0 <= t - 3 < qlim:
                        S4(t - 3)
                    if 0 <= t - 1 < qlim:
                        S2(t - 1)
                P.dma("sp", attnT[b, h * 128:(h + 1) * 128, :], aTs, r=[b_aTs], w=[b_attnT])


def phase_proj_res(kb, IN, li, src_name, wsrc, gate_off, tok_lo):
    P = kb.P
    xs, b_xs = kb.dr["xs"]
    srcT, b_srcT = kb.dr[src_name]
    ntok = T - tok_lo
    with kb.phase():
        gB = load_gate_rows(kb, li, gate_off)
        stage = [kb.sb([128, 1024], F32, "wst") for _ in range(2)]
        W, b_W = kb.sb([128, 8, 1024], BF16, "wout")
        load_wchunks_bf16(kb, W, b_W, wsrc, 8, 1024, stage, ("pool", "act", "dve"))
        a32s = [kb.sb([128, 8, 256], F32, "a32") for _ in range(2)]
        mixbs = [kb.sb([128, 8, 256], BF16, "mixb") for _ in range(2)]
        pdd = [kb.ps([128, 512], F32, "pdd") for _ in range(2)]
        xts = [kb.sb([128, 1024], F32, "xt") for _ in range(2)]
        tms = [kb.sb([128, 512], F32, "tm") for _ in range(2)]
        ui = 0
        for b in range(NB):
            for un in range(ntok // 256):
                tsl = slice(un * 256, (un + 1) * 256)
                (a32, b_a32), (mixb, b_mixb) = a32s[ui % 2], mixbs[ui % 2]
                ui += 1
                P.dma("sp", a32, srcT[b, :, tsl].rearrange("(c p) t -> p c t", p=128), r=[b_srcT], w=[b_a32])
                P.op("act", lambda e: e.copy(out=mixb, in_=a32), r=[b_a32], w=[b_mixb])
                for tt in range(2):
                    tok0 = tok_lo + un * 256 + tt * 128
                    cond = 2 if tok0 < CTX else b
                    xt, b_xt = xts[tt]
                    P.dma("sp", xt, xs[b, tok0:tok0 + 128, :], r=[b_xs], w=[b_xt])
                    for half in range(2):
                        pd, b_pd = pdd[half]
                        tm, b_tm = tms[half]
                        hs = slice(half * 512, (half + 1) * 512)
                        for k in range(8):
                            P.op("pe", lambda e: e.matmul(pd, lhsT=mixb[:, k, tt * 128:(tt + 1) * 128], rhs=W[:, k, hs],
                                                          start=(k == 0), stop=(k == 7)), r=[b_mixb, b_W], w=[b_pd])
                        P.op("dve", lambda e: e.tensor_tensor(out=tm, in0=pd, in1=gB[cond][0][:, hs], op=ALU.mult),
                             r=[b_pd, gB[cond][1]], w=[b_tm])
                        P.op("pool", lambda e: e.tensor_tensor(out=xt[:, hs], in0=xt[:, hs], in1=tm, op=ALU.add),
                             r=[b_xt, b_tm], w=[b_xt])
                    P.dma("sp", xs[b, tok0:tok0 + 128, :], xt, r=[b_xt], w=[b_xs])


def phase_final(kb, IN, out_ap, b_out):
    P = kb.P
    xs, b_xs = kb.dr["xs"]
    with kb.phase():
        gF, b_gF = kb.sb([128, 1024], F32, "gF")
        P.dma("sp", gF, IN["final_norm_g"].partition_broadcast(128), w=[b_gF])
        xts = [kb.sb([128, 1024], F32, "xt") for _ in range(2)]
        sqs = [kb.sb([128, 1024], F32, "sq") for _ in range(2)]
        sss = [kb.sb([128, 4], F32, "ss") for _ in range(2)]
        it = 0
        for b in range(NB):
            for j in range(SEQ // 128):
                (xt, b_xt), (sq, b_sq), (ss, b_ss) = xts[it % 2], sqs[it % 2], sss[it % 2]
                it += 1
                P.dma("sp", xt, xs[b, CTX + j * 128:CTX + (j + 1) * 128, :], r=[b_xs], w=[b_xt])
                P.op("act", lambda e: e.activation(out=sq, in_=xt, func=AF.Square, accum_out=ss[:, 0:1]), r=[b_xt], w=[b_sq, b_ss])
                P.op("dve", lambda e: e.tensor_scalar(out=ss[:, 1:2], in0=ss[:, 0:1], scalar1=1.0 / D, scalar2=EPS, op0=ALU.mult, op1=ALU.add), r=[b_ss], w=[b_ss])
                P.op("act", lambda e: e.activation(out=ss[:, 2:3], in_=ss[:, 1:2], func=AF.Sqrt), r=[b_ss], w=[b_ss])
                P.op("dve", lambda e: e.reciprocal(out=ss[:, 3:4], in_=ss[:, 2:3]), r=[b_ss], w=[b_ss])
                P.op("dve", lambda e: e.scalar_tensor_tensor(out=sq, in0=xt, scalar=ss[:, 3:4], in1=gF, op0=ALU.mult, op1=ALU.mult),
                     r=[b_xt, b_ss, b_gF], w=[b_sq])
                P.dma("sp", out_ap[b, j * 128:(j + 1) * 128, :], sq, r=[b_sq], w=[b_out])


def build(phases, scr_kinds=None):
    kb = KB(scr_kinds)
    nc = kb.nc
    shapes = {}

    def inp(name, shape):
        shapes[name] = list(shape)

    class LazyIn(dict):
        def __missing__(self, name):
            ap = nc.dram_tensor(name, shapes[name], F32, kind="ExternalInput").ap()
            self[name] = ap
            return ap

    IN = LazyIn()
    kb.IN = IN
    inp("x", [NB, SEQ, D]); inp("c", [NB, D]); inp("ctx", [NB, CTX, D]); inp("c_ctx", [D])
    inp("ada_w", [2, D, 6 * D]); inp("ada_b", [2, 6 * D]); inp("norm1_g", [2, D]); inp("norm2_g", [2, D])
    inp("ab_w_in", [1, D, AB_W]); inp("dn_conv_w", [1, 5, 1536]); inp("dn_A_log", [1, 2, 4]); inp("dn_dt_bias", [1, 2, 4])
    inp("dn_norm_g", [1, 128])
    inp("s5_A_re", [1, 2, 32, 64]); inp("s5_A_im", [1, 2, 32, 64]); inp("s5_log_dt", [1, 2, 32])
    inp("s5_B_re", [1, 32, 64, 16]); inp("s5_B_im", [1, 32, 64, 16]); inp("s5_C_re", [1, 32, 16, 64]); inp("s5_C_im", [1, 32, 16, 64])
    inp("s5_D", [1, 32, 16]); inp("s5_glu_w", [1, 512, 512]); inp("s5_glu_b", [1, 512]); inp("ab_w_out", [1, 1024, D])
    inp("mla_w_in", [1, D, 704]); inp("mla_q_norm_g", [1, 384]); inp("mla_w_q_up", [1, 384, 1536]); inp("mla_kv_norm_g", [1, 256])
    inp("mla_w_kv_up", [1, 256, 2048]); inp("mla_w_out", [1, 1024, D])
    inp("router_w", [D, 16]); inp("router_bias", [16])
    inp("moe_w_gate", [2, 16, D, 512]); inp("moe_w_up", [2, 16, D, 512]); inp("moe_w_down", [2, 16, 512, D])
    inp("final_norm_g", [D])
    out_ap = nc.dram_tensor("out", [NB, SEQ, D], F32, kind="ExternalOutput").ap()
    b_out = Buf("out")

    kb.dram("modd0", [3, 6144]); kb.dram("modd1", [3, 6144])
    kb.dram("xs", [NB, T, D])
    kb.dram("qkvu", [NB, 2048, T])
    kb.dram("zab", [NB, T, 528])
    kb.dram("qkvn", [NB, 1536, T])
    kb.dram("mixT", [NB, 1024, T])
    kb.dram("ygT", [NB, 512, T])
    kb.dram("attnT", [NB, 1024, SEQ])

    with ExitStack() as top:
        kb.stack = top
        kb.consts()
        P = kb.P
        if "init" in phases:
            xs, b_xs = kb.dr["xs"]
            for b in range(NB):
                P.dma("sp", xs[b, 0:CTX, :], IN["ctx"][b], w=[b_xs])
                for q in range(4):
                    P.dma("sp", xs[b, CTX + q * 512:CTX + (q + 1) * 512, :], IN["x"][b, q * 512:(q + 1) * 512, :], w=[b_xs])
        if "ada0" in phases:
            phase_adaln(kb, 0, IN)
        if "l0in" in phases:
            phase_l0_inproj(kb, IN)
        if "l0conv" in phases:
            phase_l0_conv(kb, IN)
        if "l0delta" in phases:
            phase_l0_delta(kb, IN)
        if "l0s5" in phases:
            phase_l0_s5(kb, IN)
        if "l0out" in phases:
            phase_l0_out(kb, IN)
        if "moe0" in phases:
            phase_moe(kb, IN, 0, 0)
        if "ada1" in phases and not (ADA1_IN_CONV and "l0conv" in phases):
            phase_adaln(kb, 1, IN)
        if "l1mla" in phases:
            phase_l1_mla(kb, IN)
        if "l1out" in phases:
            phase_proj_res(kb, IN, 1, "attnT", IN["mla_w_out"][0], 2048, CTX)
        if "moe1" in phases:
            phase_moe(kb, IN, 1, CTX)
        if "final" in phases:
            phase_final(kb, IN, out_ap, b_out)
        if "dumpxs" in phases:
            xs, b_xs = kb.dr["xs"]
            xd = nc.dram_tensor("xs_dump", [NB, T, D], F32, kind="ExternalOutput").ap()
            b_xd = Buf("xd")
            for b in range(NB):
                P.dma("sp", xd[b], xs[b], r=[b_xs], w=[b_xd])
        P.barrier()
    return kb


def _in_maps(inputs, names=None):
    maps = []
    for core in range(NCORES):
        m = {}
        for k, v in inputs.items():
            if names is not None and k not in names:
                continue
            a = np.asarray(v)
            if k in ("x", "c", "ctx"):
                a = a[core * NB:(core + 1) * NB]
            m[k] = np.ascontiguousarray(a, dtype=np.float32)
        maps.append(m)
    return maps


def kernel(**inputs):
    kb = build(ALL_PHASES)
    res = run_bass_kernel_spmd(kb.nc, _in_maps(inputs, set(kb.IN.keys())), core_ids=list(range(NCORES)))
    return np.concatenate([np.asarray(r["out"]) for r in res.results], axis=0).astype(np.float32)


ALL_PHASES = ("init", "ada0", "l0in", "l0conv", "l0delta", "l0s5", "l0out", "moe0",
              "ada1", "l1mla", "l1out", "moe1", "final")
```

```python
import math
from contextlib import ExitStack
import numpy as np
import concourse.bass as bass
import concourse.mybir as mybir
from concourse.bass_utils import run_bass_kernel_spmd

F32 = mybir.dt.float32
BF16 = mybir.dt.bfloat16
ALU = mybir.AluOpType
AF = mybir.ActivationFunctionType
AX = mybir.AxisListType

NCORES = 8
NB = 2
D = 1024
SEQ = 2048
CTX = 256
T = SEQ + CTX
NT = T // 128
EPS = 1e-6
KD = D // 128
MAGIC = 12582912.0
TWO_PI = 2.0 * math.pi


import os as _os
STORE_Q = _os.environ.get("STORE_Q", "pool")
OP_TRACE = bool(_os.environ.get("OP_TRACE"))
OP_LIMIT = int(_os.environ.get("OP_LIMIT", "1000000000"))


class Buf:
    __slots__ = ("name", "w", "r", "excl")

    def __init__(self, name="", excl=False):
        self.name = name
        self.w = None
        self.r = {}
        self.excl = excl


class Prog:
    SEM_ROT = 30000
    N_DMA_SEMS = 32

    def __init__(self, nc):
        self.nc = nc
        self.eng = {"pe": nc.tensor, "dve": nc.vector, "act": nc.scalar,
                    "pool": nc.gpsimd, "sp": nc.sync}
        self.sems = {}
        self.cur = {}
        self.cnt = {}
        self.waited = {e: {} for e in self.eng}
        self.gen = {e: 0 for e in self.eng}
        for e in self.eng:
            self._new_sem(e)
        self.dma_keys = []
        for i in range(self.N_DMA_SEMS):
            k = "dma%d" % i
            self.sems[k] = nc.alloc_semaphore(name=k)
            self.cnt[k] = 0
            self.dma_keys.append(k)
        self.dma_rr = 0
        self.ninst = {e: 0 for e in self.eng}

    def _new_sem(self, e):
        k = "%s_%d" % (e, self.gen[e])
        self.gen[e] += 1
        self.sems[k] = self.nc.alloc_semaphore(name=k)
        self.cnt[k] = 0
        self.cur[e] = k

    def _wait(self, e, key, val):
        if val <= 0 or self.waited[e].get(key, 0) >= val:
            return
        self.eng[e].wait_ge(self.sems[key], val)
        self.waited[e][key] = val
        self.ninst[e] += 1

    def _deps(self, e, r, w):
        need = {}
        own0 = self.cur[e]
        for b in r:
            if b.w is not None:
                k, v = b.w
                need[k] = max(need.get(k, 0), v)
            if b.excl:
                for k, v in b.r.items():
                    if k != own0:
                        need[k] = max(need.get(k, 0), v)
        for b in w:
            if b.w is not None:
                k, v = b.w
                need[k] = max(need.get(k, 0), v)
            for k, v in b.r.items():
                need[k] = max(need.get(k, 0), v)
        own = self.cur[e]
        for k, v in need.items():
            if k == own and e == "pe":
                continue
            self._wait(e, k, v)

    def op(self, e, fn, r=(), w=()):
        self.total = getattr(self, "total", 0) + 1
        if self.total > OP_LIMIT:
            return None
        if OP_TRACE:
            import sys as _sys
            print("OP", self.total, e, _sys._getframe(1).f_lineno)
        self._deps(e, r, w)
        inst = fn(self.eng[e])
        k = self.cur[e]
        inst.then_inc(self.sems[k], 1)
        self.cnt[k] += 1
        v = self.cnt[k]
        for b in r:
            b.r[k] = v
        for b in w:
            b.w = (k, v)
            b.r = {}
        self.ninst[e] += 1
        if v >= self.SEM_ROT:
            self._new_sem(e)
        return inst

    def dma(self, e, out, in_, r=(), w=(), **kw):
        self.total = getattr(self, "total", 0) + 1
        if self.total > OP_LIMIT:
            return None
        if STORE_Q and "DRAM" in str(out.space):
            e = STORE_Q
        k = self.dma_keys[self.dma_rr]
        self.dma_rr = (self.dma_rr + 1) % len(self.dma_keys)
        self._wait(e, k, self.cnt[k])
        self._deps(e, r, w)
        inst = self.eng[e].dma_start(out=out, in_=in_, **kw)
        inst.then_inc(self.sems[k], 16)
        self.cnt[k] += 16
        v = self.cnt[k]
        for b in r:
            b.r[k] = v
        for b in w:
            b.w = (k, v)
            b.r = {}
        self.ninst[e] += 1
        return inst

    def barrier(self, engines=None):
        for e in (engines or self.eng):
            for k in list(self.cnt):
                self._wait(e, k, self.cnt[k])


class KB:
    def __init__(self, scr_kinds=None):
        self.nc = bass.Bass("TRN2", target_bir_lowering=False)
        self.P = Prog(self.nc)
        self.scr_kinds = scr_kinds or {}
        self.dr = {}
        self.stack = None
        self.uid = 0
        self.dq = 0

    def dram(self, name, shape, dtype=F32, kind="Internal"):
        kind = self.scr_kinds.get(name, kind)
        ap = self.nc.dram_tensor(name, list(shape), dtype, kind=kind).ap()
        self.dr[name] = (ap, Buf(name))
        return self.dr[name]

    def sb(self, shape, dtype=F32, name=None):
        self.uid += 1
        nm = "%s_%d" % (name or "sb", self.uid)
        t = self.stack.enter_context(self.nc.sbuf_tensor(nm, list(shape), dtype))
        return t.ap(), Buf(nm)

    def ps(self, shape, dtype=F32, name=None):
        self.uid += 1
        nm = "%s_%d" % (name or "ps", self.uid)
        t = self.stack.enter_context(self.nc.psum_tensor(nm, [128, 512], F32))
        ap = t.ap()
        shape = list(shape)
        n = 1
        for v in shape[1:]:
            n *= v
        assert dtype == F32 and n <= 512, shape
        v = ap[0:shape[0], 0:n]
        if len(shape) == 3:
            v = v.rearrange("p (a b) -> p a b", b=shape[2])
        return v, Buf(nm, excl=True)

    def phase(self):
        kb = self

        class _Ph:
            def __enter__(s):
                s.saved = kb.stack
                kb.stack = ExitStack()
                kb.stack.__enter__()
                return kb.stack

            def __exit__(s, *a):
                kb.P.barrier()
                kb.stack.__exit__(*a)
                kb.stack = s.saved
        return _Ph()

    def dmaq(self):
        self.dq ^= 1
        return "sp"

    def consts(self):
        P, nc = self.P, self.nc
        self.ident, self.b_ident = self.sb([128, 128], F32, "ident")
        P.op("pool", lambda e: e.memset(self.ident, 0.0), w=[self.b_ident])
        P.op("pool", lambda e: e.affine_select(out=self.ident, in_=self.ident, pattern=[[-1, 128]],
                                               compare_op=ALU.not_equal, fill=1.0, base=0, channel_multiplier=1),
             r=[self.b_ident], w=[self.b_ident])
        self.identb, self.b_identb = self.sb([128, 128], BF16, "identb")
        P.op("dve", lambda e: e.tensor_copy(out=self.identb, in_=self.ident), r=[self.b_ident], w=[self.b_identb])
        self.ones, self.b_ones = self.sb([128, 128], F32, "ones")
        P.op("pool", lambda e: e.memset(self.ones, 1.0), w=[self.b_ones])
        self.shiftc = {}
        for sh in (0.0, math.pi / 2, MAGIC, -MAGIC):
            t, bt = self.sb([128, 1], F32, "shc")
            P.op("pool", lambda e: e.memset(t, sh), w=[bt])
            self.shiftc[sh] = (t, bt)

    def load_vec_fm(self, src1d, n, dst, b_dst, tmp_ps):
        P = self.P
        st, b_st = self.sb([n, 128], F32, "lv")
        P.dma("sp", st, src1d.rearrange("(k p) -> k p", p=128), w=[b_st])
        ps, b_ps = tmp_ps
        P.op("pe", lambda e: e.transpose(out=ps[:, 0:n], in_=st, identity=self.ident[0:n, 0:n]),
             r=[b_st, self.b_ident], w=[b_ps])
        P.op("dve", lambda e: e.tensor_copy(out=dst, in_=ps[:, 0:n]), r=[b_ps], w=[b_dst])

    def load_w_bf16(self, dst, b_dst, src, rows, cols, stage, colblk=2048):
        P = self.P
        i = 0
        for c0 in range(0, cols, colblk):
            cw = min(colblk, cols - c0)
            st, b_st = stage[self.uid % len(stage)]
            self.uid += 1
            P.dma("sp", st[0:rows, 0:cw], src[:, c0:c0 + cw], w=[b_st])
            self.castrr = getattr(self, "castrr", 0) + 1
            eng = ("pool", "act", "dve")[self.castrr % 3]
            i += 1
            if eng == "act":
                P.op("act", lambda e: e.copy(out=dst[:, c0:c0 + cw], in_=st[0:rows, 0:cw]), r=[b_st], w=[b_dst])
            else:
                P.op(eng, lambda e: e.tensor_copy(out=dst[:, c0:c0 + cw], in_=st[0:rows, 0:cw]), r=[b_st], w=[b_dst])


def phase_adaln(kb, li, IN):
    P, nc = kb.P, kb.nc
    modd, b_modd = kb.dr["modd%d" % li]
    with kb.phase():
        condT, b_condT = kb.sb([128, 8, 3], F32, "condT")
        crow, b_crow = kb.sb([3, 1024], F32, "crow")
        P.dma("sp", crow[0:2, :], IN["c"], w=[b_crow])
        P.dma("sp", crow[2:3, :], IN["c_ctx"].rearrange("(o n) -> o n", o=1), w=[b_crow])
        pst, b_pst = kb.ps([128, 8, 3], F32, "pst")
        for k in range(8):
            P.op("pe", lambda e: e.transpose(out=pst[:, k, :], in_=crow[0:3, k * 128:(k + 1) * 128],
                                             identity=kb.ident[0:3, 0:3]), r=[b_crow, kb.b_ident], w=[b_pst])
        P.op("act", lambda e: e.activation(out=condT, in_=pst, func=AF.Silu), r=[b_pst], w=[b_condT])
        wb = [kb.sb([128, 8, 512], F32, "adaw") for _ in range(2)]
        bb = [kb.sb([3, 512], F32, "adab") for _ in range(2)]
        rs = [kb.sb([3, 512], F32, "adar") for _ in range(2)]
        pm = [kb.ps([3, 512], F32, "adaps") for _ in range(2)]
        for blk in range(12):
            w, b_w = wb[blk % 2]
            bt, b_bt = bb[blk % 2]
            r_, b_r = rs[blk % 2]
            ps, b_ps = pm[blk % 2]
            cs = slice(blk * 512, (blk + 1) * 512)
            P.dma("sp", w, IN["ada_w"][li, :, cs].rearrange("(k p) n -> p k n", p=128), w=[b_w])
            P.dma("sp", bt, IN["ada_b"][li, cs].partition_broadcast(3), w=[b_bt])
            for k in range(8):
                P.op("pe", lambda e: e.matmul(ps, lhsT=condT[:, k, :], rhs=w[:, k, :], start=(k == 0), stop=(k == 7)),
                     r=[b_condT, b_w], w=[b_ps])
            P.op("dve", lambda e: e.tensor_tensor(out=r_, in0=ps, in1=bt, op=ALU.add), r=[b_ps, b_bt], w=[b_r])
            P.dma("sp", modd[:, cs], r_, r=[b_r], w=[b_modd])


def adaln_gen(kb, li, IN):
    P = kb.P
    modd, b_modd = kb.dr["modd%d" % li]
    condT, b_condT = kb.sb([128, 8, 3], F32, "condT")
    crow, b_crow = kb.sb([3, 1024], F32, "crow")
    P.dma("sp", crow[0:2, :], IN["c"], w=[b_crow])
    P.dma("sp", crow[2:3, :], IN["c_ctx"].rearrange("(o n) -> o n", o=1), w=[b_crow])
    pst, b_pst = kb.ps([128, 8, 3], F32, "pst")
    for k in range(8):
        P.op("pe", lambda e: e.transpose(out=pst[:, k, :], in_=crow[0:3, k * 128:(k + 1) * 128],
                                         identity=kb.ident[0:3, 0:3]), r=[b_crow, kb.b_ident], w=[b_pst])
    P.op("act", lambda e: e.activation(out=condT, in_=pst, func=AF.Silu), r=[b_pst], w=[b_condT])
    wb = [kb.sb([128, 8, 512], F32, "adaw") for _ in range(2)]
    bb = [kb.sb([3, 512], F32, "adab") for _ in range(2)]
    rs = [kb.sb([3, 512], F32, "adar") for _ in range(2)]
    pm = [kb.ps([3, 512], F32, "adaps") for _ in range(2)]
    yield
    for blk in range(12):
        w, b_w = wb[blk % 2]
        bt, b_bt = bb[blk % 2]
        r_, b_r = rs[blk % 2]
        ps, b_ps = pm[blk % 2]
        cs = slice(blk * 512, (blk + 1) * 512)
        P.dma("sp", w, IN["ada_w"][li, :, cs].rearrange("(k p) n -> p k n", p=128), w=[b_w])
        P.dma("sp", bt, IN["ada_b"][li, cs].partition_broadcast(3), w=[b_bt])
        for k in range(8):
            P.op("pe", lambda e: e.matmul(ps, lhsT=condT[:, k, :], rhs=w[:, k, :], start=(k == 0), stop=(k == 7)),
                 r=[b_condT, b_w], w=[b_ps])
        P.op("dve", lambda e: e.tensor_tensor(out=r_, in0=ps, in1=bt, op=ALU.add), r=[b_ps, b_bt], w=[b_r])
        P.dma("sp", modd[:, cs], r_, r=[b_r], w=[b_modd])
        yield


def load_mod_fm(kb, li, IN, gname):
    P = kb.P
    modd, b_modd = kb.dr["modd%d" % li]
    modF, b_modF = kb.sb([128, 48, 3], F32, "modF")
    As = [kb.sb([128, 8, 3], F32, "modA") for _ in range(2)]
    gs = [kb.sb([128, 8], F32, "ng") for _ in range(2)]
    with kb.phase():
        mrow, b_mrow = kb.sb([3, 6144], F32, "mrow")
        P.dma("sp", mrow, modd, r=[b_modd], w=[b_mrow])
        pst, b_pst = kb.ps([128, 48, 3], F32, "modps")
        for j in range(48):
            P.op("pe", lambda e: e.transpose(out=pst[:, j, :], in_=mrow[0:3, j * 128:(j + 1) * 128],
                                             identity=kb.ident[0:3, 0:3]), r=[b_mrow, kb.b_ident], w=[b_pst])
        P.op("dve", lambda e: e.tensor_copy(out=modF, in_=pst), r=[b_pst], w=[b_modF])
        for i, (nm, so) in enumerate(((gname[0], 8), (gname[1], 32))):
            g, b_g = gs[i]
            A, b_A = As[i]
            kb.load_vec_fm(IN[nm][li], 8, g, b_g, (pst.rearrange("p a c -> p (a c)"), b_pst))
            P.op("dve", lambda e: e.tensor_scalar(out=A, in0=modF[:, so:so + 8, :], scalar1=1.0, scalar2=None, op0=ALU.add),
                 r=[b_modF], w=[b_A])
            P.op("dve", lambda e: e.tensor_tensor(out=A, in0=A, in1=g.unsqueeze(2).to_broadcast([128, 8, 3]), op=ALU.mult),
                 r=[b_A, b_g], w=[b_A])
    return modF, b_modF, As[0], As[1]


def norm_mod_tile(kb, xs_ap, b_xs, rows, cond, A, b_A, modF, b_modF, shift_off, work, hT_list, col0, cols=None):
    P = kb.P
    (xt, b_xt), (sq, b_sq), (ss, b_ss), (pt, b_pt) = work
    P.dma("sp", xt, xs_ap[rows, :], r=[b_xs], w=[b_xt])
    P.op("act", lambda e: e.activation(out=sq, in_=xt, func=AF.Square, accum_out=ss[:, 0:1]), r=[b_xt], w=[b_sq, b_ss])
    P.op("dve", lambda e: e.tensor_scalar(out=ss[:, 1:2], in0=ss[:, 0:1], scalar1=1.0 / D, scalar2=EPS,
                                          op0=ALU.mult, op1=ALU.add), r=[b_ss], w=[b_ss])
    P.op("act", lambda e: e.activation(out=ss[:, 2:3], in_=ss[:, 1:2], func=AF.Sqrt), r=[b_ss], w=[b_ss])
    P.op("dve", lambda e: e.reciprocal(out=ss[:, 3:4], in_=ss[:, 2:3]), r=[b_ss], w=[b_ss])
    P.op("act", lambda e: e.activation(out=sq, in_=xt, func=AF.Copy, scale=ss[:, 3:4]), r=[b_xt, b_ss], w=[b_sq])
    for half in range(2):
        for k4 in range(4):
            k = half * 4 + k4
            P.op("pe", lambda e: e.transpose(out=pt[:, k4, :], in_=sq[:, k * 128:(k + 1) * 128], identity=kb.ident),
                 r=[b_sq, kb.b_ident], w=[b_pt])
        for k4 in range(4):
            k = half * 4 + k4
            for hi, (hT, b_hT) in enumerate(hT_list):
                eng = "dve"
                c0_ = cols[hi] if cols is not None else col0
                P.op(eng, lambda e: e.tensor_scalar(out=hT[:, k, c0_:c0_ + 128], in0=pt[:, k4, :],
                                                    scalar1=A[:, k, cond:cond + 1],
                                                    scalar2=modF[:, shift_off + k, cond:cond + 1],
                                                    op0=ALU.mult, op1=ALU.add),
                     r=[b_pt, b_A, b_modF], w=[b_hT])


def norm_mod_batch(kb, xs_ap, b_xs, toks, conds, A, b_A, modF, b_modF, shift_off, bufs, outs):
    P = kb.P
    G = len(toks)
    ss, b_ss = bufs["ss"]
    for i, tok0 in enumerate(toks):
        (xt, b_xt), (sq, b_sq) = bufs["xt"][i], bufs["sq"][i]
        P.dma("sp", xt, xs_ap[tok0:tok0 + 128, :], r=[b_xs], w=[b_xt])
        P.op("act", lambda e: e.activation(out=sq, in_=xt, func=AF.Square, accum_out=ss[:, 0, i:i + 1]), r=[b_xt], w=[b_sq, b_ss])
    P.op("dve", lambda e: e.tensor_scalar(out=ss[:, 1, 0:G], in0=ss[:, 0, 0:G], scalar1=1.0 / D, scalar2=EPS,
                                          op0=ALU.mult, op1=ALU.add), r=[b_ss], w=[b_ss])
    P.op("act", lambda e: e.activation(out=ss[:, 2, 0:G], in_=ss[:, 1, 0:G], func=AF.Sqrt), r=[b_ss], w=[b_ss])
    P.op("dve", lambda e: e.reciprocal(out=ss[:, 3, 0:G], in_=ss[:, 2, 0:G]), r=[b_ss], w=[b_ss])
    rot = bufs.setdefault("rot", [0])
    for i, tok0 in enumerate(toks):
        (xt, b_xt), (sq, b_sq) = bufs["xt"][i], bufs["sq"][i]
        cond = conds[i]
        P.op("act", lambda e: e.activation(out=sq, in_=xt, func=AF.Copy, scale=ss[:, 3, i:i + 1]), r=[b_xt, b_ss], w=[b_sq])
        for half in range(2):
            pt, b_pt = bufs["pts"][rot[0] % len(bufs["pts"])]
            rot[0] += 1
            for k4 in range(4):
                k = half * 4 + k4
                P.op("pe", lambda e: e.transpose(out=pt[:, k4, :], in_=sq[:, k * 128:(k + 1) * 128], identity=kb.ident),
                     r=[b_sq, kb.b_ident], w=[b_pt])
            for k4 in range(4):
                k = half * 4 + k4
                for hi, (hT, b_hT, colfn) in enumerate(outs):
                    c0_ = colfn(i)
                    if (k4 + hi) % 2 == 0:
                        P.op("dve", lambda e: e.tensor_scalar(out=hT[:, k, c0_:c0_ + 128], in0=pt[:, k4, :],
                                                              scalar1=A[:, k, cond:cond + 1],
                                                              scalar2=modF[:, shift_off + k, cond:cond + 1],
                                                              op0=ALU.mult, op1=ALU.add),
                             r=[b_pt, b_A, b_modF], w=[b_hT])
                    else:
                        P.op("act", lambda e: e.activation(out=hT[:, k, c0_:c0_ + 128], in_=pt[:, k4, :], func=AF.Identity,
                                                           scale=A[:, k, cond:cond + 1],
                                                           bias=modF[:, shift_off + k, cond:cond + 1]),
                             r=[b_pt, b_A, b_modF], w=[b_hT])


def make_nm_bufs(kb, G, npt):
    return dict(xt=[kb.sb([128, 1024], F32, "xt") for _ in range(G)], sq=[kb.sb([128, 1024], F32, "sq") for _ in range(G)],
                ss=kb.sb([128, 4, G], F32, "ss"), pts=[kb.ps([128, 4, 128], F32, "pt") for _ in range(npt)])


AB_W = 2576


def phase_l0_inproj(kb, IN):
    P = kb.P
    xs, b_xs = kb.dr["xs"]
    qkvu, b_qkvu = kb.dr["qkvu"]
    zab, b_zab = kb.dr["zab"]
    with kb.phase():
        modF, b_modF, (A1, b_A1), _ = load_mod_fm(kb, 0, IN, ("norm1_g", "norm2_g"))
        W, b_W = kb.sb([128, 8, AB_W], BF16, "win")
        stage = [kb.sb([128, 2576], F32, "wst") for _ in range(2)]
        for k in range(8):
            kb.load_w_bf16(W[:, k, :], b_W, IN["ab_w_in"][0, k * 128:(k + 1) * 128, :], 128, AB_W, stage, colblk=2576)
        pts_shared = [kb.ps([128, 4, 128], F32, "pt") for _ in range(4)]
        nmb = []
        for _i in range(2):
            bb_ = make_nm_bufs(kb, 2, 0)
            bb_["pts"] = pts_shared
            nmb.append(bb_)
        nmb[1]["rot"] = nmb[0].setdefault("rot", [0])
        hTs = [kb.sb([128, 8, 256], BF16, "hT") for _ in range(2)]
        outs = [kb.sb([128, 16, 256], F32, "ost") for _ in range(2)]
        zst = [kb.sb([128, 528], F32, "zst") for _ in range(2)]
        pf = [kb.ps([128, 512], F32, "pf") for _ in range(2)]
        pz = [kb.ps([128, 512], F32, "pz") for _ in range(1)]
        pab = kb.ps([128, 512], F32, "pab")
        fm_cols = [c * 128 for c in range(12)] + [2064 + c * 128 for c in range(4)]
        ui = 0
        for b in range(NB):
            for un in range(T // 256):
                hT, b_hT = hTs[ui % 2]
                ost, b_ost = outs[ui % 2]
                toks_ = [un * 256, un * 256 + 128]
                norm_mod_batch(kb, xs[b], b_xs, toks_, [2 if t_ < CTX else b for t_ in toks_], A1, b_A1, modF, b_modF, 0,
                               nmb[ui % 2], [(hT, b_hT, lambda i: i * 128)])
                for ci, c0 in enumerate(fm_cols):
                    ps, b_ps = pf[ci % 2]
                    for k in range(8):
                        P.op("pe", lambda e: e.matmul(ps[:, 0:256], lhsT=W[:, k, c0:c0 + 128], rhs=hT[:, k, :],
                                                      start=(k == 0), stop=(k == 7)), r=[b_W, b_hT], w=[b_ps])
                    if ci % 2 == 0:
                        P.op("act", lambda e: e.copy(out=ost[:, ci, :], in_=ps[:, 0:256]), r=[b_ps], w=[b_ost])
                    else:
                        P.op("dve", lambda e: e.tensor_copy(out=ost[:, ci, :], in_=ps[:, 0:256]), r=[b_ps], w=[b_ost])
                P.dma("sp", qkvu[b, :, un * 256:(un + 1) * 256].rearrange("(c p) t -> p c t", p=128), ost,
                      r=[b_ost], w=[b_qkvu])
                for tt in range(2):
                    z, b_z = zst[tt]
                    ps, b_ps = pz[0]
                    for k in range(8):
                        P.op("pe", lambda e: e.matmul(ps[:, 0:512], lhsT=hT[:, k, tt * 128:(tt + 1) * 128],
                                                      rhs=W[:, k, 1536:2048], start=(k == 0), stop=(k == 7)),
                             r=[b_W, b_hT], w=[b_ps])
                    for k in range(8):
                        P.op("pe", lambda e: e.matmul(pab[0][:, 0:16], lhsT=hT[:, k, tt * 128:(tt + 1) * 128],
                                                      rhs=W[:, k, 2048:2064], start=(k == 0), stop=(k == 7)),
                             r=[b_W, b_hT], w=[pab[1]])
                    P.op("act", lambda e: e.copy(out=z[:, 0:512], in_=ps), r=[b_ps], w=[b_z])
                    P.op("dve", lambda e: e.tensor_copy(out=z[:, 512:528], in_=pab[0][:, 0:16]), r=[pab[1]], w=[b_z])
                    tok0 = un * 256 + tt * 128
                    P.dma("sp", zab[b, tok0:tok0 + 128, :], z, r=[b_z], w=[b_zab])
                ui += 1


SEGS = ((0, CTX), (CTX, T))
CPIECES = [(0, CTX)] + [(CTX + 512 * q_, CTX + 512 * (q_ + 1)) for q_ in range(4)]
ADA1_IN_CONV = bool(int(_os.environ.get("ADA1_IN_CONV", "1")))
PE_CONV = bool(int(_os.environ.get("PE_CONV", "0")))


def phase_l0_conv(kb, IN):
    P = kb.P
    qkvu, b_qkvu = kb.dr["qkvu"]
    qkvn, b_qkvn = kb.dr["qkvn"]
    with kb.phase():
        cwr, b_cwr = kb.sb([5, 1536], F32, "cwr")
        P.dma("sp", cwr, IN["dn_conv_w"][0], w=[b_cwr])
        pcw, b_pcw = kb.ps([128, 12, 5], F32, "pcw")
        for c in range(12):
            P.op("pe", lambda e: e.transpose(out=pcw[:, c, :], in_=cwr[0:5, c * 128:(c + 1) * 128],
                                             identity=kb.ident[0:5, 0:5]), r=[b_cwr, kb.b_ident], w=[b_pcw])
        cw, b_cw = kb.sb([128, 12, 5], F32, "cw")
        P.op("dve", lambda e: e.tensor_copy(out=cw, in_=pcw), r=[b_pcw], w=[b_cw])
        epsc, b_epsc = kb.sb([128, 1], F32, "epsc")
        P.op("pool", lambda e: e.memset(epsc, EPS), w=[b_epsc])
        bufs = [dict(x=kb.sb([128, T], F32, "cx"), a=kb.sb([128, T], F32, "ca"), y=kb.sb([128, T], F32, "cy"),
                     q=kb.sb([128, T], F32, "cq")) for _ in range(2)]
        pss = [kb.ps([128, 512], F32, "cps") for _ in range(3)]
        pcv = [kb.ps([128, 512], F32, "pcv") for _ in range(1)] * 2
        ada_it = adaln_gen(kb, 1, IN) if ADA1_IN_CONV else iter(())
        next(ada_it, None)
        diags = [kb.sb([128, 5, 128], F32, "cdiag") for _ in range(2)]
        it = 0
        for b in range(NB):
            for c in range(12):
                B_ = bufs[it % 2]
                (x, b_x), (a, b_a), (y, b_y), (q, b_q) = B_["x"], B_["a"], B_["y"], B_["q"]
                P.dma("sp", x, qkvu[b, c * 128:(c + 1) * 128, :], r=[b_qkvu], w=[b_x])
                if PE_CONV and (it % 2 == 1):
                    (dg_, b_dg) = diags[(it // 2) % 2]
                    for j in range(5):
                        P.op("pool", lambda e: e.tensor_scalar(out=dg_[:, j, :], in0=kb.ident, scalar1=cw[:, c, j:j + 1], scalar2=None, op0=ALU.mult),
                             r=[kb.b_ident, b_cw], w=[b_dg])
                    for pi, (p0, p1) in enumerate(CPIECES):
                        ps, b_ps = pcv[pi % 2]
                        s0, s1 = (0, CTX) if p0 < CTX else (CTX, T)
                        for ji, j in enumerate((2, 0, 1, 3, 4)):
                            o = j - 2
                            t0, t1 = max(p0, s0 - o), min(p1, s1 - o)
                            P.op("pe", lambda e: e.matmul(ps[:, t0 - p0:t1 - p0], lhsT=dg_[:, j, :], rhs=x[:, t0 + o:t1 + o],
                                                          start=(ji == 0), stop=(ji == 4)), r=[b_dg, b_x], w=[b_ps])
                        P.op("act", lambda e: e.activation(out=y[:, p0:p1], in_=ps[:, 0:p1 - p0], func=AF.Silu), r=[b_ps], w=[b_y])
                else:
                    P.op("act", lambda e: e.activation(out=a, in_=x, func=AF.Copy, scale=cw[:, c, 2:3]), r=[b_x, b_cw], w=[b_a])
                    for j in (0, 1, 3, 4):
                        o = j - 2
                        for (s0, s1) in SEGS:
                            t0, t1 = max(s0, s0 - o), min(s1, s1 - o)
                            P.op("dve", lambda e: e.scalar_tensor_tensor(out=a[:, t0:t1], in0=x[:, t0 + o:t1 + o],
                                                                         scalar=cw[:, c, j:j + 1], in1=a[:, t0:t1],
                                                                         op0=ALU.mult, op1=ALU.add),
                                 r=[b_x, b_cw, b_a], w=[b_a])
                    P.op("act", lambda e: e.activation(out=y, in_=a, func=AF.Silu), r=[b_a], w=[b_y])
                if c < 8:
                    P.op("act", lambda e: e.activation(out=q, in_=y, func=AF.Square), r=[b_y], w=[b_q])
                    for pi, t0 in enumerate(range(0, T, 512)):
                        t1 = min(T, t0 + 512)
                        ps, b_ps = pss[pi % 3]
                        P.op("pe", lambda e: e.matmul(ps[:, 0:t1 - t0], lhsT=kb.ones, rhs=q[:, t0:t1], start=True, stop=True),
                             r=[kb.b_ones, b_q], w=[b_ps])
                        P.op("act", lambda e: e.activation(out=a[:, t0:t1], in_=ps[:, 0:t1 - t0], func=AF.Sqrt, bias=epsc[:, 0:1]),
                             r=[b_ps, b_epsc], w=[b_a])
                    P.op("dve", lambda e: e.reciprocal(out=q, in_=a), r=[b_a], w=[b_q])
                    sc = (128.0 ** -0.5) if c < 4 else 1.0
                    P.op("dve", lambda e: e.scalar_tensor_tensor(out=y, in0=y, scalar=sc, in1=q, op0=ALU.mult, op1=ALU.mult),
                         r=[b_y, b_q], w=[b_y])
                P.dma("sp", qkvn[b, c * 128:(c + 1) * 128, :], y, r=[b_y], w=[b_qkvn])
                it += 1
                if it % 2 == 0:
                    next(ada_it, None)
        for _ in ada_it:
            pass


BIG = 30000.0


def phase_l0_delta(kb, IN):
    P = kb.P
    qkvn, b_qkvn = kb.dr["qkvn"]
    zab, b_zab = kb.dr["zab"]
    mixT, b_mixT = kb.dr["mixT"]
    ident, b_ident, ones, b_ones = kb.ident, kb.b_ident, kb.ones, kb.b_ones
    with kb.phase():
        def mk(nm):
            return kb.sb([128, 128], F32, nm)
        (Lo, b_Lo), (Up, b_Up) = mk("Lo"), mk("Up")
        for (M, b_M, op_) in ((Lo, b_Lo, ALU.is_ge), (Up, b_Up, ALU.is_ge)):
            P.op("pool", lambda e: e.memset(M, 0.0), w=[b_M])
            for r0 in (0, 64):
                blk = M[r0:r0 + 64, r0:r0 + 64]
                P.op("pool", lambda e: e.memset(blk, 1.0), r=[b_M], w=[b_M])
                if M is Lo:
                    P.op("pool", lambda e: e.affine_select(out=blk, in_=blk, pattern=[[-1, 64]], compare_op=ALU.is_ge,
                                                           fill=0.0, base=0, channel_multiplier=1), r=[b_M], w=[b_M])
                else:
                    P.op("pool", lambda e: e.affine_select(out=blk, in_=blk, pattern=[[1, 64]], compare_op=ALU.is_ge,
                                                           fill=0.0, base=0, channel_multiplier=-1), r=[b_M], w=[b_M])
        masks = {}
        for nm, (M, b_M) in (("Lo", (Lo, b_Lo)), ("Up", (Up, b_Up))):
            for sgn in (1.0, -1.0):
                N_, b_N = mk("N" + nm)
                P.op("dve", lambda e: e.tensor_scalar(out=N_, in0=M, scalar1=-sgn * BIG, scalar2=sgn * BIG,
                                                      op0=ALU.mult, op1=ALU.add), r=[b_M], w=[b_N])
                masks[(nm, sgn)] = (N_, b_N)
        dirs = [dict(LT=(Up, b_Up), M1=masks[("Lo", 1.0)], M2=masks[("Up", -1.0)], last=(63, 127), order=(0, 64)),
                dict(LT=(Lo, b_Lo), M1=masks[("Up", 1.0)], M2=masks[("Lo", -1.0)], last=(0, 64), order=(64, 0))]
        dtb, b_dtb = kb.sb([128, 8], F32, "dtb")
        P.dma("sp", dtb, IN["dn_dt_bias"][0].rearrange("a b -> (a b)").partition_broadcast(128), w=[b_dtb])
        nA, b_nA = kb.sb([128, 8], F32, "nA")
        P.dma("sp", nA, IN["dn_A_log"][0].rearrange("a b -> (a b)").partition_broadcast(128), w=[b_nA])
        P.op("act", lambda e: e.activation(out=nA, in_=nA, func=AF.Exp), r=[b_nA], w=[b_nA])
        P.op("dve", lambda e: e.tensor_scalar(out=nA, in0=nA, scalar1=-1.0, scalar2=None, op0=ALU.mult), r=[b_nA], w=[b_nA])
        gB, b_gB = kb.sb([128, 128], F32, "gB")
        P.dma("sp", gB, IN["dn_norm_g"][0].partition_broadcast(128), w=[b_gB])

        gat, b_gat = kb.sb([128, NT, 16], F32, "gat")
        gg, b_gg = kb.sb([128, NT, 8], F32, "gg")
        bet, b_bet = kb.sb([128, NT, 8], F32, "bet")
        qT, b_qT = kb.sb([128, T], F32, "qT")
        kT, b_kT = kb.sb([128, T], F32, "kT")
        vT, b_vT = kb.sb([128, T], F32, "vT")
        zt, b_zt = kb.sb([128, NT, 128], F32, "zt")
        dst_, b_dst = kb.sb([128, 4, NT], F32, "dnst")
        qTb, b_qTb = kb.sb([128, T], BF16, "qTb")
        kTb, b_kTb = kb.sb([128, T], BF16, "kTb")
        vTb, b_vTb = kb.sb([128, T], BF16, "vTb")
        identb, b_identb = kb.identb, kb.b_identb
        osums = [kb.sb([128, NT, 128], F32, "osum") for _ in range(2)]
        aT, b_aT = kb.sb([128, T], F32, "aT")
        banks = [kb.ps([128, 4, 128], F32, "dps") for _ in range(8)]

        def slot(bk, i):
            ap, bf = banks[bk]
            return ap[:, i, :], bf
        chains = []
        NTMP = 21
        for c in range(2):
            o = 4 * c
            chains.append(dict(
                ps=[slot(o, 0), slot(o, 1), slot(o, 2), slot(o, 3), slot(o + 1, 0), slot(o + 1, 1), slot(o + 1, 2), slot(o + 1, 3),
                    slot(o + 2, 0), slot(o + 2, 1), slot(o + 2, 2), slot(o + 2, 3)],
                prec=[slot(o + 3, 0), slot(o + 3, 1), slot(o + 3, 2)],
                tset=[[kb.sb([128, 128], F32, "dt%d" % i) for i in range(NTMP)] for _ in range(2)],
                tsetb=[[kb.sb([128, 128], BF16, "db%d" % i) for i in range(14)] for _ in range(2)],
                Sb=kb.sb([128, 128], BF16, "Sb"),
                cset=[kb.sb([128, 8], F32, "dc") for _ in range(2)],
                vnew=[kb.sb([128, 128], BF16, "vnew") for _ in range(2)],
                S=kb.sb([128, 128], F32, "S"),
                osum=osums[c]))
        import os
        lim = [int(v) for v in os.environ.get("DELTA_LIM", "2,4,2").split(",")]

        def chain_gen(ch, d, h):
            dd = dirs[d]
            (LT, b_LT), (M1, b_M1), (M2, b_M2) = dd["LT"], dd["M1"], dd["M2"]
            col = d * 4 + h
            S, b_S = ch["S"]
            osum, b_osum = ch["osum"]
            P.op("pool", lambda e: e.memset(S, 0.0), w=[b_S])
            Sb_, b_Sb_ = ch["Sb"]
            P.op("pool", lambda e: e.memset(Sb_, 0.0), w=[b_Sb_])
            order = list(range(NT)) if d == 0 else [1, 0] + list(range(NT - 1, 1, -1))
            for it, j in enumerate(order):
                tm = ch["tset"][it % 2]
                (cs, b_cs) = ch["cset"][it % 2]
                tk = slice(j * 128, (j + 1) * 128)
                tb_ = ch["tsetb"][it % 2]
                (Lg, b_Lg), (Erow, b_Erow), (dec, b_dec), (decT, b_decT), (u, b_u) = tm[0:5]
                (A, b_A), (AT, b_AT), (TT0, b_TT0), (TT1, b_TT1) = tb_[0:4]
                Pm = [tb_[4], tb_[5]]
                PTm = [tb_[6], tb_[7]]
                (Xu, b_Xu), (Xw, b_Xw), (wT, b_wT), (qdT, b_qdT), (kdec, b_kdec), (qkT, b_qkT) = tb_[8:14]
                Sb, b_Sb = ch["Sb"]
                (pGr, b_pGr), (pgc, b_pgc), (pKK, b_pKK), (pQK, b_pQK) = ch["ps"][0:4]
                (pAT, b_pAT), (pP, b_pP), (pPT, b_pPT), (pTT, b_pTT) = ch["ps"][4:8]
                (pu, b_pu), (pwT, b_pwT), (pkt, b_pkt), (pvt, b_pvt) = ch["ps"][8:12]
                pATb = pAT.bitcast(BF16)[:, 0:128]
                pktb = pkt.bitcast(BF16)[:, 0:128]
                pvtb = pvt.bitcast(BF16)[:, 0:128]
                gcol = gg[:, j, col:col + 1]
                bcol = bet[:, j, col:col + 1]
                P.op("dve", lambda e: e.tensor_scalar(out=Lg, in0=LT, scalar1=gcol, scalar2=None, op0=ALU.mult),
                     r=[b_LT, b_gg], w=[b_Lg])
                P.op("pe", lambda e: e.matmul(pGr, lhsT=ones, rhs=Lg, start=True, stop=True), r=[b_ones, b_Lg], w=[b_pGr])
                P.op("pe", lambda e: e.matmul(pgc[:, 0:1], lhsT=Lg, rhs=ones[:, 0:1], start=True, stop=True),
                     r=[b_ones, b_Lg], w=[b_pgc])
                P.op("pe", lambda e: e.matmul(pKK, lhsT=kTb[:, tk], rhs=kTb[:, tk], start=True, stop=True), r=[b_kTb], w=[b_pKK])
                P.op("pe", lambda e: e.matmul(pQK, lhsT=kTb[:, tk], rhs=qTb[:, tk], start=True, stop=True), r=[b_kTb, b_qTb], w=[b_pQK])
                yield
                P.op("dve", lambda e: e.tensor_copy(out=cs[:, 0:1], in_=pgc[:, 0:1]), r=[b_pgc], w=[b_cs])
                P.op("act", lambda e: e.activation(out=cs[:, 1:2], in_=pgc[:, 0:1], func=AF.Copy, scale=-1.0), r=[b_pgc], w=[b_cs])
                P.op("dve", lambda e: e.tensor_tensor(out=dec, in0=pGr, in1=M1, op=ALU.add), r=[b_pGr, b_M1], w=[b_dec])
                P.op("dve", lambda e: e.tensor_tensor(out=decT, in0=pGr, in1=M2, op=ALU.add), r=[b_pGr, b_M2], w=[b_decT])
                P.op("act", lambda e: e.activation(out=Erow, in_=pGr, func=AF.Exp), r=[b_pGr], w=[b_Erow])
                for ci, r0 in enumerate((0, 64)):
                    lc = dd["last"][ci]
                    P.op("act", lambda e: e.activation(out=cs[r0:r0 + 64, 3:4], in_=pGr[r0:r0 + 64, lc:lc + 1], func=AF.Exp,
                                                       bias=cs[r0:r0 + 64, 1:2], scale=1.0), r=[b_pGr, b_cs], w=[b_cs])
                P.op("act", lambda e: e.activation(out=dec, in_=dec, func=AF.Exp, bias=cs[:, 0:1], scale=-1.0), r=[b_dec, b_cs], w=[b_dec])
                P.op("act", lambda e: e.activation(out=decT, in_=decT, func=AF.Exp, bias=cs[:, 1:2], scale=1.0), r=[b_decT, b_cs], w=[b_decT])
                P.op("act", lambda e: e.activation(out=cs[:, 2:3], in_=cs[:, 0:1], func=AF.Exp), r=[b_cs], w=[b_cs])
                P.op("dve", lambda e: e.tensor_tensor(out=cs[:, 4:5], in0=cs[:, 2:3], in1=bcol, op=ALU.mult), r=[b_cs, b_bet], w=[b_cs])
                yield
                P.op("pool", lambda e: e.tensor_tensor(out=dec, in0=dec, in1=ident, op=ALU.subtract), r=[b_dec, b_ident], w=[b_dec])
                P.op("dve", lambda e: e.scalar_tensor_tensor(out=A, in0=pKK, scalar=bcol, in1=dec, op0=ALU.mult, op1=ALU.mult),
                     r=[b_pKK, b_bet, b_dec], w=[b_A])
                P.op("dve", lambda e: e.tensor_tensor(out=qkT, in0=pQK, in1=decT, op=ALU.mult), r=[b_pQK, b_decT], w=[b_qkT])
                P.op("pe", lambda e: e.transpose(out=pATb, in_=A, identity=identb), r=[b_A, b_identb], w=[b_pAT])
                P.op("pe", lambda e: e.transpose(out=pktb, in_=kTb[:, tk], identity=identb), r=[b_kTb, b_identb], w=[b_pkt])
                P.op("pe", lambda e: e.transpose(out=pvtb, in_=vTb[:, tk], identity=identb), r=[b_vTb, b_identb], w=[b_pvt])
                yield
                P.op("act", lambda e: e.copy(out=AT, in_=pATb), r=[b_pAT], w=[b_AT])
                P.op("pool", lambda e: e.tensor_tensor(out=TT0, in0=ident, in1=AT, op=ALU.subtract), r=[b_ident, b_AT], w=[b_TT0])
                P.op("act", lambda e: e.activation(out=Xu, in_=pvtb, func=AF.Copy, scale=bcol), r=[b_pvt, b_bet], w=[b_Xu])
                P.op("act", lambda e: e.activation(out=Xw, in_=pktb, func=AF.Copy, scale=cs[:, 4:5]), r=[b_pkt, b_cs], w=[b_Xw])
                P.op("dve", lambda e: e.tensor_scalar(out=kdec, in0=pktb, scalar1=cs[:, 3:4], scalar2=0.0, op0=ALU.mult, op1=ALU.add),
                     r=[b_pkt, b_cs], w=[b_kdec])
                P.op("pool", lambda e: e.tensor_tensor(out=qdT, in0=qT[:, tk], in1=Erow, op=ALU.mult), r=[b_qT, b_Erow], w=[b_qdT])
                curP, curPT = (A, b_A), (AT, b_AT)
                TTc, TTn = (TT0, b_TT0), (TT1, b_TT1)
                for m in range(1, 6):
                    (nP, b_nP), (nPT, b_nPT) = Pm[m % 2], PTm[m % 2]
                    P.op("pe", lambda e: e.matmul(pP, lhsT=curPT[0], rhs=curP[0], start=True, stop=True),
                         r=[curPT[1], curP[1]], w=[b_pP])
                    if m < 5:
                        P.op("pe", lambda e: e.matmul(pPT, lhsT=curP[0], rhs=curPT[0], start=True, stop=True),
                             r=[curPT[1], curP[1]], w=[b_pPT])
                    yield
                    P.op("act", lambda e: e.copy(out=nP, in_=pP), r=[b_pP], w=[b_nP])
                    if m < 5:
                        P.op("dve", lambda e: e.tensor_copy(out=nPT, in_=pPT), r=[b_pPT], w=[b_nPT])
                    P.op("pe", lambda e: e.matmul(pTT, lhsT=nP, rhs=TTc[0], start=True, stop=True), r=[b_nP, TTc[1]], w=[b_pTT])
                    yield
                    P.op("dve", lambda e: e.tensor_tensor(out=TTn[0], in0=pTT, in1=TTc[0], op=ALU.add),
                         r=[b_pTT, TTc[1]], w=[TTn[1]])
                    curP, curPT = (nP, b_nP), (nPT, b_nPT)
                    TTc, TTn = TTn, TTc
                TT, b_TT = TTc
                P.op("pe", lambda e: e.matmul(pu, lhsT=TT, rhs=Xu, start=True, stop=True), r=[b_TT, b_Xu], w=[b_pu])
                P.op("pe", lambda e: e.matmul(pwT, lhsT=Xw, rhs=TT, start=True, stop=True), r=[b_TT, b_Xw], w=[b_pwT])
                yield
                P.op("act", lambda e: e.copy(out=u, in_=pu), r=[b_pu], w=[b_u])
                P.op("act", lambda e: e.copy(out=wT, in_=pwT), r=[b_pwT], w=[b_wT])
                for r0 in dd["order"]:
                    rs = slice(r0, r0 + 64)
                    lc = dd["last"][r0 // 64]
                    (vn, b_vn) = ch["vnew"][(r0 // 64)]
                    (p1, b_p1), (p2, b_p2), (p3, b_p3) = ch["prec"]
                    P.op("pe", lambda e: e.matmul(p1[rs, :], lhsT=wT[:, rs], rhs=Sb, start=True, stop=True), r=[b_wT, b_Sb], w=[b_p1])
                    yield
                    P.op("dve", lambda e: e.tensor_tensor(out=vn[rs, :], in0=u[rs, :], in1=p1[rs, :], op=ALU.subtract),
                         r=[b_u, b_p1], w=[b_vn])
                    P.op("pe", lambda e: e.matmul(p2[rs, :], lhsT=qdT[:, rs], rhs=Sb, start=True, stop=False), r=[b_qdT, b_Sb], w=[b_p2])
                    P.op("pe", lambda e: e.matmul(p2[rs, :], lhsT=qkT[rs, rs], rhs=vn[rs, :], start=False, stop=True),
                         r=[b_qkT, b_vn], w=[b_p2])
                    P.op("pe", lambda e: e.matmul(p3, lhsT=kdec[rs, :], rhs=vn[rs, :], start=True, stop=True), r=[b_kdec, b_vn], w=[b_p3])
                    yield
                    P.op("act", lambda e: e.copy(out=osum[rs, j, :], in_=p2[rs, :]), r=[b_p2], w=[b_osum])
                    P.op("dve", lambda e: e.scalar_tensor_tensor(out=S, in0=S, scalar=Erow[:, lc:lc + 1], in1=p3,
                                                                 op0=ALU.mult, op1=ALU.add), r=[b_S, b_Erow, b_p3], w=[b_S])
                    P.op("act", lambda e: e.copy(out=Sb, in_=S), r=[b_S], w=[b_Sb])

        for b in range(lim[0]):
            P.dma("sp", gat, zab[b, :, 512:528].rearrange("(j p) c -> p j c", p=128), r=[b_zab], w=[b_gat])
            P.op("dve", lambda e: e.tensor_tensor(out=gg, in0=gat[:, :, 0:8], in1=dtb.unsqueeze(1).to_broadcast([128, NT, 8]),
                                                  op=ALU.add), r=[b_gat, b_dtb], w=[b_gg])
            P.op("act", lambda e: e.activation(out=gg, in_=gg, func=AF.Exp), r=[b_gg], w=[b_gg])
            P.op("act", lambda e: e.activation(out=gg, in_=gg, func=AF.Ln, bias=1.0), r=[b_gg], w=[b_gg])
            P.op("dve", lambda e: e.tensor_tensor(out=gg, in0=gg, in1=nA.unsqueeze(1).to_broadcast([128, NT, 8]),
                                                  op=ALU.mult), r=[b_gg, b_nA], w=[b_gg])
            P.op("act", lambda e: e.activation(out=bet, in_=gat[:, :, 8:16], func=AF.Sigmoid), r=[b_gat], w=[b_bet])
            for h in range(lim[1]):
                P.dma("sp", qT, qkvn[b, h * 128:(h + 1) * 128, :], r=[b_qkvn], w=[b_qT])
                P.dma("sp", kT, qkvn[b, 512 + h * 128:512 + (h + 1) * 128, :], r=[b_qkvn], w=[b_kT])
                P.dma("sp", vT, qkvn[b, 1024 + h * 128:1024 + (h + 1) * 128, :], r=[b_qkvn], w=[b_vT])
                P.dma("sp", zt, zab[b, :, h * 128:(h + 1) * 128].rearrange("(j p) c -> p j c", p=128), r=[b_zab], w=[b_zt])
                P.op("act", lambda e: e.copy(out=qTb, in_=qT), r=[b_qT], w=[b_qTb])
                P.op("pool", lambda e: e.tensor_copy(out=kTb, in_=kT), r=[b_kT], w=[b_kTb])
                P.op("act", lambda e: e.copy(out=vTb, in_=vT), r=[b_vT], w=[b_vTb])
                gens = [chain_gen(chains[d], d, h) for d in range(2)]
                alive = [True, True]
                while any(alive):
                    for gi, g_ in enumerate(gens):
                        if alive[gi]:
                            try:
                                next(g_)
                            except StopIteration:
                                alive[gi] = False
                osum, b_osum = osums[0]
                P.op("pool", lambda e: e.tensor_tensor(out=osum, in0=osum, in1=osums[1][0], op=ALU.add), r=[b_osum, osums[1][1]], w=[b_osum])
                tset = chains[0]["tset"]
                cset = [[chains[0]["cset"][0]], [chains[0]["cset"][1]]]
                pset = [chains[0]["ps"], chains[1]["ps"]]
                itn = 0
                (sqj, b_sqj) = tset[0][0]
                for j in range(NT):
                    P.op("act", lambda e: e.activation(out=sqj, in_=osum[:, j, :], func=AF.Square, accum_out=dst_[:, 0, j:j + 1]),
                         r=[b_osum], w=[b_sqj, b_dst])
                P.op("dve", lambda e: e.tensor_scalar(out=dst_[:, 1, :], in0=dst_[:, 0, :], scalar1=1.0 / 128, scalar2=EPS,
                                                      op0=ALU.mult, op1=ALU.add), r=[b_dst], w=[b_dst])
                P.op("act", lambda e: e.activation(out=dst_[:, 2, :], in_=dst_[:, 1, :], func=AF.Sqrt), r=[b_dst], w=[b_dst])
                P.op("dve", lambda e: e.reciprocal(out=dst_[:, 3, :], in_=dst_[:, 2, :]), r=[b_dst], w=[b_dst])
                P.op("act", lambda e: e.activation(out=zt, in_=zt, func=AF.Silu), r=[b_zt], w=[b_zt])
                P.op("dve", lambda e: e.tensor_tensor(out=osum, in0=osum, in1=dst_[:, 3, :].unsqueeze(2).to_broadcast([128, NT, 128]), op=ALU.mult),
                     r=[b_osum, b_dst], w=[b_osum])
                P.op("pool", lambda e: e.tensor_tensor(out=zt, in0=zt, in1=gB.unsqueeze(1).to_broadcast([128, NT, 128]), op=ALU.mult),
                     r=[b_zt, b_gB], w=[b_zt])
                P.op("dve", lambda e: e.tensor_tensor(out=osum, in0=osum, in1=zt, op=ALU.mult), r=[b_osum, b_zt], w=[b_osum])
                for j0 in range(0, NT, 4):
                    nj = min(4, NT - j0)
                    bk_ap, bk_b = banks[(j0 // 4) % 8]
                    for jj in range(nj):
                        P.op("pe", lambda e: e.transpose(out=bk_ap[:, jj, :], in_=osum[:, j0 + jj, :], identity=ident), r=[b_osum, b_ident], w=[bk_b])
                    src_ = bk_ap[:, 0:nj, :]
                    dstv = aT[:, j0 * 128:(j0 + nj) * 128].rearrange("p (a b) -> p a b", b=128)
                    if (j0 // 4) % 2 == 0:
                        P.op("act", lambda e: e.copy(out=dstv, in_=src_), r=[bk_b], w=[b_aT])
                    else:
                        P.op("dve", lambda e: e.tensor_copy(out=dstv, in_=src_), r=[bk_b], w=[b_aT])
                P.dma("sp", mixT[b, h * 128:(h + 1) * 128, :], aT, r=[b_aT], w=[b_mixT])


def phase_l0_s5(kb, IN):
    P = kb.P
    qkvu, b_qkvu = kb.dr["qkvu"]
    ygT, b_ygT = kb.dr["ygT"]
    ident, b_ident = kb.ident, kb.b_ident
    with kb.phase():
        pbk = [kb.ps([128, 512], F32, "s5ps") for _ in range(7)]
        def stacked_T(src_a, src_b, nm):
            st, b_st = kb.sb([64, 128], F32, nm + "s")
            P.dma("sp", st[:, 0:64], src_a, w=[b_st])
            P.dma("sp", st[:, 64:128], src_b, w=[b_st])
            ps, b_ps = pbk[0]
            P.op("pe", lambda e: e.transpose(out=ps[:, 0:64], in_=st, identity=ident[0:64, 0:64]), r=[b_st, b_ident], w=[b_ps])
            o, b_o = kb.sb([128, 64], F32, nm)
            P.op("dve", lambda e: e.tensor_copy(out=o, in_=ps[:, 0:64]), r=[b_ps], w=[b_o])
            return o, b_o
        Are_src = IN["s5_A_re"][0].rearrange("d g p -> (d g) p")
        Aim_src = IN["s5_A_im"][0].rearrange("d g p -> (d g) p")
        Are, b_Are = stacked_T(Are_src, Are_src, "Are")
        Aim, b_Aim = stacked_T(Aim_src, Aim_src, "Aim")
        dt, b_dt = kb.sb([128, 64], F32, "dt")
        P.dma("sp", dt, IN["s5_log_dt"][0].rearrange("d g -> (d g)").partition_broadcast(128), w=[b_dt])
        P.op("act", lambda e: e.activation(out=dt, in_=dt, func=AF.Exp), r=[b_dt], w=[b_dt])
        NS = 14
        sm = [kb.sb([128, 64], F32, "s5sm%d" % i) for i in range(NS)]
        (rr, b_rr), (th, b_th), (t0_, b_t0), (t1_, b_t1), (cs_, b_cs), (sn_, b_sn), (lre, b_lre), (lim, b_lim) = sm[0:8]
        (den, b_den), (cre, b_cre), (cim, b_cim), (cimS, b_cimS), (creS, b_creS), (t2_, b_t2) = sm[8:14]

        def tt(eng, out, b_out, a, b_a, b, b_b, op):
            P.op(eng, lambda e: e.tensor_tensor(out=out, in0=a, in1=b, op=op), r=[b_a, b_b], w=[b_out])

        def ts(eng, out, b_out, a, b_a, s1, s2, op0, op1):
            P.op(eng, lambda e: e.tensor_scalar(out=out, in0=a, scalar1=s1, scalar2=s2, op0=op0, op1=op1), r=[b_a], w=[b_out])
        tt("dve", rr, b_rr, Are, b_Are, dt, b_dt, ALU.mult)
        P.op("act", lambda e: e.activation(out=rr, in_=rr, func=AF.Exp), r=[b_rr], w=[b_rr])
        tt("dve", th, b_th, Aim, b_Aim, dt, b_dt, ALU.mult)

        def sincos(dst, b_dst, shift):
            ts("dve", t0_, b_t0, th, b_th, shift, 1.0 / TWO_PI, ALU.add, ALU.mult)
            ts("dve", t1_, b_t1, t0_, b_t0, MAGIC, None, ALU.add, ALU.bypass)
            ts("dve", t1_, b_t1, t1_, b_t1, -MAGIC, -TWO_PI, ALU.add, ALU.mult)
            P.op("dve", lambda e: e.scalar_tensor_tensor(out=t0_, in0=th, scalar=shift, in1=t1_, op0=ALU.add, op1=ALU.add),
                 r=[b_th, b_t1], w=[b_t0])
            P.op("act", lambda e: e.activation(out=dst, in_=t0_, func=AF.Sin), r=[b_t0], w=[b_dst])
        sincos(cs_, b_cs, math.pi / 2)
        sincos(sn_, b_sn, 0.0)
        tt("dve", lre, b_lre, rr, b_rr, cs_, b_cs, ALU.mult)
        tt("dve", lim, b_lim, rr, b_rr, sn_, b_sn, ALU.mult)
        ts("dve", lre, b_lre, lre, b_lre, -1.0, None, ALU.add, ALU.bypass)
        tt("dve", den, b_den, Are, b_Are, Are, b_Are, ALU.mult)
        tt("dve", t2_, b_t2, Aim, b_Aim, Aim, b_Aim, ALU.mult)
        tt("dve", den, b_den, den, b_den, t2_, b_t2, ALU.add)
        P.op("dve", lambda e: e.reciprocal(out=den, in_=den), r=[b_den], w=[b_den])
        tt("dve", cre, b_cre, lre, b_lre, Are, b_Are, ALU.mult)
        tt("dve", t2_, b_t2, lim, b_lim, Aim, b_Aim, ALU.mult)
        tt("dve", cre, b_cre, cre, b_cre, t2_, b_t2, ALU.add)
        tt("dve", cre, b_cre, cre, b_cre, den, b_den, ALU.mult)
        tt("dve", cim, b_cim, lim, b_lim, Are, b_Are, ALU.mult)
        tt("dve", t2_, b_t2, lre, b_lre, Aim, b_Aim, ALU.mult)
        tt("dve", cim, b_cim, cim, b_cim, t2_, b_t2, ALU.subtract)
        tt("dve", cim, b_cim, cim, b_cim, den, b_den, ALU.mult)
        ts("dve", cimS[0:64, :], b_cimS, cim[0:64, :], b_cim, -1.0, None, ALU.mult, ALU.bypass)
        P.op("dve", lambda e: e.tensor_copy(out=cimS[64:128, :], in_=cim[64:128, :]), r=[b_cim], w=[b_cimS])
        P.op("dve", lambda e: e.tensor_copy(out=creS[0:64, :], in_=cre[0:64, :]), r=[b_cre], w=[b_creS])
        ts("dve", creS[64:128, :], b_creS, cre[64:128, :], b_cre, -1.0, None, ALU.mult, ALU.bypass)
        Bst1, b_Bst1 = kb.sb([128, 32, 16], F32, "Bst1")
        BstS, b_BstS = kb.sb([128, 32, 16], F32, "BstS")
        Bre_src = IN["s5_B_re"][0].rearrange("g p h -> p g h")
        Bim_src = IN["s5_B_im"][0].rearrange("g p h -> p g h")
        P.dma("sp", Bst1[0:64], Bre_src, w=[b_Bst1]); P.dma("sp", Bst1[64:128], Bim_src, w=[b_Bst1])
        P.dma("sp", BstS[0:64], Bim_src, w=[b_BstS]); P.dma("sp", BstS[64:128], Bre_src, w=[b_BstS])
        Wst, b_Wst = kb.sb([128, 4, 2, 2, 2, 128], BF16, "Wst")
        Wst3, b_Wst3 = kb.sb([128, 4, 2, 2, 2, 128], BF16, "Wst3")
        src, b_src = kb.sb([128, 128], F32, "wsrc")
        wtmp, b_wtmp = kb.sb([128, 16], F32, "wtmp")
        for c in range(4):
            for m in range(2):
                for d in range(2):
                    for var in range(2):
                        P.op("pool", lambda e: e.memset(src, 0.0), w=[b_src])
                        for pr in range(4):
                            g = 8 * c + 2 * pr + m
                            dg = d * 32 + g
                            dst = src[:, 32 * pr + 16 * m:32 * pr + 16 * m + 16]
                            if var == 0:
                                P.op("dve", lambda e: e.tensor_scalar(out=wtmp, in0=Bst1[:, g, :], scalar1=cre[:, dg:dg + 1], scalar2=None,
                                                                      op0=ALU.mult), r=[b_Bst1, b_cre], w=[b_wtmp])
                                P.op("dve", lambda e: e.scalar_tensor_tensor(out=dst, in0=BstS[:, g, :], scalar=cimS[:, dg:dg + 1], in1=wtmp,
                                                                             op0=ALU.mult, op1=ALU.add), r=[b_BstS, b_cimS, b_wtmp], w=[b_src])
                            else:
                                P.op("dve", lambda e: e.tensor_scalar(out=wtmp, in0=BstS[:, g, :], scalar1=creS[:, dg:dg + 1], scalar2=None,
                                                                      op0=ALU.mult), r=[b_BstS, b_creS], w=[b_wtmp])
                                P.op("dve", lambda e: e.scalar_tensor_tensor(out=dst, in0=Bst1[:, g, :], scalar=cim[:, dg:dg + 1], in1=wtmp,
                                                                             op0=ALU.mult, op1=ALU.add), r=[b_Bst1, b_cim, b_wtmp], w=[b_src])
                        ps, b_ps = pbk[1]
                        P.op("pe", lambda e: e.transpose(out=ps[:, 0:128], in_=src, identity=ident), r=[b_src, b_ident], w=[b_ps])
                        P.op("act", lambda e: e.copy(out=Wst[:, c, m, d, var, :], in_=ps[:, 0:128]), r=[b_ps], w=[b_Wst])
                        P.op("act", lambda e: e.copy(out=Wst3[64:128, c, m, d, var, :], in_=ps[64:128, 0:128]), r=[b_ps], w=[b_Wst3])
                        P.op("pool", lambda e: e.memset(Wst3[64:96, c, m, d, var, :], 0.0), r=[b_Wst3], w=[b_Wst3])
        CrPad, b_CrPad = kb.sb([128, 32, 2, 128], BF16, "CrPad")
        P.op("pool", lambda e: e.memset(CrPad, 0.0), w=[b_CrPad])
        Cre_src = IN["s5_C_re"][0].rearrange("g h p -> (g h) p")
        Cim_src = IN["s5_C_im"][0].rearrange("g h p -> (g h) p")
        cst, b_cst = kb.sb([128, 128], F32, "cst")
        for var in range(2):
            for q in range(4):
                rows = slice(q * 128, (q + 1) * 128)
                if var == 0:
                    P.dma("sp", cst[:, 0:64], Cre_src[rows, :], w=[b_cst]); P.dma("sp", cst[:, 64:128], Cim_src[rows, :], w=[b_cst])
                else:
                    P.dma("sp", cst[:, 0:64], Cim_src[rows, :], w=[b_cst]); P.dma("sp", cst[:, 64:128], Cre_src[rows, :], w=[b_cst])
                ps, b_ps = pbk[2]
                P.op("pe", lambda e: e.transpose(out=ps[:, 0:128], in_=cst, identity=ident), r=[b_cst, b_ident], w=[b_ps])
                for gl in range(8):
                    g = q * 8 + gl
                    cols = slice(gl * 16, gl * 16 + 16)
                    sgn_top = 1.0 if var == 0 else -1.0
                    P.op("act", lambda e: e.activation(out=CrPad[0:64, g, var, cols], in_=ps[0:64, cols], func=AF.Copy, scale=sgn_top),
                         r=[b_ps], w=[b_CrPad])
                    P.op("act", lambda e: e.activation(out=CrPad[64:128, g, var, cols], in_=ps[64:128, cols], func=AF.Copy, scale=-1.0),
                         r=[b_ps], w=[b_CrPad])
        Dc, b_Dc = kb.sb([128, 4], F32, "Dc")
        kb.load_vec_fm(IN["s5_D"][0].rearrange("g h -> (g h)"), 4, Dc, b_Dc, pbk[0])
        from concourse.mybir import dt as _dt
        iof, b_iof = kb.sb([128, T], F32, "iof")
        with kb.phase():
            ioi, b_ioi = kb.sb([128, T], _dt.int32, "ioi")
            P.op("pool", lambda e: e.iota(ioi, pattern=[[1, T]], base=0, channel_multiplier=0), w=[b_ioi])
            P.op("dve", lambda e: e.tensor_copy(out=iof, in_=ioi), r=[b_ioi], w=[b_iof])
        ub = [kb.sb([128, T], BF16, "ub") for _ in range(NB)]
        ysb = [kb.sb([128, T], F32, "ysb") for _ in range(NB)]
        tabs = [dict(C=kb.sb([128, T], F32, "tC"), S=kb.sb([128, T], F32, "tS")) for _ in range(2)]
        ph1, b_ph1 = kb.sb([128, T], F32, "ph1")
        ph2, b_ph2 = ph1, b_ph1
        tmps5 = [dict(bt=kb.sb([128, T], F32, "bt"), tmp=kb.sb([128, T], F32, "tmp"), ww=kb.sb([128, T], F32, "ww"),
                      Q1=kb.sb([128, T], BF16, "Q1"), Q2=kb.sb([128, T], BF16, "Q2")) for _ in range(2)]
        s5it = 0
        pieces = [(0, 256)] + [(256 + 512 * q, 256 + 512 * (q + 1)) for q in range(4)]

        def possl(d, t0, t1):
            if d == 0:
                return slice(t0, t1)
            if t1 <= CTX:
                hi, lo = CTX - 1 - t0, CTX - t1
            else:
                hi, lo = (T + CTX - 1) - t0, (T + CTX) - t1
            return slice(hi, lo - 1 if lo > 0 else None, -1)
        import os
        glim = int(os.environ.get("S5_GLIM", "32"))
        tix = 0
        for c in range(4):
            for b in range(NB):
                P.dma("sp", ysb[b][0], qkvu[b, 1536 + c * 128:1536 + (c + 1) * 128, :], r=[b_qkvu], w=[ysb[b][1]])
                P.op("act", lambda e: e.copy(out=ub[b][0], in_=ysb[b][0]), r=[ysb[b][1]], w=[ub[b][1]])
                P.op("dve", lambda e: e.tensor_scalar(out=ysb[b][0], in0=ysb[b][0], scalar1=Dc[:, c:c + 1], scalar2=None, op0=ALU.mult),
                     r=[ysb[b][1], b_Dc], w=[ysb[b][1]])
            its = [(gl, d, b) for gl in range(8) if 8 * c + gl < glim for d in range(2) for b in range(NB)]
            nit = len(its)

            def info(t):
                gl, d, b = its[t]
                g = 8 * c + gl
                pr, m = gl // 2, gl % 2
                tb = tabs[(t // 2) % 2]
                tq = tmps5[t % 2]
                return g, d, b, pr, m, d * 32 + g, tb["C"], tb["S"], tq

            def S_tables(t):
                g, d, b, pr, m, dg, (tC, b_tC), (tS, b_tS), tq = info(t)
                for (tab, b_tab, shift, ph, b_ph) in ((tC, b_tC, math.pi / 2, ph1, b_ph1), (tS, b_tS, 0.0, ph2, b_ph2)):
                    P.op("act", lambda e: e.activation(out=ph, in_=iof, func=AF.Identity, scale=th[:, dg:dg + 1],
                                                       bias=kb.shiftc[shift][0][:, 0:1]), r=[b_iof, b_th, kb.shiftc[shift][1]], w=[b_ph])
                    P.op("act", lambda e: e.activation(out=tab, in_=ph, func=AF.Identity, scale=1.0 / TWO_PI,
                                                       bias=kb.shiftc[MAGIC][0][:, 0:1]), r=[b_ph, kb.shiftc[MAGIC][1]], w=[b_tab])
                    P.op("act", lambda e: e.activation(out=tab, in_=tab, func=AF.Identity, scale=1.0,
                                                       bias=kb.shiftc[-MAGIC][0][:, 0:1]), r=[b_tab, kb.shiftc[-MAGIC][1]], w=[b_tab])
                    P.op("dve", lambda e: e.scalar_tensor_tensor(out=ph, in0=tab, scalar=-TWO_PI, in1=ph, op0=ALU.mult, op1=ALU.add),
                         r=[b_tab, b_ph], w=[b_ph])
                    P.op("act", lambda e: e.activation(out=tab, in_=ph, func=AF.Sin), r=[b_ph], w=[b_tab])

            def S_A(t):
                g, d, b, pr, m, dg, (tC, b_tC), (tS, b_tS), tq = info(t)
                (bt, b_bt), (tmp, b_tmp) = tq["bt"], tq["tmp"]
                prs = slice(32 * pr, 32 * pr + 32)
                for pi, (t0, t1) in enumerate(pieces):
                    n = t1 - t0
                    psl = possl(d, t0, t1)
                    (pA1, b_pA1), (pA2, b_pA2) = pbk[(pi % 2) * 2], pbk[(pi % 2) * 2 + 1]
                    Wuse, b_Wuse, prs_ = (Wst, b_Wst, prs) if pr < 3 else (Wst3, b_Wst3, slice(64, 128))
                    P.op("pe", lambda e: e.matmul(pA1[:, 0:n], lhsT=Wuse[prs_, c, m, d, 0, :], rhs=ub[b][0][prs_, psl], start=True, stop=True),
                         r=[b_Wuse, ub[b][1]], w=[b_pA1])
                    P.op("pe", lambda e: e.matmul(pA2[:, 0:n], lhsT=Wuse[prs_, c, m, d, 1, :], rhs=ub[b][0][prs_, psl], start=True, stop=True),
                         r=[b_Wuse, ub[b][1]], w=[b_pA2])
                    P.op("dve", lambda e: e.tensor_tensor(out=bt[:, t0:t1], in0=pA1[:, 0:n], in1=tC[:, t0:t1], op=ALU.mult),
                         r=[b_pA1, b_tC], w=[b_bt])
                    P.op("dve", lambda e: e.tensor_tensor(out=tmp[:, t0:t1], in0=pA2[:, 0:n], in1=tS[:, t0:t1], op=ALU.mult),
                         r=[b_pA2, b_tS], w=[b_tmp])

            def S_BC(t):
                g, d, b, pr, m, dg, (tC, b_tC), (tS, b_tS), tq = info(t)
                (bt, b_bt), (tmp, b_tmp), (ww, b_ww) = tq["bt"], tq["tmp"], tq["ww"]
                P.op("pool", lambda e: e.tensor_tensor(out=bt, in0=bt, in1=tmp, op=ALU.add), r=[b_bt, b_tmp], w=[b_bt])
                P.op("dve", lambda e: e.tensor_tensor_scan(out=ww, data0=rr[:, dg:dg + 1].to_broadcast([128, T]), data1=bt,
                                                           initial=0.0, op0=ALU.mult, op1=ALU.add), r=[b_bt, b_rr], w=[b_ww])

            def S_D(t):
                g, d, b, pr, m, dg, (tC, b_tC), (tS, b_tS), tq = info(t)
                (ww, b_ww), (Q1, b_Q1), (Q2, b_Q2) = tq["ww"], tq["Q1"], tq["Q2"]
                P.op("pool", lambda e: e.tensor_tensor(out=Q1, in0=ww, in1=tC, op=ALU.mult), r=[b_ww, b_tC], w=[b_Q1])
                P.op("pool", lambda e: e.tensor_tensor(out=Q2, in0=ww, in1=tS, op=ALU.mult), r=[b_ww, b_tS], w=[b_Q2])

            def S_E(t):
                g, d, b, pr, m, dg, (tC, b_tC), (tS, b_tS), tq = info(t)
                (Q1, b_Q1), (Q2, b_Q2) = tq["Q1"], tq["Q2"]
                for pi, (t0, t1) in enumerate(pieces):
                    n = t1 - t0
                    psl = possl(d, t0, t1)
                    (pY, b_pY) = pbk[4 + pi % 3]
                    P.op("pe", lambda e: e.matmul(pY[:, 0:n], lhsT=CrPad[:, g, 0, :], rhs=Q1[:, t0:t1], start=True, stop=False),
                         r=[b_CrPad, b_Q1], w=[b_pY])
                    P.op("pe", lambda e: e.matmul(pY[:, 0:n], lhsT=CrPad[:, g, 1, :], rhs=Q2[:, t0:t1], start=False, stop=True),
                         r=[b_CrPad, b_Q2], w=[b_pY])
                    yv = ysb[b][0][:, psl]
                    P.op("dve", lambda e: e.tensor_tensor(out=yv, in0=pY[:, 0:n], in1=yv, op=ALU.add),
                         r=[b_pY, ysb[b][1]], w=[ysb[b][1]])

            for step in range(nit + 3):
                if step < nit and step % 2 == 0:
                    S_tables(step)
                if step < nit:
                    S_A(step)
                if 0 <= step - 1 < nit:
                    S_BC(step - 1)
                if 0 <= step - 2 < nit:
                    S_D(step - 2)
                if 0 <= step - 3 < nit:
                    S_E(step - 3)
            for b in range(NB):
                P.op("act", lambda e: e.activation(out=ysb[b][0], in_=ysb[b][0], func=AF.Gelu), r=[ysb[b][1]], w=[ysb[b][1]])
                P.dma("sp", ygT[b, c * 128:(c + 1) * 128, :], ysb[b][0], r=[ysb[b][1]], w=[b_ygT])


def load_gate_rows(kb, li, off):
    P = kb.P
    modd, b_modd = kb.dr["modd%d" % li]
    out = []
    for c in range(3):
        g, b_g = kb.sb([128, 1024], F32, "gB")
        P.dma("sp", g, modd[c, off:off + 1024].partition_broadcast(128), r=[b_modd], w=[b_g])
        out.append((g, b_g))
    return out


def load_wchunks_bf16(kb, dst, b_dst, src2d, nk, cols, stage, engines=("pool",)):
    P = kb.P
    for k in range(nk):
        st, b_st = stage[kb.uid % len(stage)]
        kb.uid += 1
        P.dma("sp", st[:, 0:cols], src2d[k * 128:(k + 1) * 128, :], w=[b_st])
        eng = engines[k % len(engines)]
        if eng == "act":
            P.op("act", lambda e: e.copy(out=dst[:, k, :], in_=st[:, 0:cols]), r=[b_st], w=[b_dst])
        else:
            P.op(eng, lambda e: e.tensor_copy(out=dst[:, k, :], in_=st[:, 0:cols]), r=[b_st], w=[b_dst])


def phase_l0_out(kb, IN):
    P = kb.P
    xs, b_xs = kb.dr["xs"]
    mixT, b_mixT = kb.dr["mixT"]
    ygT, b_ygT = kb.dr["ygT"]
    with kb.phase():
        gB = load_gate_rows(kb, 0, 2048)
        stage = [kb.sb([128, 1024], F32, "wst") for _ in range(2)]
        Wglu, b_Wglu = kb.sb([128, 4, 512], BF16, "wglu")
        load_wchunks_bf16(kb, Wglu, b_Wglu, IN["s5_glu_w"][0], 4, 512, stage, ("pool", "act", "dve"))
        Wout, b_Wout = kb.sb([128, 8, 1024], BF16, "wout")
        load_wchunks_bf16(kb, Wout, b_Wout, IN["ab_w_out"][0], 8, 1024, stage, ("pool", "act", "dve"))
        pgl = [kb.ps([128, 512], F32, "pgl") for _ in range(2)]
        pdd = [kb.ps([128, 512], F32, "pdd") for _ in range(2)]
        glub, b_glub = kb.sb([128, 4], F32, "glub")
        kb.load_vec_fm(IN["s5_glu_b"][0], 4, glub, b_glub, pgl[0])
        ygs = [kb.sb([128, 4, 256], F32, "yg32") for _ in range(2)]
        ygbs = [kb.sb([128, 4, 256], BF16, "ygb") for _ in range(2)]
        a32s = [kb.sb([128, 4, 256], F32, "a32") for _ in range(2)]
        mixbs = [kb.sb([128, 8, 256], BF16, "mixb") for _ in range(2)]
        sigs = [kb.sb([128, 256], F32, "sig") for _ in range(2)]
        xts = [kb.sb([128, 1024], F32, "xt") for _ in range(2)]
        tms = [kb.sb([128, 512], F32, "tm") for _ in range(2)]
        ui = 0
        for b in range(NB):
            for un in range(T // 256):
                tsl = slice(un * 256, (un + 1) * 256)
                (yg, b_yg), (ygb, b_ygb), (a32, b_a32), (mixb, b_mixb) = ygs[ui % 2], ygbs[ui % 2], a32s[ui % 2], mixbs[ui % 2]
                ui += 1
                P.dma("sp", yg, ygT[b, :, tsl].rearrange("(c p) t -> p c t", p=128), r=[b_ygT], w=[b_yg])
                P.dma("sp", a32, mixT[b, 0:512, tsl].rearrange("(c p) t -> p c t", p=128), r=[b_mixT], w=[b_a32])
                P.op("act", lambda e: e.copy(out=ygb, in_=yg), r=[b_yg], w=[b_ygb])
                P.op("pool", lambda e: e.tensor_copy(out=mixb[:, 0:4, :], in_=a32), r=[b_a32], w=[b_mixb])
                for f in range(4):
                    ps, b_ps = pgl[f % 2]
                    sg, b_sg = sigs[f % 2]
                    for k in range(4):
                        P.op("pe", lambda e: e.matmul(ps[:, 0:256], lhsT=Wglu[:, k, f * 128:(f + 1) * 128], rhs=ygb[:, k, :],
                                                      start=(k == 0), stop=(k == 3)), r=[b_Wglu, b_ygb], w=[b_ps])
                    P.op("act", lambda e: e.activation(out=sg, in_=ps[:, 0:256], func=AF.Sigmoid, bias=glub[:, f:f + 1]),
                         r=[b_ps, b_glub], w=[b_sg])
                    P.op("dve", lambda e: e.tensor_tensor(out=mixb[:, 4 + f, :], in0=yg[:, f, :], in1=sg, op=ALU.mult),
                         r=[b_yg, b_sg], w=[b_mixb])
                for tt in range(2):
                    tok0 = un * 256 + tt * 128
                    cond = 2 if tok0 < CTX else b
                    xt, b_xt = xts[tt]
                    P.dma("sp", xt, xs[b, tok0:tok0 + 128, :], r=[b_xs], w=[b_xt])
                    for half in range(2):
                        pd, b_pd = pdd[half]
                        tm, b_tm = tms[half]
                        hs = slice(half * 512, (half + 1) * 512)
                        for k in range(8):
                            P.op("pe", lambda e: e.matmul(pd, lhsT=mixb[:, k, tt * 128:(tt + 1) * 128], rhs=Wout[:, k, hs],
                                                          start=(k == 0), stop=(k == 7)), r=[b_mixb, b_Wout], w=[b_pd])
                        P.op("dve", lambda e: e.tensor_tensor(out=tm, in0=pd, in1=gB[cond][0][:, hs], op=ALU.mult),
                             r=[b_pd, gB[cond][1]], w=[b_tm])
                        P.op("pool", lambda e: e.tensor_tensor(out=xt[:, hs], in0=xt[:, hs], in1=tm, op=ALU.add),
                             r=[b_xt, b_tm], w=[b_xt])
                    P.dma("sp", xs[b, tok0:tok0 + 128, :], xt, r=[b_xt], w=[b_xs])


def phase_moe(kb, IN, li, tok_lo):
    P = kb.P
    xs, b_xs = kb.dr["xs"]
    ident, b_ident = kb.ident, kb.b_ident
    ntile_all = (T - tok_lo) // 128
    nblk = int(_os.environ.get("MOE_NBLK", "1"))
    tiles_per_blk = ntile_all // nblk
    TB = tiles_per_blk * 128
    with kb.phase():
        modF, b_modF, _, (A2, b_A2) = load_mod_fm(kb, li, IN, ("norm1_g", "norm2_g"))
        rw, b_rw = kb.sb([128, 8, 16], F32, "rw")
        P.dma("sp", rw, IN["router_w"].rearrange("(k p) e -> p k e", p=128), w=[b_rw])
        rb, b_rb = kb.sb([128, 16], F32, "rb")
        P.dma("sp", rb, IN["router_bias"].partition_broadcast(128), w=[b_rb])
        Esel, b_Esel = kb.sb([16, 16, 128], F32, "Esel")
        P.op("dve", lambda e: e.tensor_copy(out=Esel, in_=ident[0:16, 0:16].unsqueeze(2).to_broadcast([16, 16, 128])),
             r=[b_ident], w=[b_Esel])
        hT, b_hT = kb.sb([128, 8, TB], BF16, "hTm")
        gT, b_gT = kb.sb([16, TB], F32, "gT")
        import os
        elim = int(os.environ.get("MOE_ELIM", "16"))
        for b in range(NB):
            for blk in range(nblk):
                base = tok_lo + blk * TB
                with kb.phase():
                    GB = 3 if tiles_per_blk % 3 == 0 else 4
                    nmb = make_nm_bufs(kb, GB, 4)
                    h32s = [kb.sb([128, 8, 128], F32, "h32") for _ in range(GB)]
                    plog, b_plog = kb.ps([128, 512], F32, "plog")
                    pgT, b_pgT = kb.ps([128, 512], F32, "pgT")
                    nt_ = tiles_per_blk
                    h32all, b_h32all = kb.sb([128, 8, GB * 128], F32, "h32all")
                    for j0 in range(0, tiles_per_blk, GB):
                        toks_ = [base + (j0 + i) * 128 for i in range(GB)]
                        norm_mod_batch(kb, xs[b], b_xs, toks_, [2 if t_ < CTX else b for t_ in toks_], A2, b_A2, modF, b_modF, 24,
                                       nmb, [(h32all, b_h32all, lambda i: i * 128)])
                        P.op("pool", lambda e: e.tensor_copy(out=hT[:, :, j0 * 128:(j0 + GB) * 128], in_=h32all), r=[b_h32all], w=[b_hT])
                        for i in range(GB):
                            j = j0 + i
                            for k in range(8):
                                P.op("pe", lambda e: e.matmul(plog[:, j * 16:(j + 1) * 16], lhsT=h32all[:, k, i * 128:(i + 1) * 128], rhs=rw[:, k, :],
                                                              start=(k == 0), stop=(k == 7)), r=[b_h32all, b_rw], w=[b_plog])
                    def T3(nm, inner=16):
                        return kb.sb([128, nt_, inner], F32, nm)
                    (sc, b_sc), (ch, b_ch), (em, b_em), (cm, b_cm), (sel, b_sel) = T3("sc"), T3("ch"), T3("em"), T3("cm"), T3("sel")
                    (q4, b_q4) = kb.sb([128, 4, nt_, 4], F32, "q4")
                    (r4, b_r4) = kb.sb([128, 4, nt_, 4], F32, "r4")
                    (gs, b_gs), (gm, b_gm) = T3("gs", 4), T3("gm", 4)
                    (col, b_col) = kb.sb([128, 6, nt_], F32, "col")

                    def tt(out, i0, i1, op, rr, ww):
                        P.op("dve", lambda e: e.tensor_tensor(out=out, in0=i0, in1=i1, op=op), r=rr, w=ww)
                    P.op("act", lambda e: e.activation(out=sc, in_=plog[:, 0:nt_ * 16].rearrange("p (j e) -> p j e", e=16), func=AF.Sigmoid),
                         r=[b_plog], w=[b_sc])
                    tt(ch, sc, rb.unsqueeze(1).to_broadcast([128, nt_, 16]), ALU.add, [b_sc, b_rb], [b_ch])
                    ch4 = ch.rearrange("p j (g e) -> p j g e", e=4)
                    a_, b_, c_, d_ = ch4[:, :, :, 0], ch4[:, :, :, 1], ch4[:, :, :, 2], ch4[:, :, :, 3]
                    tt(q4[:, 0], a_, b_, ALU.max, [b_ch], [b_q4])
                    tt(q4[:, 1], a_, b_, ALU.min, [b_ch], [b_q4])
                    tt(q4[:, 2], c_, d_, ALU.max, [b_ch], [b_q4])
                    tt(q4[:, 3], c_, d_, ALU.min, [b_ch], [b_q4])
                    tt(r4[:, 0], q4[:, 0], q4[:, 2], ALU.max, [b_q4], [b_r4])
                    tt(r4[:, 1], q4[:, 0], q4[:, 2], ALU.min, [b_q4], [b_r4])
                    tt(r4[:, 2], q4[:, 1], q4[:, 3], ALU.max, [b_q4], [b_r4])
                    tt(r4[:, 3], r4[:, 1], r4[:, 2], ALU.max, [b_r4], [b_r4])
                    tt(gs, r4[:, 0], r4[:, 3], ALU.add, [b_r4], [b_gs])
                    P.op("dve", lambda e: e.tensor_reduce(out=col[:, 0, :], in_=gs, axis=AX.X, op=ALU.max), r=[b_gs], w=[b_col])
                    tt(gm, gs, col[:, 0, :].unsqueeze(2).to_broadcast([128, nt_, 4]), ALU.is_ge, [b_gs, b_col], [b_gm])
                    em4 = em.rearrange("p j (g e) -> p j g e", e=4)
                    P.op("dve", lambda e: e.tensor_copy(out=em4, in_=gm.unsqueeze(3).to_broadcast([128, nt_, 4, 4])), r=[b_gm], w=[b_em])
                    tt(cm, ch, em, ALU.mult, [b_ch, b_em], [b_cm])
                    P.op("dve", lambda e: e.tensor_scalar(out=em, in0=em, scalar1=BIG, scalar2=-BIG, op0=ALU.mult, op1=ALU.add), r=[b_em], w=[b_em])
                    tt(cm, cm, em, ALU.add, [b_cm, b_em], [b_cm])
                    P.op("dve", lambda e: e.tensor_reduce(out=col[:, 1, :], in_=cm, axis=AX.X, op=ALU.max), r=[b_cm], w=[b_col])
                    tt(sel, cm, col[:, 1, :].unsqueeze(2).to_broadcast([128, nt_, 16]), ALU.is_ge, [b_cm, b_col], [b_sel])
                    P.op("dve", lambda e: e.scalar_tensor_tensor(out=cm, in0=sel, scalar=-4.0 * BIG, in1=cm, op0=ALU.mult, op1=ALU.add),
                         r=[b_sel, b_cm], w=[b_cm])
                    P.op("dve", lambda e: e.tensor_reduce(out=col[:, 2, :], in_=cm, axis=AX.X, op=ALU.max), r=[b_cm], w=[b_col])
                    tt(em, cm, col[:, 2, :].unsqueeze(2).to_broadcast([128, nt_, 16]), ALU.is_ge, [b_cm, b_col], [b_em])
                    tt(sel, sel, em, ALU.add, [b_sel, b_em], [b_sel])
                    tt(sel, sel, sc, ALU.mult, [b_sel, b_sc], [b_sel])
                    P.op("dve", lambda e: e.tensor_reduce(out=col[:, 3, :], in_=sel, axis=AX.X, op=ALU.add), r=[b_sel], w=[b_col])
                    P.op("dve", lambda e: e.reciprocal(out=col[:, 4, :], in_=col[:, 3, :]), r=[b_col], w=[b_col])
                    tt(sel, sel, col[:, 4, :].unsqueeze(2).to_broadcast([128, nt_, 16]), ALU.mult, [b_sel, b_col], [b_sel])
                    for j0 in range(0, nt_, 4):
                        nj = min(4, nt_ - j0)
                        for jj in range(nj):
                            P.op("pe", lambda e: e.transpose(out=pgT[0:16, jj * 128:(jj + 1) * 128], in_=sel[:, j0 + jj, :], identity=ident),
                                 r=[b_sel, b_ident], w=[b_pgT])
                        P.op("act", lambda e: e.copy(out=gT[:, j0 * 128:(j0 + nj) * 128], in_=pgT[0:16, 0:nj * 128]), r=[b_pgT], w=[b_gT])
                with kb.phase():
                  acc, b_acc = kb.sb([128, tiles_per_blk, 1024], F32, "acc")
                  with kb.phase():
                    stage = [kb.sb([128, 1024], F32, "mst") for _ in range(3)]
                    Wg = [kb.sb([128, 8, 512], BF16, "Wg") for _ in range(2)]
                    Wu = [kb.sb([128, 8, 512], BF16, "Wu") for _ in range(2)]
                    Wd = [kb.sb([128, 4, 1024], BF16, "Wd") for _ in range(2)]
                    pg = [kb.ps([128, 512], F32, "pg") for _ in range(2)]
                    pu = [kb.ps([128, 512], F32, "pu") for _ in range(2)]
                    pgb = kb.ps([128, 512], F32, "pgb")
                    pd = [kb.ps([128, 512], F32, "pd") for _ in range(2)]
                    actT = [kb.sb([128, 4, 512], BF16, "actT") for _ in range(2)]
                    sgs = [kb.sb([128, 512], F32, "sg") for _ in range(2)]
                    tms = [kb.sb([128, 512], F32, "tmm") for _ in range(2)]
                    pieces = [(t0, min(TB, t0 + 512)) for t0 in range(0, TB, 512)]
                    def gateup(ex, pi, aTb):
                        (wg, b_wg), (wu, b_wu) = Wg[ex % 2], Wu[ex % 2]
                        t0, t1 = pieces[pi]
                        n = t1 - t0
                        aT, b_aT = aTb
                        P.op("pe", lambda e: e.matmul(pgb[0][:, 0:n], lhsT=Esel[:, ex, :], rhs=gT[:, t0:t1], start=True, stop=True),
                             r=[b_Esel, b_gT], w=[pgb[1]])
                        for f in range(4):
                            (g_, b_g), (u_, b_u) = pg[f % 2], pu[f % 2]
                            sg, b_sg = sgs[f % 2]
                            tm, b_tm = tms[f % 2]
                            fs = slice(f * 128, (f + 1) * 128)
                            for k in range(8):
                                P.op("pe", lambda e: e.matmul(g_[:, 0:n], lhsT=wg[:, k, fs], rhs=hT[:, k, t0:t1], start=(k == 0), stop=(k == 7)),
                                     r=[b_wg, b_hT], w=[b_g])
                            for k in range(8):
                                P.op("pe", lambda e: e.matmul(u_[:, 0:n], lhsT=wu[:, k, fs], rhs=hT[:, k, t0:t1], start=(k == 0), stop=(k == 7)),
                                     r=[b_wu, b_hT], w=[b_u])
                            P.op("act", lambda e: e.activation(out=sg[:, 0:n], in_=g_[:, 0:n], func=AF.Silu), r=[b_g], w=[b_sg])
                            P.op("dve", lambda e: e.tensor_tensor(out=tm[:, 0:n], in0=u_[:, 0:n], in1=sg[:, 0:n], op=ALU.mult), r=[b_u, b_sg], w=[b_tm])
                            P.op("dve", lambda e: e.tensor_tensor(out=aT[:, f, 0:n], in0=pgb[0][:, 0:n], in1=tm[:, 0:n], op=ALU.mult),
                                 r=[pgb[1], b_tm], w=[b_aT])

                    def down(ex, pi, aTb):
                        (wd, b_wd) = Wd[ex % 2]
                        t0, t1 = pieces[pi]
                        n = t1 - t0
                        aT, b_aT = aTb
                        for tt_ in range(n // 128):
                            j = t0 // 128 + tt_
                            for half in range(2):
                                d_, b_d = pd[half]
                                hs = slice(half * 512, (half + 1) * 512)
                                for f in range(4):
                                    P.op("pe", lambda e: e.matmul(d_, lhsT=aT[:, f, tt_ * 128:(tt_ + 1) * 128], rhs=wd[:, f, hs],
                                                                  start=(f == 0), stop=(f == 3)), r=[b_aT, b_wd], w=[b_d])
                                if ex == 0:
                                    P.op("act", lambda e: e.copy(out=acc[:, j, hs], in_=d_), r=[b_d], w=[b_acc])
                                else:
                                    P.op("dve", lambda e: e.tensor_tensor(out=acc[:, j, hs], in0=d_, in1=acc[:, j, hs], op=ALU.add),
                                         r=[b_d, b_acc], w=[b_acc])

                    items = [(ex, pi) for ex in range(elim) for pi in range(len(pieces))]
                    prev = None
                    for idx, (ex, pi) in enumerate(items):
                        if pi == 0:
                            load_wchunks_bf16(kb, Wg[ex % 2][0], Wg[ex % 2][1], IN["moe_w_gate"][li, ex], 8, 512, stage)
                            load_wchunks_bf16(kb, Wu[ex % 2][0], Wu[ex % 2][1], IN["moe_w_up"][li, ex], 8, 512, stage)
                            load_wchunks_bf16(kb, Wd[ex % 2][0], Wd[ex % 2][1], IN["moe_w_down"][li, ex], 4, 1024, stage)
                        gateup(ex, pi, actT[idx % 2])
                        if prev is not None:
                            down(prev[0], prev[1], actT[(idx - 1) % 2])
                        prev = (ex, pi)
                    down(prev[0], prev[1], actT[(len(items) - 1) % 2])
                  with kb.phase():
                    gB = load_gate_rows(kb, li, 5 * 1024)
                    xts = [kb.sb([128, 1024], F32, "xtm") for _ in range(2)]
                    for j in range(tiles_per_blk):
                        tok0 = base + j * 128
                        cond = 2 if tok0 < CTX else b
                        xt, b_xt = xts[j % 2]
                        P.dma("sp", xt, xs[b, tok0:tok0 + 128, :], r=[b_xs], w=[b_xt])
                        P.op("dve", lambda e: e.tensor_tensor(out=acc[:, j, :], in0=acc[:, j, :], in1=gB[cond][0], op=ALU.mult),
                             r=[b_acc, gB[cond][1]], w=[b_acc])
                        P.op("dve" if j % 2 == 0 else "pool", lambda e: e.tensor_tensor(out=xt, in0=xt, in1=acc[:, j, :], op=ALU.add),
                             r=[b_xt, b_acc], w=[b_xt])
                        P.dma("sp", xs[b, tok0:tok0 + 128, :], xt, r=[b_xt], w=[b_xs])


MLA_SCALE = 192.0 ** -0.5
I32 = mybir.dt.int32


def range_reduce_sin(kb, eng, out, b_out, ang, b_ang, shift, tmp, b_tmp):
    P = kb.P
    P.op(eng, lambda e: e.tensor_scalar(out=tmp, in0=ang, scalar1=shift, scalar2=1.0 / TWO_PI, op0=ALU.add, op1=ALU.mult), r=[b_ang], w=[b_tmp])
    P.op(eng, lambda e: e.tensor_scalar(out=tmp, in0=tmp, scalar1=MAGIC, scalar2=None, op0=ALU.add), r=[b_tmp], w=[b_tmp])
    P.op(eng, lambda e: e.tensor_scalar(out=tmp, in0=tmp, scalar1=-MAGIC, scalar2=-TWO_PI, op0=ALU.add, op1=ALU.mult), r=[b_tmp], w=[b_tmp])
    P.op(eng, lambda e: e.tensor_tensor(out=tmp, in0=tmp, in1=ang, op=ALU.add), r=[b_tmp, b_ang], w=[b_tmp])
    P.op("act", lambda e: e.activation(out=out, in_=tmp, func=AF.Sin, bias=kb.shiftc[shift][0][0:out.shape[0], 0:1]), r=[b_tmp, kb.shiftc[shift][1]], w=[b_out])


def phase_l1_mla(kb, IN):
    P = kb.P
    xs, b_xs = kb.dr["xs"]
    attnT, b_attnT = kb.dr["attnT"]
    ident, b_ident, identb, b_identb, ones, b_ones = kb.ident, kb.b_ident, kb.identb, kb.b_identb, kb.ones, kb.b_ones
    with kb.phase():
        modF, b_modF, (A1, b_A1), _ = load_mod_fm(kb, 1, IN, ("norm1_g", "norm2_g"))
        pmisc, b_pmisc = kb.ps([128, 512], F32, "pmisc")
        Win, b_Win = kb.sb([128, 8, 768], BF16, "Win")
        Wq, b_Wq = kb.sb([128, 3, 2048], BF16, "Wq")
        Wkv, b_Wkv = kb.sb([128, 2, 2048], BF16, "Wkv")
        cosT, b_cosT = kb.sb([64, SEQ], F32, "cosT")
        sinT, b_sinT = kb.sb([64, SEQ], F32, "sinT")
        rc, b_rc = kb.sb([64, 4], F32, "rc")
        fr_i, b_fr_i = kb.sb([1, 128], I32, "fri")
        fr_f, b_fr_f = kb.sb([1, 128], F32, "frf")
        gq, b_gq = kb.sb([128, 3], F32, "gq")
        gkv, b_gkv = kb.sb([128, 2], F32, "gkv")
        epsc, b_epsc = kb.sb([128, 1], F32, "epsc")
        _scope = kb.phase()
        _scope.__enter__()
        stage = [kb.sb([128, 2048], F32, "mst") for _ in range(2)]
        for k in range(8):
            st, b_st = stage[k % 2]
            P.dma("sp", st[:, 0:704], IN["mla_w_in"][0, k * 128:(k + 1) * 128, :], w=[b_st])
            P.op("pool", lambda e: e.tensor_copy(out=Win[:, k, 0:704], in_=st[:, 0:704]), r=[b_st], w=[b_Win])
            P.op("pool", lambda e: e.tensor_scalar(out=Win[:, k, 704:736], in0=st[:, 672:704], scalar1=-1.0, scalar2=None, op0=ALU.mult), r=[b_st], w=[b_Win])
            P.op("pool", lambda e: e.tensor_copy(out=Win[:, k, 736:768], in_=st[:, 640:672]), r=[b_st], w=[b_Win])
        for k in range(3):
            st, b_st = stage[k % 2]
            P.dma("sp", st[:, 0:1536], IN["mla_w_q_up"][0, k * 128:(k + 1) * 128, :], w=[b_st])
            P.op("pool", lambda e: e.tensor_copy(out=Wq[:, k, 0:1536], in_=st[:, 0:1536]), r=[b_st], w=[b_Wq])
            for h in range(8):
                P.op("pool", lambda e: e.tensor_scalar(out=Wq[:, k, 1536 + h * 64:1536 + h * 64 + 32], in0=st[:, h * 192 + 160:h * 192 + 192],
                                                       scalar1=-1.0, scalar2=None, op0=ALU.mult), r=[b_st], w=[b_Wq])
                P.op("pool", lambda e: e.tensor_copy(out=Wq[:, k, 1536 + h * 64 + 32:1536 + h * 64 + 64], in_=st[:, h * 192 + 128:h * 192 + 160]),
                     r=[b_st], w=[b_Wq])
        load_wchunks_bf16(kb, Wkv, b_Wkv, IN["mla_w_kv_up"][0], 2, 2048, stage)
        kb.load_vec_fm(IN["mla_q_norm_g"][0], 3, gq, b_gq, (pmisc, b_pmisc))
        kb.load_vec_fm(IN["mla_kv_norm_g"][0], 2, gkv, b_gkv, (pmisc, b_pmisc))
        P.op("pool", lambda e: e.memset(epsc, EPS), w=[b_epsc])
        P.op("pool", lambda e: e.iota(fr_i[:, 0:64], pattern=[[0, 2], [0, 2], [1, 16]], base=0, channel_multiplier=0), w=[b_fr_i])
        P.op("pool", lambda e: e.iota(fr_i[:, 64:128], pattern=[[0, 2], [1, 2], [0, 16]], base=0, channel_multiplier=0), r=[b_fr_i], w=[b_fr_i])
        P.op("dve", lambda e: e.tensor_copy(out=fr_f, in_=fr_i), r=[b_fr_i], w=[b_fr_f])
        P.op("pe", lambda e: e.transpose(out=pmisc[0:64, 0:1], in_=fr_f[0:1, 0:64], identity=ident[0:1, 0:1]), r=[b_fr_f, b_ident], w=[b_pmisc])
        P.op("pe", lambda e: e.transpose(out=pmisc[0:64, 1:2], in_=fr_f[0:1, 64:128], identity=ident[0:1, 0:1]), r=[b_fr_f, b_ident], w=[b_pmisc])
        P.op("act", lambda e: e.activation(out=rc[:, 0:1], in_=pmisc[0:64, 0:1], func=AF.Exp, scale=-math.log(10000.0) / 16.0), r=[b_pmisc], w=[b_rc])
        P.op("dve", lambda e: e.tensor_scalar(out=rc[:, 1:2], in0=pmisc[0:64, 1:2], scalar1=-1.0, scalar2=1.0, op0=ALU.mult, op1=ALU.add), r=[b_pmisc], w=[b_rc])
        pos_i, b_pos_i = kb.sb([64, SEQ], I32, "posi")
        rowf, b_rowf = kb.sb([64, SEQ], F32, "rowf")
        colf, b_colf = kb.sb([64, SEQ], F32, "colf")
        P.op("pool", lambda e: e.iota(pos_i, pattern=[[1, 32], [0, 64]], base=0, channel_multiplier=0), w=[b_pos_i])
        P.op("dve", lambda e: e.tensor_copy(out=rowf, in_=pos_i), r=[b_pos_i], w=[b_rowf])
        P.op("pool", lambda e: e.iota(pos_i, pattern=[[0, 32], [1, 64]], base=0, channel_multiplier=0), r=[b_pos_i], w=[b_pos_i])
        P.op("dve", lambda e: e.tensor_copy(out=colf, in_=pos_i), r=[b_pos_i], w=[b_colf])
        P.op("dve", lambda e: e.tensor_tensor(out=rowf, in0=rowf, in1=colf, op=ALU.subtract), r=[b_rowf, b_colf], w=[b_rowf])
        P.op("dve", lambda e: e.scalar_tensor_tensor(out=rowf, in0=rowf, scalar=rc[:, 1:2], in1=colf, op0=ALU.mult, op1=ALU.add), r=[b_rowf, b_rc, b_colf], w=[b_rowf])
        P.op("dve", lambda e: e.tensor_scalar(out=rowf, in0=rowf, scalar1=rc[:, 0:1], scalar2=None, op0=ALU.mult), r=[b_rowf, b_rc], w=[b_rowf])
        range_reduce_sin(kb, "dve", cosT, b_cosT, rowf, b_rowf, math.pi / 2, colf, b_colf)
        range_reduce_sin(kb, "dve", sinT, b_sinT, rowf, b_rowf, 0.0, colf, b_colf)
        _scope.__exit__(None, None, None)
        cqn, b_cqn = kb.sb([128, 3, SEQ], BF16, "cqn")
        ckvn, b_ckvn = kb.sb([128, 2, T], BF16, "ckvn")
        krT, b_krT = kb.sb([64, T], BF16, "krT")
        nmb = make_nm_bufs(kb, 2, 1)
        hT, b_hT = kb.sb([128, 8, 512], BF16, "hT1")
        c32, b_c32 = kb.sb([128, 3, 512], F32, "c32")
        csq, b_csq = kb.sb([128, 3, 512], F32, "csq")
        rin, b_rin = kb.sb([128, 512], F32, "rin")
        kr1, b_kr1 = kb.sb([64, 512], F32, "kr1")
        kr2, b_kr2 = kb.sb([64, 512], F32, "kr2")
        pA, b_pA = kb.ps([128, 512], F32, "pA")
        pB, b_pB = kb.ps([128, 512], F32, "pB")
        knT, b_knT = kb.sb([128, T], BF16, "knT")
        Vh, b_Vh = kb.sb([128, NT, 128], BF16, "Vh")
        qnT, b_qnT = kb.sb([128, SEQ], BF16, "qnT")
        qrT, b_qrT = kb.sb([64, SEQ], BF16, "qrT")
        q32a, b_q32a = kb.sb([64, 512], F32, "q32a")
        q32b, b_q32b = kb.sb([64, 512], F32, "q32b")
        Pb = [kb.sb([128, T], BF16, "Pb") for _ in range(2)]
        PTs = [kb.sb([128, NT, 128], BF16, "PTs") for _ in range(2)]
        smx = [kb.sb([128, 16], F32, "smx") for _ in range(4)]
        o32 = [kb.sb([128, 128], F32, "o32") for _ in range(2)]
        aTs, b_aTs = kb.sb([128, SEQ], F32, "aTs")
        s1b = [(pA, b_pA), (pB, b_pB)] + [kb.ps([128, 512], F32, "s2b") for _ in range(1)]
        Ssb = [kb.sb([128, T], F32, "Ssb") for _ in range(2)]
        ptbs = []
        for _i in range(3):
            kb.uid += 1
            ptb_t = kb.stack.enter_context(kb.nc.psum_tensor("ptb_%d" % kb.uid, [128, 1024], BF16))
            ptbs.append((ptb_t.ap().rearrange("p (a b) -> p a b", b=128), Buf("ptb%d" % _i, excl=True)))
        po, b_po = pmisc, b_pmisc
        kpieces = [(t0, min(T, t0 + 512)) for t0 in range(0, T, 512)]
        import os
        hlim = int(os.environ.get("MLA_HLIM", "8"))
        qlim = int(os.environ.get("MLA_QLIM", "16"))

        def rms_scale(src_ps_list, nchunk, width, gcol, dst, b_dst, dcol0, dfeat):
            for c, (ps, b_ps) in enumerate(src_ps_list):
                P.op("act", lambda e: e.copy(out=c32[:, c, 0:width], in_=ps), r=[b_ps], w=[b_c32])
                P.op("act", lambda e: e.activation(out=csq[:, c, 0:width], in_=c32[:, c, 0:width], func=AF.Square), r=[b_c32], w=[b_csq])
            return

        for b in range(NB):
            units = [(0, CTX)] + [(CTX + 512 * q_, CTX + 512 * (q_ + 1)) for q_ in range(4)]
            pC_, b_pC = s1b[2]
            for (u0, u1) in units:
                W_ = u1 - u0
                for half in range(W_ // 256):
                    toks_ = [u0 + half * 256, u0 + half * 256 + 128]
                    norm_mod_batch(kb, xs[b], b_xs, toks_, [2 if t_ < CTX else b for t_ in toks_], A1, b_A1, modF, b_modF, 0,
                                   nmb, [(hT, b_hT, lambda i, half=half: half * 256 + i * 128)])
                tsl = slice(u0, u1)
                lat = u0 >= CTX
                lsl = slice(u0 - CTX, u1 - CTX)
                groups = ([("q", 0, 3, 384.0, gq, b_gq)] if lat else []) + [("kv", 3, 2, 256.0, gkv, b_gkv)]
                for (nm, c0, nch, dfeat, gcol, b_gcol) in groups:
                    for c in range(nch):
                        ps, b_ps = (pA, b_pA) if c % 2 == 0 else (pB, b_pB)
                        for k in range(8):
                            P.op("pe", lambda e: e.matmul(ps[:, 0:W_], lhsT=Win[:, k, (c0 + c) * 128:(c0 + c + 1) * 128], rhs=hT[:, k, 0:W_],
                                                          start=(k == 0), stop=(k == 7)), r=[b_Win, b_hT], w=[b_ps])
                        P.op("act", lambda e: e.copy(out=c32[:, c, 0:W_], in_=ps[:, 0:W_]), r=[b_ps], w=[b_c32])
                        P.op("act", lambda e: e.activation(out=csq[:, c, 0:W_], in_=c32[:, c, 0:W_], func=AF.Square), r=[b_c32], w=[b_csq])
                    for c in range(nch):
                        P.op("pe", lambda e: e.matmul(pC_[:, 0:W_], lhsT=ones, rhs=csq[:, c, 0:W_], start=(c == 0), stop=(c == nch - 1)),
                             r=[b_ones, b_csq], w=[b_pC])
                    P.op("dve", lambda e: e.tensor_scalar(out=rin[:, 0:W_], in0=pC_[:, 0:W_], scalar1=1.0 / dfeat, scalar2=EPS, op0=ALU.mult, op1=ALU.add),
                         r=[b_pC], w=[b_rin])
                    P.op("act", lambda e: e.activation(out=rin[:, 0:W_], in_=rin[:, 0:W_], func=AF.Sqrt), r=[b_rin], w=[b_rin])
                    P.op("dve", lambda e: e.reciprocal(out=rin[:, 0:W_], in_=rin[:, 0:W_]), r=[b_rin], w=[b_rin])
                    for c in range(nch):
                        if nm == "q":
                            dst, b_dst, dsl = cqn, b_cqn, lsl
                        else:
                            dst, b_dst, dsl = ckvn, b_ckvn, tsl
                        P.op("dve", lambda e: e.scalar_tensor_tensor(out=dst[:, c, dsl], in0=c32[:, c, 0:W_], scalar=gcol[:, c:c + 1], in1=rin[:, 0:W_],
                                                                     op0=ALU.mult, op1=ALU.mult), r=[b_c32, b_gcol, b_rin], w=[b_dst])
                for k in range(8):
                    P.op("pe", lambda e: e.matmul(pB[0:64, 0:W_], lhsT=Win[:, k, 640:704], rhs=hT[:, k, 0:W_], start=(k == 0), stop=(k == 7)),
                         r=[b_Win, b_hT], w=[b_pB])
                if not lat:
                    P.op("act", lambda e: e.copy(out=krT[:, tsl], in_=pB[0:64, 0:W_]), r=[b_pB], w=[b_krT])
                else:
                    for k in range(8):
                        P.op("pe", lambda e: e.matmul(pA[0:64, 0:W_], lhsT=Win[:, k, 704:768], rhs=hT[:, k, 0:W_], start=(k == 0), stop=(k == 7)),
                             r=[b_Win, b_hT], w=[b_pA])
                    P.op("dve", lambda e: e.tensor_tensor(out=kr1[:, 0:W_], in0=pB[0:64, 0:W_], in1=cosT[:, lsl], op=ALU.mult), r=[b_pB, b_cosT], w=[b_kr1])
                    P.op("dve", lambda e: e.tensor_tensor(out=kr2[:, 0:W_], in0=pA[0:64, 0:W_], in1=sinT[:, lsl], op=ALU.mult), r=[b_pA, b_sinT], w=[b_kr2])
                    P.op("pool", lambda e: e.tensor_tensor(out=krT[:, tsl], in0=kr1[:, 0:W_], in1=kr2[:, 0:W_], op=ALU.add), r=[b_kr1, b_kr2], w=[b_krT])
            for h in range(hlim):
                for pi, (t0, t1) in enumerate(kpieces):
                    n = t1 - t0
                    for k in range(2):
                        P.op("pe", lambda e: e.matmul(pA[:, 0:n], lhsT=Wkv[:, k, h * 256:h * 256 + 128], rhs=ckvn[:, k, t0:t1], start=(k == 0), stop=(k == 1)),
                             r=[b_Wkv, b_ckvn], w=[b_pA])
                    P.op("act", lambda e: e.copy(out=knT[:, t0:t1], in_=pA[:, 0:n]), r=[b_pA], w=[b_knT])
                for j0 in range(0, NT, 4):
                    nj = min(4, NT - j0)
                    for jj in range(nj):
                        j = j0 + jj
                        for k in range(2):
                            P.op("pe", lambda e: e.matmul(pB[:, jj * 128:(jj + 1) * 128], lhsT=ckvn[:, k, j * 128:(j + 1) * 128],
                                                          rhs=Wkv[:, k, h * 256 + 128:h * 256 + 256], start=(k == 0), stop=(k == 1)),
                                 r=[b_Wkv, b_ckvn], w=[b_pB])
                    P.op("dve", lambda e: e.tensor_copy(out=Vh[:, j0:j0 + nj, :], in_=pB[:, 0:nj * 128].rearrange("p (a b) -> p a b", b=128)),
                         r=[b_pB], w=[b_Vh])
                for qp in range(4):
                    qsl = slice(qp * 512, (qp + 1) * 512)
                    for k in range(3):
                        P.op("pe", lambda e: e.matmul(pA, lhsT=Wq[:, k, h * 192:h * 192 + 128], rhs=cqn[:, k, qsl], start=(k == 0), stop=(k == 2)),
                             r=[b_Wq, b_cqn], w=[b_pA])
                    P.op("act", lambda e: e.copy(out=qnT[:, qsl], in_=pA), r=[b_pA], w=[b_qnT])
                    for k in range(3):
                        P.op("pe", lambda e: e.matmul(pB[0:64, :], lhsT=Wq[:, k, h * 192 + 128:h * 192 + 192], rhs=cqn[:, k, qsl], start=(k == 0), stop=(k == 2)),
                             r=[b_Wq, b_cqn], w=[b_pB])
                    P.op("dve", lambda e: e.tensor_tensor(out=q32a, in0=pB[0:64, :], in1=cosT[:, qsl], op=ALU.mult), r=[b_pB, b_cosT], w=[b_q32a])
                    for k in range(3):
                        P.op("pe", lambda e: e.matmul(pB[0:64, :], lhsT=Wq[:, k, 1536 + h * 64:1536 + h * 64 + 64], rhs=cqn[:, k, qsl], start=(k == 0), stop=(k == 2)),
                             r=[b_Wq, b_cqn], w=[b_pB])
                    P.op("dve", lambda e: e.tensor_tensor(out=q32b, in0=pB[0:64, :], in1=sinT[:, qsl], op=ALU.mult), r=[b_pB, b_sinT], w=[b_q32b])
                    P.op("pool", lambda e: e.tensor_tensor(out=qrT[:, qsl], in0=q32a, in1=q32b, op=ALU.add), r=[b_q32a, b_q32b], w=[b_qrT])
                def S1(qt):
                    qs = slice(qt * 128, (qt + 1) * 128)
                    (sx, b_sx) = smx[qt % 4]
                    (ssb, b_ssb) = Ssb[qt % 2]
                    for pi, (t0, t1) in enumerate(kpieces):
                        n = t1 - t0
                        sc_, b_sc = s1b[(qt * 5 + pi) % 3]
                        P.op("pe", lambda e: e.matmul(sc_[:, 0:n], lhsT=qnT[:, qs], rhs=knT[:, t0:t1], start=True, stop=False),
                             r=[b_qnT, b_knT], w=[b_sc])
                        P.op("pe", lambda e: e.matmul(sc_[:, 0:n], lhsT=qrT[:, qs], rhs=krT[:, t0:t1], start=False, stop=True),
                             r=[b_qrT, b_krT], w=[b_sc])
                        if pi % 2 == 0:
                            P.op("act", lambda e: e.copy(out=ssb[:, t0:t1], in_=sc_[:, 0:n]), r=[b_sc], w=[b_ssb])
                        else:
                            P.op("dve", lambda e: e.tensor_copy(out=ssb[:, t0:t1], in_=sc_[:, 0:n]), r=[b_sc], w=[b_ssb])
                    P.op("dve", lambda e: e.tensor_reduce(out=sx[:, 5:6], in_=ssb, axis=AX.X, op=ALU.max), r=[b_ssb], w=[b_sx])
                    P.op("dve", lambda e: e.tensor_scalar(out=sx[:, 6:7], in0=sx[:, 5:6], scalar1=-MLA_SCALE, scalar2=None, op0=ALU.mult), r=[b_sx], w=[b_sx])

                def S2(qt):
                    (sx, b_sx) = smx[qt % 4]
                    (pb_, b_pb) = Pb[qt % 2]
                    (ssb, b_ssb) = Ssb[qt % 2]
                    P.op("act", lambda e: e.activation(out=pb_, in_=ssb, func=AF.Exp, scale=MLA_SCALE, bias=sx[:, 6:7],
                                                       accum_out=sx[:, 7:8]), r=[b_ssb, b_sx], w=[b_pb, b_sx])
                    P.op("dve", lambda e: e.reciprocal(out=sx[:, 13:14], in_=sx[:, 7:8]), r=[b_sx], w=[b_sx])

                def S3(qt):
                    (pb_, b_pb), (pts, b_pts) = Pb[qt % 2], PTs[qt % 2]
                    for gi, j0 in enumerate(range(0, NT, 8)):
                        nj = min(8, NT - j0)
                        ptb, b_ptb = ptbs[gi % 3]
                        for jj in range(nj):
                            j = j0 + jj
                            P.op("pe", lambda e: e.transpose(out=ptb[:, jj, :], in_=pb_[:, j * 128:(j + 1) * 128], identity=identb),
                                 r=[b_pb, b_identb], w=[b_ptb])
                        if gi % 2 == 0:
                            P.op("act", lambda e: e.copy(out=pts[:, j0:j0 + nj, :], in_=ptb[:, 0:nj, :]), r=[b_ptb], w=[b_pts])
                        else:
                            P.op("dve", lambda e: e.tensor_copy(out=pts[:, j0:j0 + nj, :], in_=ptb[:, 0:nj, :]), r=[b_ptb], w=[b_pts])

                def S4(qt):
                    qs = slice(qt * 128, (qt + 1) * 128)
                    (pts, b_pts), (sx, b_sx), (o3, b_o3) = PTs[qt % 2], smx[qt % 4], o32[qt % 2]
                    for j in range(NT):
                        P.op("pe", lambda e: e.matmul(po[:, 0:128], lhsT=pts[:, j, :], rhs=Vh[:, j, :], start=(j == 0), stop=(j == NT - 1)),
                             r=[b_pts, b_Vh], w=[b_po])
                    P.op("act", lambda e: e.activation(out=o3, in_=po[:, 0:128], func=AF.Copy, scale=sx[:, 13:14]), r=[b_po, b_sx], w=[b_o3])
                    P.op("pe", lambda e: e.transpose(out=po[:, 128:256], in_=o3, identity=ident), r=[b_o3, b_ident], w=[b_po])
                    P.op("dve", lambda e: e.tensor_copy(out=aTs[:, qs], in_=po[:, 128:256]), r=[b_po], w=[b_aTs])

                for t in range(qlim + 3):
                    if t < qlim:
                        S1(t)
                    if 0 <= t - 2 < qlim:
                        S3(t - 2)
                    if 0 <= t - 3 < qlim:
                        S4(t - 3)
                    if 0 <= t - 1 < qlim:
                        S2(t - 1)
                P.dma("sp", attnT[b, h * 128:(h + 1) * 128, :], aTs, r=[b_aTs], w=[b_attnT])


def phase_proj_res(kb, IN, li, src_name, wsrc, gate_off, tok_lo):
    P = kb.P
    xs, b_xs = kb.dr["xs"]
    srcT, b_srcT = kb.dr[src_name]
    ntok = T - tok_lo
    with kb.phase():
        gB = load_gate_rows(kb, li, gate_off)
        stage = [kb.sb([128, 1024], F32, "wst") for _ in range(2)]
        W, b_W = kb.sb([128, 8, 1024], BF16, "wout")
        load_wchunks_bf16(kb, W, b_W, wsrc, 8, 1024, stage, ("pool", "act", "dve"))
        a32s = [kb.sb([128, 8, 256], F32, "a32") for _ in range(2)]
        mixbs = [kb.sb([128, 8, 256], BF16, "mixb") for _ in range(2)]
        pdd = [kb.ps([128, 512], F32, "pdd") for _ in range(2)]
        xts = [kb.sb([128, 1024], F32, "xt") for _ in range(2)]
        tms = [kb.sb([128, 512], F32, "tm") for _ in range(2)]
        ui = 0
        for b in range(NB):
            for un in range(ntok // 256):
                tsl = slice(un * 256, (un + 1) * 256)
                (a32, b_a32), (mixb, b_mixb) = a32s[ui % 2], mixbs[ui % 2]
                ui += 1
                P.dma("sp", a32, srcT[b, :, tsl].rearrange("(c p) t -> p c t", p=128), r=[b_srcT], w=[b_a32])
                P.op("act", lambda e: e.copy(out=mixb, in_=a32), r=[b_a32], w=[b_mixb])
                for tt in range(2):
                    tok0 = tok_lo + un * 256 + tt * 128
                    cond = 2 if tok0 < CTX else b
                    xt, b_xt = xts[tt]
                    P.dma("sp", xt, xs[b, tok0:tok0 + 128, :], r=[b_xs], w=[b_xt])
                    for half in range(2):
                        pd, b_pd = pdd[half]
                        tm, b_tm = tms[half]
                        hs = slice(half * 512, (half + 1) * 512)
                        for k in range(8):
                            P.op("pe", lambda e: e.matmul(pd, lhsT=mixb[:, k, tt * 128:(tt + 1) * 128], rhs=W[:, k, hs],
                                                          start=(k == 0), stop=(k == 7)), r=[b_mixb, b_W], w=[b_pd])
                        P.op("dve", lambda e: e.tensor_tensor(out=tm, in0=pd, in1=gB[cond][0][:, hs], op=ALU.mult),
                             r=[b_pd, gB[cond][1]], w=[b_tm])
                        P.op("pool", lambda e: e.tensor_tensor(out=xt[:, hs], in0=xt[:, hs], in1=tm, op=ALU.add),
                             r=[b_xt, b_tm], w=[b_xt])
                    P.dma("sp", xs[b, tok0:tok0 + 128, :], xt, r=[b_xt], w=[b_xs])


def phase_final(kb, IN, out_ap, b_out):
    P = kb.P
    xs, b_xs = kb.dr["xs"]
    with kb.phase():
        gF, b_gF = kb.sb([128, 1024], F32, "gF")
        P.dma("sp", gF, IN["final_norm_g"].partition_broadcast(128), w=[b_gF])
        xts = [kb.sb([128, 1024], F32, "xt") for _ in range(2)]
        sqs = [kb.sb([128, 1024], F32, "sq") for _ in range(2)]
        sss = [kb.sb([128, 4], F32, "ss") for _ in range(2)]
        it = 0
        for b in range(NB):
            for j in range(SEQ // 128):
                (xt, b_xt), (sq, b_sq), (ss, b_ss) = xts[it % 2], sqs[it % 2], sss[it % 2]
                it += 1
                P.dma("sp", xt, xs[b, CTX + j * 128:CTX + (j + 1) * 128, :], r=[b_xs], w=[b_xt])
                P.op("act", lambda e: e.activation(out=sq, in_=xt, func=AF.Square, accum_out=ss[:, 0:1]), r=[b_xt], w=[b_sq, b_ss])
                P.op("dve", lambda e: e.tensor_scalar(out=ss[:, 1:2], in0=ss[:, 0:1], scalar1=1.0 / D, scalar2=EPS, op0=ALU.mult, op1=ALU.add), r=[b_ss], w=[b_ss])
                P.op("act", lambda e: e.activation(out=ss[:, 2:3], in_=ss[:, 1:2], func=AF.Sqrt), r=[b_ss], w=[b_ss])
                P.op("dve", lambda e: e.reciprocal(out=ss[:, 3:4], in_=ss[:, 2:3]), r=[b_ss], w=[b_ss])
                P.op("dve", lambda e: e.scalar_tensor_tensor(out=sq, in0=xt, scalar=ss[:, 3:4], in1=gF, op0=ALU.mult, op1=ALU.mult),
                     r=[b_xt, b_ss, b_gF], w=[b_sq])
                P.dma("sp", out_ap[b, j * 128:(j + 1) * 128, :], sq, r=[b_sq], w=[b_out])


def build(phases, scr_kinds=None):
    kb = KB(scr_kinds)
    nc = kb.nc
    shapes = {}

    def inp(name, shape):
        shapes[name] = list(shape)

    class LazyIn(dict):
        def __missing__(self, name):
            ap = nc.dram_tensor(name, shapes[name], F32, kind="ExternalInput").ap()
            self[name] = ap
            return ap

    IN = LazyIn()
    kb.IN = IN
    inp("x", [NB, SEQ, D]); inp("c", [NB, D]); inp("ctx", [NB, CTX, D]); inp("c_ctx", [D])
    inp("ada_w", [2, D, 6 * D]); inp("ada_b", [2, 6 * D]); inp("norm1_g", [2, D]); inp("norm2_g", [2, D])
    inp("ab_w_in", [1, D, AB_W]); inp("dn_conv_w", [1, 5, 1536]); inp("dn_A_log", [1, 2, 4]); inp("dn_dt_bias", [1, 2, 4])
    inp("dn_norm_g", [1, 128])
    inp("s5_A_re", [1, 2, 32, 64]); inp("s5_A_im", [1, 2, 32, 64]); inp("s5_log_dt", [1, 2, 32])
    inp("s5_B_re", [1, 32, 64, 16]); inp("s5_B_im", [1, 32, 64, 16]); inp("s5_C_re", [1, 32, 16, 64]); inp("s5_C_im", [1, 32, 16, 64])
    inp("s5_D", [1, 32, 16]); inp("s5_glu_w", [1, 512, 512]); inp("s5_glu_b", [1, 512]); inp("ab_w_out", [1, 1024, D])
    inp("mla_w_in", [1, D, 704]); inp("mla_q_norm_g", [1, 384]); inp("mla_w_q_up", [1, 384, 1536]); inp("mla_kv_norm_g", [1, 256])
    inp("mla_w_kv_up", [1, 256, 2048]); inp("mla_w_out", [1, 1024, D])
    inp("router_w", [D, 16]); inp("router_bias", [16])
    inp("moe_w_gate", [2, 16, D, 512]); inp("moe_w_up", [2, 16, D, 512]); inp("moe_w_down", [2, 16, 512, D])
    inp("final_norm_g", [D])
    out_ap = nc.dram_tensor("out", [NB, SEQ, D], F32, kind="ExternalOutput").ap()
    b_out = Buf("out")

    kb.dram("modd0", [3, 6144]); kb.dram("modd1", [3, 6144])
    kb.dram("xs", [NB, T, D])
    kb.dram("qkvu", [NB, 2048, T])
    kb.dram("zab", [NB, T, 528])
    kb.dram("qkvn", [NB, 1536, T])
    kb.dram("mixT", [NB, 1024, T])
    kb.dram("ygT", [NB, 512, T])
    kb.dram("attnT", [NB, 1024, SEQ])

    with ExitStack() as top:
        kb.stack = top
        kb.consts()
        P = kb.P
        if "init" in phases:
            xs, b_xs = kb.dr["xs"]
            for b in range(NB):
                P.dma("sp", xs[b, 0:CTX, :], IN["ctx"][b], w=[b_xs])
                for q in range(4):
                    P.dma("sp", xs[b, CTX + q * 512:CTX + (q + 1) * 512, :], IN["x"][b, q * 512:(q + 1) * 512, :], w=[b_xs])
        if "ada0" in phases:
            phase_adaln(kb, 0, IN)
        if "l0in" in phases:
            phase_l0_inproj(kb, IN)
        if "l0conv" in phases:
            phase_l0_conv(kb, IN)
        if "l0delta" in phases:
            phase_l0_delta(kb, IN)
        if "l0s5" in phases:
            phase_l0_s5(kb, IN)
        if "l0out" in phases:
            phase_l0_out(kb, IN)
        if "moe0" in phases:
            phase_moe(kb, IN, 0, 0)
        if "ada1" in phases and not (ADA1_IN_CONV and "l0conv" in phases):
            phase_adaln(kb, 1, IN)
        if "l1mla" in phases:
            phase_l1_mla(kb, IN)
        if "l1out" in phases:
            phase_proj_res(kb, IN, 1, "attnT", IN["mla_w_out"][0], 2048, CTX)
        if "moe1" in phases:
            phase_moe(kb, IN, 1, CTX)
        if "final" in phases:
            phase_final(kb, IN, out_ap, b_out)
        if "dumpxs" in phases:
            xs, b_xs = kb.dr["xs"]
            xd = nc.dram_tensor("xs_dump", [NB, T, D], F32, kind="ExternalOutput").ap()
            b_xd = Buf("xd")
            for b in range(NB):
                P.dma("sp", xd[b], xs[b], r=[b_xs], w=[b_xd])
        P.barrier()
    return kb


def _in_maps(inputs, names=None):
    maps = []
    for core in range(NCORES):
        m = {}
        for k, v in inputs.items():
            if names is not None and k not in names:
                continue
            a = np.asarray(v)
            if k in ("x", "c", "ctx"):
                a = a[core * NB:(core + 1) * NB]
            m[k] = np.ascontiguousarray(a, dtype=np.float32)
        maps.append(m)
    return maps


def kernel(**inputs):
    kb = build(ALL_PHASES)
    res = run_bass_kernel_spmd(kb.nc, _in_maps(inputs, set(kb.IN.keys())), core_ids=list(range(NCORES)))
    return np.concatenate([np.asarray(r["out"]) for r in res.results], axis=0).astype(np.float32)


ALL_PHASES = ("init", "ada0", "l0in", "l0conv", "l0delta", "l0s5", "l0out", "moe0",
              "ada1", "l1mla", "l1out", "moe1", "final")
```
